# Optimizing a Trainium2 kernel written in Bass

```python
import math
import jax, jax.numpy as jnp
from jax import lax
import numpy as np

D_MODEL = 1024
BATCH = 16
SEQ = 2048
DEPTH = 1

CONF_DIM = D_MODEL
CONF_KERNEL = 31
SSM_HEADS = 16
SSM_HEAD_DIM = 64
SSM_DIM = SSM_HEADS * SSM_HEAD_DIM
SSM_GROUPS = 4
SSM_STATE = 128
SSM_CONV = 4
SSD_CHUNK = 128
XBC_DIM = SSM_DIM + 2 * SSM_GROUPS * SSM_STATE
D_MIX = CONF_DIM + SSM_DIM
IN_COLS = 2 * CONF_DIM + SSM_DIM + XBC_DIM + SSM_HEADS
N_EXPERT_GROUPS = 8
EXPERTS_PER_GROUP = 8
N_EXPERTS = N_EXPERT_GROUPS * EXPERTS_PER_GROUP
TOP_K = 2
D_EXPERT = D_MODEL // 2
MOE_BLOCK = 128
DEEPNORM_ALPHA = (2.0 * DEPTH) ** 0.25
DEEPNORM_BETA = (8.0 * DEPTH) ** -0.25
EPS = 1e-5

kernel_name = "hymba_conformer_ssd_hmoe_deepnorm_adaln"


def layer_norm(x, g, b):
    xf = x.astype(jnp.float32)
    mu = jnp.mean(xf, axis=-1, keepdims=True)
    var = jnp.mean(jnp.square(xf - mu), axis=-1, keepdims=True)
    return ((xf - mu) * lax.rsqrt(var + EPS)).astype(x.dtype) * g + b


def causal_dwconv(u, w, b):
    k, ch = w.shape
    y = lax.conv_general_dilated(u, w[:, None, :].astype(u.dtype), window_strides=(1,),
                                 padding=[(k - 1, 0)], dimension_numbers=("NWC", "WIO", "NWC"),
                                 feature_group_count=ch)
    return y + b


def gated_rmsnorm(y, z, w):
    g = y * jax.nn.silu(z)
    gf = g.astype(jnp.float32).reshape(g.shape[:-1] + (SSM_GROUPS, SSM_DIM // SSM_GROUPS))
    gf = gf * lax.rsqrt(jnp.mean(jnp.square(gf), axis=-1, keepdims=True) + EPS)
    return gf.reshape(g.shape).astype(y.dtype) * w


def ssd_chunked(xs, dt, A, Bm, Cm):
    b, t, g, r, p = xs.shape
    n = Bm.shape[-1]
    c, l = t // SSD_CHUNK, SSD_CHUNK
    xc = (xs * dt[..., None]).reshape(b, c, l, g, r, p)
    a = jnp.moveaxis((dt * A).reshape(b, c, l, g, r), 2, -1)
    a_cs = jnp.cumsum(a, axis=-1)
    bc = Bm.reshape(b, c, l, g, n)
    cc = Cm.reshape(b, c, l, g, n)
    causal = jnp.tril(jnp.ones((l, l), dtype=bool))
    seg = a_cs[..., :, None] - a_cs[..., None, :]
    decay = jnp.exp(jnp.where(causal, seg, -jnp.inf))
    cb = jnp.einsum("bclgn,bcsgn->bcgls", cc, bc)
    y_diag = jnp.einsum("bcgls,bcgrls,bcsgrp->bclgrp", cb, decay, xc)
    decay_to_end = jnp.exp(a_cs[..., -1:] - a_cs)
    chunk_states = jnp.einsum("bclgn,bcgrl,bclgrp->bcgrpn", bc, decay_to_end, xc)
    chunk_decay = jnp.exp(a_cs[..., -1])

    def step(state, inp):
        s_c, d_c = inp
        return d_c[..., None, None] * state + s_c, state

    init = jnp.zeros_like(chunk_states[:, 0])
    _, prev = lax.scan(step, init, (jnp.moveaxis(chunk_states, 1, 0), jnp.moveaxis(chunk_decay, 1, 0)))
    prev = jnp.moveaxis(prev, 0, 1)
    y_off = jnp.einsum("bclgn,bcgrpn,bcgrl->bclgrp", cc, prev, jnp.exp(a_cs))
    return (y_diag + y_off).reshape(b, t, g, r, p)


def hybrid_mixer(h, w_in, conf_conv_w, conf_conv_b, conf_ln_g, conf_ln_b, ssm_conv_w, ssm_conv_b,
                 ssm_dt_bias, ssm_A_log, ssm_D, ssm_norm_w, w_out):
    proj = jnp.einsum("btd,de->bte", h, w_in)
    s1 = CONF_DIM
    s2 = 2 * CONF_DIM
    s3 = s2 + SSM_DIM
    s4 = s3 + XBC_DIM
    conf_val, conf_gate, z, xbc, dt_raw = jnp.split(proj, [s1, s2, s3, s4], axis=-1)
    u = conf_val * jax.nn.sigmoid(conf_gate)
    u = jax.nn.silu(layer_norm(causal_dwconv(u, conf_conv_w, conf_conv_b), conf_ln_g, conf_ln_b))
    xbc = jax.nn.silu(causal_dwconv(xbc, ssm_conv_w, ssm_conv_b))
    xs, bm, cm = jnp.split(xbc, [SSM_DIM, SSM_DIM + SSM_GROUPS * SSM_STATE], axis=-1)
    b, t = xs.shape[:2]
    r = SSM_HEADS // SSM_GROUPS
    xs = xs.reshape(b, t, SSM_GROUPS, r, SSM_HEAD_DIM)
    bm = bm.reshape(b, t, SSM_GROUPS, SSM_STATE)
    cm = cm.reshape(b, t, SSM_GROUPS, SSM_STATE)
    dt = jax.nn.softplus(dt_raw + ssm_dt_bias).reshape(b, t, SSM_GROUPS, r)
    A = -jnp.exp(ssm_A_log).reshape(SSM_GROUPS, r)
    y = ssd_chunked(xs, dt, A, bm, cm) + ssm_D.reshape(SSM_GROUPS, r)[:, :, None] * xs
    y = gated_rmsnorm(y.reshape(b, t, SSM_DIM), z, ssm_norm_w)
    return jnp.einsum("bte,ed->btd", jnp.concatenate([u, y], axis=-1), w_out)


def hier_moe(h, wg, bg, we, be, w1, w3, w2):
    n_tok, d = h.shape
    p_group = jax.nn.softmax((h @ wg + bg).astype(jnp.float32), axis=-1)
    pg_top, g_idx = lax.top_k(p_group, 1)
    e_logits = (h @ we + be).astype(jnp.float32).reshape(n_tok, N_EXPERT_GROUPS, EXPERTS_PER_GROUP)
    sel = jnp.take_along_axis(e_logits, g_idx[:, :, None], axis=1)[:, 0]
    pe_top, e_local = lax.top_k(jax.nn.softmax(sel, axis=-1), TOP_K)
    pe_top = pe_top / jnp.sum(pe_top, axis=-1, keepdims=True)
    gate = (pg_top * pe_top).astype(h.dtype)
    e_idx = g_idx * EXPERTS_PER_GROUP + e_local
    m = n_tok * TOP_K
    e_flat = e_idx.reshape(m)
    tok_flat = jnp.repeat(jnp.arange(n_tok, dtype=jnp.int32), TOP_K)
    w_flat = gate.reshape(m)
    order = jnp.argsort(e_flat)
    e_sorted, tok_sorted, w_sorted = e_flat[order], tok_flat[order], w_flat[order]
    counts = jnp.bincount(e_flat, length=N_EXPERTS)
    starts = jnp.cumsum(counts) - counts
    padded = ((counts + MOE_BLOCK - 1) // MOE_BLOCK) * MOE_BLOCK
    pad_ends = jnp.cumsum(padded)
    pad_starts = pad_ends - padded
    dest = pad_starts[e_sorted] + (jnp.arange(m) - starts[e_sorted])
    n_slots = ((m + MOE_BLOCK - 1) // MOE_BLOCK) * MOE_BLOCK + N_EXPERTS * MOE_BLOCK
    n_blocks = n_slots // MOE_BLOCK
    slot_tok = jnp.full((n_slots,), n_tok, dtype=jnp.int32).at[dest].set(tok_sorted)
    slot_w = jnp.zeros((n_slots,), dtype=h.dtype).at[dest].set(w_sorted)
    block_exp = jnp.minimum(jnp.searchsorted(pad_ends, jnp.arange(n_blocks) * MOE_BLOCK, side="right"),
                            N_EXPERTS - 1)
    h_pad = jnp.concatenate([h, jnp.zeros((1, d), dtype=h.dtype)], axis=0)
    xs = h_pad[slot_tok].reshape(n_blocks, MOE_BLOCK, d)

    def expert_block(args):
        xb, e = args
        return (jax.nn.silu(xb @ w1[e]) * (xb @ w3[e])) @ w2[e]

    ys = lax.map(expert_block, (xs, block_exp)).reshape(n_slots, d) * slot_w[:, None]
    return jax.ops.segment_sum(ys, slot_tok, num_segments=n_tok + 1)[:n_tok]


def setup_inputs(seed: int = 0) -> dict:
    key = jax.random.key(seed)
    ks = jax.random.split(key, 28)
    f32 = jnp.float32
    L = DEPTH

    def nrm(k, shape, std):
        return jax.random.normal(k, shape, dtype=f32) * std

    xavier_out = math.sqrt(2.0 / (D_MIX + D_MODEL)) * DEEPNORM_BETA
    xavier_exp = math.sqrt(2.0 / (D_EXPERT + D_MODEL)) * DEEPNORM_BETA
    dt0 = jnp.exp(jax.random.uniform(ks[10], (L, SSM_HEADS), minval=math.log(1e-3), maxval=math.log(1e-1)))
    return {
        "x": nrm(ks[0], (BATCH, SEQ, D_MODEL), 1.0),
        "c": nrm(ks[1], (BATCH, D_MODEL), 1.0),
        "w_ada": nrm(ks[2], (L, D_MODEL, 6 * D_MODEL), D_MODEL ** -0.5),
        "b_ada": nrm(ks[3], (L, 6 * D_MODEL), 0.02),
        "w_in": nrm(ks[4], (L, D_MODEL, IN_COLS), D_MODEL ** -0.5),
        "conf_conv_w": nrm(ks[5], (L, CONF_KERNEL, CONF_DIM), CONF_KERNEL ** -0.5),
        "conf_conv_b": nrm(ks[6], (L, CONF_DIM), 0.02),
        "conf_ln_g": 1.0 + nrm(ks[7], (L, CONF_DIM), 0.02),
        "conf_ln_b": nrm(ks[8], (L, CONF_DIM), 0.02),
        "ssm_conv_w": nrm(ks[9], (L, SSM_CONV, XBC_DIM), SSM_CONV ** -0.5),
        "ssm_conv_b": nrm(ks[11], (L, XBC_DIM), 0.02),
        "ssm_dt_bias": dt0 + jnp.log(-jnp.expm1(-dt0)),
        "ssm_A_log": jnp.log(jax.random.uniform(ks[12], (L, SSM_HEADS), minval=1.0, maxval=16.0)),
        "ssm_D": 1.0 + nrm(ks[13], (L, SSM_HEADS), 0.1),
        "ssm_norm_w": 1.0 + nrm(ks[14], (L, SSM_DIM), 0.02),
        "w_out": nrm(ks[15], (L, D_MIX, D_MODEL), xavier_out),
        "ln1_g": 1.0 + nrm(ks[16], (L, D_MODEL), 0.02),
        "ln1_b": nrm(ks[17], (L, D_MODEL), 0.02),
        "router_group_w": nrm(ks[18], (L, D_MODEL, N_EXPERT_GROUPS), D_MODEL ** -0.5),
        "router_group_b": nrm(ks[19], (L, N_EXPERT_GROUPS), 0.01),
        "router_expert_w": nrm(ks[20], (L, D_MODEL, N_EXPERTS), D_MODEL ** -0.5),
        "router_expert_b": nrm(ks[21], (L, N_EXPERTS), 0.01),
        "expert_w1": nrm(ks[22], (L, N_EXPERTS, D_MODEL, D_EXPERT), D_MODEL ** -0.5),
        "expert_w3": nrm(ks[23], (L, N_EXPERTS, D_MODEL, D_EXPERT), D_MODEL ** -0.5),
        "expert_w2": nrm(ks[24], (L, N_EXPERTS, D_EXPERT, D_MODEL), xavier_exp),
        "ln2_g": 1.0 + nrm(ks[25], (L, D_MODEL), 0.02),
        "ln2_b": nrm(ks[26], (L, D_MODEL), 0.02),
    }


def reference(x, c, w_ada, b_ada, w_in, conf_conv_w, conf_conv_b, conf_ln_g, conf_ln_b,
              ssm_conv_w, ssm_conv_b, ssm_dt_bias, ssm_A_log, ssm_D, ssm_norm_w, w_out,
              ln1_g, ln1_b, router_group_w, router_group_b, router_expert_w, router_expert_b,
              expert_w1, expert_w3, expert_w2, ln2_g, ln2_b):
    cond = jax.nn.silu(c)
    for l in range(DEPTH):
        mod = cond @ w_ada[l] + b_ada[l]
        sh1, sc1, g1, sh2, sc2, g2 = [m[:, None, :] for m in jnp.split(mod, 6, axis=-1)]
        h = x * (1.0 + sc1) + sh1
        mix = hybrid_mixer(h, w_in[l], conf_conv_w[l], conf_conv_b[l], conf_ln_g[l], conf_ln_b[l],
                           ssm_conv_w[l], ssm_conv_b[l], ssm_dt_bias[l], ssm_A_log[l], ssm_D[l],
                           ssm_norm_w[l], w_out[l])
        x = layer_norm(DEEPNORM_ALPHA * x + g1 * mix, ln1_g[l], ln1_b[l])
        h = x * (1.0 + sc2) + sh2
        ffn = hier_moe(h.reshape(-1, D_MODEL), router_group_w[l], router_group_b[l],
                       router_expert_w[l], router_expert_b[l], expert_w1[l], expert_w3[l],
                       expert_w2[l]).reshape(x.shape)
        x = layer_norm(DEEPNORM_ALPHA * x + g2 * ffn, ln2_g[l], ln2_b[l])
    return x
```

```python
import contextlib
import numpy as np
import concourse.bass as bass
import concourse.mybir as mybir
from concourse.bass_utils import run_bass_kernel_spmd

F32 = mybir.dt.float32
BF16 = mybir.dt.bfloat16
AF = mybir.ActivationFunctionType
ALU = mybir.AluOpType
AX = mybir.AxisListType

ALPHA = 2.0 ** 0.25
EPS = 1e-5
NTOK = 4096
SEQ = 2048
D = 1024
NEXP = 64
BIG = 1.0e9


ALL_DMA_RES = []


class Res:
    __slots__ = ("name", "wr", "rd", "dsem", "dcnt")

    def __init__(self, name):
        self.name = name
        self.wr = {}
        self.rd = {}
        self.dsem = None
        self.dcnt = 0


class Eng:
    def __init__(self, nc, eng, name):
        self.nc = nc
        self.eng = eng
        self.sem = nc.alloc_semaphore(name=name)
        self.cnt = 0
        self.waited = {}

    def wait(self, evs):
        for key, (sem, val) in list(evs.items()):
            if self.waited.get(key, 0) < val:
                self.eng.wait_ge(sem, val)
                self.waited[key] = val

    def deps(self, reads, writes):
        for r in reads:
            self.wait(r.wr)
        for w in writes:
            self.wait(w.wr)
            self.wait(w.rd)

    def mark(self, reads, writes, sem, val):
        key = id(sem)
        for r in reads:
            r.rd[key] = (sem, val)
        for w in writes:
            w.wr = {key: (sem, val)}
            w.rd = {}

    def op(self, fn, reads=(), writes=()):
        self.deps(reads, writes)
        ins = fn()
        self.cnt += 1
        ins.then_inc(self.sem, 1)
        self.mark(reads, writes, self.sem, self.cnt)

    def dma(self, parts, reads, writes, sres):
        self.deps(reads, writes)
        if sres.dsem is None:
            sres.dsem = self.nc.alloc_semaphore(name=f"d{len(ALL_DMA_RES)}_" + sres.name)
            ALL_DMA_RES.append(sres)
        if sres.dcnt:
            self.wait({id(sres.dsem): (sres.dsem, sres.dcnt)})
        for (o, i) in parts:
            self.eng.dma_start(out=o, in_=i).then_inc(sres.dsem, 16)
            sres.dcnt += 16
        self.mark(reads, writes, sres.dsem, sres.dcnt)


class EarlyExit(Exception):
    pass


def build_nc(stage="full"):
    nc = bass.Bass("TRN2", target_bir_lowering=False)
    ALL_DMA_RES.clear()
    st_all = contextlib.ExitStack()
    try:
        with st_all:
            _build(nc, stage, st_all)
    except EarlyExit:
        pass
    return nc


def _build(nc, stage, st_all):

    def din(name, shape, dt=F32):
        return nc.dram_tensor(name, list(shape), dt, kind="ExternalInput").ap()

    x_d = din("x", [NTOK, D])
    xT_d = din("xT", [D, NTOK])
    cT_d = din("cT", [128, 8, 2])
    wada_d = din("w_ada", [D, 6 * D])
    bada_fm_d = din("b_ada_fm", [128, 16])
    bada_row_d = din("b_ada_row", [128, 4 * D])
    win_d = din("w_in", [D, 5136])
    ccw_d = din("ccw", [128, 8, 31])
    ccb_d = din("ccb", [128, 8])
    clg_d = din("clg", [128, 8])
    clb_d = din("clb", [128, 8])
    scw_d = din("scw", [128, 16, 4])
    scb_d = din("scb", [128, 16])
    dtb_d = din("dtb", [128, 16])
    alog_d = din("alog", [128, 16])
    dsk_d = din("dsk", [128, 16])
    normw_d = din("normw", [128, D])
    wout_d = din("w_out", [2 * D, D])
    ln1g_d = din("ln1g", [128, D])
    ln1b_d = din("ln1b", [128, D])
    ln2g_d = din("ln2g", [128, D])
    ln2b_d = din("ln2b", [128, D])
    wr_d = din("wr", [128, 8, 72])
    rb_d = din("rb", [128, 72])
    nexp_decl = NEXP if stage == "full" else 1
    wall_d = din("wall", [nexp_decl * 128, 3 * 4096])
    thr_d = din("thr", [128, 64])
    iotab_d = din("iotab", [128, 128])
    pid_d = din("pid", [128, 1])
    identf_d = din("identf", [128, 128])
    tri_d = din("tri", [128, 128])
    negm_d = din("negm", [128, 512])
    out_d = nc.dram_tensor("out", [NTOK, D], F32, kind="ExternalOutput").ap()

    UF_d = nc.dram_tensor("UF", [D, NTOK], BF16, kind="Internal").ap()
    X1_d = nc.dram_tensor("X1", [NTOK, D], F32, kind="Internal").ap()
    H2_d = nc.dram_tensor("H2", [NTOK, D], BF16, kind="Internal").ap()
    Xs_d = nc.dram_tensor("Xs", [4 * NTOK, D], BF16, kind="Internal").ap()
    Ys_d = nc.dram_tensor("Ys", [4 * NTOK, D], F32, kind="Internal").ap()

    r_UF, r_X1, r_H2T, r_YN = Res("UF"), Res("X1"), Res("H2T"), Res("YN")
    YN_d = nc.dram_tensor("YN", [D, NTOK], BF16, kind="Internal").ap()
    pe = Eng(nc, nc.tensor, "s_pe")
    act = Eng(nc, nc.scalar, "s_act")
    dve = Eng(nc, nc.vector, "s_dve")
    pool = Eng(nc, nc.gpsimd, "s_pool")
    sp = Eng(nc, nc.sync, "s_sp")

    es = st_all
    engines = [pe, act, dve, pool, sp]

    def ck(n):
        if stage == f"K{n}":
            barrier()
            raise EarlyExit()

    def barrier():
        evs = {}
        for E in engines:
            if E.cnt:
                evs[id(E.sem)] = (E.sem, E.cnt)
        for r in ALL_DMA_RES:
            if r.dcnt:
                evs[id(r.dsem)] = (r.dsem, r.dcnt)
        for E in engines:
            E.wait(evs)

    cast_rr = [0]

    def load_cast(dst_aps, src_aps, stg, r_dst):
        for dst, src in zip(dst_aps, src_aps):
            t, r = stg[cast_rr[0] % len(stg)]
            n = dst.shape[-1]
            sp.dma([(t[:, 0:n], src)], [], [r], r)
            if cast_rr[0] % 2 == 0:
                act.op(lambda t=t, dst=dst, n=n: nc.scalar.activation(out=dst, in_=t[:, 0:n], func=AF.Copy), [r], [r_dst])
            else:
                pool.op(lambda t=t, dst=dst, n=n: nc.gpsimd.tensor_copy(out=dst, in_=t[:, 0:n]), [r], [r_dst])
            cast_rr[0] += 1

    sb_cnt = [0]

    def sb(st, name, shape, dt=F32):
        sb_cnt[0] += 1
        t = st.enter_context(nc.sbuf_tensor(f"sb{sb_cnt[0]}_" + name, list(shape), dt))
        return t, Res(name)

    ps = []
    psr = []
    for i in range(7):
        t = es.enter_context(nc.psum_tensor(f"ps{i}", [128, 512], F32))
        ps.append(t)
        psr.append(Res(f"ps{i}"))
    ps7b = es.enter_context(nc.psum_tensor("ps7b", [128, 1024], BF16))
    ps.append(None)
    psr.append(Res("ps7b"))

    def psbf(i):
        return ps7b[:]

    identf, r_identf = sb(es, "identf", [128, 128])
    identb, r_identb = sb(es, "identb", [128, 128], BF16)
    onesf, r_onesf = sb(es, "onesf", [128, 128])
    onesb, r_onesb = sb(es, "onesb", [128, 128], BF16)
    tri, r_tri = sb(es, "tri", [128, 128])
    negm, r_negm = sb(es, "negm", [128, 512])
    modg2, r_modg2 = sb(es, "modg2", [128, 2, D])
    rsm, r_rsm = sb(es, "rsm", [128, 32 * 8])
    d1i, r_d1i = sb(es, "d1i", [128, 32], mybir.dt.int32)
    d2i, r_d2i = sb(es, "d2i", [128, 32], mybir.dt.int32)
    widx, r_widx = sb(es, "widx", [128, 128], mybir.dt.int32)
    modfm, r_modfm = sb(es, "modfm", [128, 16, 2])
    ccw, r_ccw = sb(es, "ccw", [128, 8, 31])
    pfm, r_pfm = sb(es, "pfm", [128, 64])
    prow, r_prow = sb(es, "prow", [128, 64])
    rb, r_rb = sb(es, "rb", [128, 72])
    stMid = es.enter_context(contextlib.ExitStack())
    modrow, r_modrow = sb(stMid, "modrow", [128, 2, 3 * D])
    Lg, r_Lg = sb(stMid, "Lg", [128, 32, 72])

    sp.dma([(identf[:], identf_d[:, :])], [], [r_identf], r_identf)
    sp.dma([(tri[:], tri_d[:, :])], [], [r_tri], r_tri)
    sp.dma([(negm[:], negm_d[:, :])], [], [r_negm], r_negm)
    sp.dma([(ccw[:], ccw_d[:, :, :])], [], [r_ccw], r_ccw)
    sp.dma([(pfm[:, 0:8], ccb_d[:, :]), (pfm[:, 8:16], clg_d[:, :]), (pfm[:, 16:24], clb_d[:, :]),
            (pfm[:, 24:40], scb_d[:, :])], [], [r_pfm], r_pfm)
    sp.dma([(prow[:, 0:16], dtb_d[:, :]), (prow[:, 48:64], alog_d[:, :]), (prow[:, 32:48], dsk_d[:, :])],
           [], [r_prow], r_prow)
    sp.dma([(rb[:], rb_d[:, :])], [], [r_rb], r_rb)
    dve.op(lambda: nc.vector.tensor_copy(out=identb[:], in_=identf[:]), [r_identf], [r_identb])
    dve.op(lambda: nc.vector.memset(onesf[:], 1.0), [], [r_onesf])
    dve.op(lambda: nc.vector.memset(onesb[:], 1.0), [], [r_onesb])
    act.op(lambda: nc.scalar.activation(out=prow[:, 16:32], in_=prow[:, 48:64], func=AF.Exp), [r_prow], [r_prow])
    dve.op(lambda: nc.vector.tensor_scalar(out=prow[:, 16:32], in0=prow[:, 16:32], scalar1=-1.0, scalar2=None,
                                           op0=ALU.mult), [r_prow], [r_prow])

    with contextlib.ExitStack() as st0:
        cT, r_cT = sb(st0, "cT", [128, 8, 2])
        crep, r_crep = sb(st0, "crep", [128, 2, 8, 128])
        bfm, r_bfm = sb(st0, "bfm", [128, 16])
        brow, r_brow = sb(st0, "brow", [128, 4 * D])
        slabA = [sb(st0, f"slabA{i}", [128, 8, 1024]) for i in range(2)]
        slabB = [sb(st0, f"slabB{i}", [128, 8, 512]) for i in range(2)]
        sp.dma([(cT[:], cT_d[:, :, :])], [], [r_cT], r_cT)
        sp.dma([(bfm[:], bada_fm_d[:, :])], [], [r_bfm], r_bfm)
        sp.dma([(brow[:], bada_row_d[:, :])], [], [r_brow], r_brow)
        act.op(lambda: nc.scalar.activation(out=cT[:], in_=cT[:], func=AF.Silu), [r_cT], [r_cT])
        for b in range(2):
            dve.op(lambda b=b: nc.vector.tensor_copy(
                out=crep[:, b, :, :], in_=cT[:, :, b:b + 1].broadcast_to([128, 8, 128])), [r_cT], [r_crep])
        for s in range(2):
            t, r = slabA[s]
            sp.dma([(t[:, k, :], wada_d[k * 128:(k + 1) * 128, s * 1024:(s + 1) * 1024]) for k in range(8)],
                   [], [r], r)
            for j in range(8):
                fc = s * 8 + j

                def mm(fc=fc, j=j, t=t):
                    ins = None
                    for k in range(8):
                        ins = nc.tensor.matmul(ps[0][:, 2 * fc:2 * fc + 2], lhsT=t[:, k, j * 128:(j + 1) * 128],
                                               rhs=cT[:, k, :], start=(k == 0), stop=(k == 7))
                    return ins
                pe.op(mm, [r, r_cT], [psr[0]])
        dve.op(lambda: nc.vector.tensor_tensor(
            out=modfm[:], in0=ps[0][:, 0:32].rearrange("p (f b) -> p f b", b=2),
            in1=bfm[:].unsqueeze(2).broadcast_to([128, 16, 2]), op=ALU.add), [psr[0], r_bfm], [r_modfm])
        dve.op(lambda: nc.vector.tensor_scalar(out=modfm[:, 8:16, :], in0=modfm[:, 8:16, :], scalar1=1.0,
                                               scalar2=None, op0=ALU.add), [r_modfm], [r_modfm])
        for tsl in range(8):
            t, r = slabB[tsl % 2]
            c0 = 2048 + tsl * 512
            sp.dma([(t[:, k, :], wada_d[k * 128:(k + 1) * 128, c0:c0 + 512]) for k in range(8)], [], [r], r)
            for b in range(2):
                pb = 1 + b

                def mm(b=b, t=t, pb=pb):
                    ins = None
                    for k in range(8):
                        ins = nc.tensor.matmul(ps[pb][:, :], lhsT=crep[:, b, k, :], rhs=t[:, k, :],
                                               start=(k == 0), stop=(k == 7))
                    return ins
                pe.op(mm, [r, r_crep], [psr[pb]])
                if tsl < 6:
                    dve.op(lambda b=b, pb=pb, tsl=tsl: nc.vector.tensor_tensor(
                        out=modrow[:, b, tsl * 512:(tsl + 1) * 512], in0=ps[pb][:, :],
                        in1=brow[:, tsl * 512:(tsl + 1) * 512], op=ALU.add), [psr[pb], r_brow], [r_modrow])
                else:
                    dve.op(lambda b=b, pb=pb, tsl=tsl: nc.vector.tensor_tensor(
                        out=modg2[:, b, (tsl - 6) * 512:(tsl - 5) * 512], in0=ps[pb][:, :],
                        in1=brow[:, tsl * 512:(tsl + 1) * 512], op=ALU.add), [psr[pb], r_brow], [r_modg2])
        dve.op(lambda: nc.vector.tensor_scalar(out=modrow[:, :, 2048:3072], in0=modrow[:, :, 2048:3072],
                                               scalar1=1.0, scalar2=None, op0=ALU.add), [r_modrow], [r_modrow])

    barrier()
    if stage == "0":
        sp.dma([(out_d[0:128, :], modrow[:, 0, 0:1024])], [r_modrow], [], r_modrow)
        sp.dma([(out_d[128:256, :], modg2[:, 1, :])], [r_modg2], [], r_modg2)
        sp.dma([(out_d[256:384, 0:32], modfm[:].rearrange("p a b -> p (a b)"))], [r_modfm], [], r_modfm)
        sp.deps([], [r_modrow, r_modg2, r_modfm])
        raise EarlyExit()

    xT_v = xT_d.rearrange("(k p) t -> p k t", p=128)
    UF_v = UF_d.rearrange("(k p) t -> p k t", p=128)
    YN_v = YN_d.rearrange("(k p) t -> p k t", p=128)

    def load_hT(st_tiles, tok0, nt, bsel):
        xTf, r_xTf, hT, r_hT = st_tiles
        sp.dma([(xTf[:, :, 0:nt], xT_v[:, :, tok0:tok0 + nt])], [], [r_xTf], r_xTf)
        for k in range(8):
            eng, E = (nc.vector, dve) if k % 2 == 0 else (nc.gpsimd, pool)
            E.op(lambda k=k, eng=eng: eng.tensor_scalar(
                out=hT[:, k, 0:nt], in0=xTf[:, k, 0:nt], scalar1=modfm[:, 8 + k, bsel:bsel + 1],
                scalar2=modfm[:, k, bsel:bsel + 1], op0=ALU.mult, op1=ALU.add), [r_xTf, r_modfm], [r_hT])

    TB = 256
    with contextlib.ExitStack() as stA:
        winA, r_winA = sb(stA, "winA", [128, 8, 2048], BF16)
        dgA, r_dgA = sb(stA, "dgA", [128, 8, 31, 128], BF16)
        r_p6 = [psr[6], psr[6]]
        xTf, r_xTf = sb(stA, "xTfA", [128, 8, TB])
        hT, r_hT = sb(stA, "hTA", [128, 8, TB], BF16)
        sig = [sb(stA, f"sig{i}", [128, TB]) for i in range(2)]
        ub, r_ub = sb(stA, "ub", [128, 8, 30 + TB], BF16)
        r_uc = [Res(f"uc{c}") for c in range(8)]
        cv, _ = sb(stA, "cv", [128, 8, TB])
        r_cv = [Res(f"cv{c}") for c in range(8)]
        cvq = [sb(stA, f"cvq{i}", [128, 2, TB], BF16) for i in range(2)]
        mean, r_mean = sb(stA, "mean", [128, TB])
        var, r_var = sb(stA, "var", [128, TB])
        rstd, r_rstd = sb(stA, "rstd", [128, TB])
        uf, r_uf = sb(stA, "uf", [128, 8, TB], BF16)
        stgA = [sb(stA, f"stgA{i}", [128, 1024]) for i in range(2)]
        load_cast([winA[:, k, h * 1024:(h + 1) * 1024] for k in range(8) for h in range(2)],
                  [win_d[k * 128:(k + 1) * 128, h * 1024:(h + 1) * 1024] for k in range(8) for h in range(2)],
                  stgA, r_winA)
        for c in range(8):
            for k in range(31):
                dve.op(lambda c=c, k=k: nc.vector.tensor_scalar(
                    out=dgA[:, c, k, :], in0=identb[:], scalar1=ccw[:, c, k:k + 1], scalar2=None, op0=ALU.mult),
                    [r_identb, r_ccw], [r_dgA])
        for s in range(2):
            for c in range(8):
                dve.op(lambda c=c: nc.vector.memset(ub[:, c, 0:30], 0.0), [], [r_uc[c]])
            for j in range(SEQ // TB):
                tok0 = s * SEQ + j * TB
                load_hT((xTf, r_xTf, hT, r_hT), tok0, TB, s)
                for c in range(8):
                    pv, pg = (0, 1) if c % 2 == 0 else (2, 3)
                    for (pi, col0) in ((pv, c * 128), (pg, 1024 + c * 128)):
                        def mm(pi=pi, col0=col0):
                            ins = None
                            for k in range(8):
                                ins = nc.tensor.matmul(ps[pi][:, 0:TB], lhsT=winA[:, k, col0:col0 + 128],
                                                       rhs=hT[:, k, :], start=(k == 0), stop=(k == 7))
                            return ins
                        pe.op(mm, [r_winA, r_hT], [psr[pi]])
                    sg, r_sg = sig[c % 2]
                    act.op(lambda pg=pg, sg=sg: nc.scalar.activation(out=sg[:], in_=ps[pg][:, 0:TB], func=AF.Sigmoid),
                           [psr[pg]], [r_sg])
                    dve.op(lambda pv=pv, sg=sg, c=c: nc.vector.tensor_tensor(
                        out=ub[:, c, 30:30 + TB], in0=ps[pv][:, 0:TB], in1=sg[:], op=ALU.mult),
                        [psr[pv], r_sg], [r_uc[c]])
                ck(51)
                for c in range(8):
                    hp = c % 2

                    def mm(c=c, hp=hp):
                        ins = None
                        for k in range(31):
                            ins = nc.tensor.matmul(ps[6][:, hp * TB:(hp + 1) * TB], lhsT=dgA[:, c, k, :],
                                                   rhs=ub[:, c, k:k + TB], start=(k == 0), stop=(k == 30))
                        return ins
                    pe.op(mm, [r_dgA, r_uc[c]], [r_p6[hp]])
                    ck(52)
                    act.op(lambda c=c, hp=hp: nc.scalar.activation(
                        out=cv[:, c, :], in_=ps[6][:, hp * TB:(hp + 1) * TB], func=AF.Identity,
                        bias=pfm[:, c:c + 1]), [r_p6[hp], r_pfm], [r_cv[c]])
                    ck(53)
                ck(54)
                for c in range(8):
                    eng, E = (nc.vector, dve) if c < 5 else (nc.gpsimd, pool)
                    E.op(lambda c=c, eng=eng: eng.tensor_copy(out=ub[:, c, 0:30], in_=ub[:, c, TB:TB + 30]),
                         [r_uc[c]], [r_uc[c]])
                for c in range(8):
                    q, r_q = cvq[c % 2]
                    act.op(lambda c=c, q=q: nc.scalar.activation(out=q[:, 0, :], in_=cv[:, c, :], func=AF.Copy),
                           [r_cv[c]], [r_q])
                    act.op(lambda c=c, q=q: nc.scalar.activation(out=q[:, 1, :], in_=cv[:, c, :], func=AF.Square),
                           [r_cv[c]], [r_q])

                    def mm(c=c, q=q):
                        nc.tensor.matmul(ps[4][:, 0:TB], lhsT=onesb[:], rhs=q[:, 0, :], start=(c == 0), stop=(c == 7))
                        return nc.tensor.matmul(ps[5][:, 0:TB], lhsT=onesb[:], rhs=q[:, 1, :], start=(c == 0),
                                                stop=(c == 7))
                    pe.op(mm, [r_q, r_onesb], [psr[4], psr[5]])
                ck(55)
                dve.op(lambda: nc.vector.tensor_scalar(out=mean[:], in0=ps[4][:, 0:TB], scalar1=1.0 / 1024.0,
                                                       scalar2=None, op0=ALU.mult), [psr[4]], [r_mean])
                dve.op(lambda: nc.vector.tensor_tensor(out=var[:], in0=mean[:], in1=mean[:], op=ALU.mult),
                       [r_mean], [r_var])
                dve.op(lambda: nc.vector.scalar_tensor_tensor(out=var[:], in0=ps[5][:, 0:TB], scalar=1.0 / 1024.0,
                                                              in1=var[:], op0=ALU.mult, op1=ALU.subtract),
                       [psr[5], r_var], [r_var])
                dve.op(lambda: nc.vector.tensor_scalar(out=var[:], in0=var[:], scalar1=0.0, scalar2=EPS,
                                                       op0=ALU.max, op1=ALU.add), [r_var], [r_var])
                act.op(lambda: nc.scalar.activation(out=var[:], in_=var[:], func=AF.Sqrt), [r_var], [r_var])
                dve.op(lambda: nc.vector.reciprocal(out=rstd[:], in_=var[:]), [r_var], [r_rstd])
                for c in range(8):
                    eng, E = (nc.vector, dve) if c < 5 else (nc.gpsimd, pool)
                    E.op(lambda c=c, eng=eng: eng.tensor_tensor(out=cv[:, c, :], in0=cv[:, c, :], in1=mean[:],
                                                                op=ALU.subtract), [r_cv[c], r_mean], [r_cv[c]])
                    E.op(lambda c=c, eng=eng: eng.tensor_tensor(out=cv[:, c, :], in0=cv[:, c, :], in1=rstd[:],
                                                                op=ALU.mult), [r_cv[c], r_rstd], [r_cv[c]])
                    act.op(lambda c=c: nc.scalar.activation(out=uf[:, c, :], in_=cv[:, c, :], func=AF.Silu,
                                                            bias=pfm[:, 16 + c:17 + c], scale=pfm[:, 8 + c:9 + c]),
                           [r_cv[c], r_pfm], [r_uf])
                sp.dma([(UF_v[:, :, tok0:tok0 + TB], uf[:])], [r_uf], [r_UF], r_uf)
                ck(56)
                if stage == "A1":
                    dbg, r_dbg = sb(stA, "dbg", [128, 1024])
                    def dump(row, src_ap, n, rs):
                        dve.op(lambda: nc.vector.tensor_copy(out=dbg[:, 0:n], in_=src_ap), rs, [r_dbg])
                        sp.dma([(out_d[row:row + 128, 0:n], dbg[:, 0:n])], [r_dbg], [], r_dbg)
                    dump(0, winA[:, 0, 0:1024], 1024, [r_winA])
                    dump(128, hT[:, 0, :], 512, [r_hT])
                    dump(256, ub[:, 0, 0:542], 542, [r_uc[0]])
                    dump(384, cv[:, 0, :], 512, [r_cv[0]])
                    dump(512, mean[:], 512, [r_mean])
                    dump(640, var[:], 512, [r_var])
                    dump(768, rstd[:], 512, [r_rstd])
                    dump(896, uf[:, 0, :], 512, [r_uf])
                    sp.deps([], [r_dbg, r_uf])
                    raise EarlyExit()
                if stage == "A":
                    j4 = tok0 // 1024
                    col = tok0 % 1024
                    dve.op(lambda: nc.vector.tensor_copy(out=xTf[:], in_=uf[:]), [r_uf], [r_xTf])
                    sp.dma([(out_d[j4 * 1024:(j4 + 1) * 1024, col:col + TB].rearrange("(c p) t -> p c t", p=128),
                             xTf[:])], [r_xTf], [], r_xTf)

    barrier()
    if stage == "A":
        sp.deps([], [r_uf, r_xTf])
        raise EarlyExit()

    TBB = 128
    with contextlib.ExitStack() as stB:
        winB, r_winB = sb(stB, "winB", [128, 8, 3088], BF16)
        dg, r_dg = sb(stB, "dg", [128, 16, 4, 128], BF16)
        scw, r_scw = sb(stB, "scw", [128, 16, 4])
        normw, r_normw = sb(stB, "normw", [128, D])
        xTf, r_xTf = sb(stB, "xTfB", [128, 8, TBB])
        hT, r_hT = sb(stB, "hTB", [128, 8, TBB], BF16)
        xpre, r_xpre = sb(stB, "xpre", [128, 16, 3 + TBB], BF16)
        xpost, r_xpost = sb(stB, "xpost", [128, 16, TBB], BF16)
        tA, r_tA = sb(stB, "tA", [128, D])
        tB, r_tB = sb(stB, "tB", [128, D])
        tC, r_tC = sb(stB, "tC", [128, D])
        Rm, r_Rm = sb(stB, "Rm", [128, D])
        dec, r_dec = sb(stB, "dec", [128, D])
        MT, r_MT = sb(stB, "MT", [128, 16, 128], BF16)
        cbs, r_cbs = sb(stB, "cbs", [128, 512])
        xc, r_xc = sb(stB, "xc", [128, D], BF16)
        xcd, r_xcd = sb(stB, "xcd", [128, D], BF16)
        ynb, r_ynb = sb(stB, "ynb", [128, D], BF16)
        ynT, r_ynT = sb(stB, "ynT", [128, 8, 128], BF16)
        Btok, r_Btok = sb(stB, "Btok", [128, 512], BF16)
        S, r_S = sb(stB, "S", [128, D])
        Sb, r_Sb = sb(stB, "Sb", [128, D], BF16)
        sm, r_sm = sb(stB, "smB", [128, 16 * 12])
        dtp = sm[:, 0:16]
        dtv = sm[:, 16:32]
        av = sm[:, 32:48]
        acs = sm[:, 48:64]
        nacs = sm[:, 64:80]
        eacs = sm[:, 80:96]
        dif = sm[:, 96:112]
        dte = sm[:, 112:128]
        cdr = sm[:, 128:144]
        ss4 = sm[:, 144:148]
        rs4 = sm[:, 148:152]
        mv = sm[:, 152:154]
        rs1 = sm[:, 154:155]
        junk = dec

        stgB = [sb(stB, f"stgB{i}", [128, 1544]) for i in range(2)]
        for half in range(2):
            c0 = 2048 + half * 1544
            load_cast([winB[:, k, half * 1544:(half + 1) * 1544] for k in range(8)],
                      [win_d[k * 128:(k + 1) * 128, c0:c0 + 1544] for k in range(8)], stgB, r_winB)
        sp.dma([(scw[:], scw_d[:, :, :])], [], [r_scw], r_scw)
        sp.dma([(normw[:], normw_d[:, :])], [], [r_normw], r_normw)
        for c in range(16):
            for k in range(4):
                dve.op(lambda c=c, k=k: nc.vector.tensor_scalar(
                    out=dg[:, c, k, :], in0=identb[:], scalar1=scw[:, c, k:k + 1], scalar2=None, op0=ALU.mult),
                    [r_identb, r_scw], [r_dg])

        ZC0, XC0, DC0 = 0, 1024, 3072
        for s in range(2):
            dve.op(lambda: nc.vector.memset(xpre[:, :, 0:3], 0.0), [], [r_xpre])
            dve.op(lambda: nc.vector.memset(S[:], 0.0), [], [r_S])
            dve.op(lambda: nc.vector.memset(Sb[:], 0.0), [], [r_Sb])
            for j in range(SEQ // TBB):
                tok0 = s * SEQ + j * TBB
                load_hT((xTf, r_xTf, hT, r_hT), tok0, TBB, s)
                for cp in range(8):
                    pi = cp % 2
                    for hh in range(2):
                        c = cp * 2 + hh

                        def mm(pi=pi, hh=hh, c=c):
                            ins = None
                            for k in range(8):
                                ins = nc.tensor.matmul(ps[pi][:, hh * TBB:(hh + 1) * TBB],
                                                       lhsT=winB[:, k, XC0 + c * 128:XC0 + (c + 1) * 128],
                                                       rhs=hT[:, k, :], start=(k == 0), stop=(k == 7))
                            return ins
                        pe.op(mm, [r_winB, r_hT], [psr[pi]])
                    act.op(lambda pi=pi, cp=cp: nc.scalar.activation(
                        out=xpre[:, 2 * cp:2 * cp + 2, 3:3 + TBB],
                        in_=ps[pi][:, 0:2 * TBB].rearrange("p (a t) -> p a t", a=2), func=AF.Copy),
                        [psr[pi]], [r_xpre])
                for cp in range(8):
                    pi = 2 + cp % 2
                    for hh in range(2):
                        c = cp * 2 + hh

                        def mm(pi=pi, hh=hh, c=c):
                            ins = None
                            for k in range(4):
                                ins = nc.tensor.matmul(ps[pi][:, hh * TBB:(hh + 1) * TBB], lhsT=dg[:, c, k, :],
                                                       rhs=xpre[:, c, k:k + TBB], start=(k == 0), stop=(k == 3))
                            return ins
                        pe.op(mm, [r_dg, r_xpre], [psr[pi]])
                        act.op(lambda pi=pi, hh=hh, c=c: nc.scalar.activation(
                            out=xpost[:, c, :], in_=ps[pi][:, hh * TBB:(hh + 1) * TBB], func=AF.Silu,
                            bias=pfm[:, 24 + c:25 + c]), [psr[pi], r_pfm], [r_xpost])
                dve.op(lambda: nc.vector.tensor_copy(out=xpre[:, :, 0:3], in_=xpre[:, :, TBB:TBB + 3]),
                       [r_xpre], [r_xpre])

                ck(1)
                for q in range(TBB // 128):
                    o = q * 128
                    t0 = tok0 + o
                    tile_idx = t0 // 128
                    for hf in range(2):
                        def mm(hf=hf, o=o):
                            ins = None
                            for k in range(8):
                                ins = nc.tensor.matmul(ps[hf][:, :], lhsT=hT[:, k, o:o + 128],
                                                       rhs=winB[:, k, ZC0 + hf * 512:ZC0 + (hf + 1) * 512],
                                                       start=(k == 0), stop=(k == 7))
                            return ins
                        pe.op(mm, [r_hT, r_winB], [psr[hf]])

                    def mm(o=o):
                        ins = None
                        for k in range(8):
                            ins = nc.tensor.matmul(ps[2][:, 0:16], lhsT=hT[:, k, o:o + 128],
                                                   rhs=winB[:, k, DC0:DC0 + 16], start=(k == 0), stop=(k == 7))
                        return ins
                    pe.op(mm, [r_hT, r_winB], [psr[2]])
                    ck(2)
                    dve.op(lambda: nc.vector.tensor_tensor(out=dtp, in0=ps[2][:, 0:16], in1=prow[:, 0:16],
                                                           op=ALU.add), [psr[2], r_prow], [r_sm])
                    act.op(lambda: nc.scalar.activation(out=dtp, in_=dtp, func=AF.Exp), [r_sm], [r_sm])
                    act.op(lambda: nc.scalar.activation(out=dtv, in_=dtp, func=AF.Ln, bias=1.0), [r_sm], [r_sm])
                    dve.op(lambda: nc.vector.tensor_tensor(out=av, in0=dtv, in1=prow[:, 16:32], op=ALU.mult),
                           [r_sm, r_prow], [r_sm])

                    def mm():
                        nc.tensor.matmul(ps[2][:, 16:32], lhsT=tri[:], rhs=av, start=True, stop=True)
                        return nc.tensor.matmul(ps[2][:, 32:48], lhsT=onesf[:], rhs=av, start=True, stop=True)
                    pe.op(mm, [r_tri, r_onesf, r_sm], [psr[2]])
                    dve.op(lambda: nc.vector.tensor_copy(out=acs, in_=ps[2][:, 16:32]), [psr[2]], [r_sm])
                    dve.op(lambda: nc.vector.tensor_scalar(out=nacs, in0=acs, scalar1=-1.0, scalar2=None,
                                                           op0=ALU.mult), [r_sm], [r_sm])
                    dve.op(lambda: nc.vector.tensor_tensor(out=dif, in0=ps[2][:, 32:48], in1=acs, op=ALU.subtract),
                           [psr[2], r_sm], [r_sm])
                    act.op(lambda: nc.scalar.activation(out=eacs, in_=acs, func=AF.Exp), [r_sm], [r_sm])
                    act.op(lambda: nc.scalar.activation(out=dte, in_=dif, func=AF.Exp), [r_sm], [r_sm])
                    act.op(lambda: nc.scalar.activation(out=cdr, in_=ps[2][:, 32:48], func=AF.Exp),
                           [psr[2], r_sm], [r_sm])
                    ck(3)
                    def mm(o=o):
                        ins = None
                        for g in range(4):
                            ins = nc.tensor.matmul(ps[3][:, g * 128:(g + 1) * 128], lhsT=xpost[:, 8 + g, o:o + 128],
                                                   rhs=xpost[:, 12 + g, o:o + 128], start=True, stop=True)
                        return ins
                    pe.op(mm, [r_xpost], [psr[3]])
                    act.op(lambda: nc.scalar.activation(out=cbs[:], in_=ps[3][:, :], func=AF.Copy), [psr[3]], [r_cbs])
                    ck(4)
                    def mm(o=o):
                        ins = None
                        for c in range(8):
                            ins = nc.tensor.transpose(out=psbf(7)[:, c * 128:(c + 1) * 128], in_=xpost[:, c, o:o + 128],
                                                      identity=identb[:])
                        return ins
                    pe.op(mm, [r_xpost, r_identb], [psr[7]])
                    ck(41)
                    act.op(lambda: nc.scalar.activation(out=tB[:], in_=psbf(7)[:, :], func=AF.Copy), [psr[7]], [r_tB])
                    ck(42)
                    dve.op(lambda: nc.vector.tensor_tensor(
                        out=xc[:].rearrange("p (h d) -> p h d", d=64),
                        in0=tB[:].rearrange("p (h d) -> p h d", d=64),
                        in1=dtv.unsqueeze(2).broadcast_to([128, 16, 64]), op=ALU.mult), [r_tB, r_sm], [r_xc])
                    ck(43)
                    dve.op(lambda: nc.vector.tensor_tensor(
                        out=xcd[:].rearrange("p (h d) -> p h d", d=64),
                        in0=xc[:].rearrange("p (h d) -> p h d", d=64),
                        in1=dte.unsqueeze(2).broadcast_to([128, 16, 64]), op=ALU.mult), [r_xc, r_sm], [r_xcd])
                    ck(5)
                    for hh in range(2):
                        dve.op(lambda hh=hh: nc.vector.tensor_tensor(
                            out=Rm[:].rearrange("p (h l) -> p h l", l=128),
                            in0=tri[:].unsqueeze(1).broadcast_to([128, 8, 128]),
                            in1=av[:, 8 * hh:8 * hh + 8].unsqueeze(2).broadcast_to([128, 8, 128]), op=ALU.mult),
                            [r_tri, r_sm], [r_Rm])
                        for jb in range(2):
                            def mm(jb=jb):
                                nc.tensor.matmul(ps[4 + jb][:, :], lhsT=onesf[:], rhs=Rm[:, jb * 512:(jb + 1) * 512],
                                                 start=True, stop=False)
                                return nc.tensor.matmul(ps[4 + jb][:, :], lhsT=identf[:], rhs=negm[:],
                                                        start=False, stop=True)
                            pe.op(mm, [r_onesf, r_identf, r_negm, r_Rm], [psr[4 + jb]])
                        for jj in range(8):
                            h = 8 * hh + jj
                            act.op(lambda jj=jj, h=h: nc.scalar.activation(
                                out=dec[:, jj * 128:(jj + 1) * 128],
                                in_=ps[4 + jj // 4][:, (jj % 4) * 128:(jj % 4 + 1) * 128], func=AF.Exp,
                                bias=nacs[:, h:h + 1]), [psr[4 + jj // 4], r_sm], [r_dec])
                        for jb in range(2):
                            g = 2 * hh + jb
                            dve.op(lambda jb=jb, g=g, hh=hh: nc.vector.tensor_tensor(
                                out=MT[:, 8 * hh + 4 * jb:8 * hh + 4 * jb + 4, :],
                                in0=dec[:, jb * 512:(jb + 1) * 512].rearrange("p (h l) -> p h l", l=128),
                                in1=cbs[:, g * 128:(g + 1) * 128].unsqueeze(1).broadcast_to([128, 4, 128]),
                                op=ALU.mult), [r_dec, r_cbs], [r_MT])
                    ck(6)
                    for bk in range(2):
                        def mm(bk=bk, o=o):
                            ins = None
                            for gg in range(2):
                                g = 2 * bk + gg
                                ins = nc.tensor.matmul(ps[4 + bk][:, gg * 256:(gg + 1) * 256],
                                                       lhsT=xpost[:, 12 + g, o:o + 128],
                                                       rhs=Sb[:, g * 256:(g + 1) * 256], start=True, stop=True)
                            return ins
                        pe.op(mm, [r_xpost, r_Sb], [psr[4 + bk]])
                        dve.op(lambda bk=bk: nc.vector.tensor_tensor(
                            out=tC[:, bk * 512:(bk + 1) * 512].rearrange("p (h d) -> p h d", d=64),
                            in0=ps[4 + bk][:, :].rearrange("p (h d) -> p h d", d=64),
                            in1=eacs[:, 8 * bk:8 * bk + 8].unsqueeze(2).broadcast_to([128, 8, 64]), op=ALU.mult),
                            [psr[4 + bk], r_sm], [r_tC])
                    ck(7)
                    for bk in range(2):
                        def mm(bk=bk):
                            ins = None
                            for hh8 in range(8):
                                h = 8 * bk + hh8
                                ins = nc.tensor.matmul(ps[(6, 3)[bk]][:, hh8 * 64:(hh8 + 1) * 64], lhsT=MT[:, h, :],
                                                       rhs=xc[:, h * 64:(h + 1) * 64], start=True, stop=True)
                            return ins
                        pe.op(mm, [r_MT, r_xc], [psr[(6, 3)[bk]]])
                        dve.op(lambda bk=bk: nc.vector.tensor_tensor(
                            out=tC[:, bk * 512:(bk + 1) * 512], in0=ps[(6, 3)[bk]][:, :],
                            in1=tC[:, bk * 512:(bk + 1) * 512], op=ALU.add), [psr[(6, 3)[bk]], r_tC], [r_tC])
                    ck(8)
                    dve.op(lambda: nc.vector.tensor_tensor(
                        out=tB[:].rearrange("p (h d) -> p h d", d=64), in0=tB[:].rearrange("p (h d) -> p h d", d=64),
                        in1=prow[:, 32:48].unsqueeze(2).broadcast_to([128, 16, 64]), op=ALU.mult),
                        [r_tB, r_prow], [r_tB])
                    dve.op(lambda: nc.vector.tensor_tensor(out=tC[:], in0=tC[:], in1=tB[:], op=ALU.add),
                           [r_tC, r_tB], [r_tC])
                    ck(9)
                    for hf in range(2):
                        act.op(lambda hf=hf: nc.scalar.activation(out=tA[:, hf * 512:(hf + 1) * 512], in_=ps[hf][:, :],
                                                                  func=AF.Silu), [psr[hf]], [r_tA])
                    dve.op(lambda: nc.vector.tensor_tensor(out=tC[:], in0=tC[:], in1=tA[:], op=ALU.mult),
                           [r_tC, r_tA], [r_tC])
                    for g4 in range(4):
                        act.op(lambda g4=g4: nc.scalar.activation(
                            out=junk[:, 0:256], in_=tC[:, g4 * 256:(g4 + 1) * 256], func=AF.Square,
                            accum_out=ss4[:, g4:g4 + 1]), [r_tC], [r_dec, r_sm])
                    dve.op(lambda: nc.vector.tensor_scalar(out=ss4, in0=ss4, scalar1=1.0 / 256.0, scalar2=EPS,
                                                           op0=ALU.mult, op1=ALU.add), [r_sm], [r_sm])
                    act.op(lambda: nc.scalar.activation(out=ss4, in_=ss4, func=AF.Sqrt), [r_sm], [r_sm])
                    dve.op(lambda: nc.vector.reciprocal(out=rs4, in_=ss4), [r_sm], [r_sm])
                    dve.op(lambda: nc.vector.tensor_tensor(
                        out=tC[:].rearrange("p (g d) -> p g d", d=256), in0=tC[:].rearrange("p (g d) -> p g d", d=256),
                        in1=rs4.unsqueeze(2).broadcast_to([128, 4, 256]), op=ALU.mult), [r_tC, r_sm], [r_tC])
                    dve.op(lambda: nc.vector.tensor_tensor(out=ynb[:], in0=tC[:], in1=normw[:], op=ALU.mult),
                           [r_tC, r_normw], [r_ynb])

                    def mm():
                        ins = None
                        for c in range(8):
                            ins = nc.tensor.transpose(out=psbf(3)[:, c * 128:(c + 1) * 128],
                                                      in_=ynb[:, c * 128:(c + 1) * 128], identity=identb[:])
                        return ins
                    pe.op(mm, [r_ynb, r_identb], [psr[7]])
                    act.op(lambda: nc.scalar.activation(out=ynT[:].rearrange("p c t -> p (c t)"), in_=psbf(3)[:, :],
                                                        func=AF.Copy), [psr[7]], [r_ynT])
                    ck(10)
                    def mm(o=o):
                        ins = None
                        for g in range(4):
                            ins = nc.tensor.transpose(out=psbf(2)[:, g * 128:(g + 1) * 128],
                                                      in_=xpost[:, 8 + g, o:o + 128], identity=identb[:])
                        return ins
                    pe.op(mm, [r_xpost, r_identb], [psr[7]])
                    act.op(lambda: nc.scalar.activation(out=Btok[:], in_=psbf(2)[:, 0:512], func=AF.Copy),
                           [psr[7]], [r_Btok])
                    dve.op(lambda: nc.vector.tensor_tensor(
                        out=S[:].rearrange("p (h d) -> p h d", d=64), in0=S[:].rearrange("p (h d) -> p h d", d=64),
                        in1=cdr.unsqueeze(2).broadcast_to([128, 16, 64]), op=ALU.mult), [r_S, r_sm], [r_S])
                    for bk in range(2):
                        def mm(bk=bk):
                            ins = None
                            for gg in range(2):
                                g = 2 * bk + gg
                                ins = nc.tensor.matmul(ps[4 + bk][:, gg * 256:(gg + 1) * 256],
                                                       lhsT=Btok[:, g * 128:(g + 1) * 128],
                                                       rhs=xcd[:, g * 256:(g + 1) * 256], start=True, stop=True)
                            return ins
                        pe.op(mm, [r_Btok, r_xcd], [psr[4 + bk]])
                        dve.op(lambda bk=bk: nc.vector.tensor_tensor(
                            out=S[:, bk * 512:(bk + 1) * 512], in0=ps[4 + bk][:, :],
                            in1=S[:, bk * 512:(bk + 1) * 512], op=ALU.add), [psr[4 + bk], r_S], [r_S])
                    act.op(lambda: nc.scalar.activation(out=Sb[:], in_=S[:], func=AF.Copy), [r_S], [r_Sb])
                    sp.dma([(YN_v[:, :, t0:t0 + 128], ynT[:])], [r_ynT], [r_YN], r_ynT)
                    if stage == "B1":
                        j4 = t0 // 1024
                        col = t0 % 1024
                        dve.op(lambda: nc.vector.tensor_copy(out=xTf[:], in_=ynT[:]), [r_ynT], [r_xTf])
                        sp.dma([(out_d[j4 * 1024:(j4 + 1) * 1024, col:col + 128].rearrange("(c p) t -> p c t", p=128),
                                 xTf[:])], [r_xTf], [], r_xTf)

    barrier()
    if stage == "B1":
        raise EarlyExit()
    with contextlib.ExitStack() as stC:
        woutb, r_woutb = sb(stC, "woutb", [128, 16, 1024], BF16)
        ln1g, r_ln1g = sb(stC, "ln1g", [128, D])
        ln1b, r_ln1b = sb(stC, "ln1b", [128, D])
        wr, r_wr = sb(stC, "wr", [128, 8, 72])
        ufb, r_ufb = sb(stC, "ufb", [128, 8, 128], BF16)
        ynT, r_ynT = sb(stC, "ynTC", [128, 8, 128], BF16)
        xtok, r_xtok = sb(stC, "xtok", [128, D])
        tA, r_tA = sb(stC, "tAC", [128, D])
        tC, r_tC = sb(stC, "tCC", [128, D])
        h2Tf, r_h2Tf = sb(stC, "h2Tf", [128, 8, 128])
        h2b, r_h2b = sb(stC, "h2b", [128, D], BF16)
        bst, r_bst = sb(stC, "bst", [128, 2, 6])
        sm, r_sm = sb(stC, "smC", [128, 8])
        mv = sm[:, 0:2]
        rs1 = sm[:, 2:3]
        stgC = [sb(stC, f"stgC{i}", [128, 1024]) for i in range(2)]
        load_cast([woutb[:, c, :] for c in range(16)], [wout_d[c * 128:(c + 1) * 128, :] for c in range(16)],
                  stgC, r_woutb)
        sp.dma([(ln1g[:], ln1g_d[:, :])], [], [r_ln1g], r_ln1g)
        sp.dma([(ln1b[:], ln1b_d[:, :])], [], [r_ln1b], r_ln1b)
        sp.dma([(wr[:], wr_d[:, :, :])], [], [r_wr], r_wr)
        for tile_idx in range(32):
            s = tile_idx // 16
            t0 = tile_idx * 128
            g1row = modrow[:, s, 0:1024]
            sh2row = modrow[:, s, 1024:2048]
            sc2row = modrow[:, s, 2048:3072]
            sp.dma([(xtok[:], x_d[t0:t0 + 128, :])], [], [r_xtok], r_xtok)
            sp.dma([(ufb[:], UF_v[:, :, t0:t0 + 128])], [r_UF], [r_ufb], r_ufb)
            sp.dma([(ynT[:], YN_v[:, :, t0:t0 + 128])], [r_YN], [r_ynT], r_ynT)
            for hf in range(2):
                def mm(hf=hf):
                    ins = None
                    for c in range(8):
                        ins = nc.tensor.matmul(ps[hf][:, :], lhsT=ufb[:, c, :],
                                               rhs=woutb[:, c, hf * 512:(hf + 1) * 512],
                                               start=(c == 0), stop=False)
                    for c in range(8):
                        ins = nc.tensor.matmul(ps[hf][:, :], lhsT=ynT[:, c, :],
                                               rhs=woutb[:, 8 + c, hf * 512:(hf + 1) * 512],
                                               start=False, stop=(c == 7))
                    return ins
                pe.op(mm, [r_ufb, r_ynT, r_woutb], [psr[hf]])
                dve.op(lambda hf=hf: nc.vector.tensor_tensor(
                    out=tA[:, hf * 512:(hf + 1) * 512], in0=ps[hf][:, :],
                    in1=g1row[:, hf * 512:(hf + 1) * 512], op=ALU.mult), [psr[hf], r_modrow], [r_tA])
            dve.op(lambda: nc.vector.scalar_tensor_tensor(out=tA[:], in0=xtok[:], scalar=ALPHA, in1=tA[:],
                                                          op0=ALU.mult, op1=ALU.add), [r_xtok, r_tA], [r_tA])
            ck(20)
            dve.op(lambda: nc.vector.bn_stats(out=bst[:, 0, :], in_=tA[:, 0:512]), [r_tA], [r_bst])
            dve.op(lambda: nc.vector.bn_stats(out=bst[:, 1, :], in_=tA[:, 512:1024]), [r_tA], [r_bst])
            dve.op(lambda: nc.vector.bn_aggr(out=mv, in_=bst[:].rearrange("p a b -> p (a b)")),
                   [r_bst], [r_sm])
            ck(21)
            dve.op(lambda: nc.vector.tensor_scalar(out=rs1, in0=mv[:, 1:2], scalar1=EPS, scalar2=None,
                                                   op0=ALU.add), [r_sm], [r_sm])
            act.op(lambda: nc.scalar.activation(out=rs1, in_=rs1, func=AF.Sqrt), [r_sm], [r_sm])
            dve.op(lambda: nc.vector.reciprocal(out=rs1, in_=rs1), [r_sm], [r_sm])
            dve.op(lambda: nc.vector.tensor_scalar(out=tC[:], in0=tA[:], scalar1=mv[:, 0:1], scalar2=rs1,
                                                   op0=ALU.subtract, op1=ALU.mult), [r_tA, r_sm], [r_tC])
            dve.op(lambda: nc.vector.tensor_tensor(out=tC[:], in0=tC[:], in1=ln1g[:], op=ALU.mult),
                   [r_tC, r_ln1g], [r_tC])
            dve.op(lambda: nc.vector.tensor_tensor(out=tC[:], in0=tC[:], in1=ln1b[:], op=ALU.add),
                   [r_tC, r_ln1b], [r_tC])
            sp.dma([(X1_d[t0:t0 + 128, :], tC[:])], [r_tC], [r_X1], r_tC)
            if stage == "B":
                sp.dma([(out_d[t0:t0 + 128, :], tC[:])], [r_tC], [], r_tC)
            ck(22)
            dve.op(lambda: nc.vector.tensor_tensor(out=tA[:], in0=tC[:], in1=sc2row, op=ALU.mult),
                   [r_tC, r_modrow], [r_tA])
            dve.op(lambda: nc.vector.tensor_tensor(out=tA[:], in0=tA[:], in1=sh2row, op=ALU.add),
                   [r_tA, r_modrow], [r_tA])
            ck(23)
            for bk in range(2):
                def mm(bk=bk):
                    ins = None
                    for cc in range(4):
                        c = 4 * bk + cc
                        ins = nc.tensor.matmul(ps[2 + bk][:, cc * 128:(cc + 1) * 128],
                                               lhsT=tA[:, c * 128:(c + 1) * 128], rhs=identf[:],
                                               start=True, stop=True)
                    return ins
                pe.op(mm, [r_tA, r_identf], [psr[2 + bk]])
                ck(241)
                act.op(lambda bk=bk: nc.scalar.activation(
                    out=h2Tf[:, 4 * bk:4 * bk + 4, :].rearrange("p c t -> p (c t)"), in_=ps[2 + bk][:, :],
                    func=AF.Copy), [psr[2 + bk]], [r_h2Tf])
                ck(242)
            ck(24)
            pool.op(lambda: nc.gpsimd.tensor_copy(out=h2b[:], in_=tA[:]), [r_tA], [r_h2b])
            sp.dma([(H2_d[t0:t0 + 128, :], h2b[:])], [r_h2b], [r_H2T], r_h2b)

            def mm():
                ins = None
                for k in range(8):
                    ins = nc.tensor.matmul(ps[6][:, 0:72], lhsT=h2Tf[:, k, :], rhs=wr[:, k, :],
                                           start=(k == 0), stop=(k == 7))
                return ins
            pe.op(mm, [r_h2Tf, r_wr], [psr[6]])
            dve.op(lambda tile_idx=tile_idx: nc.vector.tensor_tensor(
                out=Lg[:, tile_idx, :], in0=ps[6][:, 0:72], in1=rb[:], op=ALU.add), [psr[6], r_rb], [r_Lg])
            ck(25)

    barrier()
    if stage == "B":
        sp.deps([], [r_tC])
        raise EarlyExit()

    I32 = mybir.dt.int32
    V = nc.vector
    gmax = rsm[:, 0:32]
    pgt = rsm[:, 32:64]
    m1 = rsm[:, 64:96]
    m2 = rsm[:, 96:128]
    pe1 = rsm[:, 128:160]
    pe2 = rsm[:, 160:192]
    d1f = rsm[:, 192:224]
    d2f = rsm[:, 224:256]
    with contextlib.ExitStack() as stM:
        stR = contextlib.ExitStack()
        wk, r_wk = sb(stR, "wk", [128, 32, 64])
        oh1, r_oh1 = sb(stR, "oh1", [128, 32, 64])
        oh2, r_oh2 = sb(stR, "oh2", [128, 32, 64])
        cum, r_cum = sb(stR, "cum", [128, 32, 64])
        cs, r_cs = sb(stR, "cs", [128, 32, 64])
        ohb, r_ohb = sb(stR, "ohb", [128, 32, 64], BF16)
        triSb, r_triSb = sb(stR, "triSb", [128, 128], BF16)
        gm, r_gm = sb(stR, "gm", [128, 32, 8])
        ohg, r_ohg = sb(stR, "ohg", [128, 32, 8])
        thr, r_thr = sb(stR, "thr", [128, 64])
        iotab, r_iotab = sb(stR, "iotab", [128, 128])
        pid, r_pid = sb(stR, "pid", [128, 1])
        e64, r_e64 = sb(stR, "e64", [128, 6, 64])
        bef, r_bef = sb(stR, "bef", [128, 128])
        big, r_big = sb(stR, "big", [128, 4096])
        sp.dma([(thr[:], thr_d[:, :])], [], [r_thr], r_thr)
        sp.dma([(iotab[:], iotab_d[:, :])], [], [r_iotab], r_iotab)
        sp.dma([(pid[:], pid_d[:, :])], [], [r_pid], r_pid)
        dve.op(lambda: V.tensor_reduce(out=gmax, in_=Lg[:, :, 0:8], axis=AX.X, op=ALU.max), [r_Lg], [r_rsm])
        dve.op(lambda: V.tensor_tensor(out=gm[:], in0=Lg[:, :, 0:8], in1=gmax.unsqueeze(2).broadcast_to([128, 32, 8]),
                                       op=ALU.subtract), [r_Lg, r_rsm], [r_gm])
        dve.op(lambda: V.tensor_single_scalar(out=ohg[:], in_=gm[:], scalar=0.0, op=ALU.is_ge), [r_gm], [r_ohg])
        act.op(lambda: nc.scalar.activation(out=gm[:], in_=gm[:], func=AF.Exp), [r_gm], [r_gm])
        dve.op(lambda: V.tensor_reduce(out=pgt, in_=gm[:], axis=AX.X, op=ALU.add), [r_gm], [r_rsm])
        dve.op(lambda: V.reciprocal(out=pgt, in_=pgt), [r_rsm], [r_rsm])
        dve.op(lambda: V.tensor_scalar(out=ohg[:], in0=ohg[:], scalar1=-1.0, scalar2=BIG, op0=ALU.add, op1=ALU.mult),
               [r_ohg], [r_ohg])
        dve.op(lambda: V.tensor_tensor(
            out=wk[:].rearrange("p t (g e) -> p t g e", e=8), in0=Lg[:, :, 8:72].rearrange("p t (g e) -> p t g e", e=8),
            in1=ohg[:].unsqueeze(3).broadcast_to([128, 32, 8, 8]), op=ALU.add), [r_Lg, r_ohg], [r_wk])
        dve.op(lambda: V.tensor_reduce(out=m1, in_=wk[:], axis=AX.X, op=ALU.max), [r_wk], [r_rsm])
        dve.op(lambda: V.tensor_tensor(out=oh1[:], in0=wk[:], in1=m1.unsqueeze(2).broadcast_to([128, 32, 64]),
                                       op=ALU.is_ge), [r_wk, r_rsm], [r_oh1])
        dve.op(lambda: V.scalar_tensor_tensor(out=wk[:], in0=oh1[:], scalar=-BIG, in1=wk[:], op0=ALU.mult,
                                              op1=ALU.add), [r_oh1, r_wk], [r_wk])
        dve.op(lambda: V.tensor_reduce(out=m2, in_=wk[:], axis=AX.X, op=ALU.max), [r_wk], [r_rsm])
        dve.op(lambda: V.tensor_tensor(out=oh2[:], in0=wk[:], in1=m2.unsqueeze(2).broadcast_to([128, 32, 64]),
                                       op=ALU.is_ge), [r_wk, r_rsm], [r_oh2])
        dve.op(lambda: V.tensor_tensor(out=pe1, in0=m2, in1=m1, op=ALU.subtract), [r_rsm], [r_rsm])
        act.op(lambda: nc.scalar.activation(out=pe1, in_=pe1, func=AF.Exp), [r_rsm], [r_rsm])
        dve.op(lambda: V.tensor_scalar(out=pe1, in0=pe1, scalar1=1.0, scalar2=None, op0=ALU.add), [r_rsm], [r_rsm])
        dve.op(lambda: V.reciprocal(out=pe1, in_=pe1), [r_rsm], [r_rsm])
        dve.op(lambda: V.tensor_scalar(out=pe2, in0=pe1, scalar1=-1.0, scalar2=1.0, op0=ALU.mult, op1=ALU.add),
               [r_rsm], [r_rsm])
        dve.op(lambda: V.tensor_tensor(out=pe1, in0=pe1, in1=pgt, op=ALU.mult), [r_rsm], [r_rsm])
        dve.op(lambda: V.tensor_tensor(out=pe2, in0=pe2, in1=pgt, op=ALU.mult), [r_rsm], [r_rsm])
        dve.op(lambda: V.tensor_tensor(out=ohb[:], in0=oh1[:], in1=oh2[:], op=ALU.add), [r_oh1, r_oh2], [r_ohb])
        dve.op(lambda: V.tensor_tensor(out=triSb[:], in0=tri[:], in1=identf[:], op=ALU.subtract),
               [r_tri, r_identf], [r_triSb])
        for q4 in range(4):
            def mm(q4=q4):
                return nc.tensor.matmul(ps[q4][:, :], lhsT=triSb[:], rhs=ohb[:, 8 * q4:8 * q4 + 8, :],
                                        start=True, stop=True)
            pe.op(mm, [r_triSb, r_ohb], [psr[q4]])
            act.op(lambda q4=q4: nc.scalar.activation(
                out=cum[:, 8 * q4:8 * q4 + 8, :].rearrange("p t e -> p (t e)"), in_=ps[q4][:, :], func=AF.Copy),
                [psr[q4]], [r_cum])
        for q4 in range(4):
            def mm(q4=q4):
                return nc.tensor.matmul(ps[q4][:, :], lhsT=onesb[:], rhs=ohb[:, 8 * q4:8 * q4 + 8, :],
                                        start=True, stop=True)
            pe.op(mm, [r_onesb, r_ohb], [psr[q4]])
            act.op(lambda q4=q4: nc.scalar.activation(
                out=cs[:, 8 * q4:8 * q4 + 8, :].rearrange("p t e -> p (t e)"), in_=ps[q4][:, :], func=AF.Copy),
                [psr[q4]], [r_cs])
        cnt = e64[:, 0, :]
        nb = e64[:, 1, :]
        ci = e64[:, 2, :]
        sbase = e64[:, 3, :]
        ones64 = e64[:, 4, :]
        dve.op(lambda: V.memset(e64[:], 0.0), [], [r_e64])
        dve.op(lambda: V.memset(ones64, 1.0), [r_e64], [r_e64])
        for t in range(32):
            dve.op(lambda t=t: V.tensor_tensor(out=cum[:, t, :], in0=cum[:, t, :], in1=cnt, op=ALU.add),
                   [r_cum, r_e64], [r_cum])
            dve.op(lambda t=t: V.tensor_tensor(out=cnt, in0=cnt, in1=cs[:, t, :], op=ALU.add), [r_cs, r_e64], [r_e64])
        dve.op(lambda: V.tensor_tensor(out=big[:].rearrange("p (e j) -> p e j", j=64),
                                       in0=cnt.unsqueeze(2).broadcast_to([128, 64, 64]),
                                       in1=thr[:].unsqueeze(1).broadcast_to([128, 64, 64]), op=ALU.is_gt),
               [r_e64, r_thr], [r_big])
        dve.op(lambda: V.tensor_reduce(out=nb, in_=big[:].rearrange("p (e j) -> p e j", j=64), axis=AX.X, op=ALU.add),
               [r_big], [r_e64])
        dve.op(lambda: V.tensor_tensor_scan(out=ci, data0=ones64, data1=nb, initial=0.0, op0=ALU.mult, op1=ALU.add),
               [r_e64], [r_e64])
        dve.op(lambda: V.tensor_tensor(out=sbase, in0=ci, in1=nb, op=ALU.subtract), [r_e64], [r_e64])
        dve.op(lambda: V.tensor_scalar(out=sbase, in0=sbase, scalar1=128.0, scalar2=None, op0=ALU.mult),
               [r_e64], [r_e64])
        dve.op(lambda: V.tensor_tensor(out=cum[:], in0=cum[:], in1=sbase.unsqueeze(1).broadcast_to([128, 32, 64]),
                                       op=ALU.add), [r_cum, r_e64], [r_cum])
        dve.op(lambda: V.tensor_tensor(out=wk[:], in0=cum[:], in1=oh1[:], op=ALU.mult), [r_cum, r_oh1], [r_wk])
        dve.op(lambda: V.tensor_reduce(out=d1f, in_=wk[:], axis=AX.X, op=ALU.add), [r_wk], [r_rsm])
        dve.op(lambda: V.tensor_tensor(out=wk[:], in0=cum[:], in1=oh2[:], op=ALU.mult), [r_cum, r_oh2], [r_wk])
        dve.op(lambda: V.tensor_reduce(out=d2f, in_=wk[:], axis=AX.X, op=ALU.add), [r_wk], [r_rsm])
        dve.op(lambda: V.tensor_copy(out=d1i[:], in_=d1f), [r_rsm], [r_d1i])
        dve.op(lambda: V.tensor_copy(out=d2i[:], in_=d2f), [r_rsm], [r_d2i])
        for hb in range(2):
            dve.op(lambda hb=hb: V.tensor_tensor(
                out=big[:].rearrange("p (b e) -> p b e", e=64),
                in0=ci.unsqueeze(1).broadcast_to([128, 64, 64]),
                in1=iotab[:, 64 * hb:64 * hb + 64].unsqueeze(2).broadcast_to([128, 64, 64]), op=ALU.is_le),
                [r_e64, r_iotab], [r_big])
            dve.op(lambda hb=hb: V.tensor_reduce(
                out=bef[:, 64 * hb:64 * hb + 64], in_=big[:].rearrange("p (b e) -> p b e", e=64),
                axis=AX.X, op=ALU.add), [r_big], [r_bef])
        dve.op(lambda: V.tensor_scalar(out=bef[:], in0=bef[:], scalar1=63.0, scalar2=None, op0=ALU.min),
               [r_bef], [r_bef])
        dve.op(lambda: V.memset(big[:, 0:128], 0.0), [r_big], [r_big])
        dve.op(lambda: V.tensor_tensor(out=big[:, 1:128], in0=bef[:, 1:128], in1=bef[:, 0:127], op=ALU.is_equal),
               [r_bef, r_big], [r_big])
        dve.op(lambda: V.tensor_scalar(out=bef[:], in0=bef[:], scalar1=128.0, scalar2=None, op0=ALU.mult),
               [r_bef], [r_bef])
        dve.op(lambda: V.scalar_tensor_tensor(out=bef[:], in0=big[:, 0:128], scalar=1.0e6, in1=bef[:], op0=ALU.mult,
                                              op1=ALU.add), [r_big, r_bef], [r_bef])
        dve.op(lambda: V.tensor_scalar(out=bef[:], in0=bef[:], scalar1=pid[:, 0:1], scalar2=None, op0=ALU.add),
               [r_bef, r_pid], [r_bef])
        dve.op(lambda: V.tensor_copy(out=widx[:], in_=bef[:]), [r_bef], [r_widx])
        barrier()
        stR.close()
        stMid.close()
        if stage == "R":
            dbg, r_dbg = sb(stM, "dbgR", [128, 256])
            dve.op(lambda: V.tensor_copy(out=dbg[:, 0:32], in_=d1i[:]), [r_d1i], [r_dbg])
            dve.op(lambda: V.tensor_copy(out=dbg[:, 32:64], in_=d2i[:]), [r_d2i], [r_dbg])
            dve.op(lambda: V.tensor_copy(out=dbg[:, 64:128], in_=rsm[:, 128:192]), [r_rsm], [r_dbg])
            dve.op(lambda: V.tensor_copy(out=dbg[:, 128:256], in_=widx[:]), [r_widx], [r_dbg])
            sp.dma([(out_d[0:128, 0:256], dbg[:])], [r_dbg], [], r_dbg)
            sp.deps([], [r_dbg])
            barrier()
            raise EarlyExit()
        r_Xs, r_Ys = Res("Xs"), Res("Ys")
        with contextlib.ExitStack() as stS:
            hb2 = [sb(stS, f"hb2_{i}", [128, D], BF16) for i in range(2)]
            for t in range(32):
                tl, r_tl = hb2[t % 2]
                sp.dma([(tl[:], H2_d[t * 128:(t + 1) * 128, :])], [r_H2T], [r_tl], r_tl)
                for (di, r_di) in ((d1i, r_d1i), (d2i, r_d2i)):
                    pool.deps([r_tl, r_di], [])
                    if r_tl.dsem is None:
                        raise RuntimeError("no dsem")
                    pool.wait({id(r_tl.dsem): (r_tl.dsem, r_tl.dcnt)})
                    nc.gpsimd.indirect_dma_start(
                        out=Xs_d[:, :], out_offset=bass.IndirectOffsetOnAxis(ap=di[:, t:t + 1], axis=0),
                        in_=tl[:, :], in_offset=None).then_inc(r_tl.dsem, 16)
                    r_tl.dcnt += 16
                    pool.mark([r_tl, r_di], [r_Xs], r_tl.dsem, r_tl.dcnt)
            barrier()
        with contextlib.ExitStack() as stE:
            stgW, r_stgW = sb(stE, "stgW", [128, 3 * 4096])
            wbf = []
            for i in range(2):
                a1, ra1 = sb(stE, f"w1b{i}", [128, 8, 512], BF16)
                a3, ra3 = sb(stE, f"w3b{i}", [128, 8, 512], BF16)
                a2, ra2 = sb(stE, f"w2b{i}", [128, 4, 1024], BF16)
                wbf.append((a1, ra1, a3, ra3, a2, ra2))
            xblk = [sb(stE, f"xblk{i}", [128, D], BF16) for i in range(2)]
            XT, r_XT = sb(stE, "XT", [128, 8, 128], BF16)
            sgE, r_sgE = sb(stE, "sgE", [128, 512])
            actb, r_actb = sb(stE, "actb", [128, 4, 128], BF16)
            yb = [sb(stE, f"yb{i}", [128, D]) for i in range(2)]
            bc_reg = nc.gpsimd.alloc_register("bc_reg")
            nc.gpsimd.reg_mov(bc_reg, nexp_decl * 128 - 1)
            r_stgW.dsem = nc.alloc_semaphore(name=f"d{len(ALL_DMA_RES)}_stgW")
            ALL_DMA_RES.append(r_stgW)
            for b in range(128):
                a1, ra1, a3, ra3, a2, ra2 = wbf[b % 2]
                pool.deps([r_widx], [r_stgW])
                if r_stgW.dcnt:
                    pool.wait({id(r_stgW.dsem): (r_stgW.dsem, r_stgW.dcnt)})
                nc.gpsimd.indirect_dma_start(
                    out=stgW[:, :], out_offset=None, in_=wall_d[:, :],
                    in_offset=bass.IndirectOffsetOnAxis(ap=widx[:, b:b + 1], axis=0),
                    bounds_check=bc_reg, oob_is_err=False).then_inc(r_stgW.dsem, 16)
                r_stgW.dcnt += 16
                pool.mark([r_widx], [r_stgW], r_stgW.dsem, r_stgW.dcnt)
                dve.op(lambda a1=a1: V.tensor_copy(out=a1[:].rearrange("p k n -> p (k n)"), in_=stgW[:, 0:4096]),
                       [r_stgW], [ra1])
                act.op(lambda a2=a2: nc.scalar.activation(out=a2[:].rearrange("p k n -> p (k n)"),
                                                          in_=stgW[:, 8192:12288], func=AF.Copy), [r_stgW], [ra2])
                dve.op(lambda a3=a3: V.tensor_copy(out=a3[:, 0:4, :].rearrange("p k n -> p (k n)"),
                                                   in_=stgW[:, 4096:6144]), [r_stgW], [ra3])
                pool.op(lambda a3=a3: nc.gpsimd.tensor_copy(out=a3[:, 4:8, :].rearrange("p k n -> p (k n)"),
                                                            in_=stgW[:, 6144:8192]), [r_stgW, ra3], [ra3])
                xb_, r_xb = xblk[b % 2]
                sp.dma([(xb_[:], Xs_d[b * 128:(b + 1) * 128, :])], [r_Xs], [r_xb], r_xb)

                def mm(xb_=xb_):
                    ins = None
                    for c in range(8):
                        ins = nc.tensor.transpose(out=ps7b[:, c * 128:(c + 1) * 128], in_=xb_[:, c * 128:(c + 1) * 128],
                                                  identity=identb[:])
                    return ins
                pe.op(mm, [r_xb, r_identb], [psr[7]])
                act.op(lambda: nc.scalar.activation(out=XT[:].rearrange("p c t -> p (c t)"), in_=ps7b[:, :],
                                                    func=AF.Copy), [psr[7]], [r_XT])
                pg_, pu_ = (0, 1) if b % 2 == 0 else (2, 3)

                def mm(a1=a1, a3=a3, pg_=pg_, pu_=pu_):
                    ins = None
                    for hc in range(4):
                        for k in range(8):
                            ins = nc.tensor.matmul(ps[pg_][:, hc * 128:(hc + 1) * 128],
                                                   lhsT=a1[:, k, hc * 128:(hc + 1) * 128], rhs=XT[:, k, :],
                                                   start=(k == 0), stop=(k == 7))
                    for hc in range(4):
                        for k in range(8):
                            ins = nc.tensor.matmul(ps[pu_][:, hc * 128:(hc + 1) * 128],
                                                   lhsT=a3[:, k, hc * 128:(hc + 1) * 128], rhs=XT[:, k, :],
                                                   start=(k == 0), stop=(k == 7))
                    return ins
                pe.op(mm, [ra1, ra3, r_XT], [psr[pg_], psr[pu_]])
                act.op(lambda pg_=pg_: nc.scalar.activation(out=sgE[:], in_=ps[pg_][:, :], func=AF.Silu),
                       [psr[pg_]], [r_sgE])
                dve.op(lambda pu_=pu_: V.tensor_tensor(out=actb[:].rearrange("p c t -> p (c t)"), in0=ps[pu_][:, :],
                                                       in1=sgE[:], op=ALU.mult), [psr[pu_], r_sgE], [r_actb])
                yb_, r_yb = yb[b % 2]
                for hf in range(2):
                    py = 4 + hf

                    def mm(hf=hf, py=py, a2=a2):
                        ins = None
                        for hc in range(4):
                            ins = nc.tensor.matmul(ps[py][:, :], lhsT=actb[:, hc, :],
                                                   rhs=a2[:, hc, hf * 512:(hf + 1) * 512],
                                                   start=(hc == 0), stop=(hc == 3))
                        return ins
                    pe.op(mm, [r_actb, ra2], [psr[py]])
                    act.op(lambda hf=hf, py=py, yb_=yb_: nc.scalar.activation(
                        out=yb_[:, hf * 512:(hf + 1) * 512], in_=ps[py][:, :], func=AF.Copy), [psr[py]], [r_yb])
                sp.dma([(Ys_d[b * 128:(b + 1) * 128, :], yb_[:])], [r_yb], [r_Ys], r_yb)
            barrier()
        with contextlib.ExitStack() as stF:
            ln2g, r_ln2g = sb(stF, "ln2g", [128, D])
            ln2b, r_ln2b = sb(stF, "ln2b", [128, D])
            x1t, r_x1t = sb(stF, "x1t", [128, D])
            fo, r_fo = sb(stF, "fo", [128, D])
            ya = [sb(stF, f"ya{i}", [128, D]) for i in range(2)]
            bst2, r_bst2 = sb(stF, "bst2", [128, 2, 6])
            sm2, r_sm2 = sb(stF, "sm2", [128, 4])
            sp.dma([(ln2g[:], ln2g_d[:, :])], [], [r_ln2g], r_ln2g)
            sp.dma([(ln2b[:], ln2b_d[:, :])], [], [r_ln2b], r_ln2b)
            for t in range(32):
                s_ = t // 16
                t0 = t * 128
                g2row = modg2[:, s_, :]
                for (yt_, r_yt), (di, r_di) in zip(ya, ((d1i, r_d1i), (d2i, r_d2i))):
                    pool.deps([r_di, r_Ys], [r_yt])
                    if r_yt.dsem is None:
                        r_yt.dsem = nc.alloc_semaphore(name=f"d{len(ALL_DMA_RES)}_" + r_yt.name)
                        ALL_DMA_RES.append(r_yt)
                    if r_yt.dcnt:
                        pool.wait({id(r_yt.dsem): (r_yt.dsem, r_yt.dcnt)})
                    nc.gpsimd.indirect_dma_start(
                        out=yt_[:, :], out_offset=None, in_=Ys_d[:, :],
                        in_offset=bass.IndirectOffsetOnAxis(ap=di[:, t:t + 1], axis=0)).then_inc(r_yt.dsem, 16)
                    r_yt.dcnt += 16
                    pool.mark([r_di, r_Ys], [r_yt], r_yt.dsem, r_yt.dcnt)
                sp.dma([(x1t[:], X1_d[t0:t0 + 128, :])], [r_X1], [r_x1t], r_x1t)
                dve.op(lambda t=t: V.tensor_scalar(out=fo[:], in0=ya[0][0][:], scalar1=pe1[:, t:t + 1], scalar2=None,
                                                   op0=ALU.mult), [ya[0][1], r_rsm], [r_fo])
                dve.op(lambda t=t: V.scalar_tensor_tensor(out=fo[:], in0=ya[1][0][:], scalar=pe2[:, t:t + 1], in1=fo[:],
                                                          op0=ALU.mult, op1=ALU.add), [ya[1][1], r_rsm, r_fo], [r_fo])
                dve.op(lambda g2row=g2row: V.tensor_tensor(out=fo[:], in0=fo[:], in1=g2row, op=ALU.mult),
                       [r_fo, r_modg2], [r_fo])
                dve.op(lambda: V.scalar_tensor_tensor(out=fo[:], in0=x1t[:], scalar=ALPHA, in1=fo[:], op0=ALU.mult,
                                                      op1=ALU.add), [r_x1t, r_fo], [r_fo])
                dve.op(lambda: V.bn_stats(out=bst2[:, 0, :], in_=fo[:, 0:512]), [r_fo], [r_bst2])
                dve.op(lambda: V.bn_stats(out=bst2[:, 1, :], in_=fo[:, 512:1024]), [r_fo], [r_bst2])
                dve.op(lambda: V.bn_aggr(out=sm2[:, 0:2], in_=bst2[:].rearrange("p a b -> p (a b)")),
                       [r_bst2], [r_sm2])
                dve.op(lambda: V.tensor_scalar(out=sm2[:, 2:3], in0=sm2[:, 1:2], scalar1=EPS, scalar2=None,
                                               op0=ALU.add), [r_sm2], [r_sm2])
                act.op(lambda: nc.scalar.activation(out=sm2[:, 2:3], in_=sm2[:, 2:3], func=AF.Sqrt), [r_sm2], [r_sm2])
                dve.op(lambda: V.reciprocal(out=sm2[:, 2:3], in_=sm2[:, 2:3]), [r_sm2], [r_sm2])
                dve.op(lambda: V.tensor_scalar(out=fo[:], in0=fo[:], scalar1=sm2[:, 0:1], scalar2=sm2[:, 2:3],
                                               op0=ALU.subtract, op1=ALU.mult), [r_fo, r_sm2], [r_fo])
                dve.op(lambda: V.tensor_tensor(out=fo[:], in0=fo[:], in1=ln2g[:], op=ALU.mult), [r_fo, r_ln2g], [r_fo])
                dve.op(lambda: V.tensor_tensor(out=fo[:], in0=fo[:], in1=ln2b[:], op=ALU.add), [r_fo, r_ln2b], [r_fo])
                sp.dma([(out_d[t0:t0 + 128, :], fo[:])], [r_fo], [], r_fo)
            sp.deps([], [r_fo])
            barrier()
    return nc


def make_inputs(core, x, c, w_ada, b_ada, w_in, conf_conv_w, conf_conv_b, conf_ln_g, conf_ln_b,
                ssm_conv_w, ssm_conv_b, ssm_dt_bias, ssm_A_log, ssm_D, ssm_norm_w, w_out,
                ln1_g, ln1_b, router_group_w, router_group_b, router_expert_w, router_expert_b,
                expert_w1, expert_w3, expert_w2, ln2_g, ln2_b, shared):
    f = np.float32
    xc = np.ascontiguousarray(x[2 * core:2 * core + 2].reshape(NTOK, D), dtype=f)
    cc = c[2 * core:2 * core + 2]
    m = dict(shared)
    m["x"] = xc
    m["xT"] = np.ascontiguousarray(xc.T)
    m["cT"] = np.ascontiguousarray(cc.reshape(2, 8, 128).transpose(2, 1, 0), dtype=f)
    return m


def rep(v, n=128):
    return np.ascontiguousarray(np.broadcast_to(np.asarray(v, dtype=np.float32).reshape(1, -1), (n, v.size)))


def fm(v, nch):
    return np.ascontiguousarray(np.asarray(v, dtype=np.float32).reshape(nch, 128).T)


def shared_inputs(w_ada, b_ada, w_in, conf_conv_w, conf_conv_b, conf_ln_g, conf_ln_b,
                  ssm_conv_w, ssm_conv_b, ssm_dt_bias, ssm_A_log, ssm_D, ssm_norm_w, w_out,
                  ln1_g, ln1_b, router_group_w, router_group_b, router_expert_w, router_expert_b,
                  expert_w1, expert_w3, expert_w2, ln2_g, ln2_b):
    f = np.float32
    sh = {}
    sh["w_ada"] = np.ascontiguousarray(w_ada[0], dtype=f)
    sh["b_ada_fm"] = fm(b_ada[0][:2048], 16)
    sh["b_ada_row"] = rep(b_ada[0][2048:])
    sh["w_in"] = np.ascontiguousarray(w_in[0], dtype=f)
    sh["ccw"] = np.ascontiguousarray(conf_conv_w[0].reshape(31, 8, 128).transpose(2, 1, 0), dtype=f)
    sh["ccb"] = fm(conf_conv_b[0], 8)
    sh["clg"] = fm(conf_ln_g[0], 8)
    sh["clb"] = fm(conf_ln_b[0], 8)
    sh["scw"] = np.ascontiguousarray(ssm_conv_w[0].reshape(4, 16, 128).transpose(2, 1, 0), dtype=f)
    sh["scb"] = fm(ssm_conv_b[0], 16)
    sh["dtb"] = rep(ssm_dt_bias[0])
    sh["alog"] = rep(ssm_A_log[0])
    sh["dsk"] = rep(ssm_D[0])
    sh["normw"] = rep(ssm_norm_w[0])
    sh["w_out"] = np.ascontiguousarray(w_out[0], dtype=f)
    sh["ln1g"] = rep(ln1_g[0])
    sh["ln1b"] = rep(ln1_b[0])
    sh["ln2g"] = rep(ln2_g[0])
    sh["ln2b"] = rep(ln2_b[0])
    wrr = np.concatenate([router_group_w[0], router_expert_w[0]], axis=1).astype(f)
    sh["wr"] = np.ascontiguousarray(wrr.reshape(8, 128, 72).transpose(1, 0, 2))
    sh["rb"] = rep(np.concatenate([router_group_b[0], router_expert_b[0]]))
    wall = np.empty((NEXP * 128, 3 * 4096), dtype=f)
    wall[:, 0:4096] = np.asarray(expert_w1[0], dtype=f).reshape(NEXP, 8, 128, 512).transpose(0, 2, 1, 3).reshape(NEXP * 128, 4096)
    wall[:, 4096:8192] = np.asarray(expert_w3[0], dtype=f).reshape(NEXP, 8, 128, 512).transpose(0, 2, 1, 3).reshape(NEXP * 128, 4096)
    wall[:, 8192:12288] = np.asarray(expert_w2[0], dtype=f).reshape(NEXP, 4, 128, 1024).transpose(0, 2, 1, 3).reshape(NEXP * 128, 4096)
    sh["wall"] = wall
    sh["thr"] = rep(np.arange(64, dtype=f) * 128.0)
    sh["iotab"] = rep(np.arange(128, dtype=f))
    sh["pid"] = np.arange(128, dtype=f).reshape(128, 1)
    sh["identf"] = np.eye(128, dtype=f)
    sh["tri"] = np.triu(np.ones((128, 128), dtype=f))
    nm = np.where(np.triu(np.ones((128, 128), dtype=bool)), 0.0, -30000.0).astype(f)
    sh["negm"] = np.ascontiguousarray(np.tile(nm, (1, 4)))
    return sh


def kernel(x, c, w_ada, b_ada, w_in, conf_conv_w, conf_conv_b, conf_ln_g, conf_ln_b,
           ssm_conv_w, ssm_conv_b, ssm_dt_bias, ssm_A_log, ssm_D, ssm_norm_w, w_out,
           ln1_g, ln1_b, router_group_w, router_group_b, router_expert_w, router_expert_b,
           expert_w1, expert_w3, expert_w2, ln2_g, ln2_b, _stage="full", _trace=False):
    args = [np.asarray(a) for a in (w_ada, b_ada, w_in, conf_conv_w, conf_conv_b, conf_ln_g, conf_ln_b,
                                    ssm_conv_w, ssm_conv_b, ssm_dt_bias, ssm_A_log, ssm_D, ssm_norm_w, w_out,
                                    ln1_g, ln1_b, router_group_w, router_group_b, router_expert_w, router_expert_b,
                                    expert_w1, expert_w3, expert_w2, ln2_g, ln2_b)]
    x = np.asarray(x)
    c = np.asarray(c)
    sh = shared_inputs(*args)
    if _stage != "full":
        sh["wall"] = np.ascontiguousarray(sh["wall"][0:128])
    in_maps = []
    for core in range(8):
        m = dict(sh)
        xc = np.ascontiguousarray(x[2 * core:2 * core + 2].reshape(NTOK, D), dtype=np.float32)
        m["x"] = xc
        m["xT"] = np.ascontiguousarray(xc.T)
        m["cT"] = np.ascontiguousarray(c[2 * core:2 * core + 2].reshape(2, 8, 128).transpose(2, 1, 0),
                                       dtype=np.float32)
        in_maps.append(m)
    nc = build_nc(_stage)
    if _trace:
        res = run_bass_kernel_spmd(nc, in_maps, core_ids=list(range(8)), trace=True)
        print("EXEC_TIME_NS", _stage, res.exec_time_ns)
    else:
        res = run_bass_kernel_spmd(nc, in_maps, core_ids=list(range(8)))
    outs = [np.asarray(r["out"], dtype=np.float32).reshape(2, SEQ, D) for r in res.results]
    return np.concatenate(outs, axis=0)
```

```python
import contextlib
import numpy as np
import concourse.bass as bass
import concourse.mybir as mybir
from concourse.bass_utils import run_bass_kernel_spmd

F32 = mybir.dt.float32
BF16 = mybir.dt.bfloat16
AF = mybir.ActivationFunctionType
ALU = mybir.AluOpType
AX = mybir.AxisListType

ALPHA = 2.0 ** 0.25
EPS = 1e-5
NTOK = 4096
SEQ = 2048
D = 1024
NEXP = 64
BIG = 1.0e9


ALL_DMA_RES = []


class Res:
    __slots__ = ("name", "wr", "rd", "dsem", "dcnt")

    def __init__(self, name):
        self.name = name
        self.wr = {}
        self.rd = {}
        self.dsem = None
        self.dcnt = 0


class Eng:
    def __init__(self, nc, eng, name):
        self.nc = nc
        self.eng = eng
        self.sem = nc.alloc_semaphore(name=name)
        self.cnt = 0
        self.waited = {}

    def wait(self, evs):
        for key, (sem, val) in list(evs.items()):
            if self.waited.get(key, 0) < val:
                self.eng.wait_ge(sem, val)
                self.waited[key] = val

    def deps(self, reads, writes):
        for r in reads:
            self.wait(r.wr)
        for w in writes:
            self.wait(w.wr)
            self.wait(w.rd)

    def mark(self, reads, writes, sem, val):
        key = id(sem)
        for r in reads:
            r.rd[key] = (sem, val)
        for w in writes:
            w.wr = {key: (sem, val)}
            w.rd = {}

    def op(self, fn, reads=(), writes=()):
        self.deps(reads, writes)
        ins = fn()
        self.cnt += 1
        ins.then_inc(self.sem, 1)
        self.mark(reads, writes, self.sem, self.cnt)

    def dma(self, parts, reads, writes, sres):
        self.deps(reads, writes)
        if sres.dsem is None:
            sres.dsem = self.nc.alloc_semaphore(name=f"d{len(ALL_DMA_RES)}_" + sres.name)
            ALL_DMA_RES.append(sres)
        if sres.dcnt:
            self.wait({id(sres.dsem): (sres.dsem, sres.dcnt)})
        for (o, i) in parts:
            self.eng.dma_start(out=o, in_=i).then_inc(sres.dsem, 16)
            sres.dcnt += 16
        self.mark(reads, writes, sres.dsem, sres.dcnt)


class EarlyExit(Exception):
    pass


def build_nc(stage="full"):
    nc = bass.Bass("TRN2", target_bir_lowering=False)
    ALL_DMA_RES.clear()
    st_all = contextlib.ExitStack()
    try:
        with st_all:
            _build(nc, stage, st_all)
    except EarlyExit:
        pass
    return nc


def _build(nc, stage, st_all):

    def din(name, shape, dt=F32):
        return nc.dram_tensor(name, list(shape), dt, kind="ExternalInput").ap()

    x_d = din("x", [NTOK, D])
    xT_d = din("xT", [D, NTOK])
    cT_d = din("cT", [128, 8, 2])
    wada_d = din("w_ada", [D, 6 * D])
    bada_fm_d = din("b_ada_fm", [128, 16])
    bada_row_d = din("b_ada_row", [128, 4 * D])
    win_d = din("w_in", [D, 5136])
    ccw_d = din("ccw", [128, 8, 31])
    ccb_d = din("ccb", [128, 8])
    clg_d = din("clg", [128, 8])
    clb_d = din("clb", [128, 8])
    scw_d = din("scw", [128, 16, 4])
    scb_d = din("scb", [128, 16])
    dtb_d = din("dtb", [128, 16])
    alog_d = din("alog", [128, 16])
    dsk_d = din("dsk", [128, 16])
    normw_d = din("normw", [128, D])
    wout_d = din("w_out", [2 * D, D])
    ln1g_d = din("ln1g", [128, D])
    ln1b_d = din("ln1b", [128, D])
    ln2g_d = din("ln2g", [128, D])
    ln2b_d = din("ln2b", [128, D])
    wr_d = din("wr", [128, 8, 72])
    rb_d = din("rb", [128, 72])
    nexp_decl = NEXP if stage == "full" else 1
    w1_d = din("w1", [nexp_decl * 128, 4096])
    w3_d = din("w3", [nexp_decl * 128, 4096])
    w2_d = din("w2", [nexp_decl * 128, 4096])
    thr_d = din("thr", [128, 64])
    iotab_d = din("iotab", [128, 128])
    pid_d = din("pid", [128, 1])
    identf_d = din("identf", [128, 128])
    tri_d = din("tri", [128, 128])
    negm_d = din("negm", [128, 512])
    out_d = nc.dram_tensor("out", [NTOK, D], F32, kind="ExternalOutput").ap()

    UF_d = nc.dram_tensor("UF", [D, NTOK], BF16, kind="Internal").ap()
    X1_d = nc.dram_tensor("X1", [NTOK, D], F32, kind="Internal").ap()
    H2_d = nc.dram_tensor("H2", [NTOK, D], BF16, kind="Internal").ap()
    Xs_d = nc.dram_tensor("Xs", [4 * NTOK, D], BF16, kind="Internal").ap()
    Ys_d = nc.dram_tensor("Ys", [4 * NTOK, D], F32, kind="Internal").ap()

    r_UF, r_X1, r_H2T, r_YN = Res("UF"), Res("X1"), Res("H2T"), Res("YN")
    YN_d = nc.dram_tensor("YN", [D, NTOK], BF16, kind="Internal").ap()
    pe = Eng(nc, nc.tensor, "s_pe")
    act = Eng(nc, nc.scalar, "s_act")
    dve = Eng(nc, nc.vector, "s_dve")
    pool = Eng(nc, nc.gpsimd, "s_pool")
    sp = Eng(nc, nc.sync, "s_sp")

    es = st_all
    engines = [pe, act, dve, pool, sp]

    def ck(n):
        if stage == f"K{n}":
            barrier()
            raise EarlyExit()

    def barrier():
        evs = {}
        for E in engines:
            if E.cnt:
                evs[id(E.sem)] = (E.sem, E.cnt)
        for r in ALL_DMA_RES:
            if r.dcnt:
                evs[id(r.dsem)] = (r.dsem, r.dcnt)
        for E in engines:
            E.wait(evs)

    cast_rr = [0]

    def load_cast(dst_aps, src_aps, stg, r_dst):
        for dst, src in zip(dst_aps, src_aps):
            t, r = stg[cast_rr[0] % len(stg)]
            n = dst.shape[-1]
            sp.dma([(t[:, 0:n], src)], [], [r], r)
            if cast_rr[0] % 2 == 0:
                act.op(lambda t=t, dst=dst, n=n: nc.scalar.activation(out=dst, in_=t[:, 0:n], func=AF.Copy), [r], [r_dst])
            else:
                pool.op(lambda t=t, dst=dst, n=n: nc.gpsimd.tensor_copy(out=dst, in_=t[:, 0:n]), [r], [r_dst])
            cast_rr[0] += 1

    sb_cnt = [0]

    def sb(st, name, shape, dt=F32):
        sb_cnt[0] += 1
        t = st.enter_context(nc.sbuf_tensor(f"sb{sb_cnt[0]}_" + name, list(shape), dt))
        return t, Res(name)

    ps = []
    psr = []
    for i in range(7):
        t = es.enter_context(nc.psum_tensor(f"ps{i}", [128, 512], F32))
        ps.append(t)
        psr.append(Res(f"ps{i}"))
    ps7b = es.enter_context(nc.psum_tensor("ps7b", [128, 1024], BF16))
    ps.append(None)
    psr.append(Res("ps7b"))

    def psbf(i):
        return ps7b[:]

    identf, r_identf = sb(es, "identf", [128, 128])
    identb, r_identb = sb(es, "identb", [128, 128], BF16)
    onesf, r_onesf = sb(es, "onesf", [128, 128])
    onesb, r_onesb = sb(es, "onesb", [128, 128], BF16)
    tri, r_tri = sb(es, "tri", [128, 128])
    negm, r_negm = sb(es, "negm", [128, 512])
    modg2, r_modg2 = sb(es, "modg2", [128, 2, D])
    rsm, r_rsm = sb(es, "rsm", [128, 32 * 8])
    d1i, r_d1i = sb(es, "d1i", [128, 32], mybir.dt.int32)
    d2i, r_d2i = sb(es, "d2i", [128, 32], mybir.dt.int32)
    widx, r_widx = sb(es, "widx", [128, 128], mybir.dt.int32)
    modfm, r_modfm = sb(es, "modfm", [128, 16, 2])
    ccw, r_ccw = sb(es, "ccw", [128, 8, 31])
    pfm, r_pfm = sb(es, "pfm", [128, 64])
    prow, r_prow = sb(es, "prow", [128, 64])
    rb, r_rb = sb(es, "rb", [128, 72])
    stMid = es.enter_context(contextlib.ExitStack())
    modrow, r_modrow = sb(stMid, "modrow", [128, 2, 3 * D])
    Lg, r_Lg = sb(stMid, "Lg", [128, 32, 72])

    sp.dma([(identf[:], identf_d[:, :])], [], [r_identf], r_identf)
    sp.dma([(tri[:], tri_d[:, :])], [], [r_tri], r_tri)
    sp.dma([(negm[:], negm_d[:, :])], [], [r_negm], r_negm)
    sp.dma([(ccw[:], ccw_d[:, :, :])], [], [r_ccw], r_ccw)
    sp.dma([(pfm[:, 0:8], ccb_d[:, :]), (pfm[:, 8:16], clg_d[:, :]), (pfm[:, 16:24], clb_d[:, :]),
            (pfm[:, 24:40], scb_d[:, :])], [], [r_pfm], r_pfm)
    sp.dma([(prow[:, 0:16], dtb_d[:, :]), (prow[:, 48:64], alog_d[:, :]), (prow[:, 32:48], dsk_d[:, :])],
           [], [r_prow], r_prow)
    sp.dma([(rb[:], rb_d[:, :])], [], [r_rb], r_rb)
    dve.op(lambda: nc.vector.tensor_copy(out=identb[:], in_=identf[:]), [r_identf], [r_identb])
    dve.op(lambda: nc.vector.memset(onesf[:], 1.0), [], [r_onesf])
    dve.op(lambda: nc.vector.memset(onesb[:], 1.0), [], [r_onesb])
    act.op(lambda: nc.scalar.activation(out=prow[:, 16:32], in_=prow[:, 48:64], func=AF.Exp), [r_prow], [r_prow])
    dve.op(lambda: nc.vector.tensor_scalar(out=prow[:, 16:32], in0=prow[:, 16:32], scalar1=-1.0, scalar2=None,
                                           op0=ALU.mult), [r_prow], [r_prow])

    with contextlib.ExitStack() as st0:
        cT, r_cT = sb(st0, "cT", [128, 8, 2])
        crep, r_crep = sb(st0, "crep", [128, 2, 8, 128])
        bfm, r_bfm = sb(st0, "bfm", [128, 16])
        brow, r_brow = sb(st0, "brow", [128, 4 * D])
        slabA = [sb(st0, f"slabA{i}", [128, 8, 1024]) for i in range(2)]
        slabB = [sb(st0, f"slabB{i}", [128, 8, 512]) for i in range(2)]
        sp.dma([(cT[:], cT_d[:, :, :])], [], [r_cT], r_cT)
        sp.dma([(bfm[:], bada_fm_d[:, :])], [], [r_bfm], r_bfm)
        sp.dma([(brow[:], bada_row_d[:, :])], [], [r_brow], r_brow)
        act.op(lambda: nc.scalar.activation(out=cT[:], in_=cT[:], func=AF.Silu), [r_cT], [r_cT])
        for b in range(2):
            dve.op(lambda b=b: nc.vector.tensor_copy(
                out=crep[:, b, :, :], in_=cT[:, :, b:b + 1].broadcast_to([128, 8, 128])), [r_cT], [r_crep])
        for s in range(2):
            t, r = slabA[s]
            sp.dma([(t[:, k, :], wada_d[k * 128:(k + 1) * 128, s * 1024:(s + 1) * 1024]) for k in range(8)],
                   [], [r], r)
            for j in range(8):
                fc = s * 8 + j

                def mm(fc=fc, j=j, t=t):
                    ins = None
                    for k in range(8):
                        ins = nc.tensor.matmul(ps[0][:, 2 * fc:2 * fc + 2], lhsT=t[:, k, j * 128:(j + 1) * 128],
                                               rhs=cT[:, k, :], start=(k == 0), stop=(k == 7))
                    return ins
                pe.op(mm, [r, r_cT], [psr[0]])
        dve.op(lambda: nc.vector.tensor_tensor(
            out=modfm[:], in0=ps[0][:, 0:32].rearrange("p (f b) -> p f b", b=2),
            in1=bfm[:].unsqueeze(2).broadcast_to([128, 16, 2]), op=ALU.add), [psr[0], r_bfm], [r_modfm])
        dve.op(lambda: nc.vector.tensor_scalar(out=modfm[:, 8:16, :], in0=modfm[:, 8:16, :], scalar1=1.0,
                                               scalar2=None, op0=ALU.add), [r_modfm], [r_modfm])
        for tsl in range(8):
            t, r = slabB[tsl % 2]
            c0 = 2048 + tsl * 512
            sp.dma([(t[:, k, :], wada_d[k * 128:(k + 1) * 128, c0:c0 + 512]) for k in range(8)], [], [r], r)
            for b in range(2):
                pb = 1 + b

                def mm(b=b, t=t, pb=pb):
                    ins = None
                    for k in range(8):
                        ins = nc.tensor.matmul(ps[pb][:, :], lhsT=crep[:, b, k, :], rhs=t[:, k, :],
                                               start=(k == 0), stop=(k == 7))
                    return ins
                pe.op(mm, [r, r_crep], [psr[pb]])
                if tsl < 6:
                    dve.op(lambda b=b, pb=pb, tsl=tsl: nc.vector.tensor_tensor(
                        out=modrow[:, b, tsl * 512:(tsl + 1) * 512], in0=ps[pb][:, :],
                        in1=brow[:, tsl * 512:(tsl + 1) * 512], op=ALU.add), [psr[pb], r_brow], [r_modrow])
                else:
                    dve.op(lambda b=b, pb=pb, tsl=tsl: nc.vector.tensor_tensor(
                        out=modg2[:, b, (tsl - 6) * 512:(tsl - 5) * 512], in0=ps[pb][:, :],
                        in1=brow[:, tsl * 512:(tsl + 1) * 512], op=ALU.add), [psr[pb], r_brow], [r_modg2])
        dve.op(lambda: nc.vector.tensor_scalar(out=modrow[:, :, 2048:3072], in0=modrow[:, :, 2048:3072],
                                               scalar1=1.0, scalar2=None, op0=ALU.add), [r_modrow], [r_modrow])

    barrier()
    if stage == "0":
        sp.dma([(out_d[0:128, :], modrow[:, 0, 0:1024])], [r_modrow], [], r_modrow)
        sp.dma([(out_d[128:256, :], modg2[:, 1, :])], [r_modg2], [], r_modg2)
        sp.dma([(out_d[256:384, 0:32], modfm[:].rearrange("p a b -> p (a b)"))], [r_modfm], [], r_modfm)
        sp.deps([], [r_modrow, r_modg2, r_modfm])
        raise EarlyExit()

    xT_v = xT_d.rearrange("(k p) t -> p k t", p=128)
    UF_v = UF_d.rearrange("(k p) t -> p k t", p=128)
    YN_v = YN_d.rearrange("(k p) t -> p k t", p=128)

    def load_hT(st_tiles, tok0, nt, bsel):
        xTf, r_xTf, hT, r_hT = st_tiles
        sp.dma([(xTf[:, :, 0:nt], xT_v[:, :, tok0:tok0 + nt])], [], [r_xTf], r_xTf)
        for k in range(8):
            eng, E = (nc.vector, dve) if k % 2 == 0 else (nc.gpsimd, pool)
            E.op(lambda k=k, eng=eng: eng.tensor_scalar(
                out=hT[:, k, 0:nt], in0=xTf[:, k, 0:nt], scalar1=modfm[:, 8 + k, bsel:bsel + 1],
                scalar2=modfm[:, k, bsel:bsel + 1], op0=ALU.mult, op1=ALU.add), [r_xTf, r_modfm], [r_hT])

    TB = 256
    with contextlib.ExitStack() as stA:
        winA, r_winA = sb(stA, "winA", [128, 8, 2048], BF16)
        dgA, r_dgA = sb(stA, "dgA", [128, 8, 31, 128], BF16)
        r_p6 = [psr[6], psr[6]]
        xTf, r_xTf = sb(stA, "xTfA", [128, 8, TB])
        hT, r_hT = sb(stA, "hTA", [128, 8, TB], BF16)
        sig = [sb(stA, f"sig{i}", [128, TB]) for i in range(2)]
        ub, r_ub = sb(stA, "ub", [128, 8, 30 + TB], BF16)
        r_uc = [Res(f"uc{c}") for c in range(8)]
        cv, _ = sb(stA, "cv", [128, 8, TB])
        r_cv = [Res(f"cv{c}") for c in range(8)]
        cvq = [sb(stA, f"cvq{i}", [128, 2, TB], BF16) for i in range(2)]
        mean, r_mean = sb(stA, "mean", [128, TB])
        var, r_var = sb(stA, "var", [128, TB])
        rstd, r_rstd = sb(stA, "rstd", [128, TB])
        uf, r_uf = sb(stA, "uf", [128, 8, TB], BF16)
        stgA = [sb(stA, f"stgA{i}", [128, 1024]) for i in range(2)]
        load_cast([winA[:, k, h * 1024:(h + 1) * 1024] for k in range(8) for h in range(2)],
                  [win_d[k * 128:(k + 1) * 128, h * 1024:(h + 1) * 1024] for k in range(8) for h in range(2)],
                  stgA, r_winA)
        for c in range(8):
            for k in range(31):
                dve.op(lambda c=c, k=k: nc.vector.tensor_scalar(
                    out=dgA[:, c, k, :], in0=identb[:], scalar1=ccw[:, c, k:k + 1], scalar2=None, op0=ALU.mult),
                    [r_identb, r_ccw], [r_dgA])
        for s in range(2):
            for c in range(8):
                dve.op(lambda c=c: nc.vector.memset(ub[:, c, 0:30], 0.0), [], [r_uc[c]])
            for j in range(SEQ // TB):
                tok0 = s * SEQ + j * TB
                load_hT((xTf, r_xTf, hT, r_hT), tok0, TB, s)
                for c in range(8):
                    pv, pg = (0, 1) if c % 2 == 0 else (2, 3)
                    for (pi, col0) in ((pv, c * 128), (pg, 1024 + c * 128)):
                        def mm(pi=pi, col0=col0):
                            ins = None
                            for k in range(8):
                                ins = nc.tensor.matmul(ps[pi][:, 0:TB], lhsT=winA[:, k, col0:col0 + 128],
                                                       rhs=hT[:, k, :], start=(k == 0), stop=(k == 7))
                            return ins
                        pe.op(mm, [r_winA, r_hT], [psr[pi]])
                    sg, r_sg = sig[c % 2]
                    act.op(lambda pg=pg, sg=sg: nc.scalar.activation(out=sg[:], in_=ps[pg][:, 0:TB], func=AF.Sigmoid),
                           [psr[pg]], [r_sg])
                    dve.op(lambda pv=pv, sg=sg, c=c: nc.vector.tensor_tensor(
                        out=ub[:, c, 30:30 + TB], in0=ps[pv][:, 0:TB], in1=sg[:], op=ALU.mult),
                        [psr[pv], r_sg], [r_uc[c]])
                ck(51)
                for c in range(8):
                    hp = c % 2

                    def mm(c=c, hp=hp):
                        ins = None
                        for k in range(31):
                            ins = nc.tensor.matmul(ps[6][:, hp * TB:(hp + 1) * TB], lhsT=dgA[:, c, k, :],
                                                   rhs=ub[:, c, k:k + TB], start=(k == 0), stop=(k == 30))
                        return ins
                    pe.op(mm, [r_dgA, r_uc[c]], [r_p6[hp]])
                    ck(52)
                    act.op(lambda c=c, hp=hp: nc.scalar.activation(
                        out=cv[:, c, :], in_=ps[6][:, hp * TB:(hp + 1) * TB], func=AF.Identity,
                        bias=pfm[:, c:c + 1]), [r_p6[hp], r_pfm], [r_cv[c]])
                    ck(53)
                ck(54)
                for c in range(8):
                    eng, E = (nc.vector, dve) if c < 5 else (nc.gpsimd, pool)
                    E.op(lambda c=c, eng=eng: eng.tensor_copy(out=ub[:, c, 0:30], in_=ub[:, c, TB:TB + 30]),
                         [r_uc[c]], [r_uc[c]])
                for c in range(8):
                    q, r_q = cvq[c % 2]
                    act.op(lambda c=c, q=q: nc.scalar.activation(out=q[:, 0, :], in_=cv[:, c, :], func=AF.Copy),
                           [r_cv[c]], [r_q])
                    act.op(lambda c=c, q=q: nc.scalar.activation(out=q[:, 1, :], in_=cv[:, c, :], func=AF.Square),
                           [r_cv[c]], [r_q])

                    def mm(c=c, q=q):
                        nc.tensor.matmul(ps[4][:, 0:TB], lhsT=onesb[:], rhs=q[:, 0, :], start=(c == 0), stop=(c == 7))
                        return nc.tensor.matmul(ps[5][:, 0:TB], lhsT=onesb[:], rhs=q[:, 1, :], start=(c == 0),
                                                stop=(c == 7))
                    pe.op(mm, [r_q, r_onesb], [psr[4], psr[5]])
                ck(55)
                dve.op(lambda: nc.vector.tensor_scalar(out=mean[:], in0=ps[4][:, 0:TB], scalar1=1.0 / 1024.0,
                                                       scalar2=None, op0=ALU.mult), [psr[4]], [r_mean])
                dve.op(lambda: nc.vector.tensor_tensor(out=var[:], in0=mean[:], in1=mean[:], op=ALU.mult),
                       [r_mean], [r_var])
                dve.op(lambda: nc.vector.scalar_tensor_tensor(out=var[:], in0=ps[5][:, 0:TB], scalar=1.0 / 1024.0,
                                                              in1=var[:], op0=ALU.mult, op1=ALU.subtract),
                       [psr[5], r_var], [r_var])
                dve.op(lambda: nc.vector.tensor_scalar(out=var[:], in0=var[:], scalar1=0.0, scalar2=EPS,
                                                       op0=ALU.max, op1=ALU.add), [r_var], [r_var])
                act.op(lambda: nc.scalar.activation(out=var[:], in_=var[:], func=AF.Sqrt), [r_var], [r_var])
                dve.op(lambda: nc.vector.reciprocal(out=rstd[:], in_=var[:]), [r_var], [r_rstd])
                for c in range(8):
                    eng, E = (nc.vector, dve) if c < 5 else (nc.gpsimd, pool)
                    E.op(lambda c=c, eng=eng: eng.tensor_tensor(out=cv[:, c, :], in0=cv[:, c, :], in1=mean[:],
                                                                op=ALU.subtract), [r_cv[c], r_mean], [r_cv[c]])
                    E.op(lambda c=c, eng=eng: eng.tensor_tensor(out=cv[:, c, :], in0=cv[:, c, :], in1=rstd[:],
                                                                op=ALU.mult), [r_cv[c], r_rstd], [r_cv[c]])
                    act.op(lambda c=c: nc.scalar.activation(out=uf[:, c, :], in_=cv[:, c, :], func=AF.Silu,
                                                            bias=pfm[:, 16 + c:17 + c], scale=pfm[:, 8 + c:9 + c]),
                           [r_cv[c], r_pfm], [r_uf])
                sp.dma([(UF_v[:, :, tok0:tok0 + TB], uf[:])], [r_uf], [r_UF], r_uf)
                ck(56)
                if stage == "A1":
                    dbg, r_dbg = sb(stA, "dbg", [128, 1024])
                    def dump(row, src_ap, n, rs):
                        dve.op(lambda: nc.vector.tensor_copy(out=dbg[:, 0:n], in_=src_ap), rs, [r_dbg])
                        sp.dma([(out_d[row:row + 128, 0:n], dbg[:, 0:n])], [r_dbg], [], r_dbg)
                    dump(0, winA[:, 0, 0:1024], 1024, [r_winA])
                    dump(128, hT[:, 0, :], 512, [r_hT])
                    dump(256, ub[:, 0, 0:542], 542, [r_uc[0]])
                    dump(384, cv[:, 0, :], 512, [r_cv[0]])
                    dump(512, mean[:], 512, [r_mean])
                    dump(640, var[:], 512, [r_var])
                    dump(768, rstd[:], 512, [r_rstd])
                    dump(896, uf[:, 0, :], 512, [r_uf])
                    sp.deps([], [r_dbg, r_uf])
                    raise EarlyExit()
                if stage == "A":
                    j4 = tok0 // 1024
                    col = tok0 % 1024
                    dve.op(lambda: nc.vector.tensor_copy(out=xTf[:], in_=uf[:]), [r_uf], [r_xTf])
                    sp.dma([(out_d[j4 * 1024:(j4 + 1) * 1024, col:col + TB].rearrange("(c p) t -> p c t", p=128),
                             xTf[:])], [r_xTf], [], r_xTf)

    barrier()
    if stage == "A":
        sp.deps([], [r_uf, r_xTf])
        raise EarlyExit()

    TBB = 128
    with contextlib.ExitStack() as stB:
        winB, r_winB = sb(stB, "winB", [128, 8, 3088], BF16)
        dg, r_dg = sb(stB, "dg", [128, 16, 4, 128], BF16)
        scw, r_scw = sb(stB, "scw", [128, 16, 4])
        normw, r_normw = sb(stB, "normw", [128, D])
        xTf, r_xTf = sb(stB, "xTfB", [128, 8, TBB])
        hT, r_hT = sb(stB, "hTB", [128, 8, TBB], BF16)
        xpre, r_xpre = sb(stB, "xpre", [128, 16, 3 + TBB], BF16)
        xpost, r_xpost = sb(stB, "xpost", [128, 16, TBB], BF16)
        tA, r_tA = sb(stB, "tA", [128, D])
        tB, r_tB = sb(stB, "tB", [128, D])
        tC, r_tC = sb(stB, "tC", [128, D])
        Rm, r_Rm = sb(stB, "Rm", [128, D])
        dec, r_dec = sb(stB, "dec", [128, D])
        MT, r_MT = sb(stB, "MT", [128, 16, 128], BF16)
        cbs, r_cbs = sb(stB, "cbs", [128, 512])
        xc, r_xc = sb(stB, "xc", [128, D], BF16)
        xcd, r_xcd = sb(stB, "xcd", [128, D], BF16)
        ynb, r_ynb = sb(stB, "ynb", [128, D], BF16)
        ynT, r_ynT = sb(stB, "ynT", [128, 8, 128], BF16)
        Btok, r_Btok = sb(stB, "Btok", [128, 512], BF16)
        S, r_S = sb(stB, "S", [128, D])
        Sb, r_Sb = sb(stB, "Sb", [128, D], BF16)
        sm, r_sm = sb(stB, "smB", [128, 16 * 12])
        dtp = sm[:, 0:16]
        dtv = sm[:, 16:32]
        av = sm[:, 32:48]
        acs = sm[:, 48:64]
        nacs = sm[:, 64:80]
        eacs = sm[:, 80:96]
        dif = sm[:, 96:112]
        dte = sm[:, 112:128]
        cdr = sm[:, 128:144]
        ss4 = sm[:, 144:148]
        rs4 = sm[:, 148:152]
        mv = sm[:, 152:154]
        rs1 = sm[:, 154:155]
        junk = dec

        stgB = [sb(stB, f"stgB{i}", [128, 1544]) for i in range(2)]
        for half in range(2):
            c0 = 2048 + half * 1544
            load_cast([winB[:, k, half * 1544:(half + 1) * 1544] for k in range(8)],
                      [win_d[k * 128:(k + 1) * 128, c0:c0 + 1544] for k in range(8)], stgB, r_winB)
        sp.dma([(scw[:], scw_d[:, :, :])], [], [r_scw], r_scw)
        sp.dma([(normw[:], normw_d[:, :])], [], [r_normw], r_normw)
        for c in range(16):
            for k in range(4):
                dve.op(lambda c=c, k=k: nc.vector.tensor_scalar(
                    out=dg[:, c, k, :], in0=identb[:], scalar1=scw[:, c, k:k + 1], scalar2=None, op0=ALU.mult),
                    [r_identb, r_scw], [r_dg])

        ZC0, XC0, DC0 = 0, 1024, 3072
        for s in range(2):
            dve.op(lambda: nc.vector.memset(xpre[:, :, 0:3], 0.0), [], [r_xpre])
            dve.op(lambda: nc.vector.memset(S[:], 0.0), [], [r_S])
            dve.op(lambda: nc.vector.memset(Sb[:], 0.0), [], [r_Sb])
            for j in range(SEQ // TBB):
                tok0 = s * SEQ + j * TBB
                load_hT((xTf, r_xTf, hT, r_hT), tok0, TBB, s)
                for cp in range(8):
                    pi = cp % 2
                    for hh in range(2):
                        c = cp * 2 + hh

                        def mm(pi=pi, hh=hh, c=c):
                            ins = None
                            for k in range(8):
                                ins = nc.tensor.matmul(ps[pi][:, hh * TBB:(hh + 1) * TBB],
                                                       lhsT=winB[:, k, XC0 + c * 128:XC0 + (c + 1) * 128],
                                                       rhs=hT[:, k, :], start=(k == 0), stop=(k == 7))
                            return ins
                        pe.op(mm, [r_winB, r_hT], [psr[pi]])
                    act.op(lambda pi=pi, cp=cp: nc.scalar.activation(
                        out=xpre[:, 2 * cp:2 * cp + 2, 3:3 + TBB],
                        in_=ps[pi][:, 0:2 * TBB].rearrange("p (a t) -> p a t", a=2), func=AF.Copy),
                        [psr[pi]], [r_xpre])
                for cp in range(8):
                    pi = 2 + cp % 2
                    for hh in range(2):
                        c = cp * 2 + hh

                        def mm(pi=pi, hh=hh, c=c):
                            ins = None
                            for k in range(4):
                                ins = nc.tensor.matmul(ps[pi][:, hh * TBB:(hh + 1) * TBB], lhsT=dg[:, c, k, :],
                                                       rhs=xpre[:, c, k:k + TBB], start=(k == 0), stop=(k == 3))
                            return ins
                        pe.op(mm, [r_dg, r_xpre], [psr[pi]])
                        act.op(lambda pi=pi, hh=hh, c=c: nc.scalar.activation(
                            out=xpost[:, c, :], in_=ps[pi][:, hh * TBB:(hh + 1) * TBB], func=AF.Silu,
                            bias=pfm[:, 24 + c:25 + c]), [psr[pi], r_pfm], [r_xpost])
                dve.op(lambda: nc.vector.tensor_copy(out=xpre[:, :, 0:3], in_=xpre[:, :, TBB:TBB + 3]),
                       [r_xpre], [r_xpre])

                ck(1)
                for q in range(TBB // 128):
                    o = q * 128
                    t0 = tok0 + o
                    tile_idx = t0 // 128
                    for hf in range(2):
                        def mm(hf=hf, o=o):
                            ins = None
                            for k in range(8):
                                ins = nc.tensor.matmul(ps[hf][:, :], lhsT=hT[:, k, o:o + 128],
                                                       rhs=winB[:, k, ZC0 + hf * 512:ZC0 + (hf + 1) * 512],
                                                       start=(k == 0), stop=(k == 7))
                            return ins
                        pe.op(mm, [r_hT, r_winB], [psr[hf]])

                    def mm(o=o):
                        ins = None
                        for k in range(8):
                            ins = nc.tensor.matmul(ps[2][:, 0:16], lhsT=hT[:, k, o:o + 128],
                                                   rhs=winB[:, k, DC0:DC0 + 16], start=(k == 0), stop=(k == 7))
                        return ins
                    pe.op(mm, [r_hT, r_winB], [psr[2]])
                    ck(2)
                    dve.op(lambda: nc.vector.tensor_tensor(out=dtp, in0=ps[2][:, 0:16], in1=prow[:, 0:16],
                                                           op=ALU.add), [psr[2], r_prow], [r_sm])
                    act.op(lambda: nc.scalar.activation(out=dtp, in_=dtp, func=AF.Exp), [r_sm], [r_sm])
                    act.op(lambda: nc.scalar.activation(out=dtv, in_=dtp, func=AF.Ln, bias=1.0), [r_sm], [r_sm])
                    dve.op(lambda: nc.vector.tensor_tensor(out=av, in0=dtv, in1=prow[:, 16:32], op=ALU.mult),
                           [r_sm, r_prow], [r_sm])

                    def mm():
                        nc.tensor.matmul(ps[2][:, 16:32], lhsT=tri[:], rhs=av, start=True, stop=True)
                        return nc.tensor.matmul(ps[2][:, 32:48], lhsT=onesf[:], rhs=av, start=True, stop=True)
                    pe.op(mm, [r_tri, r_onesf, r_sm], [psr[2]])
                    dve.op(lambda: nc.vector.tensor_copy(out=acs, in_=ps[2][:, 16:32]), [psr[2]], [r_sm])
                    dve.op(lambda: nc.vector.tensor_scalar(out=nacs, in0=acs, scalar1=-1.0, scalar2=None,
                                                           op0=ALU.mult), [r_sm], [r_sm])
                    dve.op(lambda: nc.vector.tensor_tensor(out=dif, in0=ps[2][:, 32:48], in1=acs, op=ALU.subtract),
                           [psr[2], r_sm], [r_sm])
                    act.op(lambda: nc.scalar.activation(out=eacs, in_=acs, func=AF.Exp), [r_sm], [r_sm])
                    act.op(lambda: nc.scalar.activation(out=dte, in_=dif, func=AF.Exp), [r_sm], [r_sm])
                    act.op(lambda: nc.scalar.activation(out=cdr, in_=ps[2][:, 32:48], func=AF.Exp),
                           [psr[2], r_sm], [r_sm])
                    ck(3)
                    def mm(o=o):
                        ins = None
                        for g in range(4):
                            ins = nc.tensor.matmul(ps[3][:, g * 128:(g + 1) * 128], lhsT=xpost[:, 8 + g, o:o + 128],
                                                   rhs=xpost[:, 12 + g, o:o + 128], start=True, stop=True)
                        return ins
                    pe.op(mm, [r_xpost], [psr[3]])
                    act.op(lambda: nc.scalar.activation(out=cbs[:], in_=ps[3][:, :], func=AF.Copy), [psr[3]], [r_cbs])
                    ck(4)
                    def mm(o=o):
                        ins = None
                        for c in range(8):
                            ins = nc.tensor.transpose(out=psbf(7)[:, c * 128:(c + 1) * 128], in_=xpost[:, c, o:o + 128],
                                                      identity=identb[:])
                        return ins
                    pe.op(mm, [r_xpost, r_identb], [psr[7]])
                    ck(41)
                    act.op(lambda: nc.scalar.activation(out=tB[:], in_=psbf(7)[:, :], func=AF.Copy), [psr[7]], [r_tB])
                    ck(42)
                    dve.op(lambda: nc.vector.tensor_tensor(
                        out=xc[:].rearrange("p (h d) -> p h d", d=64),
                        in0=tB[:].rearrange("p (h d) -> p h d", d=64),
                        in1=dtv.unsqueeze(2).broadcast_to([128, 16, 64]), op=ALU.mult), [r_tB, r_sm], [r_xc])
                    ck(43)
                    dve.op(lambda: nc.vector.tensor_tensor(
                        out=xcd[:].rearrange("p (h d) -> p h d", d=64),
                        in0=xc[:].rearrange("p (h d) -> p h d", d=64),
                        in1=dte.unsqueeze(2).broadcast_to([128, 16, 64]), op=ALU.mult), [r_xc, r_sm], [r_xcd])
                    ck(5)
                    for hh in range(2):
                        dve.op(lambda hh=hh: nc.vector.tensor_tensor(
                            out=Rm[:].rearrange("p (h l) -> p h l", l=128),
                            in0=tri[:].unsqueeze(1).broadcast_to([128, 8, 128]),
                            in1=av[:, 8 * hh:8 * hh + 8].unsqueeze(2).broadcast_to([128, 8, 128]), op=ALU.mult),
                            [r_tri, r_sm], [r_Rm])
                        for jb in range(2):
                            def mm(jb=jb):
                                nc.tensor.matmul(ps[4 + jb][:, :], lhsT=onesf[:], rhs=Rm[:, jb * 512:(jb + 1) * 512],
                                                 start=True, stop=False)
                                return nc.tensor.matmul(ps[4 + jb][:, :], lhsT=identf[:], rhs=negm[:],
                                                        start=False, stop=True)
                            pe.op(mm, [r_onesf, r_identf, r_negm, r_Rm], [psr[4 + jb]])
                        for jj in range(8):
                            h = 8 * hh + jj
                            act.op(lambda jj=jj, h=h: nc.scalar.activation(
                                out=dec[:, jj * 128:(jj + 1) * 128],
                                in_=ps[4 + jj // 4][:, (jj % 4) * 128:(jj % 4 + 1) * 128], func=AF.Exp,
                                bias=nacs[:, h:h + 1]), [psr[4 + jj // 4], r_sm], [r_dec])
                        for jb in range(2):
                            g = 2 * hh + jb
                            dve.op(lambda jb=jb, g=g, hh=hh: nc.vector.tensor_tensor(
                                out=MT[:, 8 * hh + 4 * jb:8 * hh + 4 * jb + 4, :],
                                in0=dec[:, jb * 512:(jb + 1) * 512].rearrange("p (h l) -> p h l", l=128),
                                in1=cbs[:, g * 128:(g + 1) * 128].unsqueeze(1).broadcast_to([128, 4, 128]),
                                op=ALU.mult), [r_dec, r_cbs], [r_MT])
                    ck(6)
                    for bk in range(2):
                        def mm(bk=bk, o=o):
                            ins = None
                            for gg in range(2):
                                g = 2 * bk + gg
                                ins = nc.tensor.matmul(ps[4 + bk][:, gg * 256:(gg + 1) * 256],
                                                       lhsT=xpost[:, 12 + g, o:o + 128],
                                                       rhs=Sb[:, g * 256:(g + 1) * 256], start=True, stop=True)
                            return ins
                        pe.op(mm, [r_xpost, r_Sb], [psr[4 + bk]])
                        dve.op(lambda bk=bk: nc.vector.tensor_tensor(
                            out=tC[:, bk * 512:(bk + 1) * 512].rearrange("p (h d) -> p h d", d=64),
                            in0=ps[4 + bk][:, :].rearrange("p (h d) -> p h d", d=64),
                            in1=eacs[:, 8 * bk:8 * bk + 8].unsqueeze(2).broadcast_to([128, 8, 64]), op=ALU.mult),
                            [psr[4 + bk], r_sm], [r_tC])
                    ck(7)
                    for bk in range(2):
                        def mm(bk=bk):
                            ins = None
                            for hh8 in range(8):
                                h = 8 * bk + hh8
                                ins = nc.tensor.matmul(ps[(6, 3)[bk]][:, hh8 * 64:(hh8 + 1) * 64], lhsT=MT[:, h, :],
                                                       rhs=xc[:, h * 64:(h + 1) * 64], start=True, stop=True)
                            return ins
                        pe.op(mm, [r_MT, r_xc], [psr[(6, 3)[bk]]])
                        dve.op(lambda bk=bk: nc.vector.tensor_tensor(
                            out=tC[:, bk * 512:(bk + 1) * 512], in0=ps[(6, 3)[bk]][:, :],
                            in1=tC[:, bk * 512:(bk + 1) * 512], op=ALU.add), [psr[(6, 3)[bk]], r_tC], [r_tC])
                    ck(8)
                    dve.op(lambda: nc.vector.tensor_tensor(
                        out=tB[:].rearrange("p (h d) -> p h d", d=64), in0=tB[:].rearrange("p (h d) -> p h d", d=64),
                        in1=prow[:, 32:48].unsqueeze(2).broadcast_to([128, 16, 64]), op=ALU.mult),
                        [r_tB, r_prow], [r_tB])
                    dve.op(lambda: nc.vector.tensor_tensor(out=tC[:], in0=tC[:], in1=tB[:], op=ALU.add),
                           [r_tC, r_tB], [r_tC])
                    ck(9)
                    for hf in range(2):
                        act.op(lambda hf=hf: nc.scalar.activation(out=tA[:, hf * 512:(hf + 1) * 512], in_=ps[hf][:, :],
                                                                  func=AF.Silu), [psr[hf]], [r_tA])
                    dve.op(lambda: nc.vector.tensor_tensor(out=tC[:], in0=tC[:], in1=tA[:], op=ALU.mult),
                           [r_tC, r_tA], [r_tC])
                    for g4 in range(4):
                        act.op(lambda g4=g4: nc.scalar.activation(
                            out=junk[:, 0:256], in_=tC[:, g4 * 256:(g4 + 1) * 256], func=AF.Square,
                            accum_out=ss4[:, g4:g4 + 1]), [r_tC], [r_dec, r_sm])
                    dve.op(lambda: nc.vector.tensor_scalar(out=ss4, in0=ss4, scalar1=1.0 / 256.0, scalar2=EPS,
                                                           op0=ALU.mult, op1=ALU.add), [r_sm], [r_sm])
                    act.op(lambda: nc.scalar.activation(out=ss4, in_=ss4, func=AF.Sqrt), [r_sm], [r_sm])
                    dve.op(lambda: nc.vector.reciprocal(out=rs4, in_=ss4), [r_sm], [r_sm])
                    dve.op(lambda: nc.vector.tensor_tensor(
                        out=tC[:].rearrange("p (g d) -> p g d", d=256), in0=tC[:].rearrange("p (g d) -> p g d", d=256),
                        in1=rs4.unsqueeze(2).broadcast_to([128, 4, 256]), op=ALU.mult), [r_tC, r_sm], [r_tC])
                    dve.op(lambda: nc.vector.tensor_tensor(out=ynb[:], in0=tC[:], in1=normw[:], op=ALU.mult),
                           [r_tC, r_normw], [r_ynb])

                    def mm():
                        ins = None
                        for c in range(8):
                            ins = nc.tensor.transpose(out=psbf(3)[:, c * 128:(c + 1) * 128],
                                                      in_=ynb[:, c * 128:(c + 1) * 128], identity=identb[:])
                        return ins
                    pe.op(mm, [r_ynb, r_identb], [psr[7]])
                    act.op(lambda: nc.scalar.activation(out=ynT[:].rearrange("p c t -> p (c t)"), in_=psbf(3)[:, :],
                                                        func=AF.Copy), [psr[7]], [r_ynT])
                    ck(10)
                    def mm(o=o):
                        ins = None
                        for g in range(4):
                            ins = nc.tensor.transpose(out=psbf(2)[:, g * 128:(g + 1) * 128],
                                                      in_=xpost[:, 8 + g, o:o + 128], identity=identb[:])
                        return ins
                    pe.op(mm, [r_xpost, r_identb], [psr[7]])
                    act.op(lambda: nc.scalar.activation(out=Btok[:], in_=psbf(2)[:, 0:512], func=AF.Copy),
                           [psr[7]], [r_Btok])
                    dve.op(lambda: nc.vector.tensor_tensor(
                        out=S[:].rearrange("p (h d) -> p h d", d=64), in0=S[:].rearrange("p (h d) -> p h d", d=64),
                        in1=cdr.unsqueeze(2).broadcast_to([128, 16, 64]), op=ALU.mult), [r_S, r_sm], [r_S])
                    for bk in range(2):
                        def mm(bk=bk):
                            ins = None
                            for gg in range(2):
                                g = 2 * bk + gg
                                ins = nc.tensor.matmul(ps[4 + bk][:, gg * 256:(gg + 1) * 256],
                                                       lhsT=Btok[:, g * 128:(g + 1) * 128],
                                                       rhs=xcd[:, g * 256:(g + 1) * 256], start=True, stop=True)
                            return ins
                        pe.op(mm, [r_Btok, r_xcd], [psr[4 + bk]])
                        dve.op(lambda bk=bk: nc.vector.tensor_tensor(
                            out=S[:, bk * 512:(bk + 1) * 512], in0=ps[4 + bk][:, :],
                            in1=S[:, bk * 512:(bk + 1) * 512], op=ALU.add), [psr[4 + bk], r_S], [r_S])
                    act.op(lambda: nc.scalar.activation(out=Sb[:], in_=S[:], func=AF.Copy), [r_S], [r_Sb])
                    sp.dma([(YN_v[:, :, t0:t0 + 128], ynT[:])], [r_ynT], [r_YN], r_ynT)
                    if stage == "B1":
                        j4 = t0 // 1024
                        col = t0 % 1024
                        dve.op(lambda: nc.vector.tensor_copy(out=xTf[:], in_=ynT[:]), [r_ynT], [r_xTf])
                        sp.dma([(out_d[j4 * 1024:(j4 + 1) * 1024, col:col + 128].rearrange("(c p) t -> p c t", p=128),
                                 xTf[:])], [r_xTf], [], r_xTf)

    barrier()
    if stage == "B1":
        raise EarlyExit()
    with contextlib.ExitStack() as stC:
        woutb, r_woutb = sb(stC, "woutb", [128, 16, 1024], BF16)
        ln1g, r_ln1g = sb(stC, "ln1g", [128, D])
        ln1b, r_ln1b = sb(stC, "ln1b", [128, D])
        wr, r_wr = sb(stC, "wr", [128, 8, 72])
        setsC = []
        for i in range(2):
            setsC.append((sb(stC, f"ufb{i}", [128, 8, 128], BF16), sb(stC, f"ynTC{i}", [128, 8, 128], BF16),
                          sb(stC, f"xtok{i}", [128, D]), sb(stC, f"tAC{i}", [128, D]), sb(stC, f"tCC{i}", [128, D]),
                          sb(stC, f"h2Tf{i}", [128, 8, 128]), sb(stC, f"h2b{i}", [128, D], BF16),
                          sb(stC, f"bst{i}", [128, 2, 6]), sb(stC, f"smC{i}", [128, 8])))
        stgC = [sb(stC, f"stgC{i}", [128, 1024]) for i in range(2)]
        load_cast([woutb[:, c, :] for c in range(16)], [wout_d[c * 128:(c + 1) * 128, :] for c in range(16)],
                  stgC, r_woutb)
        sp.dma([(ln1g[:], ln1g_d[:, :])], [], [r_ln1g], r_ln1g)
        sp.dma([(ln1b[:], ln1b_d[:, :])], [], [r_ln1b], r_ln1b)
        sp.dma([(wr[:], wr_d[:, :, :])], [], [r_wr], r_wr)
        for tile_idx in range(32):
            ((ufb, r_ufb), (ynT, r_ynT), (xtok, r_xtok), (tA, r_tA), (tC, r_tC), (h2Tf, r_h2Tf), (h2b, r_h2b),
             (bst, r_bst), (sm, r_sm)) = setsC[tile_idx % 2]
            mv = sm[:, 0:2]
            rs1 = sm[:, 2:3]
            po = 0 if tile_idx % 2 == 0 else 4
            s = tile_idx // 16
            t0 = tile_idx * 128
            g1row = modrow[:, s, 0:1024]
            sh2row = modrow[:, s, 1024:2048]
            sc2row = modrow[:, s, 2048:3072]
            sp.dma([(xtok[:], x_d[t0:t0 + 128, :])], [], [r_xtok], r_xtok)
            sp.dma([(ufb[:], UF_v[:, :, t0:t0 + 128])], [r_UF], [r_ufb], r_ufb)
            sp.dma([(ynT[:], YN_v[:, :, t0:t0 + 128])], [r_YN], [r_ynT], r_ynT)
            for hf in range(2):
                def mm(hf=hf):
                    ins = None
                    for c in range(8):
                        ins = nc.tensor.matmul(ps[po + hf][:, :], lhsT=ufb[:, c, :],
                                               rhs=woutb[:, c, hf * 512:(hf + 1) * 512],
                                               start=(c == 0), stop=False)
                    for c in range(8):
                        ins = nc.tensor.matmul(ps[po + hf][:, :], lhsT=ynT[:, c, :],
                                               rhs=woutb[:, 8 + c, hf * 512:(hf + 1) * 512],
                                               start=False, stop=(c == 7))
                    return ins
                pe.op(mm, [r_ufb, r_ynT, r_woutb], [psr[po + hf]])
                dve.op(lambda hf=hf: nc.vector.tensor_tensor(
                    out=tA[:, hf * 512:(hf + 1) * 512], in0=ps[po + hf][:, :],
                    in1=g1row[:, hf * 512:(hf + 1) * 512], op=ALU.mult), [psr[po + hf], r_modrow], [r_tA])
            dve.op(lambda: nc.vector.scalar_tensor_tensor(out=tA[:], in0=xtok[:], scalar=ALPHA, in1=tA[:],
                                                          op0=ALU.mult, op1=ALU.add), [r_xtok, r_tA], [r_tA])
            ck(20)
            dve.op(lambda: nc.vector.bn_stats(out=bst[:, 0, :], in_=tA[:, 0:512]), [r_tA], [r_bst])
            dve.op(lambda: nc.vector.bn_stats(out=bst[:, 1, :], in_=tA[:, 512:1024]), [r_tA], [r_bst])
            dve.op(lambda: nc.vector.bn_aggr(out=mv, in_=bst[:].rearrange("p a b -> p (a b)")),
                   [r_bst], [r_sm])
            ck(21)
            dve.op(lambda: nc.vector.tensor_scalar(out=rs1, in0=mv[:, 1:2], scalar1=EPS, scalar2=None,
                                                   op0=ALU.add), [r_sm], [r_sm])
            act.op(lambda: nc.scalar.activation(out=rs1, in_=rs1, func=AF.Sqrt), [r_sm], [r_sm])
            dve.op(lambda: nc.vector.reciprocal(out=rs1, in_=rs1), [r_sm], [r_sm])
            dve.op(lambda: nc.vector.tensor_scalar(out=tC[:], in0=tA[:], scalar1=mv[:, 0:1], scalar2=rs1,
                                                   op0=ALU.subtract, op1=ALU.mult), [r_tA, r_sm], [r_tC])
            dve.op(lambda: nc.vector.tensor_tensor(out=tC[:], in0=tC[:], in1=ln1g[:], op=ALU.mult),
                   [r_tC, r_ln1g], [r_tC])
            dve.op(lambda: nc.vector.tensor_tensor(out=tC[:], in0=tC[:], in1=ln1b[:], op=ALU.add),
                   [r_tC, r_ln1b], [r_tC])
            pool.dma([(X1_d[t0:t0 + 128, :], tC[:])], [r_tC], [r_X1], r_tC)
            if stage == "B":
                sp.dma([(out_d[t0:t0 + 128, :], tC[:])], [r_tC], [], r_tC)
            ck(22)
            dve.op(lambda: nc.vector.tensor_tensor(out=tA[:], in0=tC[:], in1=sc2row, op=ALU.mult),
                   [r_tC, r_modrow], [r_tA])
            dve.op(lambda: nc.vector.tensor_tensor(out=tA[:], in0=tA[:], in1=sh2row, op=ALU.add),
                   [r_tA, r_modrow], [r_tA])
            ck(23)
            for bk in range(2):
                def mm(bk=bk):
                    ins = None
                    for cc in range(4):
                        c = 4 * bk + cc
                        ins = nc.tensor.matmul(ps[2 + bk][:, cc * 128:(cc + 1) * 128],
                                               lhsT=tA[:, c * 128:(c + 1) * 128], rhs=identf[:],
                                               start=True, stop=True)
                    return ins
                pe.op(mm, [r_tA, r_identf], [psr[2 + bk]])
                ck(241)
                act.op(lambda bk=bk: nc.scalar.activation(
                    out=h2Tf[:, 4 * bk:4 * bk + 4, :].rearrange("p c t -> p (c t)"), in_=ps[2 + bk][:, :],
                    func=AF.Copy), [psr[2 + bk]], [r_h2Tf])
                ck(242)
            ck(24)
            pool.op(lambda: nc.gpsimd.tensor_copy(out=h2b[:], in_=tA[:]), [r_tA], [r_h2b])
            pool.dma([(H2_d[t0:t0 + 128, :], h2b[:])], [r_h2b], [r_H2T], r_h2b)

            def mm():
                ins = None
                for k in range(8):
                    ins = nc.tensor.matmul(ps[6][:, 0:72], lhsT=h2Tf[:, k, :], rhs=wr[:, k, :],
                                           start=(k == 0), stop=(k == 7))
                return ins
            pe.op(mm, [r_h2Tf, r_wr], [psr[6]])
            dve.op(lambda tile_idx=tile_idx: nc.vector.tensor_tensor(
                out=Lg[:, tile_idx, :], in0=ps[6][:, 0:72], in1=rb[:], op=ALU.add), [psr[6], r_rb], [r_Lg])
            ck(25)

    barrier()
    if stage == "B":
        sp.deps([], [r_tC])
        raise EarlyExit()

    I32 = mybir.dt.int32
    V = nc.vector
    gmax = rsm[:, 0:32]
    pgt = rsm[:, 32:64]
    m1 = rsm[:, 64:96]
    m2 = rsm[:, 96:128]
    pe1 = rsm[:, 128:160]
    pe2 = rsm[:, 160:192]
    d1f = rsm[:, 192:224]
    d2f = rsm[:, 224:256]
    with contextlib.ExitStack() as stM:
        stR = contextlib.ExitStack()
        wk, r_wk = sb(stR, "wk", [128, 32, 64])
        oh1, r_oh1 = sb(stR, "oh1", [128, 32, 64])
        oh2, r_oh2 = sb(stR, "oh2", [128, 32, 64])
        cum, r_cum = sb(stR, "cum", [128, 32, 64])
        cs, r_cs = sb(stR, "cs", [128, 32, 64])
        ohb, r_ohb = sb(stR, "ohb", [128, 32, 64], BF16)
        triSb, r_triSb = sb(stR, "triSb", [128, 128], BF16)
        gm, r_gm = sb(stR, "gm", [128, 32, 8])
        ohg, r_ohg = sb(stR, "ohg", [128, 32, 8])
        thr, r_thr = sb(stR, "thr", [128, 64])
        iotab, r_iotab = sb(stR, "iotab", [128, 128])
        pid, r_pid = sb(stR, "pid", [128, 1])
        e64, r_e64 = sb(stR, "e64", [128, 6, 64])
        bef, r_bef = sb(stR, "bef", [128, 128])
        big, r_big = sb(stR, "big", [128, 4096])
        sp.dma([(thr[:], thr_d[:, :])], [], [r_thr], r_thr)
        sp.dma([(iotab[:], iotab_d[:, :])], [], [r_iotab], r_iotab)
        sp.dma([(pid[:], pid_d[:, :])], [], [r_pid], r_pid)
        dve.op(lambda: V.tensor_reduce(out=gmax, in_=Lg[:, :, 0:8], axis=AX.X, op=ALU.max), [r_Lg], [r_rsm])
        dve.op(lambda: V.tensor_tensor(out=gm[:], in0=Lg[:, :, 0:8], in1=gmax.unsqueeze(2).broadcast_to([128, 32, 8]),
                                       op=ALU.subtract), [r_Lg, r_rsm], [r_gm])
        dve.op(lambda: V.tensor_single_scalar(out=ohg[:], in_=gm[:], scalar=0.0, op=ALU.is_ge), [r_gm], [r_ohg])
        act.op(lambda: nc.scalar.activation(out=gm[:], in_=gm[:], func=AF.Exp), [r_gm], [r_gm])
        dve.op(lambda: V.tensor_reduce(out=pgt, in_=gm[:], axis=AX.X, op=ALU.add), [r_gm], [r_rsm])
        dve.op(lambda: V.reciprocal(out=pgt, in_=pgt), [r_rsm], [r_rsm])
        dve.op(lambda: V.tensor_scalar(out=ohg[:], in0=ohg[:], scalar1=-1.0, scalar2=BIG, op0=ALU.add, op1=ALU.mult),
               [r_ohg], [r_ohg])
        dve.op(lambda: V.tensor_tensor(
            out=wk[:].rearrange("p t (g e) -> p t g e", e=8), in0=Lg[:, :, 8:72].rearrange("p t (g e) -> p t g e", e=8),
            in1=ohg[:].unsqueeze(3).broadcast_to([128, 32, 8, 8]), op=ALU.add), [r_Lg, r_ohg], [r_wk])
        dve.op(lambda: V.tensor_reduce(out=m1, in_=wk[:], axis=AX.X, op=ALU.max), [r_wk], [r_rsm])
        dve.op(lambda: V.tensor_tensor(out=oh1[:], in0=wk[:], in1=m1.unsqueeze(2).broadcast_to([128, 32, 64]),
                                       op=ALU.is_ge), [r_wk, r_rsm], [r_oh1])
        dve.op(lambda: V.scalar_tensor_tensor(out=wk[:], in0=oh1[:], scalar=-BIG, in1=wk[:], op0=ALU.mult,
                                              op1=ALU.add), [r_oh1, r_wk], [r_wk])
        dve.op(lambda: V.tensor_reduce(out=m2, in_=wk[:], axis=AX.X, op=ALU.max), [r_wk], [r_rsm])
        dve.op(lambda: V.tensor_tensor(out=oh2[:], in0=wk[:], in1=m2.unsqueeze(2).broadcast_to([128, 32, 64]),
                                       op=ALU.is_ge), [r_wk, r_rsm], [r_oh2])
        dve.op(lambda: V.tensor_tensor(out=pe1, in0=m2, in1=m1, op=ALU.subtract), [r_rsm], [r_rsm])
        act.op(lambda: nc.scalar.activation(out=pe1, in_=pe1, func=AF.Exp), [r_rsm], [r_rsm])
        dve.op(lambda: V.tensor_scalar(out=pe1, in0=pe1, scalar1=1.0, scalar2=None, op0=ALU.add), [r_rsm], [r_rsm])
        dve.op(lambda: V.reciprocal(out=pe1, in_=pe1), [r_rsm], [r_rsm])
        dve.op(lambda: V.tensor_scalar(out=pe2, in0=pe1, scalar1=-1.0, scalar2=1.0, op0=ALU.mult, op1=ALU.add),
               [r_rsm], [r_rsm])
        dve.op(lambda: V.tensor_tensor(out=pe1, in0=pe1, in1=pgt, op=ALU.mult), [r_rsm], [r_rsm])
        dve.op(lambda: V.tensor_tensor(out=pe2, in0=pe2, in1=pgt, op=ALU.mult), [r_rsm], [r_rsm])
        dve.op(lambda: V.tensor_tensor(out=ohb[:], in0=oh1[:], in1=oh2[:], op=ALU.add), [r_oh1, r_oh2], [r_ohb])
        dve.op(lambda: V.tensor_tensor(out=triSb[:], in0=tri[:], in1=identf[:], op=ALU.subtract),
               [r_tri, r_identf], [r_triSb])
        for q4 in range(4):
            def mm(q4=q4):
                return nc.tensor.matmul(ps[q4][:, :], lhsT=triSb[:], rhs=ohb[:, 8 * q4:8 * q4 + 8, :],
                                        start=True, stop=True)
            pe.op(mm, [r_triSb, r_ohb], [psr[q4]])
            act.op(lambda q4=q4: nc.scalar.activation(
                out=cum[:, 8 * q4:8 * q4 + 8, :].rearrange("p t e -> p (t e)"), in_=ps[q4][:, :], func=AF.Copy),
                [psr[q4]], [r_cum])
        for q4 in range(4):
            def mm(q4=q4):
                return nc.tensor.matmul(ps[q4][:, :], lhsT=onesb[:], rhs=ohb[:, 8 * q4:8 * q4 + 8, :],
                                        start=True, stop=True)
            pe.op(mm, [r_onesb, r_ohb], [psr[q4]])
            act.op(lambda q4=q4: nc.scalar.activation(
                out=cs[:, 8 * q4:8 * q4 + 8, :].rearrange("p t e -> p (t e)"), in_=ps[q4][:, :], func=AF.Copy),
                [psr[q4]], [r_cs])
        cnt = e64[:, 0, :]
        nb = e64[:, 1, :]
        ci = e64[:, 2, :]
        sbase = e64[:, 3, :]
        ones64 = e64[:, 4, :]
        dve.op(lambda: V.memset(e64[:], 0.0), [], [r_e64])
        dve.op(lambda: V.memset(ones64, 1.0), [r_e64], [r_e64])
        for t in range(32):
            dve.op(lambda t=t: V.tensor_tensor(out=cum[:, t, :], in0=cum[:, t, :], in1=cnt, op=ALU.add),
                   [r_cum, r_e64], [r_cum])
            dve.op(lambda t=t: V.tensor_tensor(out=cnt, in0=cnt, in1=cs[:, t, :], op=ALU.add), [r_cs, r_e64], [r_e64])
        dve.op(lambda: V.tensor_tensor(out=big[:].rearrange("p (e j) -> p e j", j=64),
                                       in0=cnt.unsqueeze(2).broadcast_to([128, 64, 64]),
                                       in1=thr[:].unsqueeze(1).broadcast_to([128, 64, 64]), op=ALU.is_gt),
               [r_e64, r_thr], [r_big])
        dve.op(lambda: V.tensor_reduce(out=nb, in_=big[:].rearrange("p (e j) -> p e j", j=64), axis=AX.X, op=ALU.add),
               [r_big], [r_e64])
        dve.op(lambda: V.tensor_tensor_scan(out=ci, data0=ones64, data1=nb, initial=0.0, op0=ALU.mult, op1=ALU.add),
               [r_e64], [r_e64])
        dve.op(lambda: V.tensor_tensor(out=sbase, in0=ci, in1=nb, op=ALU.subtract), [r_e64], [r_e64])
        dve.op(lambda: V.tensor_scalar(out=sbase, in0=sbase, scalar1=128.0, scalar2=None, op0=ALU.mult),
               [r_e64], [r_e64])
        dve.op(lambda: V.tensor_tensor(out=cum[:], in0=cum[:], in1=sbase.unsqueeze(1).broadcast_to([128, 32, 64]),
                                       op=ALU.add), [r_cum, r_e64], [r_cum])
        dve.op(lambda: V.tensor_tensor(out=wk[:], in0=cum[:], in1=oh1[:], op=ALU.mult), [r_cum, r_oh1], [r_wk])
        dve.op(lambda: V.tensor_reduce(out=d1f, in_=wk[:], axis=AX.X, op=ALU.add), [r_wk], [r_rsm])
        dve.op(lambda: V.tensor_tensor(out=wk[:], in0=cum[:], in1=oh2[:], op=ALU.mult), [r_cum, r_oh2], [r_wk])
        dve.op(lambda: V.tensor_reduce(out=d2f, in_=wk[:], axis=AX.X, op=ALU.add), [r_wk], [r_rsm])
        dve.op(lambda: V.tensor_copy(out=d1i[:], in_=d1f), [r_rsm], [r_d1i])
        dve.op(lambda: V.tensor_copy(out=d2i[:], in_=d2f), [r_rsm], [r_d2i])
        for hb in range(2):
            dve.op(lambda hb=hb: V.tensor_tensor(
                out=big[:].rearrange("p (b e) -> p b e", e=64),
                in0=ci.unsqueeze(1).broadcast_to([128, 64, 64]),
                in1=iotab[:, 64 * hb:64 * hb + 64].unsqueeze(2).broadcast_to([128, 64, 64]), op=ALU.is_le),
                [r_e64, r_iotab], [r_big])
            dve.op(lambda hb=hb: V.tensor_reduce(
                out=bef[:, 64 * hb:64 * hb + 64], in_=big[:].rearrange("p (b e) -> p b e", e=64),
                axis=AX.X, op=ALU.add), [r_big], [r_bef])
        dve.op(lambda: V.tensor_scalar(out=bef[:], in0=bef[:], scalar1=63.0, scalar2=None, op0=ALU.min),
               [r_bef], [r_bef])
        dve.op(lambda: V.memset(big[:, 0:128], 0.0), [r_big], [r_big])
        dve.op(lambda: V.tensor_tensor(out=big[:, 1:128], in0=bef[:, 1:128], in1=bef[:, 0:127], op=ALU.is_equal),
               [r_bef, r_big], [r_big])
        dve.op(lambda: V.tensor_scalar(out=bef[:], in0=bef[:], scalar1=128.0, scalar2=None, op0=ALU.mult),
               [r_bef], [r_bef])
        dve.op(lambda: V.scalar_tensor_tensor(out=bef[:], in0=big[:, 0:128], scalar=1.0e6, in1=bef[:], op0=ALU.mult,
                                              op1=ALU.add), [r_big, r_bef], [r_bef])
        dve.op(lambda: V.tensor_scalar(out=bef[:], in0=bef[:], scalar1=pid[:, 0:1], scalar2=None, op0=ALU.add),
               [r_bef, r_pid], [r_bef])
        dve.op(lambda: V.tensor_copy(out=widx[:], in_=bef[:]), [r_bef], [r_widx])
        barrier()
        stR.close()
        stMid.close()
        if stage == "R":
            dbg, r_dbg = sb(stM, "dbgR", [128, 256])
            dve.op(lambda: V.tensor_copy(out=dbg[:, 0:32], in_=d1i[:]), [r_d1i], [r_dbg])
            dve.op(lambda: V.tensor_copy(out=dbg[:, 32:64], in_=d2i[:]), [r_d2i], [r_dbg])
            dve.op(lambda: V.tensor_copy(out=dbg[:, 64:128], in_=rsm[:, 128:192]), [r_rsm], [r_dbg])
            dve.op(lambda: V.tensor_copy(out=dbg[:, 128:256], in_=widx[:]), [r_widx], [r_dbg])
            sp.dma([(out_d[0:128, 0:256], dbg[:])], [r_dbg], [], r_dbg)
            sp.deps([], [r_dbg])
            barrier()
            raise EarlyExit()
        r_Xs, r_Ys = Res("Xs"), Res("Ys")
        with contextlib.ExitStack() as stS:
            hb2 = [sb(stS, f"hb2_{i}", [128, D], BF16) for i in range(2)]
            for t in range(32):
                tl, r_tl = hb2[t % 2]
                sp.dma([(tl[:], H2_d[t * 128:(t + 1) * 128, :])], [r_H2T], [r_tl], r_tl)
                for (di, r_di) in ((d1i, r_d1i), (d2i, r_d2i)):
                    pool.deps([r_tl, r_di], [])
                    if r_tl.dsem is None:
                        raise RuntimeError("no dsem")
                    pool.wait({id(r_tl.dsem): (r_tl.dsem, r_tl.dcnt)})
                    nc.gpsimd.indirect_dma_start(
                        out=Xs_d[:, :], out_offset=bass.IndirectOffsetOnAxis(ap=di[:, t:t + 1], axis=0),
                        in_=tl[:, :], in_offset=None).then_inc(r_tl.dsem, 16)
                    r_tl.dcnt += 16
                    pool.mark([r_tl, r_di], [r_Xs], r_tl.dsem, r_tl.dcnt)
            barrier()
        with contextlib.ExitStack() as stE:
            stg = [sb(stE, f"stgE{i}", [128, 4096]) for i in range(3)]
            wbf = []
            for i in range(2):
                a1, ra1 = sb(stE, f"w1b{i}", [128, 8, 512], BF16)
                a3, ra3 = sb(stE, f"w3b{i}", [128, 8, 512], BF16)
                a2, ra2 = sb(stE, f"w2b{i}", [128, 4, 1024], BF16)
                wbf.append((a1, ra1, a3, ra3, a2, ra2))
            xblk = [sb(stE, f"xblk{i}", [128, D], BF16) for i in range(2)]
            XT, r_XT = sb(stE, "XT", [128, 8, 128], BF16)
            sgE, r_sgE = sb(stE, "sgE", [128, 512])
            actb, r_actb = sb(stE, "actb", [128, 4, 128], BF16)
            yb = [sb(stE, f"yb{i}", [128, D]) for i in range(2)]
            bc_reg = nc.gpsimd.alloc_register("bc_reg")
            nc.gpsimd.reg_mov(bc_reg, nexp_decl * 128 - 1)
            for b in range(128):
                a1, ra1, a3, ra3, a2, ra2 = wbf[b % 2]
                for (wd, (tg, r_tg), (ab, r_ab), nk) in ((w1_d, stg[0], (a1, ra1), 8), (w3_d, stg[1], (a3, ra3), 8),
                                                        (w2_d, stg[2], (a2, ra2), 4)):
                    pool.deps([r_widx], [r_tg])
                    if r_tg.dsem is None:
                        r_tg.dsem = nc.alloc_semaphore(name=f"d{len(ALL_DMA_RES)}_" + r_tg.name)
                        ALL_DMA_RES.append(r_tg)
                    if r_tg.dcnt:
                        pool.wait({id(r_tg.dsem): (r_tg.dsem, r_tg.dcnt)})
                    nc.gpsimd.indirect_dma_start(
                        out=tg[:, :], out_offset=None, in_=wd[:, :],
                        in_offset=bass.IndirectOffsetOnAxis(ap=widx[:, b:b + 1], axis=0),
                        bounds_check=bc_reg, oob_is_err=False).then_inc(r_tg.dsem, 16)
                    r_tg.dcnt += 16
                    pool.mark([r_widx], [r_tg], r_tg.dsem, r_tg.dcnt)
                    if nk == 8:
                        dve.op(lambda ab=ab, tg=tg: V.tensor_copy(out=ab[:].rearrange("p k n -> p (k n)"), in_=tg[:]),
                               [r_tg], [r_ab])
                    else:
                        act.op(lambda ab=ab, tg=tg: nc.scalar.activation(out=ab[:].rearrange("p k n -> p (k n)"),
                                                                         in_=tg[:], func=AF.Copy), [r_tg], [r_ab])
                xb_, r_xb = xblk[b % 2]
                sp.dma([(xb_[:], Xs_d[b * 128:(b + 1) * 128, :])], [r_Xs], [r_xb], r_xb)

                def mm(xb_=xb_):
                    ins = None
                    for c in range(8):
                        ins = nc.tensor.transpose(out=ps7b[:, c * 128:(c + 1) * 128], in_=xb_[:, c * 128:(c + 1) * 128],
                                                  identity=identb[:])
                    return ins
                pe.op(mm, [r_xb, r_identb], [psr[7]])
                act.op(lambda: nc.scalar.activation(out=XT[:].rearrange("p c t -> p (c t)"), in_=ps7b[:, :],
                                                    func=AF.Copy), [psr[7]], [r_XT])
                pg_, pu_ = (0, 1) if b % 2 == 0 else (2, 3)

                def mm(a1=a1, a3=a3, pg_=pg_, pu_=pu_):
                    ins = None
                    for hc in range(4):
                        for k in range(8):
                            ins = nc.tensor.matmul(ps[pg_][:, hc * 128:(hc + 1) * 128],
                                                   lhsT=a1[:, k, hc * 128:(hc + 1) * 128], rhs=XT[:, k, :],
                                                   start=(k == 0), stop=(k == 7))
                    for hc in range(4):
                        for k in range(8):
                            ins = nc.tensor.matmul(ps[pu_][:, hc * 128:(hc + 1) * 128],
                                                   lhsT=a3[:, k, hc * 128:(hc + 1) * 128], rhs=XT[:, k, :],
                                                   start=(k == 0), stop=(k == 7))
                    return ins
                pe.op(mm, [ra1, ra3, r_XT], [psr[pg_], psr[pu_]])
                act.op(lambda pg_=pg_: nc.scalar.activation(out=sgE[:], in_=ps[pg_][:, :], func=AF.Silu),
                       [psr[pg_]], [r_sgE])
                dve.op(lambda pu_=pu_: V.tensor_tensor(out=actb[:].rearrange("p c t -> p (c t)"), in0=ps[pu_][:, :],
                                                       in1=sgE[:], op=ALU.mult), [psr[pu_], r_sgE], [r_actb])
                yb_, r_yb = yb[b % 2]
                for hf in range(2):
                    py = 4 + hf

                    def mm(hf=hf, py=py, a2=a2):
                        ins = None
                        for hc in range(4):
                            ins = nc.tensor.matmul(ps[py][:, :], lhsT=actb[:, hc, :],
                                                   rhs=a2[:, hc, hf * 512:(hf + 1) * 512],
                                                   start=(hc == 0), stop=(hc == 3))
                        return ins
                    pe.op(mm, [r_actb, ra2], [psr[py]])
                    act.op(lambda hf=hf, py=py, yb_=yb_: nc.scalar.activation(
                        out=yb_[:, hf * 512:(hf + 1) * 512], in_=ps[py][:, :], func=AF.Copy), [psr[py]], [r_yb])
                sp.dma([(Ys_d[b * 128:(b + 1) * 128, :], yb_[:])], [r_yb], [r_Ys], r_yb)
            barrier()
        with contextlib.ExitStack() as stF:
            ln2g, r_ln2g = sb(stF, "ln2g", [128, D])
            ln2b, r_ln2b = sb(stF, "ln2b", [128, D])
            x1t, r_x1t = sb(stF, "x1t", [128, D])
            fo, r_fo = sb(stF, "fo", [128, D])
            ya = [sb(stF, f"ya{i}", [128, D]) for i in range(2)]
            bst2, r_bst2 = sb(stF, "bst2", [128, 2, 6])
            sm2, r_sm2 = sb(stF, "sm2", [128, 4])
            sp.dma([(ln2g[:], ln2g_d[:, :])], [], [r_ln2g], r_ln2g)
            sp.dma([(ln2b[:], ln2b_d[:, :])], [], [r_ln2b], r_ln2b)
            for t in range(32):
                s_ = t // 16
                t0 = t * 128
                g2row = modg2[:, s_, :]
                for (yt_, r_yt), (di, r_di) in zip(ya, ((d1i, r_d1i), (d2i, r_d2i))):
                    pool.deps([r_di, r_Ys], [r_yt])
                    if r_yt.dsem is None:
                        r_yt.dsem = nc.alloc_semaphore(name=f"d{len(ALL_DMA_RES)}_" + r_yt.name)
                        ALL_DMA_RES.append(r_yt)
                    if r_yt.dcnt:
                        pool.wait({id(r_yt.dsem): (r_yt.dsem, r_yt.dcnt)})
                    nc.gpsimd.indirect_dma_start(
                        out=yt_[:, :], out_offset=None, in_=Ys_d[:, :],
                        in_offset=bass.IndirectOffsetOnAxis(ap=di[:, t:t + 1], axis=0)).then_inc(r_yt.dsem, 16)
                    r_yt.dcnt += 16
                    pool.mark([r_di, r_Ys], [r_yt], r_yt.dsem, r_yt.dcnt)
                sp.dma([(x1t[:], X1_d[t0:t0 + 128, :])], [r_X1], [r_x1t], r_x1t)
                dve.op(lambda t=t: V.tensor_scalar(out=fo[:], in0=ya[0][0][:], scalar1=pe1[:, t:t + 1], scalar2=None,
                                                   op0=ALU.mult), [ya[0][1], r_rsm], [r_fo])
                dve.op(lambda t=t: V.scalar_tensor_tensor(out=fo[:], in0=ya[1][0][:], scalar=pe2[:, t:t + 1], in1=fo[:],
                                                          op0=ALU.mult, op1=ALU.add), [ya[1][1], r_rsm, r_fo], [r_fo])
                dve.op(lambda g2row=g2row: V.tensor_tensor(out=fo[:], in0=fo[:], in1=g2row, op=ALU.mult),
                       [r_fo, r_modg2], [r_fo])
                dve.op(lambda: V.scalar_tensor_tensor(out=fo[:], in0=x1t[:], scalar=ALPHA, in1=fo[:], op0=ALU.mult,
                                                      op1=ALU.add), [r_x1t, r_fo], [r_fo])
                dve.op(lambda: V.bn_stats(out=bst2[:, 0, :], in_=fo[:, 0:512]), [r_fo], [r_bst2])
                dve.op(lambda: V.bn_stats(out=bst2[:, 1, :], in_=fo[:, 512:1024]), [r_fo], [r_bst2])
                dve.op(lambda: V.bn_aggr(out=sm2[:, 0:2], in_=bst2[:].rearrange("p a b -> p (a b)")),
                       [r_bst2], [r_sm2])
                dve.op(lambda: V.tensor_scalar(out=sm2[:, 2:3], in0=sm2[:, 1:2], scalar1=EPS, scalar2=None,
                                               op0=ALU.add), [r_sm2], [r_sm2])
                act.op(lambda: nc.scalar.activation(out=sm2[:, 2:3], in_=sm2[:, 2:3], func=AF.Sqrt), [r_sm2], [r_sm2])
                dve.op(lambda: V.reciprocal(out=sm2[:, 2:3], in_=sm2[:, 2:3]), [r_sm2], [r_sm2])
                dve.op(lambda: V.tensor_scalar(out=fo[:], in0=fo[:], scalar1=sm2[:, 0:1], scalar2=sm2[:, 2:3],
                                               op0=ALU.subtract, op1=ALU.mult), [r_fo, r_sm2], [r_fo])
                dve.op(lambda: V.tensor_tensor(out=fo[:], in0=fo[:], in1=ln2g[:], op=ALU.mult), [r_fo, r_ln2g], [r_fo])
                dve.op(lambda: V.tensor_tensor(out=fo[:], in0=fo[:], in1=ln2b[:], op=ALU.add), [r_fo, r_ln2b], [r_fo])
                sp.dma([(out_d[t0:t0 + 128, :], fo[:])], [r_fo], [], r_fo)
            sp.deps([], [r_fo])
            barrier()
    return nc


def make_inputs(core, x, c, w_ada, b_ada, w_in, conf_conv_w, conf_conv_b, conf_ln_g, conf_ln_b,
                ssm_conv_w, ssm_conv_b, ssm_dt_bias, ssm_A_log, ssm_D, ssm_norm_w, w_out,
                ln1_g, ln1_b, router_group_w, router_group_b, router_expert_w, router_expert_b,
                expert_w1, expert_w3, expert_w2, ln2_g, ln2_b, shared):
    f = np.float32
    xc = np.ascontiguousarray(x[2 * core:2 * core + 2].reshape(NTOK, D), dtype=f)
    cc = c[2 * core:2 * core + 2]
    m = dict(shared)
    m["x"] = xc
    m["xT"] = np.ascontiguousarray(xc.T)
    m["cT"] = np.ascontiguousarray(cc.reshape(2, 8, 128).transpose(2, 1, 0), dtype=f)
    return m


def rep(v, n=128):
    return np.ascontiguousarray(np.broadcast_to(np.asarray(v, dtype=np.float32).reshape(1, -1), (n, v.size)))


def fm(v, nch):
    return np.ascontiguousarray(np.asarray(v, dtype=np.float32).reshape(nch, 128).T)


def shared_inputs(w_ada, b_ada, w_in, conf_conv_w, conf_conv_b, conf_ln_g, conf_ln_b,
                  ssm_conv_w, ssm_conv_b, ssm_dt_bias, ssm_A_log, ssm_D, ssm_norm_w, w_out,
                  ln1_g, ln1_b, router_group_w, router_group_b, router_expert_w, router_expert_b,
                  expert_w1, expert_w3, expert_w2, ln2_g, ln2_b):
    f = np.float32
    sh = {}
    sh["w_ada"] = np.ascontiguousarray(w_ada[0], dtype=f)
    sh["b_ada_fm"] = fm(b_ada[0][:2048], 16)
    sh["b_ada_row"] = rep(b_ada[0][2048:])
    sh["w_in"] = np.ascontiguousarray(w_in[0], dtype=f)
    sh["ccw"] = np.ascontiguousarray(conf_conv_w[0].reshape(31, 8, 128).transpose(2, 1, 0), dtype=f)
    sh["ccb"] = fm(conf_conv_b[0], 8)
    sh["clg"] = fm(conf_ln_g[0], 8)
    sh["clb"] = fm(conf_ln_b[0], 8)
    sh["scw"] = np.ascontiguousarray(ssm_conv_w[0].reshape(4, 16, 128).transpose(2, 1, 0), dtype=f)
    sh["scb"] = fm(ssm_conv_b[0], 16)
    sh["dtb"] = rep(ssm_dt_bias[0])
    sh["alog"] = rep(ssm_A_log[0])
    sh["dsk"] = rep(ssm_D[0])
    sh["normw"] = rep(ssm_norm_w[0])
    sh["w_out"] = np.ascontiguousarray(w_out[0], dtype=f)
    sh["ln1g"] = rep(ln1_g[0])
    sh["ln1b"] = rep(ln1_b[0])
    sh["ln2g"] = rep(ln2_g[0])
    sh["ln2b"] = rep(ln2_b[0])
    wrr = np.concatenate([router_group_w[0], router_expert_w[0]], axis=1).astype(f)
    sh["wr"] = np.ascontiguousarray(wrr.reshape(8, 128, 72).transpose(1, 0, 2))
    sh["rb"] = rep(np.concatenate([router_group_b[0], router_expert_b[0]]))
    sh["w1"] = np.ascontiguousarray(np.asarray(expert_w1[0], dtype=f).reshape(NEXP, 8, 128, 512).transpose(0, 2, 1, 3)
                                    ).reshape(NEXP * 128, 4096)
    sh["w3"] = np.ascontiguousarray(np.asarray(expert_w3[0], dtype=f).reshape(NEXP, 8, 128, 512).transpose(0, 2, 1, 3)
                                    ).reshape(NEXP * 128, 4096)
    sh["w2"] = np.ascontiguousarray(np.asarray(expert_w2[0], dtype=f).reshape(NEXP, 4, 128, 1024).transpose(0, 2, 1, 3)
                                    ).reshape(NEXP * 128, 4096)
    sh["thr"] = rep(np.arange(64, dtype=f) * 128.0)
    sh["iotab"] = rep(np.arange(128, dtype=f))
    sh["pid"] = np.arange(128, dtype=f).reshape(128, 1)
    sh["identf"] = np.eye(128, dtype=f)
    sh["tri"] = np.triu(np.ones((128, 128), dtype=f))
    nm = np.where(np.triu(np.ones((128, 128), dtype=bool)), 0.0, -30000.0).astype(f)
    sh["negm"] = np.ascontiguousarray(np.tile(nm, (1, 4)))
    return sh


def kernel(x, c, w_ada, b_ada, w_in, conf_conv_w, conf_conv_b, conf_ln_g, conf_ln_b,
           ssm_conv_w, ssm_conv_b, ssm_dt_bias, ssm_A_log, ssm_D, ssm_norm_w, w_out,
           ln1_g, ln1_b, router_group_w, router_group_b, router_expert_w, router_expert_b,
           expert_w1, expert_w3, expert_w2, ln2_g, ln2_b, _stage="full", _trace=False):
    args = [np.asarray(a) for a in (w_ada, b_ada, w_in, conf_conv_w, conf_conv_b, conf_ln_g, conf_ln_b,
                                    ssm_conv_w, ssm_conv_b, ssm_dt_bias, ssm_A_log, ssm_D, ssm_norm_w, w_out,
                                    ln1_g, ln1_b, router_group_w, router_group_b, router_expert_w, router_expert_b,
                                    expert_w1, expert_w3, expert_w2, ln2_g, ln2_b)]
    x = np.asarray(x)
    c = np.asarray(c)
    sh = shared_inputs(*args)
    if _stage != "full":
        for k in ("w1", "w3", "w2"):
            sh[k] = np.ascontiguousarray(sh[k][0:128])
    in_maps = []
    for core in range(8):
        m = dict(sh)
        xc = np.ascontiguousarray(x[2 * core:2 * core + 2].reshape(NTOK, D), dtype=np.float32)
        m["x"] = xc
        m["xT"] = np.ascontiguousarray(xc.T)
        m["cT"] = np.ascontiguousarray(c[2 * core:2 * core + 2].reshape(2, 8, 128).transpose(2, 1, 0),
                                       dtype=np.float32)
        in_maps.append(m)
    nc = build_nc(_stage)
    if _trace:
        res = run_bass_kernel_spmd(nc, in_maps, core_ids=list(range(8)), trace=True)
        print("EXEC_TIME_NS", _stage, res.exec_time_ns)
    else:
        res = run_bass_kernel_spmd(nc, in_maps, core_ids=list(range(8)))
    outs = [np.asarray(r["out"], dtype=np.float32).reshape(2, SEQ, D) for r in res.results]
    return np.concatenate(outs, axis=0)
```

```python
import contextlib
import numpy as np
import concourse.bass as bass
import concourse.mybir as mybir
from concourse.bass_utils import run_bass_kernel_spmd

F32 = mybir.dt.float32
BF16 = mybir.dt.bfloat16
AF = mybir.ActivationFunctionType
ALU = mybir.AluOpType
AX = mybir.AxisListType

ALPHA = 2.0 ** 0.25
EPS = 1e-5
NTOK = 4096
SEQ = 2048
D = 1024
NEXP = 64
BIG = 1.0e9


ALL_DMA_RES = []


class Res:
    __slots__ = ("name", "wr", "rd", "dsem", "dcnt")

    def __init__(self, name):
        self.name = name
        self.wr = {}
        self.rd = {}
        self.dsem = None
        self.dcnt = 0


class Eng:
    def __init__(self, nc, eng, name):
        self.nc = nc
        self.eng = eng
        self.sem = nc.alloc_semaphore(name=name)
        self.cnt = 0
        self.waited = {}

    def wait(self, evs):
        for key, (sem, val) in list(evs.items()):
            if self.waited.get(key, 0) < val:
                self.eng.wait_ge(sem, val)
                self.waited[key] = val

    def deps(self, reads, writes):
        for r in reads:
            self.wait(r.wr)
        for w in writes:
            self.wait(w.wr)
            self.wait(w.rd)

    def mark(self, reads, writes, sem, val):
        key = id(sem)
        for r in reads:
            r.rd[key] = (sem, val)
        for w in writes:
            w.wr = {key: (sem, val)}
            w.rd = {}

    def op(self, fn, reads=(), writes=()):
        self.deps(reads, writes)
        ins = fn()
        self.cnt += 1
        ins.then_inc(self.sem, 1)
        self.mark(reads, writes, self.sem, self.cnt)

    def dma(self, parts, reads, writes, sres):
        self.deps(reads, writes)
        if sres.dsem is None:
            sres.dsem = self.nc.alloc_semaphore(name=f"d{len(ALL_DMA_RES)}_" + sres.name)
            ALL_DMA_RES.append(sres)
        if sres.dcnt:
            self.wait({id(sres.dsem): (sres.dsem, sres.dcnt)})
        for (o, i) in parts:
            self.eng.dma_start(out=o, in_=i).then_inc(sres.dsem, 16)
            sres.dcnt += 16
        self.mark(reads, writes, sres.dsem, sres.dcnt)


class EarlyExit(Exception):
    pass


def build_nc(stage="full"):
    nc = bass.Bass("TRN2", target_bir_lowering=False)
    ALL_DMA_RES.clear()
    st_all = contextlib.ExitStack()
    try:
        with st_all:
            _build(nc, stage, st_all)
    except EarlyExit:
        pass
    return nc


def _build(nc, stage, st_all):

    def din(name, shape, dt=F32):
        return nc.dram_tensor(name, list(shape), dt, kind="ExternalInput").ap()

    x_d = din("x", [NTOK, D])
    xT_d = din("xT", [D, NTOK])
    cT_d = din("cT", [128, 8, 2])
    wada_d = din("w_ada", [D, 6 * D])
    bada_fm_d = din("b_ada_fm", [128, 16])
    bada_row_d = din("b_ada_row", [128, 4 * D])
    win_d = din("w_in", [D, 5136])
    ccw_d = din("ccw", [128, 8, 31])
    ccb_d = din("ccb", [128, 8])
    clg_d = din("clg", [128, 8])
    clb_d = din("clb", [128, 8])
    scw_d = din("scw", [128, 16, 4])
    scb_d = din("scb", [128, 16])
    dtb_d = din("dtb", [128, 16])
    alog_d = din("alog", [128, 16])
    dsk_d = din("dsk", [128, 16])
    normw_d = din("normw", [128, D])
    wout_d = din("w_out", [2 * D, D])
    ln1g_d = din("ln1g", [128, D])
    ln1b_d = din("ln1b", [128, D])
    ln2g_d = din("ln2g", [128, D])
    ln2b_d = din("ln2b", [128, D])
    wr_d = din("wr", [128, 8, 72])
    rb_d = din("rb", [128, 72])
    nexp_decl = NEXP if stage == "full" else 1
    w1_d = din("w1", [nexp_decl * 128, 4096])
    w3_d = din("w3", [nexp_decl * 128, 4096])
    w2_d = din("w2", [nexp_decl * 128, 4096])
    thr_d = din("thr", [128, 64])
    iotab_d = din("iotab", [128, 128])
    pid_d = din("pid", [128, 1])
    identf_d = din("identf", [128, 128])
    tri_d = din("tri", [128, 128])
    negm_d = din("negm", [128, 512])
    out_d = nc.dram_tensor("out", [NTOK, D], F32, kind="ExternalOutput").ap()

    UF_d = nc.dram_tensor("UF", [D, NTOK], BF16, kind="Internal").ap()
    X1_d = nc.dram_tensor("X1", [NTOK, D], F32, kind="Internal").ap()
    H2_d = nc.dram_tensor("H2", [NTOK, D], BF16, kind="Internal").ap()
    Xs_d = nc.dram_tensor("Xs", [4 * NTOK, D], BF16, kind="Internal").ap()
    Ys_d = nc.dram_tensor("Ys", [4 * NTOK, D], F32, kind="Internal").ap()

    r_UF, r_X1, r_H2T, r_YN = Res("UF"), Res("X1"), Res("H2T"), Res("YN")
    YN_d = nc.dram_tensor("YN", [D, NTOK], BF16, kind="Internal").ap()
    pe = Eng(nc, nc.tensor, "s_pe")
    act = Eng(nc, nc.scalar, "s_act")
    dve = Eng(nc, nc.vector, "s_dve")
    pool = Eng(nc, nc.gpsimd, "s_pool")
    sp = Eng(nc, nc.sync, "s_sp")

    es = st_all
    engines = [pe, act, dve, pool, sp]

    def ck(n):
        if stage == f"K{n}":
            barrier()
            raise EarlyExit()

    def barrier():
        evs = {}
        for E in engines:
            if E.cnt:
                evs[id(E.sem)] = (E.sem, E.cnt)
        for r in ALL_DMA_RES:
            if r.dcnt:
                evs[id(r.dsem)] = (r.dsem, r.dcnt)
        for E in engines:
            E.wait(evs)

    cast_rr = [0]

    def load_cast(dst_aps, src_aps, stg, r_dst):
        for dst, src in zip(dst_aps, src_aps):
            t, r = stg[cast_rr[0] % len(stg)]
            n = dst.shape[-1]
            sp.dma([(t[:, 0:n], src)], [], [r], r)
            if cast_rr[0] % 2 == 0:
                act.op(lambda t=t, dst=dst, n=n: nc.scalar.activation(out=dst, in_=t[:, 0:n], func=AF.Copy), [r], [r_dst])
            else:
                pool.op(lambda t=t, dst=dst, n=n: nc.gpsimd.tensor_copy(out=dst, in_=t[:, 0:n]), [r], [r_dst])
            cast_rr[0] += 1

    sb_cnt = [0]

    def sb(st, name, shape, dt=F32):
        sb_cnt[0] += 1
        t = st.enter_context(nc.sbuf_tensor(f"sb{sb_cnt[0]}_" + name, list(shape), dt))
        return t, Res(name)

    ps = []
    psr = []
    for i in range(7):
        t = es.enter_context(nc.psum_tensor(f"ps{i}", [128, 512], F32))
        ps.append(t)
        psr.append(Res(f"ps{i}"))
    ps7b = es.enter_context(nc.psum_tensor("ps7b", [128, 1024], BF16))
    ps.append(None)
    psr.append(Res("ps7b"))

    def psbf(i):
        return ps7b[:]

    identf, r_identf = sb(es, "identf", [128, 128])
    identb, r_identb = sb(es, "identb", [128, 128], BF16)
    onesf, r_onesf = sb(es, "onesf", [128, 128])
    onesb, r_onesb = sb(es, "onesb", [128, 128], BF16)
    tri, r_tri = sb(es, "tri", [128, 128])
    negm, r_negm = sb(es, "negm", [128, 512])
    modg2, r_modg2 = sb(es, "modg2", [128, 2, D])
    rsm, r_rsm = sb(es, "rsm", [128, 32 * 8])
    d1i, r_d1i = sb(es, "d1i", [128, 32], mybir.dt.int32)
    d2i, r_d2i = sb(es, "d2i", [128, 32], mybir.dt.int32)
    widx, r_widx = sb(es, "widx", [128, 128], mybir.dt.int32)
    modfm, r_modfm = sb(es, "modfm", [128, 16, 2])
    ccw, r_ccw = sb(es, "ccw", [128, 8, 31])
    pfm, r_pfm = sb(es, "pfm", [128, 64])
    prow, r_prow = sb(es, "prow", [128, 64])
    rb, r_rb = sb(es, "rb", [128, 72])
    stMid = es.enter_context(contextlib.ExitStack())
    modrow, r_modrow = sb(stMid, "modrow", [128, 2, 3 * D])
    Lg, r_Lg = sb(stMid, "Lg", [128, 32, 72])

    sp.dma([(identf[:], identf_d[:, :])], [], [r_identf], r_identf)
    sp.dma([(tri[:], tri_d[:, :])], [], [r_tri], r_tri)
    sp.dma([(negm[:], negm_d[:, :])], [], [r_negm], r_negm)
    sp.dma([(ccw[:], ccw_d[:, :, :])], [], [r_ccw], r_ccw)
    sp.dma([(pfm[:, 0:8], ccb_d[:, :]), (pfm[:, 8:16], clg_d[:, :]), (pfm[:, 16:24], clb_d[:, :]),
            (pfm[:, 24:40], scb_d[:, :])], [], [r_pfm], r_pfm)
    sp.dma([(prow[:, 0:16], dtb_d[:, :]), (prow[:, 48:64], alog_d[:, :]), (prow[:, 32:48], dsk_d[:, :])],
           [], [r_prow], r_prow)
    sp.dma([(rb[:], rb_d[:, :])], [], [r_rb], r_rb)
    dve.op(lambda: nc.vector.tensor_copy(out=identb[:], in_=identf[:]), [r_identf], [r_identb])
    dve.op(lambda: nc.vector.memset(onesf[:], 1.0), [], [r_onesf])
    dve.op(lambda: nc.vector.memset(onesb[:], 1.0), [], [r_onesb])
    act.op(lambda: nc.scalar.activation(out=prow[:, 16:32], in_=prow[:, 48:64], func=AF.Exp), [r_prow], [r_prow])
    dve.op(lambda: nc.vector.tensor_scalar(out=prow[:, 16:32], in0=prow[:, 16:32], scalar1=-1.0, scalar2=None,
                                           op0=ALU.mult), [r_prow], [r_prow])

    with contextlib.ExitStack() as st0:
        cT, r_cT = sb(st0, "cT", [128, 8, 2])
        crep, r_crep = sb(st0, "crep", [128, 2, 8, 128])
        bfm, r_bfm = sb(st0, "bfm", [128, 16])
        brow, r_brow = sb(st0, "brow", [128, 4 * D])
        slabA = [sb(st0, f"slabA{i}", [128, 8, 1024]) for i in range(2)]
        slabB = [sb(st0, f"slabB{i}", [128, 8, 512]) for i in range(2)]
        sp.dma([(cT[:], cT_d[:, :, :])], [], [r_cT], r_cT)
        sp.dma([(bfm[:], bada_fm_d[:, :])], [], [r_bfm], r_bfm)
        sp.dma([(brow[:], bada_row_d[:, :])], [], [r_brow], r_brow)
        act.op(lambda: nc.scalar.activation(out=cT[:], in_=cT[:], func=AF.Silu), [r_cT], [r_cT])
        for b in range(2):
            dve.op(lambda b=b: nc.vector.tensor_copy(
                out=crep[:, b, :, :], in_=cT[:, :, b:b + 1].broadcast_to([128, 8, 128])), [r_cT], [r_crep])
        for s in range(2):
            t, r = slabA[s]
            sp.dma([(t[:, k, :], wada_d[k * 128:(k + 1) * 128, s * 1024:(s + 1) * 1024]) for k in range(8)],
                   [], [r], r)
            for j in range(8):
                fc = s * 8 + j

                def mm(fc=fc, j=j, t=t):
                    ins = None
                    for k in range(8):
                        ins = nc.tensor.matmul(ps[0][:, 2 * fc:2 * fc + 2], lhsT=t[:, k, j * 128:(j + 1) * 128],
                                               rhs=cT[:, k, :], start=(k == 0), stop=(k == 7))
                    return ins
                pe.op(mm, [r, r_cT], [psr[0]])
        dve.op(lambda: nc.vector.tensor_tensor(
            out=modfm[:], in0=ps[0][:, 0:32].rearrange("p (f b) -> p f b", b=2),
            in1=bfm[:].unsqueeze(2).broadcast_to([128, 16, 2]), op=ALU.add), [psr[0], r_bfm], [r_modfm])
        dve.op(lambda: nc.vector.tensor_scalar(out=modfm[:, 8:16, :], in0=modfm[:, 8:16, :], scalar1=1.0,
                                               scalar2=None, op0=ALU.add), [r_modfm], [r_modfm])
        for tsl in range(8):
            t, r = slabB[tsl % 2]
            c0 = 2048 + tsl * 512
            sp.dma([(t[:, k, :], wada_d[k * 128:(k + 1) * 128, c0:c0 + 512]) for k in range(8)], [], [r], r)
            for b in range(2):
                pb = 1 + b

                def mm(b=b, t=t, pb=pb):
                    ins = None
                    for k in range(8):
                        ins = nc.tensor.matmul(ps[pb][:, :], lhsT=crep[:, b, k, :], rhs=t[:, k, :],
                                               start=(k == 0), stop=(k == 7))
                    return ins
                pe.op(mm, [r, r_crep], [psr[pb]])
                if tsl < 6:
                    dve.op(lambda b=b, pb=pb, tsl=tsl: nc.vector.tensor_tensor(
                        out=modrow[:, b, tsl * 512:(tsl + 1) * 512], in0=ps[pb][:, :],
                        in1=brow[:, tsl * 512:(tsl + 1) * 512], op=ALU.add), [psr[pb], r_brow], [r_modrow])
                else:
                    dve.op(lambda b=b, pb=pb, tsl=tsl: nc.vector.tensor_tensor(
                        out=modg2[:, b, (tsl - 6) * 512:(tsl - 5) * 512], in0=ps[pb][:, :],
                        in1=brow[:, tsl * 512:(tsl + 1) * 512], op=ALU.add), [psr[pb], r_brow], [r_modg2])
        dve.op(lambda: nc.vector.tensor_scalar(out=modrow[:, :, 2048:3072], in0=modrow[:, :, 2048:3072],
                                               scalar1=1.0, scalar2=None, op0=ALU.add), [r_modrow], [r_modrow])

    barrier()
    if stage == "0":
        sp.dma([(out_d[0:128, :], modrow[:, 0, 0:1024])], [r_modrow], [], r_modrow)
        sp.dma([(out_d[128:256, :], modg2[:, 1, :])], [r_modg2], [], r_modg2)
        sp.dma([(out_d[256:384, 0:32], modfm[:].rearrange("p a b -> p (a b)"))], [r_modfm], [], r_modfm)
        sp.deps([], [r_modrow, r_modg2, r_modfm])
        raise EarlyExit()

    xT_v = xT_d.rearrange("(k p) t -> p k t", p=128)
    UF_v = UF_d.rearrange("(k p) t -> p k t", p=128)
    YN_v = YN_d.rearrange("(k p) t -> p k t", p=128)

    def load_hT(st_tiles, tok0, nt, bsel):
        xTf, r_xTf, hT, r_hT = st_tiles
        sp.dma([(xTf[:, :, 0:nt], xT_v[:, :, tok0:tok0 + nt])], [], [r_xTf], r_xTf)
        for k in range(8):
            eng, E = (nc.vector, dve) if k % 2 == 0 else (nc.gpsimd, pool)
            E.op(lambda k=k, eng=eng: eng.tensor_scalar(
                out=hT[:, k, 0:nt], in0=xTf[:, k, 0:nt], scalar1=modfm[:, 8 + k, bsel:bsel + 1],
                scalar2=modfm[:, k, bsel:bsel + 1], op0=ALU.mult, op1=ALU.add), [r_xTf, r_modfm], [r_hT])

    TB = 256
    with contextlib.ExitStack() as stA:
        winA, r_winA = sb(stA, "winA", [128, 8, 2048], BF16)
        dgA, r_dgA = sb(stA, "dgA", [128, 8, 31, 128], BF16)
        r_p6 = [psr[6], psr[6]]
        xTf, r_xTf = sb(stA, "xTfA", [128, 8, TB])
        hT, r_hT = sb(stA, "hTA", [128, 8, TB], BF16)
        sig = [sb(stA, f"sig{i}", [128, TB]) for i in range(2)]
        ub, r_ub = sb(stA, "ub", [128, 8, 30 + TB], BF16)
        r_uc = [Res(f"uc{c}") for c in range(8)]
        cv, _ = sb(stA, "cv", [128, 8, TB])
        r_cv = [Res(f"cv{c}") for c in range(8)]
        cvq = [sb(stA, f"cvq{i}", [128, 2, TB], BF16) for i in range(2)]
        mean, r_mean = sb(stA, "mean", [128, TB])
        var, r_var = sb(stA, "var", [128, TB])
        rstd, r_rstd = sb(stA, "rstd", [128, TB])
        uf, r_uf = sb(stA, "uf", [128, 8, TB], BF16)
        stgA = [sb(stA, f"stgA{i}", [128, 1024]) for i in range(2)]
        load_cast([winA[:, k, h * 1024:(h + 1) * 1024] for k in range(8) for h in range(2)],
                  [win_d[k * 128:(k + 1) * 128, h * 1024:(h + 1) * 1024] for k in range(8) for h in range(2)],
                  stgA, r_winA)
        for c in range(8):
            for k in range(31):
                dve.op(lambda c=c, k=k: nc.vector.tensor_scalar(
                    out=dgA[:, c, k, :], in0=identb[:], scalar1=ccw[:, c, k:k + 1], scalar2=None, op0=ALU.mult),
                    [r_identb, r_ccw], [r_dgA])
        for s in range(2):
            for c in range(8):
                dve.op(lambda c=c: nc.vector.memset(ub[:, c, 0:30], 0.0), [], [r_uc[c]])
            for j in range(SEQ // TB):
                tok0 = s * SEQ + j * TB
                load_hT((xTf, r_xTf, hT, r_hT), tok0, TB, s)
                for c in range(8):
                    pv, pg = (0, 1) if c % 2 == 0 else (2, 3)
                    for (pi, col0) in ((pv, c * 128), (pg, 1024 + c * 128)):
                        def mm(pi=pi, col0=col0):
                            ins = None
                            for k in range(8):
                                ins = nc.tensor.matmul(ps[pi][:, 0:TB], lhsT=winA[:, k, col0:col0 + 128],
                                                       rhs=hT[:, k, :], start=(k == 0), stop=(k == 7))
                            return ins
                        pe.op(mm, [r_winA, r_hT], [psr[pi]])
                    sg, r_sg = sig[c % 2]
                    act.op(lambda pg=pg, sg=sg: nc.scalar.activation(out=sg[:], in_=ps[pg][:, 0:TB], func=AF.Sigmoid),
                           [psr[pg]], [r_sg])
                    dve.op(lambda pv=pv, sg=sg, c=c: nc.vector.tensor_tensor(
                        out=ub[:, c, 30:30 + TB], in0=ps[pv][:, 0:TB], in1=sg[:], op=ALU.mult),
                        [psr[pv], r_sg], [r_uc[c]])
                ck(51)
                for c in range(8):
                    hp = c % 2

                    def mm(c=c, hp=hp):
                        ins = None
                        for k in range(31):
                            ins = nc.tensor.matmul(ps[6][:, hp * TB:(hp + 1) * TB], lhsT=dgA[:, c, k, :],
                                                   rhs=ub[:, c, k:k + TB], start=(k == 0), stop=(k == 30))
                        return ins
                    pe.op(mm, [r_dgA, r_uc[c]], [r_p6[hp]])
                    ck(52)
                    act.op(lambda c=c, hp=hp: nc.scalar.activation(
                        out=cv[:, c, :], in_=ps[6][:, hp * TB:(hp + 1) * TB], func=AF.Identity,
                        bias=pfm[:, c:c + 1]), [r_p6[hp], r_pfm], [r_cv[c]])
                    ck(53)
                ck(54)
                for c in range(8):
                    eng, E = (nc.vector, dve) if c < 5 else (nc.gpsimd, pool)
                    E.op(lambda c=c, eng=eng: eng.tensor_copy(out=ub[:, c, 0:30], in_=ub[:, c, TB:TB + 30]),
                         [r_uc[c]], [r_uc[c]])
                for c in range(8):
                    q, r_q = cvq[c % 2]
                    act.op(lambda c=c, q=q: nc.scalar.activation(out=q[:, 0, :], in_=cv[:, c, :], func=AF.Copy),
                           [r_cv[c]], [r_q])
                    act.op(lambda c=c, q=q: nc.scalar.activation(out=q[:, 1, :], in_=cv[:, c, :], func=AF.Square),
                           [r_cv[c]], [r_q])

                    def mm(c=c, q=q):
                        nc.tensor.matmul(ps[4][:, 0:TB], lhsT=onesb[:], rhs=q[:, 0, :], start=(c == 0), stop=(c == 7))
                        return nc.tensor.matmul(ps[5][:, 0:TB], lhsT=onesb[:], rhs=q[:, 1, :], start=(c == 0),
                                                stop=(c == 7))
                    pe.op(mm, [r_q, r_onesb], [psr[4], psr[5]])
                ck(55)
                dve.op(lambda: nc.vector.tensor_scalar(out=mean[:], in0=ps[4][:, 0:TB], scalar1=1.0 / 1024.0,
                                                       scalar2=None, op0=ALU.mult), [psr[4]], [r_mean])
                dve.op(lambda: nc.vector.tensor_tensor(out=var[:], in0=mean[:], in1=mean[:], op=ALU.mult),
                       [r_mean], [r_var])
                dve.op(lambda: nc.vector.scalar_tensor_tensor(out=var[:], in0=ps[5][:, 0:TB], scalar=1.0 / 1024.0,
                                                              in1=var[:], op0=ALU.mult, op1=ALU.subtract),
                       [psr[5], r_var], [r_var])
                dve.op(lambda: nc.vector.tensor_scalar(out=var[:], in0=var[:], scalar1=0.0, scalar2=EPS,
                                                       op0=ALU.max, op1=ALU.add), [r_var], [r_var])
                act.op(lambda: nc.scalar.activation(out=var[:], in_=var[:], func=AF.Sqrt), [r_var], [r_var])
                dve.op(lambda: nc.vector.reciprocal(out=rstd[:], in_=var[:]), [r_var], [r_rstd])
                for c in range(8):
                    eng, E = (nc.vector, dve) if c < 5 else (nc.gpsimd, pool)
                    E.op(lambda c=c, eng=eng: eng.tensor_tensor(out=cv[:, c, :], in0=cv[:, c, :], in1=mean[:],
                                                                op=ALU.subtract), [r_cv[c], r_mean], [r_cv[c]])
                    E.op(lambda c=c, eng=eng: eng.tensor_tensor(out=cv[:, c, :], in0=cv[:, c, :], in1=rstd[:],
                                                                op=ALU.mult), [r_cv[c], r_rstd], [r_cv[c]])
                    act.op(lambda c=c: nc.scalar.activation(out=uf[:, c, :], in_=cv[:, c, :], func=AF.Silu,
                                                            bias=pfm[:, 16 + c:17 + c], scale=pfm[:, 8 + c:9 + c]),
                           [r_cv[c], r_pfm], [r_uf])
                act.dma([(UF_v[:, :, tok0:tok0 + TB], uf[:])], [r_uf], [r_UF], r_uf)
                ck(56)
                if stage == "A1":
                    dbg, r_dbg = sb(stA, "dbg", [128, 1024])
                    def dump(row, src_ap, n, rs):
                        dve.op(lambda: nc.vector.tensor_copy(out=dbg[:, 0:n], in_=src_ap), rs, [r_dbg])
                        sp.dma([(out_d[row:row + 128, 0:n], dbg[:, 0:n])], [r_dbg], [], r_dbg)
                    dump(0, winA[:, 0, 0:1024], 1024, [r_winA])
                    dump(128, hT[:, 0, :], 512, [r_hT])
                    dump(256, ub[:, 0, 0:542], 542, [r_uc[0]])
                    dump(384, cv[:, 0, :], 512, [r_cv[0]])
                    dump(512, mean[:], 512, [r_mean])
                    dump(640, var[:], 512, [r_var])
                    dump(768, rstd[:], 512, [r_rstd])
                    dump(896, uf[:, 0, :], 512, [r_uf])
                    sp.deps([], [r_dbg, r_uf])
                    raise EarlyExit()
                if stage == "A":
                    j4 = tok0 // 1024
                    col = tok0 % 1024
                    dve.op(lambda: nc.vector.tensor_copy(out=xTf[:], in_=uf[:]), [r_uf], [r_xTf])
                    sp.dma([(out_d[j4 * 1024:(j4 + 1) * 1024, col:col + TB].rearrange("(c p) t -> p c t", p=128),
                             xTf[:])], [r_xTf], [], r_xTf)

    barrier()
    if stage == "A":
        sp.deps([], [r_uf, r_xTf])
        raise EarlyExit()

    TBB = 128
    with contextlib.ExitStack() as stB:
        winB, r_winB = sb(stB, "winB", [128, 8, 3088], BF16)
        dg, r_dg = sb(stB, "dg", [128, 16, 4, 128], BF16)
        scw, r_scw = sb(stB, "scw", [128, 16, 4])
        normw, r_normw = sb(stB, "normw", [128, D])
        xTf, r_xTf = sb(stB, "xTfB", [128, 8, TBB])
        hT, r_hT = sb(stB, "hTB", [128, 8, TBB], BF16)
        xpre, r_xpre = sb(stB, "xpre", [128, 16, 3 + TBB], BF16)
        xpost, r_xpost = sb(stB, "xpost", [128, 16, TBB], BF16)
        tA, r_tA = sb(stB, "tA", [128, D])
        tB, r_tB = sb(stB, "tB", [128, D])
        tC, r_tC = sb(stB, "tC", [128, D])
        Rm, r_Rm = sb(stB, "Rm", [128, D])
        dec, r_dec = sb(stB, "dec", [128, D])
        MT, r_MT = sb(stB, "MT", [128, 16, 128], BF16)
        cbs, r_cbs = sb(stB, "cbs", [128, 512])
        xc, r_xc = sb(stB, "xc", [128, D], BF16)
        xcd, r_xcd = sb(stB, "xcd", [128, D], BF16)
        ynb, r_ynb = sb(stB, "ynb", [128, D], BF16)
        ynT, r_ynT = sb(stB, "ynT", [128, 8, 128], BF16)
        Btok, r_Btok = sb(stB, "Btok", [128, 512], BF16)
        S, r_S = sb(stB, "S", [128, D])
        Sb, r_Sb = sb(stB, "Sb", [128, D], BF16)
        sm, r_sm = sb(stB, "smB", [128, 16 * 12])
        dtp = sm[:, 0:16]
        dtv = sm[:, 16:32]
        av = sm[:, 32:48]
        acs = sm[:, 48:64]
        nacs = sm[:, 64:80]
        eacs = sm[:, 80:96]
        dif = sm[:, 96:112]
        dte = sm[:, 112:128]
        cdr = sm[:, 128:144]
        ss4 = sm[:, 144:148]
        rs4 = sm[:, 148:152]
        mv = sm[:, 152:154]
        rs1 = sm[:, 154:155]
        junk = dec

        stgB = [sb(stB, f"stgB{i}", [128, 1544]) for i in range(2)]
        for half in range(2):
            c0 = 2048 + half * 1544
            load_cast([winB[:, k, half * 1544:(half + 1) * 1544] for k in range(8)],
                      [win_d[k * 128:(k + 1) * 128, c0:c0 + 1544] for k in range(8)], stgB, r_winB)
        sp.dma([(scw[:], scw_d[:, :, :])], [], [r_scw], r_scw)
        sp.dma([(normw[:], normw_d[:, :])], [], [r_normw], r_normw)
        for c in range(16):
            for k in range(4):
                dve.op(lambda c=c, k=k: nc.vector.tensor_scalar(
                    out=dg[:, c, k, :], in0=identb[:], scalar1=scw[:, c, k:k + 1], scalar2=None, op0=ALU.mult),
                    [r_identb, r_scw], [r_dg])

        ZC0, XC0, DC0 = 0, 1024, 3072
        for s in range(2):
            dve.op(lambda: nc.vector.memset(xpre[:, :, 0:3], 0.0), [], [r_xpre])
            dve.op(lambda: nc.vector.memset(S[:], 0.0), [], [r_S])
            dve.op(lambda: nc.vector.memset(Sb[:], 0.0), [], [r_Sb])
            for j in range(SEQ // TBB):
                tok0 = s * SEQ + j * TBB
                load_hT((xTf, r_xTf, hT, r_hT), tok0, TBB, s)
                for cp in range(8):
                    pi = cp % 2
                    for hh in range(2):
                        c = cp * 2 + hh

                        def mm(pi=pi, hh=hh, c=c):
                            ins = None
                            for k in range(8):
                                ins = nc.tensor.matmul(ps[pi][:, hh * TBB:(hh + 1) * TBB],
                                                       lhsT=winB[:, k, XC0 + c * 128:XC0 + (c + 1) * 128],
                                                       rhs=hT[:, k, :], start=(k == 0), stop=(k == 7))
                            return ins
                        pe.op(mm, [r_winB, r_hT], [psr[pi]])
                    act.op(lambda pi=pi, cp=cp: nc.scalar.activation(
                        out=xpre[:, 2 * cp:2 * cp + 2, 3:3 + TBB],
                        in_=ps[pi][:, 0:2 * TBB].rearrange("p (a t) -> p a t", a=2), func=AF.Copy),
                        [psr[pi]], [r_xpre])
                for cp in range(8):
                    pi = 2 + cp % 2
                    for hh in range(2):
                        c = cp * 2 + hh

                        def mm(pi=pi, hh=hh, c=c):
                            ins = None
                            for k in range(4):
                                ins = nc.tensor.matmul(ps[pi][:, hh * TBB:(hh + 1) * TBB], lhsT=dg[:, c, k, :],
                                                       rhs=xpre[:, c, k:k + TBB], start=(k == 0), stop=(k == 3))
                            return ins
                        pe.op(mm, [r_dg, r_xpre], [psr[pi]])
                        act.op(lambda pi=pi, hh=hh, c=c: nc.scalar.activation(
                            out=xpost[:, c, :], in_=ps[pi][:, hh * TBB:(hh + 1) * TBB], func=AF.Silu,
                            bias=pfm[:, 24 + c:25 + c]), [psr[pi], r_pfm], [r_xpost])
                dve.op(lambda: nc.vector.tensor_copy(out=xpre[:, :, 0:3], in_=xpre[:, :, TBB:TBB + 3]),
                       [r_xpre], [r_xpre])

                ck(1)
                for q in range(TBB // 128):
                    o = q * 128
                    t0 = tok0 + o
                    tile_idx = t0 // 128
                    for hf in range(2):
                        def mm(hf=hf, o=o):
                            ins = None
                            for k in range(8):
                                ins = nc.tensor.matmul(ps[hf][:, :], lhsT=hT[:, k, o:o + 128],
                                                       rhs=winB[:, k, ZC0 + hf * 512:ZC0 + (hf + 1) * 512],
                                                       start=(k == 0), stop=(k == 7))
                            return ins
                        pe.op(mm, [r_hT, r_winB], [psr[hf]])

                    def mm(o=o):
                        ins = None
                        for k in range(8):
                            ins = nc.tensor.matmul(ps[2][:, 0:16], lhsT=hT[:, k, o:o + 128],
                                                   rhs=winB[:, k, DC0:DC0 + 16], start=(k == 0), stop=(k == 7))
                        return ins
                    pe.op(mm, [r_hT, r_winB], [psr[2]])
                    ck(2)
                    dve.op(lambda: nc.vector.tensor_tensor(out=dtp, in0=ps[2][:, 0:16], in1=prow[:, 0:16],
                                                           op=ALU.add), [psr[2], r_prow], [r_sm])
                    act.op(lambda: nc.scalar.activation(out=dtp, in_=dtp, func=AF.Exp), [r_sm], [r_sm])
                    act.op(lambda: nc.scalar.activation(out=dtv, in_=dtp, func=AF.Ln, bias=1.0), [r_sm], [r_sm])
                    dve.op(lambda: nc.vector.tensor_tensor(out=av, in0=dtv, in1=prow[:, 16:32], op=ALU.mult),
                           [r_sm, r_prow], [r_sm])

                    def mm():
                        nc.tensor.matmul(ps[2][:, 16:32], lhsT=tri[:], rhs=av, start=True, stop=True)
                        return nc.tensor.matmul(ps[2][:, 32:48], lhsT=onesf[:], rhs=av, start=True, stop=True)
                    pe.op(mm, [r_tri, r_onesf, r_sm], [psr[2]])
                    dve.op(lambda: nc.vector.tensor_copy(out=acs, in_=ps[2][:, 16:32]), [psr[2]], [r_sm])
                    dve.op(lambda: nc.vector.tensor_scalar(out=nacs, in0=acs, scalar1=-1.0, scalar2=None,
                                                           op0=ALU.mult), [r_sm], [r_sm])
                    dve.op(lambda: nc.vector.tensor_tensor(out=dif, in0=ps[2][:, 32:48], in1=acs, op=ALU.subtract),
                           [psr[2], r_sm], [r_sm])
                    act.op(lambda: nc.scalar.activation(out=eacs, in_=acs, func=AF.Exp), [r_sm], [r_sm])
                    act.op(lambda: nc.scalar.activation(out=dte, in_=dif, func=AF.Exp), [r_sm], [r_sm])
                    act.op(lambda: nc.scalar.activation(out=cdr, in_=ps[2][:, 32:48], func=AF.Exp),
                           [psr[2], r_sm], [r_sm])
                    ck(3)
                    def mm(o=o):
                        ins = None
                        for g in range(4):
                            ins = nc.tensor.matmul(ps[3][:, g * 128:(g + 1) * 128], lhsT=xpost[:, 8 + g, o:o + 128],
                                                   rhs=xpost[:, 12 + g, o:o + 128], start=True, stop=True)
                        return ins
                    pe.op(mm, [r_xpost], [psr[3]])
                    act.op(lambda: nc.scalar.activation(out=cbs[:], in_=ps[3][:, :], func=AF.Copy), [psr[3]], [r_cbs])
                    ck(4)
                    def mm(o=o):
                        ins = None
                        for c in range(8):
                            ins = nc.tensor.transpose(out=psbf(7)[:, c * 128:(c + 1) * 128], in_=xpost[:, c, o:o + 128],
                                                      identity=identb[:])
                        return ins
                    pe.op(mm, [r_xpost, r_identb], [psr[7]])
                    ck(41)
                    act.op(lambda: nc.scalar.activation(out=tB[:], in_=psbf(7)[:, :], func=AF.Copy), [psr[7]], [r_tB])
                    ck(42)
                    dve.op(lambda: nc.vector.tensor_tensor(
                        out=xc[:].rearrange("p (h d) -> p h d", d=64),
                        in0=tB[:].rearrange("p (h d) -> p h d", d=64),
                        in1=dtv.unsqueeze(2).broadcast_to([128, 16, 64]), op=ALU.mult), [r_tB, r_sm], [r_xc])
                    ck(43)
                    dve.op(lambda: nc.vector.tensor_tensor(
                        out=xcd[:].rearrange("p (h d) -> p h d", d=64),
                        in0=xc[:].rearrange("p (h d) -> p h d", d=64),
                        in1=dte.unsqueeze(2).broadcast_to([128, 16, 64]), op=ALU.mult), [r_xc, r_sm], [r_xcd])
                    ck(5)
                    for hh in range(2):
                        dve.op(lambda hh=hh: nc.vector.tensor_tensor(
                            out=Rm[:].rearrange("p (h l) -> p h l", l=128),
                            in0=tri[:].unsqueeze(1).broadcast_to([128, 8, 128]),
                            in1=av[:, 8 * hh:8 * hh + 8].unsqueeze(2).broadcast_to([128, 8, 128]), op=ALU.mult),
                            [r_tri, r_sm], [r_Rm])
                        for jb in range(2):
                            def mm(jb=jb):
                                nc.tensor.matmul(ps[4 + jb][:, :], lhsT=onesf[:], rhs=Rm[:, jb * 512:(jb + 1) * 512],
                                                 start=True, stop=False)
                                return nc.tensor.matmul(ps[4 + jb][:, :], lhsT=identf[:], rhs=negm[:],
                                                        start=False, stop=True)
                            pe.op(mm, [r_onesf, r_identf, r_negm, r_Rm], [psr[4 + jb]])
                        for jj in range(8):
                            h = 8 * hh + jj
                            act.op(lambda jj=jj, h=h: nc.scalar.activation(
                                out=dec[:, jj * 128:(jj + 1) * 128],
                                in_=ps[4 + jj // 4][:, (jj % 4) * 128:(jj % 4 + 1) * 128], func=AF.Exp,
                                bias=nacs[:, h:h + 1]), [psr[4 + jj // 4], r_sm], [r_dec])
                        for jb in range(2):
                            g = 2 * hh + jb
                            dve.op(lambda jb=jb, g=g, hh=hh: nc.vector.tensor_tensor(
                                out=MT[:, 8 * hh + 4 * jb:8 * hh + 4 * jb + 4, :],
                                in0=dec[:, jb * 512:(jb + 1) * 512].rearrange("p (h l) -> p h l", l=128),
                                in1=cbs[:, g * 128:(g + 1) * 128].unsqueeze(1).broadcast_to([128, 4, 128]),
                                op=ALU.mult), [r_dec, r_cbs], [r_MT])
                    ck(6)
                    for bk in range(2):
                        def mm(bk=bk, o=o):
                            ins = None
                            for gg in range(2):
                                g = 2 * bk + gg
                                ins = nc.tensor.matmul(ps[4 + bk][:, gg * 256:(gg + 1) * 256],
                                                       lhsT=xpost[:, 12 + g, o:o + 128],
                                                       rhs=Sb[:, g * 256:(g + 1) * 256], start=True, stop=True)
                            return ins
                        pe.op(mm, [r_xpost, r_Sb], [psr[4 + bk]])
                        dve.op(lambda bk=bk: nc.vector.tensor_tensor(
                            out=tC[:, bk * 512:(bk + 1) * 512].rearrange("p (h d) -> p h d", d=64),
                            in0=ps[4 + bk][:, :].rearrange("p (h d) -> p h d", d=64),
                            in1=eacs[:, 8 * bk:8 * bk + 8].unsqueeze(2).broadcast_to([128, 8, 64]), op=ALU.mult),
                            [psr[4 + bk], r_sm], [r_tC])
                    ck(7)
                    for bk in range(2):
                        def mm(bk=bk):
                            ins = None
                            for hh8 in range(8):
                                h = 8 * bk + hh8
                                ins = nc.tensor.matmul(ps[(6, 3)[bk]][:, hh8 * 64:(hh8 + 1) * 64], lhsT=MT[:, h, :],
                                                       rhs=xc[:, h * 64:(h + 1) * 64], start=True, stop=True)
                            return ins
                        pe.op(mm, [r_MT, r_xc], [psr[(6, 3)[bk]]])
                        dve.op(lambda bk=bk: nc.vector.tensor_tensor(
                            out=tC[:, bk * 512:(bk + 1) * 512], in0=ps[(6, 3)[bk]][:, :],
                            in1=tC[:, bk * 512:(bk + 1) * 512], op=ALU.add), [psr[(6, 3)[bk]], r_tC], [r_tC])
                    ck(8)
                    dve.op(lambda: nc.vector.tensor_tensor(
                        out=tB[:].rearrange("p (h d) -> p h d", d=64), in0=tB[:].rearrange("p (h d) -> p h d", d=64),
                        in1=prow[:, 32:48].unsqueeze(2).broadcast_to([128, 16, 64]), op=ALU.mult),
                        [r_tB, r_prow], [r_tB])
                    dve.op(lambda: nc.vector.tensor_tensor(out=tC[:], in0=tC[:], in1=tB[:], op=ALU.add),
                           [r_tC, r_tB], [r_tC])
                    ck(9)
                    for hf in range(2):
                        act.op(lambda hf=hf: nc.scalar.activation(out=tA[:, hf * 512:(hf + 1) * 512], in_=ps[hf][:, :],
                                                                  func=AF.Silu), [psr[hf]], [r_tA])
                    dve.op(lambda: nc.vector.tensor_tensor(out=tC[:], in0=tC[:], in1=tA[:], op=ALU.mult),
                           [r_tC, r_tA], [r_tC])
                    for g4 in range(4):
                        act.op(lambda g4=g4: nc.scalar.activation(
                            out=junk[:, 0:256], in_=tC[:, g4 * 256:(g4 + 1) * 256], func=AF.Square,
                            accum_out=ss4[:, g4:g4 + 1]), [r_tC], [r_dec, r_sm])
                    dve.op(lambda: nc.vector.tensor_scalar(out=ss4, in0=ss4, scalar1=1.0 / 256.0, scalar2=EPS,
                                                           op0=ALU.mult, op1=ALU.add), [r_sm], [r_sm])
                    act.op(lambda: nc.scalar.activation(out=ss4, in_=ss4, func=AF.Sqrt), [r_sm], [r_sm])
                    dve.op(lambda: nc.vector.reciprocal(out=rs4, in_=ss4), [r_sm], [r_sm])
                    dve.op(lambda: nc.vector.tensor_tensor(
                        out=tC[:].rearrange("p (g d) -> p g d", d=256), in0=tC[:].rearrange("p (g d) -> p g d", d=256),
                        in1=rs4.unsqueeze(2).broadcast_to([128, 4, 256]), op=ALU.mult), [r_tC, r_sm], [r_tC])
                    dve.op(lambda: nc.vector.tensor_tensor(out=ynb[:], in0=tC[:], in1=normw[:], op=ALU.mult),
                           [r_tC, r_normw], [r_ynb])

                    def mm():
                        ins = None
                        for c in range(8):
                            ins = nc.tensor.transpose(out=psbf(3)[:, c * 128:(c + 1) * 128],
                                                      in_=ynb[:, c * 128:(c + 1) * 128], identity=identb[:])
                        return ins
                    pe.op(mm, [r_ynb, r_identb], [psr[7]])
                    act.op(lambda: nc.scalar.activation(out=ynT[:].rearrange("p c t -> p (c t)"), in_=psbf(3)[:, :],
                                                        func=AF.Copy), [psr[7]], [r_ynT])
                    ck(10)
                    def mm(o=o):
                        ins = None
                        for g in range(4):
                            ins = nc.tensor.transpose(out=psbf(2)[:, g * 128:(g + 1) * 128],
                                                      in_=xpost[:, 8 + g, o:o + 128], identity=identb[:])
                        return ins
                    pe.op(mm, [r_xpost, r_identb], [psr[7]])
                    act.op(lambda: nc.scalar.activation(out=Btok[:], in_=psbf(2)[:, 0:512], func=AF.Copy),
                           [psr[7]], [r_Btok])
                    dve.op(lambda: nc.vector.tensor_tensor(
                        out=S[:].rearrange("p (h d) -> p h d", d=64), in0=S[:].rearrange("p (h d) -> p h d", d=64),
                        in1=cdr.unsqueeze(2).broadcast_to([128, 16, 64]), op=ALU.mult), [r_S, r_sm], [r_S])
                    for bk in range(2):
                        def mm(bk=bk):
                            ins = None
                            for gg in range(2):
                                g = 2 * bk + gg
                                ins = nc.tensor.matmul(ps[4 + bk][:, gg * 256:(gg + 1) * 256],
                                                       lhsT=Btok[:, g * 128:(g + 1) * 128],
                                                       rhs=xcd[:, g * 256:(g + 1) * 256], start=True, stop=True)
                            return ins
                        pe.op(mm, [r_Btok, r_xcd], [psr[4 + bk]])
                        dve.op(lambda bk=bk: nc.vector.tensor_tensor(
                            out=S[:, bk * 512:(bk + 1) * 512], in0=ps[4 + bk][:, :],
                            in1=S[:, bk * 512:(bk + 1) * 512], op=ALU.add), [psr[4 + bk], r_S], [r_S])
                    act.op(lambda: nc.scalar.activation(out=Sb[:], in_=S[:], func=AF.Copy), [r_S], [r_Sb])
                    act.dma([(YN_v[:, :, t0:t0 + 128], ynT[:])], [r_ynT], [r_YN], r_ynT)
                    if stage == "B1":
                        j4 = t0 // 1024
                        col = t0 % 1024
                        dve.op(lambda: nc.vector.tensor_copy(out=xTf[:], in_=ynT[:]), [r_ynT], [r_xTf])
                        sp.dma([(out_d[j4 * 1024:(j4 + 1) * 1024, col:col + 128].rearrange("(c p) t -> p c t", p=128),
                                 xTf[:])], [r_xTf], [], r_xTf)

    barrier()
    if stage == "B1":
        raise EarlyExit()
    with contextlib.ExitStack() as stC:
        woutb, r_woutb = sb(stC, "woutb", [128, 16, 1024], BF16)
        ln1g, r_ln1g = sb(stC, "ln1g", [128, D])
        ln1b, r_ln1b = sb(stC, "ln1b", [128, D])
        wr, r_wr = sb(stC, "wr", [128, 8, 72])
        setsC = []
        for i in range(2):
            setsC.append((sb(stC, f"ufb{i}", [128, 8, 128], BF16), sb(stC, f"ynTC{i}", [128, 8, 128], BF16),
                          sb(stC, f"xtok{i}", [128, D]), sb(stC, f"tAC{i}", [128, D]), sb(stC, f"tCC{i}", [128, D]),
                          sb(stC, f"h2Tf{i}", [128, 8, 128]), sb(stC, f"h2b{i}", [128, D], BF16),
                          sb(stC, f"bst{i}", [128, 2, 6]), sb(stC, f"smC{i}", [128, 8])))
        stgC = [sb(stC, f"stgC{i}", [128, 1024]) for i in range(2)]
        load_cast([woutb[:, c, :] for c in range(16)], [wout_d[c * 128:(c + 1) * 128, :] for c in range(16)],
                  stgC, r_woutb)
        sp.dma([(ln1g[:], ln1g_d[:, :])], [], [r_ln1g], r_ln1g)
        sp.dma([(ln1b[:], ln1b_d[:, :])], [], [r_ln1b], r_ln1b)
        sp.dma([(wr[:], wr_d[:, :, :])], [], [r_wr], r_wr)
        for tile_idx in range(32):
            ((ufb, r_ufb), (ynT, r_ynT), (xtok, r_xtok), (tA, r_tA), (tC, r_tC), (h2Tf, r_h2Tf), (h2b, r_h2b),
             (bst, r_bst), (sm, r_sm)) = setsC[tile_idx % 2]
            mv = sm[:, 0:2]
            rs1 = sm[:, 2:3]
            po = 0 if tile_idx % 2 == 0 else 4
            s = tile_idx // 16
            t0 = tile_idx * 128
            g1row = modrow[:, s, 0:1024]
            sh2row = modrow[:, s, 1024:2048]
            sc2row = modrow[:, s, 2048:3072]
            sp.dma([(xtok[:], x_d[t0:t0 + 128, :])], [], [r_xtok], r_xtok)
            sp.dma([(ufb[:], UF_v[:, :, t0:t0 + 128])], [r_UF], [r_ufb], r_ufb)
            sp.dma([(ynT[:], YN_v[:, :, t0:t0 + 128])], [r_YN], [r_ynT], r_ynT)
            for hf in range(2):
                def mm(hf=hf):
                    ins = None
                    for c in range(8):
                        ins = nc.tensor.matmul(ps[po + hf][:, :], lhsT=ufb[:, c, :],
                                               rhs=woutb[:, c, hf * 512:(hf + 1) * 512],
                                               start=(c == 0), stop=False)
                    for c in range(8):
                        ins = nc.tensor.matmul(ps[po + hf][:, :], lhsT=ynT[:, c, :],
                                               rhs=woutb[:, 8 + c, hf * 512:(hf + 1) * 512],
                                               start=False, stop=(c == 7))
                    return ins
                pe.op(mm, [r_ufb, r_ynT, r_woutb], [psr[po + hf]])
                dve.op(lambda hf=hf: nc.vector.tensor_tensor(
                    out=tA[:, hf * 512:(hf + 1) * 512], in0=ps[po + hf][:, :],
                    in1=g1row[:, hf * 512:(hf + 1) * 512], op=ALU.mult), [psr[po + hf], r_modrow], [r_tA])
            dve.op(lambda: nc.vector.scalar_tensor_tensor(out=tA[:], in0=xtok[:], scalar=ALPHA, in1=tA[:],
                                                          op0=ALU.mult, op1=ALU.add), [r_xtok, r_tA], [r_tA])
            ck(20)
            dve.op(lambda: nc.vector.bn_stats(out=bst[:, 0, :], in_=tA[:, 0:512]), [r_tA], [r_bst])
            dve.op(lambda: nc.vector.bn_stats(out=bst[:, 1, :], in_=tA[:, 512:1024]), [r_tA], [r_bst])
            dve.op(lambda: nc.vector.bn_aggr(out=mv, in_=bst[:].rearrange("p a b -> p (a b)")),
                   [r_bst], [r_sm])
            ck(21)
            dve.op(lambda: nc.vector.tensor_scalar(out=rs1, in0=mv[:, 1:2], scalar1=EPS, scalar2=None,
                                                   op0=ALU.add), [r_sm], [r_sm])
            act.op(lambda: nc.scalar.activation(out=rs1, in_=rs1, func=AF.Sqrt), [r_sm], [r_sm])
            dve.op(lambda: nc.vector.reciprocal(out=rs1, in_=rs1), [r_sm], [r_sm])
            dve.op(lambda: nc.vector.tensor_scalar(out=tC[:], in0=tA[:], scalar1=mv[:, 0:1], scalar2=rs1,
                                                   op0=ALU.subtract, op1=ALU.mult), [r_tA, r_sm], [r_tC])
            dve.op(lambda: nc.vector.tensor_tensor(out=tC[:], in0=tC[:], in1=ln1g[:], op=ALU.mult),
                   [r_tC, r_ln1g], [r_tC])
            dve.op(lambda: nc.vector.tensor_tensor(out=tC[:], in0=tC[:], in1=ln1b[:], op=ALU.add),
                   [r_tC, r_ln1b], [r_tC])
            pool.dma([(X1_d[t0:t0 + 128, :], tC[:])], [r_tC], [r_X1], r_tC)
            if stage == "B":
                sp.dma([(out_d[t0:t0 + 128, :], tC[:])], [r_tC], [], r_tC)
            ck(22)
            dve.op(lambda: nc.vector.tensor_tensor(out=tA[:], in0=tC[:], in1=sc2row, op=ALU.mult),
                   [r_tC, r_modrow], [r_tA])
            dve.op(lambda: nc.vector.tensor_tensor(out=tA[:], in0=tA[:], in1=sh2row, op=ALU.add),
                   [r_tA, r_modrow], [r_tA])
            ck(23)
            for bk in range(2):
                def mm(bk=bk):
                    ins = None
                    for cc in range(4):
                        c = 4 * bk + cc
                        ins = nc.tensor.matmul(ps[2 + bk][:, cc * 128:(cc + 1) * 128],
                                               lhsT=tA[:, c * 128:(c + 1) * 128], rhs=identf[:],
                                               start=True, stop=True)
                    return ins
                pe.op(mm, [r_tA, r_identf], [psr[2 + bk]])
                ck(241)
                act.op(lambda bk=bk: nc.scalar.activation(
                    out=h2Tf[:, 4 * bk:4 * bk + 4, :].rearrange("p c t -> p (c t)"), in_=ps[2 + bk][:, :],
                    func=AF.Copy), [psr[2 + bk]], [r_h2Tf])
                ck(242)
            ck(24)
            pool.op(lambda: nc.gpsimd.tensor_copy(out=h2b[:], in_=tA[:]), [r_tA], [r_h2b])
            pool.dma([(H2_d[t0:t0 + 128, :], h2b[:])], [r_h2b], [r_H2T], r_h2b)

            def mm():
                ins = None
                for k in range(8):
                    ins = nc.tensor.matmul(ps[6][:, 0:72], lhsT=h2Tf[:, k, :], rhs=wr[:, k, :],
                                           start=(k == 0), stop=(k == 7))
                return ins
            pe.op(mm, [r_h2Tf, r_wr], [psr[6]])
            dve.op(lambda tile_idx=tile_idx: nc.vector.tensor_tensor(
                out=Lg[:, tile_idx, :], in0=ps[6][:, 0:72], in1=rb[:], op=ALU.add), [psr[6], r_rb], [r_Lg])
            ck(25)

    barrier()
    if stage == "B":
        sp.deps([], [r_tC])
        raise EarlyExit()

    I32 = mybir.dt.int32
    V = nc.vector
    gmax = rsm[:, 0:32]
    pgt = rsm[:, 32:64]
    m1 = rsm[:, 64:96]
    m2 = rsm[:, 96:128]
    pe1 = rsm[:, 128:160]
    pe2 = rsm[:, 160:192]
    d1f = rsm[:, 192:224]
    d2f = rsm[:, 224:256]
    with contextlib.ExitStack() as stM:
        stR = contextlib.ExitStack()
        wk, r_wk = sb(stR, "wk", [128, 32, 64])
        oh1, r_oh1 = sb(stR, "oh1", [128, 32, 64])
        oh2, r_oh2 = sb(stR, "oh2", [128, 32, 64])
        cum, r_cum = sb(stR, "cum", [128, 32, 64])
        cs, r_cs = sb(stR, "cs", [128, 32, 64])
        ohb, r_ohb = sb(stR, "ohb", [128, 32, 64], BF16)
        triSb, r_triSb = sb(stR, "triSb", [128, 128], BF16)
        gm, r_gm = sb(stR, "gm", [128, 32, 8])
        ohg, r_ohg = sb(stR, "ohg", [128, 32, 8])
        thr, r_thr = sb(stR, "thr", [128, 64])
        iotab, r_iotab = sb(stR, "iotab", [128, 128])
        pid, r_pid = sb(stR, "pid", [128, 1])
        e64, r_e64 = sb(stR, "e64", [128, 6, 64])
        bef, r_bef = sb(stR, "bef", [128, 128])
        big, r_big = sb(stR, "big", [128, 4096])
        sp.dma([(thr[:], thr_d[:, :])], [], [r_thr], r_thr)
        sp.dma([(iotab[:], iotab_d[:, :])], [], [r_iotab], r_iotab)
        sp.dma([(pid[:], pid_d[:, :])], [], [r_pid], r_pid)
        dve.op(lambda: V.tensor_reduce(out=gmax, in_=Lg[:, :, 0:8], axis=AX.X, op=ALU.max), [r_Lg], [r_rsm])
        dve.op(lambda: V.tensor_tensor(out=gm[:], in0=Lg[:, :, 0:8], in1=gmax.unsqueeze(2).broadcast_to([128, 32, 8]),
                                       op=ALU.subtract), [r_Lg, r_rsm], [r_gm])
        dve.op(lambda: V.tensor_single_scalar(out=ohg[:], in_=gm[:], scalar=0.0, op=ALU.is_ge), [r_gm], [r_ohg])
        act.op(lambda: nc.scalar.activation(out=gm[:], in_=gm[:], func=AF.Exp), [r_gm], [r_gm])
        dve.op(lambda: V.tensor_reduce(out=pgt, in_=gm[:], axis=AX.X, op=ALU.add), [r_gm], [r_rsm])
        dve.op(lambda: V.reciprocal(out=pgt, in_=pgt), [r_rsm], [r_rsm])
        dve.op(lambda: V.tensor_scalar(out=ohg[:], in0=ohg[:], scalar1=-1.0, scalar2=BIG, op0=ALU.add, op1=ALU.mult),
               [r_ohg], [r_ohg])
        dve.op(lambda: V.tensor_tensor(
            out=wk[:].rearrange("p t (g e) -> p t g e", e=8), in0=Lg[:, :, 8:72].rearrange("p t (g e) -> p t g e", e=8),
            in1=ohg[:].unsqueeze(3).broadcast_to([128, 32, 8, 8]), op=ALU.add), [r_Lg, r_ohg], [r_wk])
        dve.op(lambda: V.tensor_reduce(out=m1, in_=wk[:], axis=AX.X, op=ALU.max), [r_wk], [r_rsm])
        dve.op(lambda: V.tensor_tensor(out=oh1[:], in0=wk[:], in1=m1.unsqueeze(2).broadcast_to([128, 32, 64]),
                                       op=ALU.is_ge), [r_wk, r_rsm], [r_oh1])
        dve.op(lambda: V.scalar_tensor_tensor(out=wk[:], in0=oh1[:], scalar=-BIG, in1=wk[:], op0=ALU.mult,
                                              op1=ALU.add), [r_oh1, r_wk], [r_wk])
        dve.op(lambda: V.tensor_reduce(out=m2, in_=wk[:], axis=AX.X, op=ALU.max), [r_wk], [r_rsm])
        dve.op(lambda: V.tensor_tensor(out=oh2[:], in0=wk[:], in1=m2.unsqueeze(2).broadcast_to([128, 32, 64]),
                                       op=ALU.is_ge), [r_wk, r_rsm], [r_oh2])
        dve.op(lambda: V.tensor_tensor(out=pe1, in0=m2, in1=m1, op=ALU.subtract), [r_rsm], [r_rsm])
        act.op(lambda: nc.scalar.activation(out=pe1, in_=pe1, func=AF.Exp), [r_rsm], [r_rsm])
        dve.op(lambda: V.tensor_scalar(out=pe1, in0=pe1, scalar1=1.0, scalar2=None, op0=ALU.add), [r_rsm], [r_rsm])
        dve.op(lambda: V.reciprocal(out=pe1, in_=pe1), [r_rsm], [r_rsm])
        dve.op(lambda: V.tensor_scalar(out=pe2, in0=pe1, scalar1=-1.0, scalar2=1.0, op0=ALU.mult, op1=ALU.add),
               [r_rsm], [r_rsm])
        dve.op(lambda: V.tensor_tensor(out=pe1, in0=pe1, in1=pgt, op=ALU.mult), [r_rsm], [r_rsm])
        dve.op(lambda: V.tensor_tensor(out=pe2, in0=pe2, in1=pgt, op=ALU.mult), [r_rsm], [r_rsm])
        dve.op(lambda: V.tensor_tensor(out=ohb[:], in0=oh1[:], in1=oh2[:], op=ALU.add), [r_oh1, r_oh2], [r_ohb])
        dve.op(lambda: V.tensor_tensor(out=triSb[:], in0=tri[:], in1=identf[:], op=ALU.subtract),
               [r_tri, r_identf], [r_triSb])
        for q4 in range(4):
            def mm(q4=q4):
                return nc.tensor.matmul(ps[q4][:, :], lhsT=triSb[:], rhs=ohb[:, 8 * q4:8 * q4 + 8, :],
                                        start=True, stop=True)
            pe.op(mm, [r_triSb, r_ohb], [psr[q4]])
            act.op(lambda q4=q4: nc.scalar.activation(
                out=cum[:, 8 * q4:8 * q4 + 8, :].rearrange("p t e -> p (t e)"), in_=ps[q4][:, :], func=AF.Copy),
                [psr[q4]], [r_cum])
        for q4 in range(4):
            def mm(q4=q4):
                return nc.tensor.matmul(ps[q4][:, :], lhsT=onesb[:], rhs=ohb[:, 8 * q4:8 * q4 + 8, :],
                                        start=True, stop=True)
            pe.op(mm, [r_onesb, r_ohb], [psr[q4]])
            act.op(lambda q4=q4: nc.scalar.activation(
                out=cs[:, 8 * q4:8 * q4 + 8, :].rearrange("p t e -> p (t e)"), in_=ps[q4][:, :], func=AF.Copy),
                [psr[q4]], [r_cs])
        cnt = e64[:, 0, :]
        nb = e64[:, 1, :]
        ci = e64[:, 2, :]
        sbase = e64[:, 3, :]
        ones64 = e64[:, 4, :]
        dve.op(lambda: V.memset(e64[:], 0.0), [], [r_e64])
        dve.op(lambda: V.memset(ones64, 1.0), [r_e64], [r_e64])
        for t in range(32):
            dve.op(lambda t=t: V.tensor_tensor(out=cum[:, t, :], in0=cum[:, t, :], in1=cnt, op=ALU.add),
                   [r_cum, r_e64], [r_cum])
            dve.op(lambda t=t: V.tensor_tensor(out=cnt, in0=cnt, in1=cs[:, t, :], op=ALU.add), [r_cs, r_e64], [r_e64])
        dve.op(lambda: V.tensor_tensor(out=big[:].rearrange("p (e j) -> p e j", j=64),
                                       in0=cnt.unsqueeze(2).broadcast_to([128, 64, 64]),
                                       in1=thr[:].unsqueeze(1).broadcast_to([128, 64, 64]), op=ALU.is_gt),
               [r_e64, r_thr], [r_big])
        dve.op(lambda: V.tensor_reduce(out=nb, in_=big[:].rearrange("p (e j) -> p e j", j=64), axis=AX.X, op=ALU.add),
               [r_big], [r_e64])
        dve.op(lambda: V.tensor_tensor_scan(out=ci, data0=ones64, data1=nb, initial=0.0, op0=ALU.mult, op1=ALU.add),
               [r_e64], [r_e64])
        dve.op(lambda: V.tensor_tensor(out=sbase, in0=ci, in1=nb, op=ALU.subtract), [r_e64], [r_e64])
        dve.op(lambda: V.tensor_scalar(out=sbase, in0=sbase, scalar1=128.0, scalar2=None, op0=ALU.mult),
               [r_e64], [r_e64])
        dve.op(lambda: V.tensor_tensor(out=cum[:], in0=cum[:], in1=sbase.unsqueeze(1).broadcast_to([128, 32, 64]),
                                       op=ALU.add), [r_cum, r_e64], [r_cum])
        dve.op(lambda: V.tensor_tensor(out=wk[:], in0=cum[:], in1=oh1[:], op=ALU.mult), [r_cum, r_oh1], [r_wk])
        dve.op(lambda: V.tensor_reduce(out=d1f, in_=wk[:], axis=AX.X, op=ALU.add), [r_wk], [r_rsm])
        dve.op(lambda: V.tensor_tensor(out=wk[:], in0=cum[:], in1=oh2[:], op=ALU.mult), [r_cum, r_oh2], [r_wk])
        dve.op(lambda: V.tensor_reduce(out=d2f, in_=wk[:], axis=AX.X, op=ALU.add), [r_wk], [r_rsm])
        dve.op(lambda: V.tensor_copy(out=d1i[:], in_=d1f), [r_rsm], [r_d1i])
        dve.op(lambda: V.tensor_copy(out=d2i[:], in_=d2f), [r_rsm], [r_d2i])
        for hb in range(2):
            dve.op(lambda hb=hb: V.tensor_tensor(
                out=big[:].rearrange("p (b e) -> p b e", e=64),
                in0=ci.unsqueeze(1).broadcast_to([128, 64, 64]),
                in1=iotab[:, 64 * hb:64 * hb + 64].unsqueeze(2).broadcast_to([128, 64, 64]), op=ALU.is_le),
                [r_e64, r_iotab], [r_big])
            dve.op(lambda hb=hb: V.tensor_reduce(
                out=bef[:, 64 * hb:64 * hb + 64], in_=big[:].rearrange("p (b e) -> p b e", e=64),
                axis=AX.X, op=ALU.add), [r_big], [r_bef])
        dve.op(lambda: V.tensor_scalar(out=bef[:], in0=bef[:], scalar1=63.0, scalar2=None, op0=ALU.min),
               [r_bef], [r_bef])
        dve.op(lambda: V.memset(big[:, 0:128], 0.0), [r_big], [r_big])
        dve.op(lambda: V.tensor_tensor(out=big[:, 1:128], in0=bef[:, 1:128], in1=bef[:, 0:127], op=ALU.is_equal),
               [r_bef, r_big], [r_big])
        dve.op(lambda: V.tensor_scalar(out=bef[:], in0=bef[:], scalar1=128.0, scalar2=None, op0=ALU.mult),
               [r_bef], [r_bef])
        dve.op(lambda: V.scalar_tensor_tensor(out=bef[:], in0=big[:, 0:128], scalar=1.0e6, in1=bef[:], op0=ALU.mult,
                                              op1=ALU.add), [r_big, r_bef], [r_bef])
        dve.op(lambda: V.tensor_scalar(out=bef[:], in0=bef[:], scalar1=pid[:, 0:1], scalar2=None, op0=ALU.add),
               [r_bef, r_pid], [r_bef])
        dve.op(lambda: V.tensor_copy(out=widx[:], in_=bef[:]), [r_bef], [r_widx])
        barrier()
        stR.close()
        stMid.close()
        if stage == "R":
            dbg, r_dbg = sb(stM, "dbgR", [128, 256])
            dve.op(lambda: V.tensor_copy(out=dbg[:, 0:32], in_=d1i[:]), [r_d1i], [r_dbg])
            dve.op(lambda: V.tensor_copy(out=dbg[:, 32:64], in_=d2i[:]), [r_d2i], [r_dbg])
            dve.op(lambda: V.tensor_copy(out=dbg[:, 64:128], in_=rsm[:, 128:192]), [r_rsm], [r_dbg])
            dve.op(lambda: V.tensor_copy(out=dbg[:, 128:256], in_=widx[:]), [r_widx], [r_dbg])
            sp.dma([(out_d[0:128, 0:256], dbg[:])], [r_dbg], [], r_dbg)
            sp.deps([], [r_dbg])
            barrier()
            raise EarlyExit()
        r_Xs, r_Ys = Res("Xs"), Res("Ys")
        with contextlib.ExitStack() as stS:
            hb2 = [sb(stS, f"hb2_{i}", [128, D], BF16) for i in range(2)]
            for t in range(32):
                tl, r_tl = hb2[t % 2]
                sp.dma([(tl[:], H2_d[t * 128:(t + 1) * 128, :])], [r_H2T], [r_tl], r_tl)
                for (di, r_di) in ((d1i, r_d1i), (d2i, r_d2i)):
                    pool.deps([r_tl, r_di], [])
                    if r_tl.dsem is None:
                        raise RuntimeError("no dsem")
                    pool.wait({id(r_tl.dsem): (r_tl.dsem, r_tl.dcnt)})
                    nc.gpsimd.indirect_dma_start(
                        out=Xs_d[:, :], out_offset=bass.IndirectOffsetOnAxis(ap=di[:, t:t + 1], axis=0),
                        in_=tl[:, :], in_offset=None).then_inc(r_tl.dsem, 16)
                    r_tl.dcnt += 16
                    pool.mark([r_tl, r_di], [r_Xs], r_tl.dsem, r_tl.dcnt)
            barrier()
        with contextlib.ExitStack() as stE:
            stg = [sb(stE, f"stgE{i}", [128, 4096]) for i in range(3)]
            wbf = []
            for i in range(2):
                a1, ra1 = sb(stE, f"w1b{i}", [128, 8, 512], BF16)
                a3, ra3 = sb(stE, f"w3b{i}", [128, 8, 512], BF16)
                a2, ra2 = sb(stE, f"w2b{i}", [128, 4, 1024], BF16)
                wbf.append((a1, ra1, a3, ra3, a2, ra2))
            xblk = [sb(stE, f"xblk{i}", [128, D], BF16) for i in range(2)]
            XT, r_XT = sb(stE, "XT", [128, 8, 128], BF16)
            sgE, r_sgE = sb(stE, "sgE", [128, 512])
            actb, r_actb = sb(stE, "actb", [128, 4, 128], BF16)
            yb = [sb(stE, f"yb{i}", [128, D]) for i in range(2)]
            bc_reg = nc.gpsimd.alloc_register("bc_reg")
            nc.gpsimd.reg_mov(bc_reg, nexp_decl * 128 - 1)
            for b in range(128):
                a1, ra1, a3, ra3, a2, ra2 = wbf[b % 2]
                for (wd, (tg, r_tg), (ab, r_ab), nk) in ((w1_d, stg[0], (a1, ra1), 8), (w3_d, stg[1], (a3, ra3), 8),
                                                        (w2_d, stg[2], (a2, ra2), 4)):
                    pool.deps([r_widx], [r_tg])
                    if r_tg.dsem is None:
                        r_tg.dsem = nc.alloc_semaphore(name=f"d{len(ALL_DMA_RES)}_" + r_tg.name)
                        ALL_DMA_RES.append(r_tg)
                    if r_tg.dcnt:
                        pool.wait({id(r_tg.dsem): (r_tg.dsem, r_tg.dcnt)})
                    nc.gpsimd.indirect_dma_start(
                        out=tg[:, :], out_offset=None, in_=wd[:, :],
                        in_offset=bass.IndirectOffsetOnAxis(ap=widx[:, b:b + 1], axis=0),
                        bounds_check=bc_reg, oob_is_err=False).then_inc(r_tg.dsem, 16)
                    r_tg.dcnt += 16
                    pool.mark([r_widx], [r_tg], r_tg.dsem, r_tg.dcnt)
                    if nk == 8:
                        dve.op(lambda ab=ab, tg=tg: V.tensor_copy(out=ab[:].rearrange("p k n -> p (k n)"), in_=tg[:]),
                               [r_tg], [r_ab])
                    else:
                        act.op(lambda ab=ab, tg=tg: nc.scalar.activation(out=ab[:].rearrange("p k n -> p (k n)"),
                                                                         in_=tg[:], func=AF.Copy), [r_tg], [r_ab])
                xb_, r_xb = xblk[b % 2]
                sp.dma([(xb_[:], Xs_d[b * 128:(b + 1) * 128, :])], [r_Xs], [r_xb], r_xb)

                def mm(xb_=xb_):
                    ins = None
                    for c in range(8):
                        ins = nc.tensor.transpose(out=ps7b[:, c * 128:(c + 1) * 128], in_=xb_[:, c * 128:(c + 1) * 128],
                                                  identity=identb[:])
                    return ins
                pe.op(mm, [r_xb, r_identb], [psr[7]])
                act.op(lambda: nc.scalar.activation(out=XT[:].rearrange("p c t -> p (c t)"), in_=ps7b[:, :],
                                                    func=AF.Copy), [psr[7]], [r_XT])
                pg_, pu_ = (0, 1) if b % 2 == 0 else (2, 3)

                def mm(a1=a1, a3=a3, pg_=pg_, pu_=pu_):
                    ins = None
                    for hc in range(4):
                        for k in range(8):
                            ins = nc.tensor.matmul(ps[pg_][:, hc * 128:(hc + 1) * 128],
                                                   lhsT=a1[:, k, hc * 128:(hc + 1) * 128], rhs=XT[:, k, :],
                                                   start=(k == 0), stop=(k == 7))
                    for hc in range(4):
                        for k in range(8):
                            ins = nc.tensor.matmul(ps[pu_][:, hc * 128:(hc + 1) * 128],
                                                   lhsT=a3[:, k, hc * 128:(hc + 1) * 128], rhs=XT[:, k, :],
                                                   start=(k == 0), stop=(k == 7))
                    return ins
                pe.op(mm, [ra1, ra3, r_XT], [psr[pg_], psr[pu_]])
                act.op(lambda pg_=pg_: nc.scalar.activation(out=sgE[:], in_=ps[pg_][:, :], func=AF.Silu),
                       [psr[pg_]], [r_sgE])
                dve.op(lambda pu_=pu_: V.tensor_tensor(out=actb[:].rearrange("p c t -> p (c t)"), in0=ps[pu_][:, :],
                                                       in1=sgE[:], op=ALU.mult), [psr[pu_], r_sgE], [r_actb])
                yb_, r_yb = yb[b % 2]
                for hf in range(2):
                    py = 4 + hf

                    def mm(hf=hf, py=py, a2=a2):
                        ins = None
                        for hc in range(4):
                            ins = nc.tensor.matmul(ps[py][:, :], lhsT=actb[:, hc, :],
                                                   rhs=a2[:, hc, hf * 512:(hf + 1) * 512],
                                                   start=(hc == 0), stop=(hc == 3))
                        return ins
                    pe.op(mm, [r_actb, ra2], [psr[py]])
                    act.op(lambda hf=hf, py=py, yb_=yb_: nc.scalar.activation(
                        out=yb_[:, hf * 512:(hf + 1) * 512], in_=ps[py][:, :], func=AF.Copy), [psr[py]], [r_yb])
                act.dma([(Ys_d[b * 128:(b + 1) * 128, :], yb_[:])], [r_yb], [r_Ys], r_yb)
            barrier()
        with contextlib.ExitStack() as stF:
            ln2g, r_ln2g = sb(stF, "ln2g", [128, D])
            ln2b, r_ln2b = sb(stF, "ln2b", [128, D])
            x1t, r_x1t = sb(stF, "x1t", [128, D])
            fo, r_fo = sb(stF, "fo", [128, D])
            ya = [sb(stF, f"ya{i}", [128, D]) for i in range(2)]
            bst2, r_bst2 = sb(stF, "bst2", [128, 2, 6])
            sm2, r_sm2 = sb(stF, "sm2", [128, 4])
            sp.dma([(ln2g[:], ln2g_d[:, :])], [], [r_ln2g], r_ln2g)
            sp.dma([(ln2b[:], ln2b_d[:, :])], [], [r_ln2b], r_ln2b)
            for t in range(32):
                s_ = t // 16
                t0 = t * 128
                g2row = modg2[:, s_, :]
                for (yt_, r_yt), (di, r_di) in zip(ya, ((d1i, r_d1i), (d2i, r_d2i))):
                    pool.deps([r_di, r_Ys], [r_yt])
                    if r_yt.dsem is None:
                        r_yt.dsem = nc.alloc_semaphore(name=f"d{len(ALL_DMA_RES)}_" + r_yt.name)
                        ALL_DMA_RES.append(r_yt)
                    if r_yt.dcnt:
                        pool.wait({id(r_yt.dsem): (r_yt.dsem, r_yt.dcnt)})
                    nc.gpsimd.indirect_dma_start(
                        out=yt_[:, :], out_offset=None, in_=Ys_d[:, :],
                        in_offset=bass.IndirectOffsetOnAxis(ap=di[:, t:t + 1], axis=0)).then_inc(r_yt.dsem, 16)
                    r_yt.dcnt += 16
                    pool.mark([r_di, r_Ys], [r_yt], r_yt.dsem, r_yt.dcnt)
                sp.dma([(x1t[:], X1_d[t0:t0 + 128, :])], [r_X1], [r_x1t], r_x1t)
                dve.op(lambda t=t: V.tensor_scalar(out=fo[:], in0=ya[0][0][:], scalar1=pe1[:, t:t + 1], scalar2=None,
                                                   op0=ALU.mult), [ya[0][1], r_rsm], [r_fo])
                dve.op(lambda t=t: V.scalar_tensor_tensor(out=fo[:], in0=ya[1][0][:], scalar=pe2[:, t:t + 1], in1=fo[:],
                                                          op0=ALU.mult, op1=ALU.add), [ya[1][1], r_rsm, r_fo], [r_fo])
                dve.op(lambda g2row=g2row: V.tensor_tensor(out=fo[:], in0=fo[:], in1=g2row, op=ALU.mult),
                       [r_fo, r_modg2], [r_fo])
                dve.op(lambda: V.scalar_tensor_tensor(out=fo[:], in0=x1t[:], scalar=ALPHA, in1=fo[:], op0=ALU.mult,
                                                      op1=ALU.add), [r_x1t, r_fo], [r_fo])
                dve.op(lambda: V.bn_stats(out=bst2[:, 0, :], in_=fo[:, 0:512]), [r_fo], [r_bst2])
                dve.op(lambda: V.bn_stats(out=bst2[:, 1, :], in_=fo[:, 512:1024]), [r_fo], [r_bst2])
                dve.op(lambda: V.bn_aggr(out=sm2[:, 0:2], in_=bst2[:].rearrange("p a b -> p (a b)")),
                       [r_bst2], [r_sm2])
                dve.op(lambda: V.tensor_scalar(out=sm2[:, 2:3], in0=sm2[:, 1:2], scalar1=EPS, scalar2=None,
                                               op0=ALU.add), [r_sm2], [r_sm2])
                act.op(lambda: nc.scalar.activation(out=sm2[:, 2:3], in_=sm2[:, 2:3], func=AF.Sqrt), [r_sm2], [r_sm2])
                dve.op(lambda: V.reciprocal(out=sm2[:, 2:3], in_=sm2[:, 2:3]), [r_sm2], [r_sm2])
                dve.op(lambda: V.tensor_scalar(out=fo[:], in0=fo[:], scalar1=sm2[:, 0:1], scalar2=sm2[:, 2:3],
                                               op0=ALU.subtract, op1=ALU.mult), [r_fo, r_sm2], [r_fo])
                dve.op(lambda: V.tensor_tensor(out=fo[:], in0=fo[:], in1=ln2g[:], op=ALU.mult), [r_fo, r_ln2g], [r_fo])
                dve.op(lambda: V.tensor_tensor(out=fo[:], in0=fo[:], in1=ln2b[:], op=ALU.add), [r_fo, r_ln2b], [r_fo])
                sp.dma([(out_d[t0:t0 + 128, :], fo[:])], [r_fo], [], r_fo)
            sp.deps([], [r_fo])
            barrier()
    return nc


def make_inputs(core, x, c, w_ada, b_ada, w_in, conf_conv_w, conf_conv_b, conf_ln_g, conf_ln_b,
                ssm_conv_w, ssm_conv_b, ssm_dt_bias, ssm_A_log, ssm_D, ssm_norm_w, w_out,
                ln1_g, ln1_b, router_group_w, router_group_b, router_expert_w, router_expert_b,
                expert_w1, expert_w3, expert_w2, ln2_g, ln2_b, shared):
    f = np.float32
    xc = np.ascontiguousarray(x[2 * core:2 * core + 2].reshape(NTOK, D), dtype=f)
    cc = c[2 * core:2 * core + 2]
    m = dict(shared)
    m["x"] = xc
    m["xT"] = np.ascontiguousarray(xc.T)
    m["cT"] = np.ascontiguousarray(cc.reshape(2, 8, 128).transpose(2, 1, 0), dtype=f)
    return m


def rep(v, n=128):
    return np.ascontiguousarray(np.broadcast_to(np.asarray(v, dtype=np.float32).reshape(1, -1), (n, v.size)))


def fm(v, nch):
    return np.ascontiguousarray(np.asarray(v, dtype=np.float32).reshape(nch, 128).T)


def shared_inputs(w_ada, b_ada, w_in, conf_conv_w, conf_conv_b, conf_ln_g, conf_ln_b,
                  ssm_conv_w, ssm_conv_b, ssm_dt_bias, ssm_A_log, ssm_D, ssm_norm_w, w_out,
                  ln1_g, ln1_b, router_group_w, router_group_b, router_expert_w, router_expert_b,
                  expert_w1, expert_w3, expert_w2, ln2_g, ln2_b):
    f = np.float32
    sh = {}
    sh["w_ada"] = np.ascontiguousarray(w_ada[0], dtype=f)
    sh["b_ada_fm"] = fm(b_ada[0][:2048], 16)
    sh["b_ada_row"] = rep(b_ada[0][2048:])
    sh["w_in"] = np.ascontiguousarray(w_in[0], dtype=f)
    sh["ccw"] = np.ascontiguousarray(conf_conv_w[0].reshape(31, 8, 128).transpose(2, 1, 0), dtype=f)
    sh["ccb"] = fm(conf_conv_b[0], 8)
    sh["clg"] = fm(conf_ln_g[0], 8)
    sh["clb"] = fm(conf_ln_b[0], 8)
    sh["scw"] = np.ascontiguousarray(ssm_conv_w[0].reshape(4, 16, 128).transpose(2, 1, 0), dtype=f)
    sh["scb"] = fm(ssm_conv_b[0], 16)
    sh["dtb"] = rep(ssm_dt_bias[0])
    sh["alog"] = rep(ssm_A_log[0])
    sh["dsk"] = rep(ssm_D[0])
    sh["normw"] = rep(ssm_norm_w[0])
    sh["w_out"] = np.ascontiguousarray(w_out[0], dtype=f)
    sh["ln1g"] = rep(ln1_g[0])
    sh["ln1b"] = rep(ln1_b[0])
    sh["ln2g"] = rep(ln2_g[0])
    sh["ln2b"] = rep(ln2_b[0])
    wrr = np.concatenate([router_group_w[0], router_expert_w[0]], axis=1).astype(f)
    sh["wr"] = np.ascontiguousarray(wrr.reshape(8, 128, 72).transpose(1, 0, 2))
    sh["rb"] = rep(np.concatenate([router_group_b[0], router_expert_b[0]]))
    sh["w1"] = np.ascontiguousarray(np.asarray(expert_w1[0], dtype=f).reshape(NEXP, 8, 128, 512).transpose(0, 2, 1, 3)
                                    ).reshape(NEXP * 128, 4096)
    sh["w3"] = np.ascontiguousarray(np.asarray(expert_w3[0], dtype=f).reshape(NEXP, 8, 128, 512).transpose(0, 2, 1, 3)
                                    ).reshape(NEXP * 128, 4096)
    sh["w2"] = np.ascontiguousarray(np.asarray(expert_w2[0], dtype=f).reshape(NEXP, 4, 128, 1024).transpose(0, 2, 1, 3)
                                    ).reshape(NEXP * 128, 4096)
    sh["thr"] = rep(np.arange(64, dtype=f) * 128.0)
    sh["iotab"] = rep(np.arange(128, dtype=f))
    sh["pid"] = np.arange(128, dtype=f).reshape(128, 1)
    sh["identf"] = np.eye(128, dtype=f)
    sh["tri"] = np.triu(np.ones((128, 128), dtype=f))
    nm = np.where(np.triu(np.ones((128, 128), dtype=bool)), 0.0, -30000.0).astype(f)
    sh["negm"] = np.ascontiguousarray(np.tile(nm, (1, 4)))
    return sh


def kernel(x, c, w_ada, b_ada, w_in, conf_conv_w, conf_conv_b, conf_ln_g, conf_ln_b,
           ssm_conv_w, ssm_conv_b, ssm_dt_bias, ssm_A_log, ssm_D, ssm_norm_w, w_out,
           ln1_g, ln1_b, router_group_w, router_group_b, router_expert_w, router_expert_b,
           expert_w1, expert_w3, expert_w2, ln2_g, ln2_b, _stage="full", _trace=False):
    args = [np.asarray(a) for a in (w_ada, b_ada, w_in, conf_conv_w, conf_conv_b, conf_ln_g, conf_ln_b,
                                    ssm_conv_w, ssm_conv_b, ssm_dt_bias, ssm_A_log, ssm_D, ssm_norm_w, w_out,
                                    ln1_g, ln1_b, router_group_w, router_group_b, router_expert_w, router_expert_b,
                                    expert_w1, expert_w3, expert_w2, ln2_g, ln2_b)]
    x = np.asarray(x)
    c = np.asarray(c)
    sh = shared_inputs(*args)
    if _stage != "full":
        for k in ("w1", "w3", "w2"):
            sh[k] = np.ascontiguousarray(sh[k][0:128])
    in_maps = []
    for core in range(8):
        m = dict(sh)
        xc = np.ascontiguousarray(x[2 * core:2 * core + 2].reshape(NTOK, D), dtype=np.float32)
        m["x"] = xc
        m["xT"] = np.ascontiguousarray(xc.T)
        m["cT"] = np.ascontiguousarray(c[2 * core:2 * core + 2].reshape(2, 8, 128).transpose(2, 1, 0),
                                       dtype=np.float32)
        in_maps.append(m)
    nc = build_nc(_stage)
    if _trace:
        res = run_bass_kernel_spmd(nc, in_maps, core_ids=list(range(8)), trace=True)
        print("EXEC_TIME_NS", _stage, res.exec_time_ns)
    else:
        res = run_bass_kernel_spmd(nc, in_maps, core_ids=list(range(8)))
    outs = [np.asarray(r["out"], dtype=np.float32).reshape(2, SEQ, D) for r in res.results]
    return np.concatenate(outs, axis=0)
```

```python
import contextlib
import numpy as np
import concourse.bass as bass
import concourse.mybir as mybir
from concourse.bass_utils import run_bass_kernel_spmd

F32 = mybir.dt.float32
BF16 = mybir.dt.bfloat16
AF = mybir.ActivationFunctionType
ALU = mybir.AluOpType
AX = mybir.AxisListType

ALPHA = 2.0 ** 0.25
EPS = 1e-5
NTOK = 4096
SEQ = 2048
D = 1024
NEXP = 64
BIG = 1.0e9


ALL_DMA_RES = []


class Res:
    __slots__ = ("name", "wr", "rd", "dsem", "dcnt")

    def __init__(self, name):
        self.name = name
        self.wr = {}
        self.rd = {}
        self.dsem = None
        self.dcnt = 0


class Eng:
    def __init__(self, nc, eng, name):
        self.nc = nc
        self.eng = eng
        self.sem = nc.alloc_semaphore(name=name)
        self.cnt = 0
        self.waited = {}

    def wait(self, evs):
        for key, (sem, val) in list(evs.items()):
            if self.waited.get(key, 0) < val:
                self.eng.wait_ge(sem, val)
                self.waited[key] = val

    def deps(self, reads, writes):
        for r in reads:
            self.wait(r.wr)
        for w in writes:
            self.wait(w.wr)
            self.wait(w.rd)

    def mark(self, reads, writes, sem, val):
        key = id(sem)
        for r in reads:
            r.rd[key] = (sem, val)
        for w in writes:
            w.wr = {key: (sem, val)}
            w.rd = {}

    def op(self, fn, reads=(), writes=()):
        self.deps(reads, writes)
        ins = fn()
        self.cnt += 1
        ins.then_inc(self.sem, 1)
        self.mark(reads, writes, self.sem, self.cnt)

    def dma(self, parts, reads, writes, sres):
        self.deps(reads, writes)
        if sres.dsem is None:
            sres.dsem = self.nc.alloc_semaphore(name=f"d{len(ALL_DMA_RES)}_" + sres.name)
            ALL_DMA_RES.append(sres)
        if sres.dcnt:
            self.wait({id(sres.dsem): (sres.dsem, sres.dcnt)})
        for (o, i) in parts:
            self.eng.dma_start(out=o, in_=i).then_inc(sres.dsem, 16)
            sres.dcnt += 16
        self.mark(reads, writes, sres.dsem, sres.dcnt)


class EarlyExit(Exception):
    pass


def build_nc(stage="full"):
    nc = bass.Bass("TRN2", target_bir_lowering=False)
    ALL_DMA_RES.clear()
    st_all = contextlib.ExitStack()
    try:
        with st_all:
            _build(nc, stage, st_all)
    except EarlyExit:
        pass
    return nc


def _build(nc, stage, st_all):

    def din(name, shape, dt=F32):
        return nc.dram_tensor(name, list(shape), dt, kind="ExternalInput").ap()

    x_d = din("x", [NTOK, D])
    xT_d = din("xT", [D, NTOK])
    cT_d = din("cT", [128, 8, 2])
    wada_d = din("w_ada", [D, 6 * D])
    bada_fm_d = din("b_ada_fm", [128, 16])
    bada_row_d = din("b_ada_row", [128, 4 * D])
    win_d = din("w_in", [D, 5136])
    ccw_d = din("ccw", [128, 8, 31])
    ccb_d = din("ccb", [128, 8])
    clg_d = din("clg", [128, 8])
    clb_d = din("clb", [128, 8])
    scw_d = din("scw", [128, 16, 4])
    scb_d = din("scb", [128, 16])
    dtb_d = din("dtb", [128, 16])
    alog_d = din("alog", [128, 16])
    dsk_d = din("dsk", [128, 16])
    normw_d = din("normw", [128, D])
    wout_d = din("w_out", [2 * D, D])
    ln1g_d = din("ln1g", [128, D])
    ln1b_d = din("ln1b", [128, D])
    ln2g_d = din("ln2g", [128, D])
    ln2b_d = din("ln2b", [128, D])
    wr_d = din("wr", [128, 8, 72])
    rb_d = din("rb", [128, 72])
    nexp_decl = NEXP if stage == "full" else 1
    w1_d = din("w1", [nexp_decl * 128, 4096])
    w3_d = din("w3", [nexp_decl * 128, 4096])
    w2_d = din("w2", [nexp_decl * 128, 4096])
    thr_d = din("thr", [128, 64])
    iotab_d = din("iotab", [128, 128])
    pid_d = din("pid", [128, 1])
    identf_d = din("identf", [128, 128])
    tri_d = din("tri", [128, 128])
    negm_d = din("negm", [128, 512])
    out_d = nc.dram_tensor("out", [NTOK, D], F32, kind="ExternalOutput").ap()

    UF_d = nc.dram_tensor("UF", [D, NTOK], BF16, kind="Internal").ap()
    X1_d = nc.dram_tensor("X1", [NTOK, D], F32, kind="Internal").ap()
    H2_d = nc.dram_tensor("H2", [NTOK, D], BF16, kind="Internal").ap()
    Xs_d = nc.dram_tensor("Xs", [4 * NTOK, D], BF16, kind="Internal").ap()
    Ys_d = nc.dram_tensor("Ys", [4 * NTOK, D], F32, kind="Internal").ap()

    r_UF, r_X1, r_H2T, r_YN = Res("UF"), Res("X1"), Res("H2T"), Res("YN")
    YN_d = nc.dram_tensor("YN", [D, NTOK], BF16, kind="Internal").ap()
    pe = Eng(nc, nc.tensor, "s_pe")
    act = Eng(nc, nc.scalar, "s_act")
    dve = Eng(nc, nc.vector, "s_dve")
    pool = Eng(nc, nc.gpsimd, "s_pool")
    sp = Eng(nc, nc.sync, "s_sp")

    es = st_all
    engines = [pe, act, dve, pool, sp]

    def ck(n):
        if stage == f"K{n}":
            barrier()
            raise EarlyExit()

    def barrier():
        evs = {}
        for E in engines:
            if E.cnt:
                evs[id(E.sem)] = (E.sem, E.cnt)
        for r in ALL_DMA_RES:
            if r.dcnt:
                evs[id(r.dsem)] = (r.dsem, r.dcnt)
        for E in engines:
            E.wait(evs)

    cast_rr = [0]

    def load_cast(dst_aps, src_aps, stg, r_dst):
        for dst, src in zip(dst_aps, src_aps):
            t, r = stg[cast_rr[0] % len(stg)]
            n = dst.shape[-1]
            sp.dma([(t[:, 0:n], src)], [], [r], r)
            if cast_rr[0] % 2 == 0:
                act.op(lambda t=t, dst=dst, n=n: nc.scalar.activation(out=dst, in_=t[:, 0:n], func=AF.Copy), [r], [r_dst])
            else:
                pool.op(lambda t=t, dst=dst, n=n: nc.gpsimd.tensor_copy(out=dst, in_=t[:, 0:n]), [r], [r_dst])
            cast_rr[0] += 1

    sb_cnt = [0]

    def sb(st, name, shape, dt=F32):
        sb_cnt[0] += 1
        t = st.enter_context(nc.sbuf_tensor(f"sb{sb_cnt[0]}_" + name, list(shape), dt))
        return t, Res(name)

    ps = []
    psr = []
    for i in range(7):
        t = es.enter_context(nc.psum_tensor(f"ps{i}", [128, 512], F32))
        ps.append(t)
        psr.append(Res(f"ps{i}"))
    ps7b = es.enter_context(nc.psum_tensor("ps7b", [128, 1024], BF16))
    ps.append(None)
    psr.append(Res("ps7b"))

    def psbf(i):
        return ps7b[:]

    identf, r_identf = sb(es, "identf", [128, 128])
    identb, r_identb = sb(es, "identb", [128, 128], BF16)
    onesf, r_onesf = sb(es, "onesf", [128, 128])
    onesb, r_onesb = sb(es, "onesb", [128, 128], BF16)
    tri, r_tri = sb(es, "tri", [128, 128])
    negm, r_negm = sb(es, "negm", [128, 512])
    modg2, r_modg2 = sb(es, "modg2", [128, 2, D])
    rsm, r_rsm = sb(es, "rsm", [128, 32 * 8])
    d1i, r_d1i = sb(es, "d1i", [128, 32], mybir.dt.int32)
    d2i, r_d2i = sb(es, "d2i", [128, 32], mybir.dt.int32)
    widx, r_widx = sb(es, "widx", [128, 128], mybir.dt.int32)
    modfm, r_modfm = sb(es, "modfm", [128, 16, 2])
    ccw, r_ccw = sb(es, "ccw", [128, 8, 31])
    pfm, r_pfm = sb(es, "pfm", [128, 64])
    prow, r_prow = sb(es, "prow", [128, 64])
    rb, r_rb = sb(es, "rb", [128, 72])
    stMid = es.enter_context(contextlib.ExitStack())
    modrow, r_modrow = sb(stMid, "modrow", [128, 2, 3 * D])
    Lg, r_Lg = sb(stMid, "Lg", [128, 32, 72])

    sp.dma([(identf[:], identf_d[:, :])], [], [r_identf], r_identf)
    sp.dma([(tri[:], tri_d[:, :])], [], [r_tri], r_tri)
    sp.dma([(negm[:], negm_d[:, :])], [], [r_negm], r_negm)
    sp.dma([(ccw[:], ccw_d[:, :, :])], [], [r_ccw], r_ccw)
    sp.dma([(pfm[:, 0:8], ccb_d[:, :]), (pfm[:, 8:16], clg_d[:, :]), (pfm[:, 16:24], clb_d[:, :]),
            (pfm[:, 24:40], scb_d[:, :])], [], [r_pfm], r_pfm)
    sp.dma([(prow[:, 0:16], dtb_d[:, :]), (prow[:, 48:64], alog_d[:, :]), (prow[:, 32:48], dsk_d[:, :])],
           [], [r_prow], r_prow)
    sp.dma([(rb[:], rb_d[:, :])], [], [r_rb], r_rb)
    dve.op(lambda: nc.vector.tensor_copy(out=identb[:], in_=identf[:]), [r_identf], [r_identb])
    dve.op(lambda: nc.vector.memset(onesf[:], 1.0), [], [r_onesf])
    dve.op(lambda: nc.vector.memset(onesb[:], 1.0), [], [r_onesb])
    act.op(lambda: nc.scalar.activation(out=prow[:, 16:32], in_=prow[:, 48:64], func=AF.Exp), [r_prow], [r_prow])
    dve.op(lambda: nc.vector.tensor_scalar(out=prow[:, 16:32], in0=prow[:, 16:32], scalar1=-1.0, scalar2=None,
                                           op0=ALU.mult), [r_prow], [r_prow])

    with contextlib.ExitStack() as st0:
        cT, r_cT = sb(st0, "cT", [128, 8, 2])
        crep, r_crep = sb(st0, "crep", [128, 2, 8, 128])
        bfm, r_bfm = sb(st0, "bfm", [128, 16])
        brow, r_brow = sb(st0, "brow", [128, 4 * D])
        slabA = [sb(st0, f"slabA{i}", [128, 8, 1024]) for i in range(2)]
        slabB = [sb(st0, f"slabB{i}", [128, 8, 512]) for i in range(2)]
        sp.dma([(cT[:], cT_d[:, :, :])], [], [r_cT], r_cT)
        sp.dma([(bfm[:], bada_fm_d[:, :])], [], [r_bfm], r_bfm)
        sp.dma([(brow[:], bada_row_d[:, :])], [], [r_brow], r_brow)
        act.op(lambda: nc.scalar.activation(out=cT[:], in_=cT[:], func=AF.Silu), [r_cT], [r_cT])
        for b in range(2):
            dve.op(lambda b=b: nc.vector.tensor_copy(
                out=crep[:, b, :, :], in_=cT[:, :, b:b + 1].broadcast_to([128, 8, 128])), [r_cT], [r_crep])
        for s in range(2):
            t, r = slabA[s]
            sp.dma([(t[:, k, :], wada_d[k * 128:(k + 1) * 128, s * 1024:(s + 1) * 1024]) for k in range(8)],
                   [], [r], r)
            for j in range(8):
                fc = s * 8 + j

                def mm(fc=fc, j=j, t=t):
                    ins = None
                    for k in range(8):
                        ins = nc.tensor.matmul(ps[0][:, 2 * fc:2 * fc + 2], lhsT=t[:, k, j * 128:(j + 1) * 128],
                                               rhs=cT[:, k, :], start=(k == 0), stop=(k == 7))
                    return ins
                pe.op(mm, [r, r_cT], [psr[0]])
        dve.op(lambda: nc.vector.tensor_tensor(
            out=modfm[:], in0=ps[0][:, 0:32].rearrange("p (f b) -> p f b", b=2),
            in1=bfm[:].unsqueeze(2).broadcast_to([128, 16, 2]), op=ALU.add), [psr[0], r_bfm], [r_modfm])
        dve.op(lambda: nc.vector.tensor_scalar(out=modfm[:, 8:16, :], in0=modfm[:, 8:16, :], scalar1=1.0,
                                               scalar2=None, op0=ALU.add), [r_modfm], [r_modfm])
        for tsl in range(8):
            t, r = slabB[tsl % 2]
            c0 = 2048 + tsl * 512
            sp.dma([(t[:, k, :], wada_d[k * 128:(k + 1) * 128, c0:c0 + 512]) for k in range(8)], [], [r], r)
            for b in range(2):
                pb = 1 + b

                def mm(b=b, t=t, pb=pb):
                    ins = None
                    for k in range(8):
                        ins = nc.tensor.matmul(ps[pb][:, :], lhsT=crep[:, b, k, :], rhs=t[:, k, :],
                                               start=(k == 0), stop=(k == 7))
                    return ins
                pe.op(mm, [r, r_crep], [psr[pb]])
                if tsl < 6:
                    dve.op(lambda b=b, pb=pb, tsl=tsl: nc.vector.tensor_tensor(
                        out=modrow[:, b, tsl * 512:(tsl + 1) * 512], in0=ps[pb][:, :],
                        in1=brow[:, tsl * 512:(tsl + 1) * 512], op=ALU.add), [psr[pb], r_brow], [r_modrow])
                else:
                    dve.op(lambda b=b, pb=pb, tsl=tsl: nc.vector.tensor_tensor(
                        out=modg2[:, b, (tsl - 6) * 512:(tsl - 5) * 512], in0=ps[pb][:, :],
                        in1=brow[:, tsl * 512:(tsl + 1) * 512], op=ALU.add), [psr[pb], r_brow], [r_modg2])
        dve.op(lambda: nc.vector.tensor_scalar(out=modrow[:, :, 2048:3072], in0=modrow[:, :, 2048:3072],
                                               scalar1=1.0, scalar2=None, op0=ALU.add), [r_modrow], [r_modrow])

    barrier()
    if stage == "0":
        sp.dma([(out_d[0:128, :], modrow[:, 0, 0:1024])], [r_modrow], [], r_modrow)
        sp.dma([(out_d[128:256, :], modg2[:, 1, :])], [r_modg2], [], r_modg2)
        sp.dma([(out_d[256:384, 0:32], modfm[:].rearrange("p a b -> p (a b)"))], [r_modfm], [], r_modfm)
        sp.deps([], [r_modrow, r_modg2, r_modfm])
        raise EarlyExit()

    xT_v = xT_d.rearrange("(k p) t -> p k t", p=128)
    UF_v = UF_d.rearrange("(k p) t -> p k t", p=128)
    YN_v = YN_d.rearrange("(k p) t -> p k t", p=128)

    def load_hT(st_tiles, tok0, nt, bsel):
        xTf, r_xTf, hT, r_hT = st_tiles
        sp.dma([(xTf[:, :, 0:nt], xT_v[:, :, tok0:tok0 + nt])], [], [r_xTf], r_xTf)
        for k in range(8):
            eng, E = (nc.vector, dve) if k % 2 == 0 else (nc.gpsimd, pool)
            E.op(lambda k=k, eng=eng: eng.tensor_scalar(
                out=hT[:, k, 0:nt], in0=xTf[:, k, 0:nt], scalar1=modfm[:, 8 + k, bsel:bsel + 1],
                scalar2=modfm[:, k, bsel:bsel + 1], op0=ALU.mult, op1=ALU.add), [r_xTf, r_modfm], [r_hT])

    TB = 256
    with contextlib.ExitStack() as stA:
        winA, r_winA = sb(stA, "winA", [128, 8, 2048], BF16)
        dgA, r_dgA = sb(stA, "dgA", [128, 8, 31, 128], BF16)
        r_p6 = [psr[6], psr[6]]
        xTf, r_xTf = sb(stA, "xTfA", [128, 8, TB])
        hT, r_hT = sb(stA, "hTA", [128, 8, TB], BF16)
        sig = [sb(stA, f"sig{i}", [128, TB]) for i in range(2)]
        ub, r_ub = sb(stA, "ub", [128, 8, 30 + TB], BF16)
        r_uc = [Res(f"uc{c}") for c in range(8)]
        cv, _ = sb(stA, "cv", [128, 8, TB])
        r_cv = [Res(f"cv{c}") for c in range(8)]
        cvq = [sb(stA, f"cvq{i}", [128, 2, TB], BF16) for i in range(2)]
        mean, r_mean = sb(stA, "mean", [128, TB])
        var, r_var = sb(stA, "var", [128, TB])
        rstd, r_rstd = sb(stA, "rstd", [128, TB])
        uf, r_uf = sb(stA, "uf", [128, 8, TB], BF16)
        stgA = [sb(stA, f"stgA{i}", [128, 1024]) for i in range(2)]
        load_cast([winA[:, k, h * 1024:(h + 1) * 1024] for k in range(8) for h in range(2)],
                  [win_d[k * 128:(k + 1) * 128, h * 1024:(h + 1) * 1024] for k in range(8) for h in range(2)],
                  stgA, r_winA)
        for c in range(8):
            for k in range(31):
                dve.op(lambda c=c, k=k: nc.vector.tensor_scalar(
                    out=dgA[:, c, k, :], in0=identb[:], scalar1=ccw[:, c, k:k + 1], scalar2=None, op0=ALU.mult),
                    [r_identb, r_ccw], [r_dgA])
        for s in range(2):
            for c in range(8):
                dve.op(lambda c=c: nc.vector.memset(ub[:, c, 0:30], 0.0), [], [r_uc[c]])
            for j in range(SEQ // TB):
                tok0 = s * SEQ + j * TB
                load_hT((xTf, r_xTf, hT, r_hT), tok0, TB, s)
                for c in range(8):
                    pv, pg = (0, 1) if c % 2 == 0 else (2, 3)
                    for (pi, col0) in ((pv, c * 128), (pg, 1024 + c * 128)):
                        def mm(pi=pi, col0=col0):
                            ins = None
                            for k in range(8):
                                ins = nc.tensor.matmul(ps[pi][:, 0:TB], lhsT=winA[:, k, col0:col0 + 128],
                                                       rhs=hT[:, k, :], start=(k == 0), stop=(k == 7))
                            return ins
                        pe.op(mm, [r_winA, r_hT], [psr[pi]])
                    sg, r_sg = sig[c % 2]
                    act.op(lambda pg=pg, sg=sg: nc.scalar.activation(out=sg[:], in_=ps[pg][:, 0:TB], func=AF.Sigmoid),
                           [psr[pg]], [r_sg])
                    dve.op(lambda pv=pv, sg=sg, c=c: nc.vector.tensor_tensor(
                        out=ub[:, c, 30:30 + TB], in0=ps[pv][:, 0:TB], in1=sg[:], op=ALU.mult),
                        [psr[pv], r_sg], [r_uc[c]])
                ck(51)
                for c in range(8):
                    hp = c % 2

                    def mm(c=c, hp=hp):
                        ins = None
                        for k in range(31):
                            ins = nc.tensor.matmul(ps[6][:, hp * TB:(hp + 1) * TB], lhsT=dgA[:, c, k, :],
                                                   rhs=ub[:, c, k:k + TB], start=(k == 0), stop=(k == 30))
                        return ins
                    pe.op(mm, [r_dgA, r_uc[c]], [r_p6[hp]])
                    ck(52)
                    act.op(lambda c=c, hp=hp: nc.scalar.activation(
                        out=cv[:, c, :], in_=ps[6][:, hp * TB:(hp + 1) * TB], func=AF.Identity,
                        bias=pfm[:, c:c + 1]), [r_p6[hp], r_pfm], [r_cv[c]])
                    ck(53)
                ck(54)
                for c in range(8):
                    eng, E = (nc.vector, dve) if c < 5 else (nc.gpsimd, pool)
                    E.op(lambda c=c, eng=eng: eng.tensor_copy(out=ub[:, c, 0:30], in_=ub[:, c, TB:TB + 30]),
                         [r_uc[c]], [r_uc[c]])
                for c in range(8):
                    q, r_q = cvq[c % 2]
                    act.op(lambda c=c, q=q: nc.scalar.activation(out=q[:, 0, :], in_=cv[:, c, :], func=AF.Copy),
                           [r_cv[c]], [r_q])
                    act.op(lambda c=c, q=q: nc.scalar.activation(out=q[:, 1, :], in_=cv[:, c, :], func=AF.Square),
                           [r_cv[c]], [r_q])

                    def mm(c=c, q=q):
                        nc.tensor.matmul(ps[4][:, 0:TB], lhsT=onesb[:], rhs=q[:, 0, :], start=(c == 0), stop=(c == 7))
                        return nc.tensor.matmul(ps[5][:, 0:TB], lhsT=onesb[:], rhs=q[:, 1, :], start=(c == 0),
                                                stop=(c == 7))
                    pe.op(mm, [r_q, r_onesb], [psr[4], psr[5]])
                ck(55)
                dve.op(lambda: nc.vector.tensor_scalar(out=mean[:], in0=ps[4][:, 0:TB], scalar1=1.0 / 1024.0,
                                                       scalar2=None, op0=ALU.mult), [psr[4]], [r_mean])
                dve.op(lambda: nc.vector.tensor_tensor(out=var[:], in0=mean[:], in1=mean[:], op=ALU.mult),
                       [r_mean], [r_var])
                dve.op(lambda: nc.vector.scalar_tensor_tensor(out=var[:], in0=ps[5][:, 0:TB], scalar=1.0 / 1024.0,
                                                              in1=var[:], op0=ALU.mult, op1=ALU.subtract),
                       [psr[5], r_var], [r_var])
                dve.op(lambda: nc.vector.tensor_scalar(out=var[:], in0=var[:], scalar1=0.0, scalar2=EPS,
                                                       op0=ALU.max, op1=ALU.add), [r_var], [r_var])
                act.op(lambda: nc.scalar.activation(out=var[:], in_=var[:], func=AF.Sqrt), [r_var], [r_var])
                dve.op(lambda: nc.vector.reciprocal(out=rstd[:], in_=var[:]), [r_var], [r_rstd])
                for c in range(8):
                    eng, E = (nc.vector, dve) if c < 5 else (nc.gpsimd, pool)
                    E.op(lambda c=c, eng=eng: eng.tensor_tensor(out=cv[:, c, :], in0=cv[:, c, :], in1=mean[:],
                                                                op=ALU.subtract), [r_cv[c], r_mean], [r_cv[c]])
                    E.op(lambda c=c, eng=eng: eng.tensor_tensor(out=cv[:, c, :], in0=cv[:, c, :], in1=rstd[:],
                                                                op=ALU.mult), [r_cv[c], r_rstd], [r_cv[c]])
                    act.op(lambda c=c: nc.scalar.activation(out=uf[:, c, :], in_=cv[:, c, :], func=AF.Silu,
                                                            bias=pfm[:, 16 + c:17 + c], scale=pfm[:, 8 + c:9 + c]),
                           [r_cv[c], r_pfm], [r_uf])
                act.dma([(UF_v[:, :, tok0:tok0 + TB], uf[:])], [r_uf], [r_UF], r_uf)
                ck(56)
                if stage == "A1":
                    dbg, r_dbg = sb(stA, "dbg", [128, 1024])
                    def dump(row, src_ap, n, rs):
                        dve.op(lambda: nc.vector.tensor_copy(out=dbg[:, 0:n], in_=src_ap), rs, [r_dbg])
                        sp.dma([(out_d[row:row + 128, 0:n], dbg[:, 0:n])], [r_dbg], [], r_dbg)
                    dump(0, winA[:, 0, 0:1024], 1024, [r_winA])
                    dump(128, hT[:, 0, :], 512, [r_hT])
                    dump(256, ub[:, 0, 0:542], 542, [r_uc[0]])
                    dump(384, cv[:, 0, :], 512, [r_cv[0]])
                    dump(512, mean[:], 512, [r_mean])
                    dump(640, var[:], 512, [r_var])
                    dump(768, rstd[:], 512, [r_rstd])
                    dump(896, uf[:, 0, :], 512, [r_uf])
                    sp.deps([], [r_dbg, r_uf])
                    raise EarlyExit()
                if stage == "A":
                    j4 = tok0 // 1024
                    col = tok0 % 1024
                    dve.op(lambda: nc.vector.tensor_copy(out=xTf[:], in_=uf[:]), [r_uf], [r_xTf])
                    sp.dma([(out_d[j4 * 1024:(j4 + 1) * 1024, col:col + TB].rearrange("(c p) t -> p c t", p=128),
                             xTf[:])], [r_xTf], [], r_xTf)

    barrier()
    if stage == "A":
        sp.deps([], [r_uf, r_xTf])
        raise EarlyExit()

    TBB = 128
    with contextlib.ExitStack() as stB:
        winB, r_winB = sb(stB, "winB", [128, 8, 3088], BF16)
        dg, r_dg = sb(stB, "dg", [128, 16, 4, 128], BF16)
        scw, r_scw = sb(stB, "scw", [128, 16, 4])
        normw, r_normw = sb(stB, "normw", [128, D])
        xTf, r_xTf = sb(stB, "xTfB", [128, 8, TBB])
        hT, r_hT = sb(stB, "hTB", [128, 8, TBB], BF16)
        xpre, r_xpre = sb(stB, "xpre", [128, 16, 3 + TBB], BF16)
        xpost, r_xpost = sb(stB, "xpost", [128, 16, TBB], BF16)
        tA, r_tA = sb(stB, "tA", [128, D])
        tB, r_tB = sb(stB, "tB", [128, D])
        tC, r_tC = sb(stB, "tC", [128, D])
        Rm, r_Rm = sb(stB, "Rm", [128, D])
        dec, r_dec = sb(stB, "dec", [128, D])
        MT, r_MT = sb(stB, "MT", [128, 16, 128], BF16)
        cbs, r_cbs = sb(stB, "cbs", [128, 512])
        xc, r_xc = sb(stB, "xc", [128, D], BF16)
        xcd, r_xcd = sb(stB, "xcd", [128, D], BF16)
        ynb, r_ynb = sb(stB, "ynb", [128, D], BF16)
        ynT, r_ynT = sb(stB, "ynT", [128, 8, 128], BF16)
        Btok, r_Btok = sb(stB, "Btok", [128, 512], BF16)
        S, r_S = sb(stB, "S", [128, D])
        Sb, r_Sb = sb(stB, "Sb", [128, D], BF16)
        sm, r_sm = sb(stB, "smB", [128, 16 * 12])
        dtp = sm[:, 0:16]
        dtv = sm[:, 16:32]
        av = sm[:, 32:48]
        acs = sm[:, 48:64]
        nacs = sm[:, 64:80]
        eacs = sm[:, 80:96]
        dif = sm[:, 96:112]
        dte = sm[:, 112:128]
        cdr = sm[:, 128:144]
        ss4 = sm[:, 144:148]
        rs4 = sm[:, 148:152]
        mv = sm[:, 152:154]
        rs1 = sm[:, 154:155]
        junk = dec

        stgB = [sb(stB, f"stgB{i}", [128, 1544]) for i in range(2)]
        for half in range(2):
            c0 = 2048 + half * 1544
            load_cast([winB[:, k, half * 1544:(half + 1) * 1544] for k in range(8)],
                      [win_d[k * 128:(k + 1) * 128, c0:c0 + 1544] for k in range(8)], stgB, r_winB)
        sp.dma([(scw[:], scw_d[:, :, :])], [], [r_scw], r_scw)
        sp.dma([(normw[:], normw_d[:, :])], [], [r_normw], r_normw)
        for c in range(16):
            for k in range(4):
                dve.op(lambda c=c, k=k: nc.vector.tensor_scalar(
                    out=dg[:, c, k, :], in0=identb[:], scalar1=scw[:, c, k:k + 1], scalar2=None, op0=ALU.mult),
                    [r_identb, r_scw], [r_dg])

        ZC0, XC0, DC0 = 0, 1024, 3072
        for s in range(2):
            dve.op(lambda: nc.vector.memset(xpre[:, :, 0:3], 0.0), [], [r_xpre])
            dve.op(lambda: nc.vector.memset(S[:], 0.0), [], [r_S])
            dve.op(lambda: nc.vector.memset(Sb[:], 0.0), [], [r_Sb])
            for j in range(SEQ // TBB):
                tok0 = s * SEQ + j * TBB
                load_hT((xTf, r_xTf, hT, r_hT), tok0, TBB, s)
                for cp in range(8):
                    pi = cp % 2
                    for hh in range(2):
                        c = cp * 2 + hh

                        def mm(pi=pi, hh=hh, c=c):
                            ins = None
                            for k in range(8):
                                ins = nc.tensor.matmul(ps[pi][:, hh * TBB:(hh + 1) * TBB],
                                                       lhsT=winB[:, k, XC0 + c * 128:XC0 + (c + 1) * 128],
                                                       rhs=hT[:, k, :], start=(k == 0), stop=(k == 7))
                            return ins
                        pe.op(mm, [r_winB, r_hT], [psr[pi]])
                    act.op(lambda pi=pi, cp=cp: nc.scalar.activation(
                        out=xpre[:, 2 * cp:2 * cp + 2, 3:3 + TBB],
                        in_=ps[pi][:, 0:2 * TBB].rearrange("p (a t) -> p a t", a=2), func=AF.Copy),
                        [psr[pi]], [r_xpre])
                for cp in range(8):
                    pi = 2 + cp % 2
                    for hh in range(2):
                        c = cp * 2 + hh

                        def mm(pi=pi, hh=hh, c=c):
                            ins = None
                            for k in range(4):
                                ins = nc.tensor.matmul(ps[pi][:, hh * TBB:(hh + 1) * TBB], lhsT=dg[:, c, k, :],
                                                       rhs=xpre[:, c, k:k + TBB], start=(k == 0), stop=(k == 3))
                            return ins
                        pe.op(mm, [r_dg, r_xpre], [psr[pi]])
                        act.op(lambda pi=pi, hh=hh, c=c: nc.scalar.activation(
                            out=xpost[:, c, :], in_=ps[pi][:, hh * TBB:(hh + 1) * TBB], func=AF.Silu,
                            bias=pfm[:, 24 + c:25 + c]), [psr[pi], r_pfm], [r_xpost])
                dve.op(lambda: nc.vector.tensor_copy(out=xpre[:, :, 0:3], in_=xpre[:, :, TBB:TBB + 3]),
                       [r_xpre], [r_xpre])

                ck(1)
                for q in range(TBB // 128):
                    o = q * 128
                    t0 = tok0 + o
                    tile_idx = t0 // 128
                    for hf in range(2):
                        def mm(hf=hf, o=o):
                            ins = None
                            for k in range(8):
                                ins = nc.tensor.matmul(ps[hf][:, :], lhsT=hT[:, k, o:o + 128],
                                                       rhs=winB[:, k, ZC0 + hf * 512:ZC0 + (hf + 1) * 512],
                                                       start=(k == 0), stop=(k == 7))
                            return ins
                        pe.op(mm, [r_hT, r_winB], [psr[hf]])

                    def mm(o=o):
                        ins = None
                        for k in range(8):
                            ins = nc.tensor.matmul(ps[2][:, 0:16], lhsT=hT[:, k, o:o + 128],
                                                   rhs=winB[:, k, DC0:DC0 + 16], start=(k == 0), stop=(k == 7))
                        return ins
                    pe.op(mm, [r_hT, r_winB], [psr[2]])
                    ck(2)
                    dve.op(lambda: nc.vector.tensor_tensor(out=dtp, in0=ps[2][:, 0:16], in1=prow[:, 0:16],
                                                           op=ALU.add), [psr[2], r_prow], [r_sm])
                    act.op(lambda: nc.scalar.activation(out=dtp, in_=dtp, func=AF.Exp), [r_sm], [r_sm])
                    act.op(lambda: nc.scalar.activation(out=dtv, in_=dtp, func=AF.Ln, bias=1.0), [r_sm], [r_sm])
                    dve.op(lambda: nc.vector.tensor_tensor(out=av, in0=dtv, in1=prow[:, 16:32], op=ALU.mult),
                           [r_sm, r_prow], [r_sm])

                    def mm():
                        nc.tensor.matmul(ps[2][:, 16:32], lhsT=tri[:], rhs=av, start=True, stop=True)
                        return nc.tensor.matmul(ps[2][:, 32:48], lhsT=onesf[:], rhs=av, start=True, stop=True)
                    pe.op(mm, [r_tri, r_onesf, r_sm], [psr[2]])
                    dve.op(lambda: nc.vector.tensor_copy(out=acs, in_=ps[2][:, 16:32]), [psr[2]], [r_sm])
                    dve.op(lambda: nc.vector.tensor_scalar(out=nacs, in0=acs, scalar1=-1.0, scalar2=None,
                                                           op0=ALU.mult), [r_sm], [r_sm])
                    dve.op(lambda: nc.vector.tensor_tensor(out=dif, in0=ps[2][:, 32:48], in1=acs, op=ALU.subtract),
                           [psr[2], r_sm], [r_sm])
                    act.op(lambda: nc.scalar.activation(out=eacs, in_=acs, func=AF.Exp), [r_sm], [r_sm])
                    act.op(lambda: nc.scalar.activation(out=dte, in_=dif, func=AF.Exp), [r_sm], [r_sm])
                    act.op(lambda: nc.scalar.activation(out=cdr, in_=ps[2][:, 32:48], func=AF.Exp),
                           [psr[2], r_sm], [r_sm])
                    ck(3)
                    def mm(o=o):
                        ins = None
                        for g in range(4):
                            ins = nc.tensor.matmul(ps[3][:, g * 128:(g + 1) * 128], lhsT=xpost[:, 8 + g, o:o + 128],
                                                   rhs=xpost[:, 12 + g, o:o + 128], start=True, stop=True)
                        return ins
                    pe.op(mm, [r_xpost], [psr[3]])
                    act.op(lambda: nc.scalar.activation(out=cbs[:], in_=ps[3][:, :], func=AF.Copy), [psr[3]], [r_cbs])
                    ck(4)
                    def mm(o=o):
                        ins = None
                        for c in range(8):
                            ins = nc.tensor.transpose(out=psbf(7)[:, c * 128:(c + 1) * 128], in_=xpost[:, c, o:o + 128],
                                                      identity=identb[:])
                        return ins
                    pe.op(mm, [r_xpost, r_identb], [psr[7]])
                    ck(41)
                    act.op(lambda: nc.scalar.activation(out=tB[:], in_=psbf(7)[:, :], func=AF.Copy), [psr[7]], [r_tB])
                    ck(42)
                    dve.op(lambda: nc.vector.tensor_tensor(
                        out=xc[:].rearrange("p (h d) -> p h d", d=64),
                        in0=tB[:].rearrange("p (h d) -> p h d", d=64),
                        in1=dtv.unsqueeze(2).broadcast_to([128, 16, 64]), op=ALU.mult), [r_tB, r_sm], [r_xc])
                    ck(43)
                    dve.op(lambda: nc.vector.tensor_tensor(
                        out=xcd[:].rearrange("p (h d) -> p h d", d=64),
                        in0=xc[:].rearrange("p (h d) -> p h d", d=64),
                        in1=dte.unsqueeze(2).broadcast_to([128, 16, 64]), op=ALU.mult), [r_xc, r_sm], [r_xcd])
                    ck(5)
                    for hh in range(2):
                        dve.op(lambda hh=hh: nc.vector.tensor_tensor(
                            out=Rm[:].rearrange("p (h l) -> p h l", l=128),
                            in0=tri[:].unsqueeze(1).broadcast_to([128, 8, 128]),
                            in1=av[:, 8 * hh:8 * hh + 8].unsqueeze(2).broadcast_to([128, 8, 128]), op=ALU.mult),
                            [r_tri, r_sm], [r_Rm])
                        for jb in range(2):
                            def mm(jb=jb):
                                nc.tensor.matmul(ps[4 + jb][:, :], lhsT=onesf[:], rhs=Rm[:, jb * 512:(jb + 1) * 512],
                                                 start=True, stop=False)
                                return nc.tensor.matmul(ps[4 + jb][:, :], lhsT=identf[:], rhs=negm[:],
                                                        start=False, stop=True)
                            pe.op(mm, [r_onesf, r_identf, r_negm, r_Rm], [psr[4 + jb]])
                        for jj in range(8):
                            h = 8 * hh + jj
                            act.op(lambda jj=jj, h=h: nc.scalar.activation(
                                out=dec[:, jj * 128:(jj + 1) * 128],
                                in_=ps[4 + jj // 4][:, (jj % 4) * 128:(jj % 4 + 1) * 128], func=AF.Exp,
                                bias=nacs[:, h:h + 1]), [psr[4 + jj // 4], r_sm], [r_dec])
                        for jb in range(2):
                            g = 2 * hh + jb
                            dve.op(lambda jb=jb, g=g, hh=hh: nc.vector.tensor_tensor(
                                out=MT[:, 8 * hh + 4 * jb:8 * hh + 4 * jb + 4, :],
                                in0=dec[:, jb * 512:(jb + 1) * 512].rearrange("p (h l) -> p h l", l=128),
                                in1=cbs[:, g * 128:(g + 1) * 128].unsqueeze(1).broadcast_to([128, 4, 128]),
                                op=ALU.mult), [r_dec, r_cbs], [r_MT])
                    ck(6)
                    for bk in range(2):
                        def mm(bk=bk, o=o):
                            ins = None
                            for gg in range(2):
                                g = 2 * bk + gg
                                ins = nc.tensor.matmul(ps[4 + bk][:, gg * 256:(gg + 1) * 256],
                                                       lhsT=xpost[:, 12 + g, o:o + 128],
                                                       rhs=Sb[:, g * 256:(g + 1) * 256], start=True, stop=True)
                            return ins
                        pe.op(mm, [r_xpost, r_Sb], [psr[4 + bk]])
                        dve.op(lambda bk=bk: nc.vector.tensor_tensor(
                            out=tC[:, bk * 512:(bk + 1) * 512].rearrange("p (h d) -> p h d", d=64),
                            in0=ps[4 + bk][:, :].rearrange("p (h d) -> p h d", d=64),
                            in1=eacs[:, 8 * bk:8 * bk + 8].unsqueeze(2).broadcast_to([128, 8, 64]), op=ALU.mult),
                            [psr[4 + bk], r_sm], [r_tC])
                    ck(7)
                    for bk in range(2):
                        def mm(bk=bk):
                            ins = None
                            for hh8 in range(8):
                                h = 8 * bk + hh8
                                ins = nc.tensor.matmul(ps[(6, 3)[bk]][:, hh8 * 64:(hh8 + 1) * 64], lhsT=MT[:, h, :],
                                                       rhs=xc[:, h * 64:(h + 1) * 64], start=True, stop=True)
                            return ins
                        pe.op(mm, [r_MT, r_xc], [psr[(6, 3)[bk]]])
                        dve.op(lambda bk=bk: nc.vector.tensor_tensor(
                            out=tC[:, bk * 512:(bk + 1) * 512], in0=ps[(6, 3)[bk]][:, :],
                            in1=tC[:, bk * 512:(bk + 1) * 512], op=ALU.add), [psr[(6, 3)[bk]], r_tC], [r_tC])
                    ck(8)
                    dve.op(lambda: nc.vector.tensor_tensor(
                        out=tB[:].rearrange("p (h d) -> p h d", d=64), in0=tB[:].rearrange("p (h d) -> p h d", d=64),
                        in1=prow[:, 32:48].unsqueeze(2).broadcast_to([128, 16, 64]), op=ALU.mult),
                        [r_tB, r_prow], [r_tB])
                    dve.op(lambda: nc.vector.tensor_tensor(out=tC[:], in0=tC[:], in1=tB[:], op=ALU.add),
                           [r_tC, r_tB], [r_tC])
                    ck(9)
                    for hf in range(2):
                        act.op(lambda hf=hf: nc.scalar.activation(out=tA[:, hf * 512:(hf + 1) * 512], in_=ps[hf][:, :],
                                                                  func=AF.Silu), [psr[hf]], [r_tA])
                    dve.op(lambda: nc.vector.tensor_tensor(out=tC[:], in0=tC[:], in1=tA[:], op=ALU.mult),
                           [r_tC, r_tA], [r_tC])
                    for g4 in range(4):
                        act.op(lambda g4=g4: nc.scalar.activation(
                            out=junk[:, 0:256], in_=tC[:, g4 * 256:(g4 + 1) * 256], func=AF.Square,
                            accum_out=ss4[:, g4:g4 + 1]), [r_tC], [r_dec, r_sm])
                    dve.op(lambda: nc.vector.tensor_scalar(out=ss4, in0=ss4, scalar1=1.0 / 256.0, scalar2=EPS,
                                                           op0=ALU.mult, op1=ALU.add), [r_sm], [r_sm])
                    act.op(lambda: nc.scalar.activation(out=ss4, in_=ss4, func=AF.Sqrt), [r_sm], [r_sm])
                    dve.op(lambda: nc.vector.reciprocal(out=rs4, in_=ss4), [r_sm], [r_sm])
                    dve.op(lambda: nc.vector.tensor_tensor(
                        out=tC[:].rearrange("p (g d) -> p g d", d=256), in0=tC[:].rearrange("p (g d) -> p g d", d=256),
                        in1=rs4.unsqueeze(2).broadcast_to([128, 4, 256]), op=ALU.mult), [r_tC, r_sm], [r_tC])
                    dve.op(lambda: nc.vector.tensor_tensor(out=ynb[:], in0=tC[:], in1=normw[:], op=ALU.mult),
                           [r_tC, r_normw], [r_ynb])

                    def mm():
                        ins = None
                        for c in range(8):
                            ins = nc.tensor.transpose(out=psbf(3)[:, c * 128:(c + 1) * 128],
                                                      in_=ynb[:, c * 128:(c + 1) * 128], identity=identb[:])
                        return ins
                    pe.op(mm, [r_ynb, r_identb], [psr[7]])
                    act.op(lambda: nc.scalar.activation(out=ynT[:].rearrange("p c t -> p (c t)"), in_=psbf(3)[:, :],
                                                        func=AF.Copy), [psr[7]], [r_ynT])
                    ck(10)
                    def mm(o=o):
                        ins = None
                        for g in range(4):
                            ins = nc.tensor.transpose(out=psbf(2)[:, g * 128:(g + 1) * 128],
                                                      in_=xpost[:, 8 + g, o:o + 128], identity=identb[:])
                        return ins
                    pe.op(mm, [r_xpost, r_identb], [psr[7]])
                    act.op(lambda: nc.scalar.activation(out=Btok[:], in_=psbf(2)[:, 0:512], func=AF.Copy),
                           [psr[7]], [r_Btok])
                    dve.op(lambda: nc.vector.tensor_tensor(
                        out=S[:].rearrange("p (h d) -> p h d", d=64), in0=S[:].rearrange("p (h d) -> p h d", d=64),
                        in1=cdr.unsqueeze(2).broadcast_to([128, 16, 64]), op=ALU.mult), [r_S, r_sm], [r_S])
                    for bk in range(2):
                        def mm(bk=bk):
                            ins = None
                            for gg in range(2):
                                g = 2 * bk + gg
                                ins = nc.tensor.matmul(ps[4 + bk][:, gg * 256:(gg + 1) * 256],
                                                       lhsT=Btok[:, g * 128:(g + 1) * 128],
                                                       rhs=xcd[:, g * 256:(g + 1) * 256], start=True, stop=True)
                            return ins
                        pe.op(mm, [r_Btok, r_xcd], [psr[4 + bk]])
                        dve.op(lambda bk=bk: nc.vector.tensor_tensor(
                            out=S[:, bk * 512:(bk + 1) * 512], in0=ps[4 + bk][:, :],
                            in1=S[:, bk * 512:(bk + 1) * 512], op=ALU.add), [psr[4 + bk], r_S], [r_S])
                    act.op(lambda: nc.scalar.activation(out=Sb[:], in_=S[:], func=AF.Copy), [r_S], [r_Sb])
                    act.dma([(YN_v[:, :, t0:t0 + 128], ynT[:])], [r_ynT], [r_YN], r_ynT)
                    if stage == "B1":
                        j4 = t0 // 1024
                        col = t0 % 1024
                        dve.op(lambda: nc.vector.tensor_copy(out=xTf[:], in_=ynT[:]), [r_ynT], [r_xTf])
                        sp.dma([(out_d[j4 * 1024:(j4 + 1) * 1024, col:col + 128].rearrange("(c p) t -> p c t", p=128),
                                 xTf[:])], [r_xTf], [], r_xTf)

    barrier()
    if stage == "B1":
        raise EarlyExit()
    with contextlib.ExitStack() as stC:
        woutb, r_woutb = sb(stC, "woutb", [128, 16, 1024], BF16)
        ln1g, r_ln1g = sb(stC, "ln1g", [128, D])
        ln1b, r_ln1b = sb(stC, "ln1b", [128, D])
        wr, r_wr = sb(stC, "wr", [128, 8, 72])
        setsC = []
        for i in range(2):
            setsC.append((sb(stC, f"ufb{i}", [128, 8, 128], BF16), sb(stC, f"ynTC{i}", [128, 8, 128], BF16),
                          sb(stC, f"xtok{i}", [128, D]), sb(stC, f"tAC{i}", [128, D]), sb(stC, f"tCC{i}", [128, D]),
                          sb(stC, f"h2Tf{i}", [128, 8, 128]), sb(stC, f"h2b{i}", [128, D], BF16),
                          sb(stC, f"bst{i}", [128, 2, 6]), sb(stC, f"smC{i}", [128, 8])))
        stgC = [sb(stC, f"stgC{i}", [128, 1024]) for i in range(2)]
        load_cast([woutb[:, c, :] for c in range(16)], [wout_d[c * 128:(c + 1) * 128, :] for c in range(16)],
                  stgC, r_woutb)
        sp.dma([(ln1g[:], ln1g_d[:, :])], [], [r_ln1g], r_ln1g)
        sp.dma([(ln1b[:], ln1b_d[:, :])], [], [r_ln1b], r_ln1b)
        sp.dma([(wr[:], wr_d[:, :, :])], [], [r_wr], r_wr)
        for tile_idx in range(32):
            ((ufb, r_ufb), (ynT, r_ynT), (xtok, r_xtok), (tA, r_tA), (tC, r_tC), (h2Tf, r_h2Tf), (h2b, r_h2b),
             (bst, r_bst), (sm, r_sm)) = setsC[tile_idx % 2]
            mv = sm[:, 0:2]
            rs1 = sm[:, 2:3]
            po = 0 if tile_idx % 2 == 0 else 4
            s = tile_idx // 16
            t0 = tile_idx * 128
            g1row = modrow[:, s, 0:1024]
            sh2row = modrow[:, s, 1024:2048]
            sc2row = modrow[:, s, 2048:3072]
            sp.dma([(xtok[:], x_d[t0:t0 + 128, :])], [], [r_xtok], r_xtok)
            sp.dma([(ufb[:], UF_v[:, :, t0:t0 + 128])], [r_UF], [r_ufb], r_ufb)
            sp.dma([(ynT[:], YN_v[:, :, t0:t0 + 128])], [r_YN], [r_ynT], r_ynT)
            for hf in range(2):
                def mm(hf=hf):
                    ins = None
                    for c in range(8):
                        ins = nc.tensor.matmul(ps[po + hf][:, :], lhsT=ufb[:, c, :],
                                               rhs=woutb[:, c, hf * 512:(hf + 1) * 512],
                                               start=(c == 0), stop=False)
                    for c in range(8):
                        ins = nc.tensor.matmul(ps[po + hf][:, :], lhsT=ynT[:, c, :],
                                               rhs=woutb[:, 8 + c, hf * 512:(hf + 1) * 512],
                                               start=False, stop=(c == 7))
                    return ins
                pe.op(mm, [r_ufb, r_ynT, r_woutb], [psr[po + hf]])
                dve.op(lambda hf=hf: nc.vector.tensor_tensor(
                    out=tA[:, hf * 512:(hf + 1) * 512], in0=ps[po + hf][:, :],
                    in1=g1row[:, hf * 512:(hf + 1) * 512], op=ALU.mult), [psr[po + hf], r_modrow], [r_tA])
            dve.op(lambda: nc.vector.scalar_tensor_tensor(out=tA[:], in0=xtok[:], scalar=ALPHA, in1=tA[:],
                                                          op0=ALU.mult, op1=ALU.add), [r_xtok, r_tA], [r_tA])
            ck(20)
            dve.op(lambda: nc.vector.bn_stats(out=bst[:, 0, :], in_=tA[:, 0:512]), [r_tA], [r_bst])
            dve.op(lambda: nc.vector.bn_stats(out=bst[:, 1, :], in_=tA[:, 512:1024]), [r_tA], [r_bst])
            dve.op(lambda: nc.vector.bn_aggr(out=mv, in_=bst[:].rearrange("p a b -> p (a b)")),
                   [r_bst], [r_sm])
            ck(21)
            dve.op(lambda: nc.vector.tensor_scalar(out=rs1, in0=mv[:, 1:2], scalar1=EPS, scalar2=None,
                                                   op0=ALU.add), [r_sm], [r_sm])
            act.op(lambda: nc.scalar.activation(out=rs1, in_=rs1, func=AF.Sqrt), [r_sm], [r_sm])
            dve.op(lambda: nc.vector.reciprocal(out=rs1, in_=rs1), [r_sm], [r_sm])
            dve.op(lambda: nc.vector.tensor_scalar(out=tC[:], in0=tA[:], scalar1=mv[:, 0:1], scalar2=rs1,
                                                   op0=ALU.subtract, op1=ALU.mult), [r_tA, r_sm], [r_tC])
            dve.op(lambda: nc.vector.tensor_tensor(out=tC[:], in0=tC[:], in1=ln1g[:], op=ALU.mult),
                   [r_tC, r_ln1g], [r_tC])
            dve.op(lambda: nc.vector.tensor_tensor(out=tC[:], in0=tC[:], in1=ln1b[:], op=ALU.add),
                   [r_tC, r_ln1b], [r_tC])
            pool.dma([(X1_d[t0:t0 + 128, :], tC[:])], [r_tC], [r_X1], r_tC)
            if stage == "B":
                sp.dma([(out_d[t0:t0 + 128, :], tC[:])], [r_tC], [], r_tC)
            ck(22)
            dve.op(lambda: nc.vector.tensor_tensor(out=tA[:], in0=tC[:], in1=sc2row, op=ALU.mult),
                   [r_tC, r_modrow], [r_tA])
            dve.op(lambda: nc.vector.tensor_tensor(out=tA[:], in0=tA[:], in1=sh2row, op=ALU.add),
                   [r_tA, r_modrow], [r_tA])
            ck(23)
            for bk in range(2):
                def mm(bk=bk):
                    ins = None
                    for cc in range(4):
                        c = 4 * bk + cc
                        ins = nc.tensor.matmul(ps[2 + bk][:, cc * 128:(cc + 1) * 128],
                                               lhsT=tA[:, c * 128:(c + 1) * 128], rhs=identf[:],
                                               start=True, stop=True)
                    return ins
                pe.op(mm, [r_tA, r_identf], [psr[2 + bk]])
                ck(241)
                act.op(lambda bk=bk: nc.scalar.activation(
                    out=h2Tf[:, 4 * bk:4 * bk + 4, :].rearrange("p c t -> p (c t)"), in_=ps[2 + bk][:, :],
                    func=AF.Copy), [psr[2 + bk]], [r_h2Tf])
                ck(242)
            ck(24)
            pool.op(lambda: nc.gpsimd.tensor_copy(out=h2b[:], in_=tA[:]), [r_tA], [r_h2b])
            pool.dma([(H2_d[t0:t0 + 128, :], h2b[:])], [r_h2b], [r_H2T], r_h2b)

            def mm():
                ins = None
                for k in range(8):
                    ins = nc.tensor.matmul(ps[6][:, 0:72], lhsT=h2Tf[:, k, :], rhs=wr[:, k, :],
                                           start=(k == 0), stop=(k == 7))
                return ins
            pe.op(mm, [r_h2Tf, r_wr], [psr[6]])
            dve.op(lambda tile_idx=tile_idx: nc.vector.tensor_tensor(
                out=Lg[:, tile_idx, :], in0=ps[6][:, 0:72], in1=rb[:], op=ALU.add), [psr[6], r_rb], [r_Lg])
            ck(25)

    barrier()
    if stage == "B":
        sp.deps([], [r_tC])
        raise EarlyExit()

    I32 = mybir.dt.int32
    V = nc.vector
    gmax = rsm[:, 0:32]
    pgt = rsm[:, 32:64]
    m1 = rsm[:, 64:96]
    m2 = rsm[:, 96:128]
    pe1 = rsm[:, 128:160]
    pe2 = rsm[:, 160:192]
    d1f = rsm[:, 192:224]
    d2f = rsm[:, 224:256]
    with contextlib.ExitStack() as stM:
        stR = contextlib.ExitStack()
        wk, r_wk = sb(stR, "wk", [128, 32, 64])
        oh1, r_oh1 = sb(stR, "oh1", [128, 32, 64])
        oh2, r_oh2 = sb(stR, "oh2", [128, 32, 64])
        cum, r_cum = sb(stR, "cum", [128, 32, 64])
        cs, r_cs = sb(stR, "cs", [128, 32, 64])
        ohb, r_ohb = sb(stR, "ohb", [128, 32, 64], BF16)
        triSb, r_triSb = sb(stR, "triSb", [128, 128], BF16)
        gm, r_gm = sb(stR, "gm", [128, 32, 8])
        ohg, r_ohg = sb(stR, "ohg", [128, 32, 8])
        thr, r_thr = sb(stR, "thr", [128, 64])
        iotab, r_iotab = sb(stR, "iotab", [128, 128])
        pid, r_pid = sb(stR, "pid", [128, 1])
        e64, r_e64 = sb(stR, "e64", [128, 6, 64])
        bef, r_bef = sb(stR, "bef", [128, 128])
        big, r_big = sb(stR, "big", [128, 4096])
        sp.dma([(thr[:], thr_d[:, :])], [], [r_thr], r_thr)
        sp.dma([(iotab[:], iotab_d[:, :])], [], [r_iotab], r_iotab)
        sp.dma([(pid[:], pid_d[:, :])], [], [r_pid], r_pid)
        dve.op(lambda: V.tensor_reduce(out=gmax, in_=Lg[:, :, 0:8], axis=AX.X, op=ALU.max), [r_Lg], [r_rsm])
        dve.op(lambda: V.tensor_tensor(out=gm[:], in0=Lg[:, :, 0:8], in1=gmax.unsqueeze(2).broadcast_to([128, 32, 8]),
                                       op=ALU.subtract), [r_Lg, r_rsm], [r_gm])
        dve.op(lambda: V.tensor_single_scalar(out=ohg[:], in_=gm[:], scalar=0.0, op=ALU.is_ge), [r_gm], [r_ohg])
        act.op(lambda: nc.scalar.activation(out=gm[:], in_=gm[:], func=AF.Exp), [r_gm], [r_gm])
        dve.op(lambda: V.tensor_reduce(out=pgt, in_=gm[:], axis=AX.X, op=ALU.add), [r_gm], [r_rsm])
        dve.op(lambda: V.reciprocal(out=pgt, in_=pgt), [r_rsm], [r_rsm])
        dve.op(lambda: V.tensor_scalar(out=ohg[:], in0=ohg[:], scalar1=-1.0, scalar2=BIG, op0=ALU.add, op1=ALU.mult),
               [r_ohg], [r_ohg])
        dve.op(lambda: V.tensor_tensor(
            out=wk[:].rearrange("p t (g e) -> p t g e", e=8), in0=Lg[:, :, 8:72].rearrange("p t (g e) -> p t g e", e=8),
            in1=ohg[:].unsqueeze(3).broadcast_to([128, 32, 8, 8]), op=ALU.add), [r_Lg, r_ohg], [r_wk])
        dve.op(lambda: V.tensor_reduce(out=m1, in_=wk[:], axis=AX.X, op=ALU.max), [r_wk], [r_rsm])
        dve.op(lambda: V.tensor_tensor(out=oh1[:], in0=wk[:], in1=m1.unsqueeze(2).broadcast_to([128, 32, 64]),
                                       op=ALU.is_ge), [r_wk, r_rsm], [r_oh1])
        dve.op(lambda: V.scalar_tensor_tensor(out=wk[:], in0=oh1[:], scalar=-BIG, in1=wk[:], op0=ALU.mult,
                                              op1=ALU.add), [r_oh1, r_wk], [r_wk])
        dve.op(lambda: V.tensor_reduce(out=m2, in_=wk[:], axis=AX.X, op=ALU.max), [r_wk], [r_rsm])
        dve.op(lambda: V.tensor_tensor(out=oh2[:], in0=wk[:], in1=m2.unsqueeze(2).broadcast_to([128, 32, 64]),
                                       op=ALU.is_ge), [r_wk, r_rsm], [r_oh2])
        dve.op(lambda: V.tensor_tensor(out=pe1, in0=m2, in1=m1, op=ALU.subtract), [r_rsm], [r_rsm])
        act.op(lambda: nc.scalar.activation(out=pe1, in_=pe1, func=AF.Exp), [r_rsm], [r_rsm])
        dve.op(lambda: V.tensor_scalar(out=pe1, in0=pe1, scalar1=1.0, scalar2=None, op0=ALU.add), [r_rsm], [r_rsm])
        dve.op(lambda: V.reciprocal(out=pe1, in_=pe1), [r_rsm], [r_rsm])
        dve.op(lambda: V.tensor_scalar(out=pe2, in0=pe1, scalar1=-1.0, scalar2=1.0, op0=ALU.mult, op1=ALU.add),
               [r_rsm], [r_rsm])
        dve.op(lambda: V.tensor_tensor(out=pe1, in0=pe1, in1=pgt, op=ALU.mult), [r_rsm], [r_rsm])
        dve.op(lambda: V.tensor_tensor(out=pe2, in0=pe2, in1=pgt, op=ALU.mult), [r_rsm], [r_rsm])
        dve.op(lambda: V.tensor_tensor(out=ohb[:], in0=oh1[:], in1=oh2[:], op=ALU.add), [r_oh1, r_oh2], [r_ohb])
        dve.op(lambda: V.tensor_tensor(out=triSb[:], in0=tri[:], in1=identf[:], op=ALU.subtract),
               [r_tri, r_identf], [r_triSb])
        for q4 in range(4):
            def mm(q4=q4):
                return nc.tensor.matmul(ps[q4][:, :], lhsT=triSb[:], rhs=ohb[:, 8 * q4:8 * q4 + 8, :],
                                        start=True, stop=True)
            pe.op(mm, [r_triSb, r_ohb], [psr[q4]])
            act.op(lambda q4=q4: nc.scalar.activation(
                out=cum[:, 8 * q4:8 * q4 + 8, :].rearrange("p t e -> p (t e)"), in_=ps[q4][:, :], func=AF.Copy),
                [psr[q4]], [r_cum])
        for q4 in range(4):
            def mm(q4=q4):
                return nc.tensor.matmul(ps[q4][:, :], lhsT=onesb[:], rhs=ohb[:, 8 * q4:8 * q4 + 8, :],
                                        start=True, stop=True)
            pe.op(mm, [r_onesb, r_ohb], [psr[q4]])
            act.op(lambda q4=q4: nc.scalar.activation(
                out=cs[:, 8 * q4:8 * q4 + 8, :].rearrange("p t e -> p (t e)"), in_=ps[q4][:, :], func=AF.Copy),
                [psr[q4]], [r_cs])
        cnt = e64[:, 0, :]
        nb = e64[:, 1, :]
        ci = e64[:, 2, :]
        sbase = e64[:, 3, :]
        ones64 = e64[:, 4, :]
        dve.op(lambda: V.memset(e64[:], 0.0), [], [r_e64])
        dve.op(lambda: V.memset(ones64, 1.0), [r_e64], [r_e64])
        for t in range(32):
            dve.op(lambda t=t: V.tensor_tensor(out=cum[:, t, :], in0=cum[:, t, :], in1=cnt, op=ALU.add),
                   [r_cum, r_e64], [r_cum])
            dve.op(lambda t=t: V.tensor_tensor(out=cnt, in0=cnt, in1=cs[:, t, :], op=ALU.add), [r_cs, r_e64], [r_e64])
        dve.op(lambda: V.tensor_tensor(out=big[:].rearrange("p (e j) -> p e j", j=64),
                                       in0=cnt.unsqueeze(2).broadcast_to([128, 64, 64]),
                                       in1=thr[:].unsqueeze(1).broadcast_to([128, 64, 64]), op=ALU.is_gt),
               [r_e64, r_thr], [r_big])
        dve.op(lambda: V.tensor_reduce(out=nb, in_=big[:].rearrange("p (e j) -> p e j", j=64), axis=AX.X, op=ALU.add),
               [r_big], [r_e64])
        dve.op(lambda: V.tensor_tensor_scan(out=ci, data0=ones64, data1=nb, initial=0.0, op0=ALU.mult, op1=ALU.add),
               [r_e64], [r_e64])
        dve.op(lambda: V.tensor_tensor(out=sbase, in0=ci, in1=nb, op=ALU.subtract), [r_e64], [r_e64])
        dve.op(lambda: V.tensor_scalar(out=sbase, in0=sbase, scalar1=128.0, scalar2=None, op0=ALU.mult),
               [r_e64], [r_e64])
        dve.op(lambda: V.tensor_tensor(out=cum[:], in0=cum[:], in1=sbase.unsqueeze(1).broadcast_to([128, 32, 64]),
                                       op=ALU.add), [r_cum, r_e64], [r_cum])
        dve.op(lambda: V.tensor_tensor(out=wk[:], in0=cum[:], in1=oh1[:], op=ALU.mult), [r_cum, r_oh1], [r_wk])
        dve.op(lambda: V.tensor_reduce(out=d1f, in_=wk[:], axis=AX.X, op=ALU.add), [r_wk], [r_rsm])
        dve.op(lambda: V.tensor_tensor(out=wk[:], in0=cum[:], in1=oh2[:], op=ALU.mult), [r_cum, r_oh2], [r_wk])
        dve.op(lambda: V.tensor_reduce(out=d2f, in_=wk[:], axis=AX.X, op=ALU.add), [r_wk], [r_rsm])
        dve.op(lambda: V.tensor_copy(out=d1i[:], in_=d1f), [r_rsm], [r_d1i])
        dve.op(lambda: V.tensor_copy(out=d2i[:], in_=d2f), [r_rsm], [r_d2i])
        for hb in range(2):
            dve.op(lambda hb=hb: V.tensor_tensor(
                out=big[:].rearrange("p (b e) -> p b e", e=64),
                in0=ci.unsqueeze(1).broadcast_to([128, 64, 64]),
                in1=iotab[:, 64 * hb:64 * hb + 64].unsqueeze(2).broadcast_to([128, 64, 64]), op=ALU.is_le),
                [r_e64, r_iotab], [r_big])
            dve.op(lambda hb=hb: V.tensor_reduce(
                out=bef[:, 64 * hb:64 * hb + 64], in_=big[:].rearrange("p (b e) -> p b e", e=64),
                axis=AX.X, op=ALU.add), [r_big], [r_bef])
        dve.op(lambda: V.tensor_scalar(out=bef[:], in0=bef[:], scalar1=63.0, scalar2=None, op0=ALU.min),
               [r_bef], [r_bef])
        dve.op(lambda: V.memset(big[:, 0:128], 0.0), [r_big], [r_big])
        dve.op(lambda: V.tensor_tensor(out=big[:, 1:128], in0=bef[:, 1:128], in1=bef[:, 0:127], op=ALU.is_equal),
               [r_bef, r_big], [r_big])
        dve.op(lambda: V.tensor_scalar(out=bef[:], in0=bef[:], scalar1=128.0, scalar2=None, op0=ALU.mult),
               [r_bef], [r_bef])
        dve.op(lambda: V.scalar_tensor_tensor(out=bef[:], in0=big[:, 0:128], scalar=1.0e6, in1=bef[:], op0=ALU.mult,
                                              op1=ALU.add), [r_big, r_bef], [r_bef])
        dve.op(lambda: V.tensor_scalar(out=bef[:], in0=bef[:], scalar1=pid[:, 0:1], scalar2=None, op0=ALU.add),
               [r_bef, r_pid], [r_bef])
        dve.op(lambda: V.tensor_copy(out=widx[:], in_=bef[:]), [r_bef], [r_widx])
        barrier()
        stR.close()
        stMid.close()
        if stage == "R":
            dbg, r_dbg = sb(stM, "dbgR", [128, 256])
            dve.op(lambda: V.tensor_copy(out=dbg[:, 0:32], in_=d1i[:]), [r_d1i], [r_dbg])
            dve.op(lambda: V.tensor_copy(out=dbg[:, 32:64], in_=d2i[:]), [r_d2i], [r_dbg])
            dve.op(lambda: V.tensor_copy(out=dbg[:, 64:128], in_=rsm[:, 128:192]), [r_rsm], [r_dbg])
            dve.op(lambda: V.tensor_copy(out=dbg[:, 128:256], in_=widx[:]), [r_widx], [r_dbg])
            sp.dma([(out_d[0:128, 0:256], dbg[:])], [r_dbg], [], r_dbg)
            sp.deps([], [r_dbg])
            barrier()
            raise EarlyExit()
        r_Xs, r_Ys = Res("Xs"), Res("Ys")
        with contextlib.ExitStack() as stS:
            hb2 = [sb(stS, f"hb2_{i}", [128, D], BF16) for i in range(2)]
            for t in range(32):
                tl, r_tl = hb2[t % 2]
                sp.dma([(tl[:], H2_d[t * 128:(t + 1) * 128, :])], [r_H2T], [r_tl], r_tl)
                for (di, r_di) in ((d1i, r_d1i), (d2i, r_d2i)):
                    pool.deps([r_tl, r_di], [])
                    if r_tl.dsem is None:
                        raise RuntimeError("no dsem")
                    pool.wait({id(r_tl.dsem): (r_tl.dsem, r_tl.dcnt)})
                    nc.gpsimd.indirect_dma_start(
                        out=Xs_d[:, :], out_offset=bass.IndirectOffsetOnAxis(ap=di[:, t:t + 1], axis=0),
                        in_=tl[:, :], in_offset=None).then_inc(r_tl.dsem, 16)
                    r_tl.dcnt += 16
                    pool.mark([r_tl, r_di], [r_Xs], r_tl.dsem, r_tl.dcnt)
            barrier()
        with contextlib.ExitStack() as stE:
            stg = [sb(stE, f"stgE{i}", [128, 4096]) for i in range(3)]
            wbf = []
            for i in range(2):
                a1, ra1 = sb(stE, f"w1b{i}", [128, 8, 512], BF16)
                a3, ra3 = sb(stE, f"w3b{i}", [128, 8, 512], BF16)
                a2, ra2 = sb(stE, f"w2b{i}", [128, 4, 1024], BF16)
                wbf.append((a1, ra1, a3, ra3, a2, ra2))
            xblk = [sb(stE, f"xblk{i}", [128, D], BF16) for i in range(2)]
            XT, r_XT = sb(stE, "XT", [128, 8, 128], BF16)
            sgE, r_sgE = sb(stE, "sgE", [128, 512])
            actb, r_actb = sb(stE, "actb", [128, 4, 128], BF16)
            yb = [sb(stE, f"yb{i}", [128, D]) for i in range(2)]
            bc_reg = nc.gpsimd.alloc_register("bc_reg")
            nc.gpsimd.reg_mov(bc_reg, nexp_decl * 128 - 1)
            for b in range(128):
                a1, ra1, a3, ra3, a2, ra2 = wbf[b % 2]
                for (wd, (tg, r_tg), (ab, r_ab), nk) in ((w1_d, stg[0], (a1, ra1), 8), (w3_d, stg[1], (a3, ra3), 8),
                                                        (w2_d, stg[2], (a2, ra2), 4)):
                    pool.deps([r_widx], [r_tg])
                    if r_tg.dsem is None:
                        r_tg.dsem = nc.alloc_semaphore(name=f"d{len(ALL_DMA_RES)}_" + r_tg.name)
                        ALL_DMA_RES.append(r_tg)
                    if r_tg.dcnt:
                        pool.wait({id(r_tg.dsem): (r_tg.dsem, r_tg.dcnt)})
                    nc.gpsimd.indirect_dma_start(
                        out=tg[:, :], out_offset=None, in_=wd[:, :],
                        in_offset=bass.IndirectOffsetOnAxis(ap=widx[:, b:b + 1], axis=0),
                        bounds_check=bc_reg, oob_is_err=False).then_inc(r_tg.dsem, 16)
                    r_tg.dcnt += 16
                    pool.mark([r_widx], [r_tg], r_tg.dsem, r_tg.dcnt)
                    if nk == 8:
                        dve.op(lambda ab=ab, tg=tg: V.tensor_copy(out=ab[:].rearrange("p k n -> p (k n)"), in_=tg[:]),
                               [r_tg], [r_ab])
                    else:
                        act.op(lambda ab=ab, tg=tg: nc.scalar.activation(out=ab[:].rearrange("p k n -> p (k n)"),
                                                                         in_=tg[:], func=AF.Copy), [r_tg], [r_ab])
                xb_, r_xb = xblk[b % 2]
                sp.dma([(xb_[:], Xs_d[b * 128:(b + 1) * 128, :])], [r_Xs], [r_xb], r_xb)

                def mm(xb_=xb_):
                    ins = None
                    for c in range(8):
                        ins = nc.tensor.transpose(out=ps7b[:, c * 128:(c + 1) * 128], in_=xb_[:, c * 128:(c + 1) * 128],
                                                  identity=identb[:])
                    return ins
                pe.op(mm, [r_xb, r_identb], [psr[7]])
                act.op(lambda: nc.scalar.activation(out=XT[:].rearrange("p c t -> p (c t)"), in_=ps7b[:, :],
                                                    func=AF.Copy), [psr[7]], [r_XT])
                pg_, pu_ = (0, 1) if b % 2 == 0 else (2, 3)

                def mm(a1=a1, a3=a3, pg_=pg_, pu_=pu_):
                    ins = None
                    for hc in range(4):
                        for k in range(8):
                            ins = nc.tensor.matmul(ps[pg_][:, hc * 128:(hc + 1) * 128],
                                                   lhsT=a1[:, k, hc * 128:(hc + 1) * 128], rhs=XT[:, k, :],
                                                   start=(k == 0), stop=(k == 7))
                    for hc in range(4):
                        for k in range(8):
                            ins = nc.tensor.matmul(ps[pu_][:, hc * 128:(hc + 1) * 128],
                                                   lhsT=a3[:, k, hc * 128:(hc + 1) * 128], rhs=XT[:, k, :],
                                                   start=(k == 0), stop=(k == 7))
                    return ins
                pe.op(mm, [ra1, ra3, r_XT], [psr[pg_], psr[pu_]])
                act.op(lambda pg_=pg_: nc.scalar.activation(out=sgE[:], in_=ps[pg_][:, :], func=AF.Silu),
                       [psr[pg_]], [r_sgE])
                dve.op(lambda pu_=pu_: V.tensor_tensor(out=actb[:].rearrange("p c t -> p (c t)"), in0=ps[pu_][:, :],
                                                       in1=sgE[:], op=ALU.mult), [psr[pu_], r_sgE], [r_actb])
                yb_, r_yb = yb[b % 2]
                for hf in range(2):
                    py = 4 + hf

                    def mm(hf=hf, py=py, a2=a2):
                        ins = None
                        for hc in range(4):
                            ins = nc.tensor.matmul(ps[py][:, :], lhsT=actb[:, hc, :],
                                                   rhs=a2[:, hc, hf * 512:(hf + 1) * 512],
                                                   start=(hc == 0), stop=(hc == 3))
                        return ins
                    pe.op(mm, [r_actb, ra2], [psr[py]])
                    act.op(lambda hf=hf, py=py, yb_=yb_: nc.scalar.activation(
                        out=yb_[:, hf * 512:(hf + 1) * 512], in_=ps[py][:, :], func=AF.Copy), [psr[py]], [r_yb])
                act.dma([(Ys_d[b * 128:(b + 1) * 128, :], yb_[:])], [r_yb], [r_Ys], r_yb)
            barrier()
        with contextlib.ExitStack() as stF:
            ln2g, r_ln2g = sb(stF, "ln2g", [128, D])
            ln2b, r_ln2b = sb(stF, "ln2b", [128, D])
            x1t, r_x1t = sb(stF, "x1t", [128, D])
            fo, r_fo = sb(stF, "fo", [128, D])
            ya = [sb(stF, f"ya{i}", [128, D]) for i in range(2)]
            bst2, r_bst2 = sb(stF, "bst2", [128, 2, 6])
            sm2, r_sm2 = sb(stF, "sm2", [128, 4])
            sp.dma([(ln2g[:], ln2g_d[:, :])], [], [r_ln2g], r_ln2g)
            sp.dma([(ln2b[:], ln2b_d[:, :])], [], [r_ln2b], r_ln2b)
            for t in range(32):
                s_ = t // 16
                t0 = t * 128
                g2row = modg2[:, s_, :]
                for (yt_, r_yt), (di, r_di) in zip(ya, ((d1i, r_d1i), (d2i, r_d2i))):
                    pool.deps([r_di, r_Ys], [r_yt])
                    if r_yt.dsem is None:
                        r_yt.dsem = nc.alloc_semaphore(name=f"d{len(ALL_DMA_RES)}_" + r_yt.name)
                        ALL_DMA_RES.append(r_yt)
                    if r_yt.dcnt:
                        pool.wait({id(r_yt.dsem): (r_yt.dsem, r_yt.dcnt)})
                    nc.gpsimd.indirect_dma_start(
                        out=yt_[:, :], out_offset=None, in_=Ys_d[:, :],
                        in_offset=bass.IndirectOffsetOnAxis(ap=di[:, t:t + 1], axis=0)).then_inc(r_yt.dsem, 16)
                    r_yt.dcnt += 16
                    pool.mark([r_di, r_Ys], [r_yt], r_yt.dsem, r_yt.dcnt)
                sp.dma([(x1t[:], X1_d[t0:t0 + 128, :])], [r_X1], [r_x1t], r_x1t)
                dve.op(lambda t=t: V.tensor_scalar(out=fo[:], in0=ya[0][0][:], scalar1=pe1[:, t:t + 1], scalar2=None,
                                                   op0=ALU.mult), [ya[0][1], r_rsm], [r_fo])
                dve.op(lambda t=t: V.scalar_tensor_tensor(out=fo[:], in0=ya[1][0][:], scalar=pe2[:, t:t + 1], in1=fo[:],
                                                          op0=ALU.mult, op1=ALU.add), [ya[1][1], r_rsm, r_fo], [r_fo])
                dve.op(lambda g2row=g2row: V.tensor_tensor(out=fo[:], in0=fo[:], in1=g2row, op=ALU.mult),
                       [r_fo, r_modg2], [r_fo])
                dve.op(lambda: V.scalar_tensor_tensor(out=fo[:], in0=x1t[:], scalar=ALPHA, in1=fo[:], op0=ALU.mult,
                                                      op1=ALU.add), [r_x1t, r_fo], [r_fo])
                dve.op(lambda: V.bn_stats(out=bst2[:, 0, :], in_=fo[:, 0:512]), [r_fo], [r_bst2])
                dve.op(lambda: V.bn_stats(out=bst2[:, 1, :], in_=fo[:, 512:1024]), [r_fo], [r_bst2])
                dve.op(lambda: V.bn_aggr(out=sm2[:, 0:2], in_=bst2[:].rearrange("p a b -> p (a b)")),
                       [r_bst2], [r_sm2])
                dve.op(lambda: V.tensor_scalar(out=sm2[:, 2:3], in0=sm2[:, 1:2], scalar1=EPS, scalar2=None,
                                               op0=ALU.add), [r_sm2], [r_sm2])
                act.op(lambda: nc.scalar.activation(out=sm2[:, 2:3], in_=sm2[:, 2:3], func=AF.Sqrt), [r_sm2], [r_sm2])
                dve.op(lambda: V.reciprocal(out=sm2[:, 2:3], in_=sm2[:, 2:3]), [r_sm2], [r_sm2])
                dve.op(lambda: V.tensor_scalar(out=fo[:], in0=fo[:], scalar1=sm2[:, 0:1], scalar2=sm2[:, 2:3],
                                               op0=ALU.subtract, op1=ALU.mult), [r_fo, r_sm2], [r_fo])
                dve.op(lambda: V.tensor_tensor(out=fo[:], in0=fo[:], in1=ln2g[:], op=ALU.mult), [r_fo, r_ln2g], [r_fo])
                dve.op(lambda: V.tensor_tensor(out=fo[:], in0=fo[:], in1=ln2b[:], op=ALU.add), [r_fo, r_ln2b], [r_fo])
                act.dma([(out_d[t0:t0 + 128, :], fo[:])], [r_fo], [], r_fo)
            sp.deps([], [r_fo])
            barrier()
    return nc


def make_inputs(core, x, c, w_ada, b_ada, w_in, conf_conv_w, conf_conv_b, conf_ln_g, conf_ln_b,
                ssm_conv_w, ssm_conv_b, ssm_dt_bias, ssm_A_log, ssm_D, ssm_norm_w, w_out,
                ln1_g, ln1_b, router_group_w, router_group_b, router_expert_w, router_expert_b,
                expert_w1, expert_w3, expert_w2, ln2_g, ln2_b, shared):
    f = np.float32
    xc = np.ascontiguousarray(x[2 * core:2 * core + 2].reshape(NTOK, D), dtype=f)
    cc = c[2 * core:2 * core + 2]
    m = dict(shared)
    m["x"] = xc
    m["xT"] = np.ascontiguousarray(xc.T)
    m["cT"] = np.ascontiguousarray(cc.reshape(2, 8, 128).transpose(2, 1, 0), dtype=f)
    return m


def rep(v, n=128):
    return np.ascontiguousarray(np.broadcast_to(np.asarray(v, dtype=np.float32).reshape(1, -1), (n, v.size)))


def fm(v, nch):
    return np.ascontiguousarray(np.asarray(v, dtype=np.float32).reshape(nch, 128).T)


def shared_inputs(w_ada, b_ada, w_in, conf_conv_w, conf_conv_b, conf_ln_g, conf_ln_b,
                  ssm_conv_w, ssm_conv_b, ssm_dt_bias, ssm_A_log, ssm_D, ssm_norm_w, w_out,
                  ln1_g, ln1_b, router_group_w, router_group_b, router_expert_w, router_expert_b,
                  expert_w1, expert_w3, expert_w2, ln2_g, ln2_b):
    f = np.float32
    sh = {}
    sh["w_ada"] = np.ascontiguousarray(w_ada[0], dtype=f)
    sh["b_ada_fm"] = fm(b_ada[0][:2048], 16)
    sh["b_ada_row"] = rep(b_ada[0][2048:])
    sh["w_in"] = np.ascontiguousarray(w_in[0], dtype=f)
    sh["ccw"] = np.ascontiguousarray(conf_conv_w[0].reshape(31, 8, 128).transpose(2, 1, 0), dtype=f)
    sh["ccb"] = fm(conf_conv_b[0], 8)
    sh["clg"] = fm(conf_ln_g[0], 8)
    sh["clb"] = fm(conf_ln_b[0], 8)
    sh["scw"] = np.ascontiguousarray(ssm_conv_w[0].reshape(4, 16, 128).transpose(2, 1, 0), dtype=f)
    sh["scb"] = fm(ssm_conv_b[0], 16)
    sh["dtb"] = rep(ssm_dt_bias[0])
    sh["alog"] = rep(ssm_A_log[0])
    sh["dsk"] = rep(ssm_D[0])
    sh["normw"] = rep(ssm_norm_w[0])
    sh["w_out"] = np.ascontiguousarray(w_out[0], dtype=f)
    sh["ln1g"] = rep(ln1_g[0])
    sh["ln1b"] = rep(ln1_b[0])
    sh["ln2g"] = rep(ln2_g[0])
    sh["ln2b"] = rep(ln2_b[0])
    wrr = np.concatenate([router_group_w[0], router_expert_w[0]], axis=1).astype(f)
    sh["wr"] = np.ascontiguousarray(wrr.reshape(8, 128, 72).transpose(1, 0, 2))
    sh["rb"] = rep(np.concatenate([router_group_b[0], router_expert_b[0]]))
    sh["w1"] = np.ascontiguousarray(np.asarray(expert_w1[0], dtype=f).reshape(NEXP, 8, 128, 512).transpose(0, 2, 1, 3)
                                    ).reshape(NEXP * 128, 4096)
    sh["w3"] = np.ascontiguousarray(np.asarray(expert_w3[0], dtype=f).reshape(NEXP, 8, 128, 512).transpose(0, 2, 1, 3)
                                    ).reshape(NEXP * 128, 4096)
    sh["w2"] = np.ascontiguousarray(np.asarray(expert_w2[0], dtype=f).reshape(NEXP, 4, 128, 1024).transpose(0, 2, 1, 3)
                                    ).reshape(NEXP * 128, 4096)
    sh["thr"] = rep(np.arange(64, dtype=f) * 128.0)
    sh["iotab"] = rep(np.arange(128, dtype=f))
    sh["pid"] = np.arange(128, dtype=f).reshape(128, 1)
    sh["identf"] = np.eye(128, dtype=f)
    sh["tri"] = np.triu(np.ones((128, 128), dtype=f))
    nm = np.where(np.triu(np.ones((128, 128), dtype=bool)), 0.0, -30000.0).astype(f)
    sh["negm"] = np.ascontiguousarray(np.tile(nm, (1, 4)))
    return sh


def kernel(x, c, w_ada, b_ada, w_in, conf_conv_w, conf_conv_b, conf_ln_g, conf_ln_b,
           ssm_conv_w, ssm_conv_b, ssm_dt_bias, ssm_A_log, ssm_D, ssm_norm_w, w_out,
           ln1_g, ln1_b, router_group_w, router_group_b, router_expert_w, router_expert_b,
           expert_w1, expert_w3, expert_w2, ln2_g, ln2_b, _stage="full", _trace=False):
    args = [np.asarray(a) for a in (w_ada, b_ada, w_in, conf_conv_w, conf_conv_b, conf_ln_g, conf_ln_b,
                                    ssm_conv_w, ssm_conv_b, ssm_dt_bias, ssm_A_log, ssm_D, ssm_norm_w, w_out,
                                    ln1_g, ln1_b, router_group_w, router_group_b, router_expert_w, router_expert_b,
                                    expert_w1, expert_w3, expert_w2, ln2_g, ln2_b)]
    x = np.asarray(x)
    c = np.asarray(c)
    sh = shared_inputs(*args)
    if _stage != "full":
        for k in ("w1", "w3", "w2"):
            sh[k] = np.ascontiguousarray(sh[k][0:128])
    in_maps = []
    for core in range(8):
        m = dict(sh)
        xc = np.ascontiguousarray(x[2 * core:2 * core + 2].reshape(NTOK, D), dtype=np.float32)
        m["x"] = xc
        m["xT"] = np.ascontiguousarray(xc.T)
        m["cT"] = np.ascontiguousarray(c[2 * core:2 * core + 2].reshape(2, 8, 128).transpose(2, 1, 0),
                                       dtype=np.float32)
        in_maps.append(m)
    nc = build_nc(_stage)
    if _trace:
        res = run_bass_kernel_spmd(nc, in_maps, core_ids=list(range(8)), trace=True)
        print("EXEC_TIME_NS", _stage, res.exec_time_ns)
    else:
        res = run_bass_kernel_spmd(nc, in_maps, core_ids=list(range(8)))
    outs = [np.asarray(r["out"], dtype=np.float32).reshape(2, SEQ, D) for r in res.results]
    return np.concatenate(outs, axis=0)
```

```python
import contextlib
import numpy as np
import concourse.bass as bass
import concourse.mybir as mybir
from concourse.bass_utils import run_bass_kernel_spmd

F32 = mybir.dt.float32
BF16 = mybir.dt.bfloat16
AF = mybir.ActivationFunctionType
ALU = mybir.AluOpType
AX = mybir.AxisListType

ALPHA = 2.0 ** 0.25
EPS = 1e-5
NTOK = 4096
SEQ = 2048
D = 1024
NEXP = 64
BIG = 1.0e9


ALL_DMA_RES = []


class Res:
    __slots__ = ("name", "wr", "rd", "dsem", "dcnt")

    def __init__(self, name):
        self.name = name
        self.wr = {}
        self.rd = {}
        self.dsem = None
        self.dcnt = 0


class Eng:
    def __init__(self, nc, eng, name):
        self.nc = nc
        self.eng = eng
        self.sem = nc.alloc_semaphore(name=name)
        self.cnt = 0
        self.waited = {}

    def wait(self, evs):
        for key, (sem, val) in list(evs.items()):
            if self.waited.get(key, 0) < val:
                self.eng.wait_ge(sem, val)
                self.waited[key] = val

    def deps(self, reads, writes):
        for r in reads:
            self.wait(r.wr)
        for w in writes:
            self.wait(w.wr)
            self.wait(w.rd)

    def mark(self, reads, writes, sem, val):
        key = id(sem)
        for r in reads:
            r.rd[key] = (sem, val)
        for w in writes:
            w.wr = {key: (sem, val)}
            w.rd = {}

    def op(self, fn, reads=(), writes=()):
        self.deps(reads, writes)
        ins = fn()
        self.cnt += 1
        ins.then_inc(self.sem, 1)
        self.mark(reads, writes, self.sem, self.cnt)

    def dma(self, parts, reads, writes, sres):
        self.deps(reads, writes)
        if sres.dsem is None:
            sres.dsem = self.nc.alloc_semaphore(name=f"d{len(ALL_DMA_RES)}_" + sres.name)
            ALL_DMA_RES.append(sres)
        if sres.dcnt:
            self.wait({id(sres.dsem): (sres.dsem, sres.dcnt)})
        for (o, i) in parts:
            self.eng.dma_start(out=o, in_=i).then_inc(sres.dsem, 16)
            sres.dcnt += 16
        self.mark(reads, writes, sres.dsem, sres.dcnt)


class EarlyExit(Exception):
    pass


def build_nc(stage="full"):
    nc = bass.Bass("TRN2", target_bir_lowering=False)
    ALL_DMA_RES.clear()
    st_all = contextlib.ExitStack()
    try:
        with st_all:
            _build(nc, stage, st_all)
    except EarlyExit:
        pass
    return nc


def _build(nc, stage, st_all):

    def din(name, shape, dt=F32):
        return nc.dram_tensor(name, list(shape), dt, kind="ExternalInput").ap()

    x_d = din("x", [NTOK, D])
    xT_d = din("xT", [D, NTOK])
    cT_d = din("cT", [128, 8, 2])
    wada_d = din("w_ada", [D, 6 * D])
    bada_fm_d = din("b_ada_fm", [128, 16])
    bada_row_d = din("b_ada_row", [128, 4 * D])
    win_d = din("w_in", [D, 5136])
    ccw_d = din("ccw", [128, 8, 31])
    ccb_d = din("ccb", [128, 8])
    clg_d = din("clg", [128, 8])
    clb_d = din("clb", [128, 8])
    scw_d = din("scw", [128, 16, 4])
    scb_d = din("scb", [128, 16])
    dtb_d = din("dtb", [128, 16])
    alog_d = din("alog", [128, 16])
    dsk_d = din("dsk", [128, 16])
    normw_d = din("normw", [128, D])
    wout_d = din("w_out", [2 * D, D])
    ln1g_d = din("ln1g", [128, D])
    ln1b_d = din("ln1b", [128, D])
    ln2g_d = din("ln2g", [128, D])
    ln2b_d = din("ln2b", [128, D])
    wr_d = din("wr", [128, 8, 72])
    rb_d = din("rb", [128, 72])
    nexp_decl = NEXP if stage == "full" else 1
    w1_d = din("w1", [nexp_decl * 128, 4096])
    w3_d = din("w3", [nexp_decl * 128, 4096])
    w2_d = din("w2", [nexp_decl * 128, 4096])
    thr_d = din("thr", [128, 64])
    iotab_d = din("iotab", [128, 128])
    pid_d = din("pid", [128, 1])
    identf_d = din("identf", [128, 128])
    tri_d = din("tri", [128, 128])
    negm_d = din("negm", [128, 512])
    out_d = nc.dram_tensor("out", [NTOK, D], F32, kind="ExternalOutput").ap()

    UF_d = nc.dram_tensor("UF", [D, NTOK], BF16, kind="Internal").ap()
    X1_d = nc.dram_tensor("X1", [NTOK, D], F32, kind="Internal").ap()
    H2_d = nc.dram_tensor("H2", [NTOK, D], BF16, kind="Internal").ap()
    Xs_d = nc.dram_tensor("Xs", [4 * NTOK, D], BF16, kind="Internal").ap()
    Ys_d = nc.dram_tensor("Ys", [4 * NTOK, D], F32, kind="Internal").ap()

    r_UF, r_X1, r_H2T, r_YN = Res("UF"), Res("X1"), Res("H2T"), Res("YN")
    YN_d = nc.dram_tensor("YN", [D, NTOK], BF16, kind="Internal").ap()
    pe = Eng(nc, nc.tensor, "s_pe")
    act = Eng(nc, nc.scalar, "s_act")
    dve = Eng(nc, nc.vector, "s_dve")
    pool = Eng(nc, nc.gpsimd, "s_pool")
    sp = Eng(nc, nc.sync, "s_sp")

    es = st_all
    engines = [pe, act, dve, pool, sp]

    def ck(n):
        if stage == f"K{n}":
            barrier()
            raise EarlyExit()

    def barrier():
        evs = {}
        for E in engines:
            if E.cnt:
                evs[id(E.sem)] = (E.sem, E.cnt)
        for r in ALL_DMA_RES:
            if r.dcnt:
                evs[id(r.dsem)] = (r.dsem, r.dcnt)
        for E in engines:
            E.wait(evs)

    cast_rr = [0]

    def load_cast(dst_aps, src_aps, stg, r_dst):
        for dst, src in zip(dst_aps, src_aps):
            t, r = stg[cast_rr[0] % len(stg)]
            n = dst.shape[-1]
            sp.dma([(t[:, 0:n], src)], [], [r], r)
            if cast_rr[0] % 2 == 0:
                act.op(lambda t=t, dst=dst, n=n: nc.scalar.activation(out=dst, in_=t[:, 0:n], func=AF.Copy), [r], [r_dst])
            else:
                pool.op(lambda t=t, dst=dst, n=n: nc.gpsimd.tensor_copy(out=dst, in_=t[:, 0:n]), [r], [r_dst])
            cast_rr[0] += 1

    sb_cnt = [0]

    def sb(st, name, shape, dt=F32):
        sb_cnt[0] += 1
        t = st.enter_context(nc.sbuf_tensor(f"sb{sb_cnt[0]}_" + name, list(shape), dt))
        return t, Res(name)

    ps = []
    psr = []
    for i in range(7):
        t = es.enter_context(nc.psum_tensor(f"ps{i}", [128, 512], F32))
        ps.append(t)
        psr.append(Res(f"ps{i}"))
    ps7b = es.enter_context(nc.psum_tensor("ps7b", [128, 1024], BF16))
    ps.append(None)
    psr.append(Res("ps7b"))

    def psbf(i):
        return ps7b[:]

    identf, r_identf = sb(es, "identf", [128, 128])
    identb, r_identb = sb(es, "identb", [128, 128], BF16)
    onesf, r_onesf = sb(es, "onesf", [128, 128])
    onesb, r_onesb = sb(es, "onesb", [128, 128], BF16)
    tri, r_tri = sb(es, "tri", [128, 128])
    negm, r_negm = sb(es, "negm", [128, 512])
    modg2, r_modg2 = sb(es, "modg2", [128, 2, D])
    rsm, r_rsm = sb(es, "rsm", [128, 32 * 8])
    d1i, r_d1i = sb(es, "d1i", [128, 32], mybir.dt.int32)
    d2i, r_d2i = sb(es, "d2i", [128, 32], mybir.dt.int32)
    widx, r_widx = sb(es, "widx", [128, 128], mybir.dt.int32)
    modfm, r_modfm = sb(es, "modfm", [128, 16, 2])
    ccw, r_ccw = sb(es, "ccw", [128, 8, 31])
    pfm, r_pfm = sb(es, "pfm", [128, 64])
    prow, r_prow = sb(es, "prow", [128, 64])
    rb, r_rb = sb(es, "rb", [128, 72])
    stMid = es.enter_context(contextlib.ExitStack())
    modrow, r_modrow = sb(stMid, "modrow", [128, 2, 3 * D])
    Lg, r_Lg = sb(stMid, "Lg", [128, 32, 72])

    sp.dma([(identf[:], identf_d[:, :])], [], [r_identf], r_identf)
    sp.dma([(tri[:], tri_d[:, :])], [], [r_tri], r_tri)
    sp.dma([(negm[:], negm_d[:, :])], [], [r_negm], r_negm)
    sp.dma([(ccw[:], ccw_d[:, :, :])], [], [r_ccw], r_ccw)
    sp.dma([(pfm[:, 0:8], ccb_d[:, :]), (pfm[:, 8:16], clg_d[:, :]), (pfm[:, 16:24], clb_d[:, :]),
            (pfm[:, 24:40], scb_d[:, :])], [], [r_pfm], r_pfm)
    sp.dma([(prow[:, 0:16], dtb_d[:, :]), (prow[:, 48:64], alog_d[:, :]), (prow[:, 32:48], dsk_d[:, :])],
           [], [r_prow], r_prow)
    sp.dma([(rb[:], rb_d[:, :])], [], [r_rb], r_rb)
    dve.op(lambda: nc.vector.tensor_copy(out=identb[:], in_=identf[:]), [r_identf], [r_identb])
    dve.op(lambda: nc.vector.memset(onesf[:], 1.0), [], [r_onesf])
    dve.op(lambda: nc.vector.memset(onesb[:], 1.0), [], [r_onesb])
    act.op(lambda: nc.scalar.activation(out=prow[:, 16:32], in_=prow[:, 48:64], func=AF.Exp), [r_prow], [r_prow])
    dve.op(lambda: nc.vector.tensor_scalar(out=prow[:, 16:32], in0=prow[:, 16:32], scalar1=-1.0, scalar2=None,
                                           op0=ALU.mult), [r_prow], [r_prow])

    with contextlib.ExitStack() as st0:
        cT, r_cT = sb(st0, "cT", [128, 8, 2])
        crep, r_crep = sb(st0, "crep", [128, 2, 8, 128])
        bfm, r_bfm = sb(st0, "bfm", [128, 16])
        brow, r_brow = sb(st0, "brow", [128, 4 * D])
        slabA = [sb(st0, f"slabA{i}", [128, 8, 1024]) for i in range(2)]
        slabB = [sb(st0, f"slabB{i}", [128, 8, 512]) for i in range(2)]
        sp.dma([(cT[:], cT_d[:, :, :])], [], [r_cT], r_cT)
        sp.dma([(bfm[:], bada_fm_d[:, :])], [], [r_bfm], r_bfm)
        sp.dma([(brow[:], bada_row_d[:, :])], [], [r_brow], r_brow)
        act.op(lambda: nc.scalar.activation(out=cT[:], in_=cT[:], func=AF.Silu), [r_cT], [r_cT])
        for b in range(2):
            dve.op(lambda b=b: nc.vector.tensor_copy(
                out=crep[:, b, :, :], in_=cT[:, :, b:b + 1].broadcast_to([128, 8, 128])), [r_cT], [r_crep])
        for s in range(2):
            t, r = slabA[s]
            sp.dma([(t[:, k, :], wada_d[k * 128:(k + 1) * 128, s * 1024:(s + 1) * 1024]) for k in range(8)],
                   [], [r], r)
            for j in range(8):
                fc = s * 8 + j

                def mm(fc=fc, j=j, t=t):
                    ins = None
                    for k in range(8):
                        ins = nc.tensor.matmul(ps[0][:, 2 * fc:2 * fc + 2], lhsT=t[:, k, j * 128:(j + 1) * 128],
                                               rhs=cT[:, k, :], start=(k == 0), stop=(k == 7))
                    return ins
                pe.op(mm, [r, r_cT], [psr[0]])
        dve.op(lambda: nc.vector.tensor_tensor(
            out=modfm[:], in0=ps[0][:, 0:32].rearrange("p (f b) -> p f b", b=2),
            in1=bfm[:].unsqueeze(2).broadcast_to([128, 16, 2]), op=ALU.add), [psr[0], r_bfm], [r_modfm])
        dve.op(lambda: nc.vector.tensor_scalar(out=modfm[:, 8:16, :], in0=modfm[:, 8:16, :], scalar1=1.0,
                                               scalar2=None, op0=ALU.add), [r_modfm], [r_modfm])
        for tsl in range(8):
            t, r = slabB[tsl % 2]
            c0 = 2048 + tsl * 512
            sp.dma([(t[:, k, :], wada_d[k * 128:(k + 1) * 128, c0:c0 + 512]) for k in range(8)], [], [r], r)
            for b in range(2):
                pb = 1 + b

                def mm(b=b, t=t, pb=pb):
                    ins = None
                    for k in range(8):
                        ins = nc.tensor.matmul(ps[pb][:, :], lhsT=crep[:, b, k, :], rhs=t[:, k, :],
                                               start=(k == 0), stop=(k == 7))
                    return ins
                pe.op(mm, [r, r_crep], [psr[pb]])
                if tsl < 6:
                    dve.op(lambda b=b, pb=pb, tsl=tsl: nc.vector.tensor_tensor(
                        out=modrow[:, b, tsl * 512:(tsl + 1) * 512], in0=ps[pb][:, :],
                        in1=brow[:, tsl * 512:(tsl + 1) * 512], op=ALU.add), [psr[pb], r_brow], [r_modrow])
                else:
                    dve.op(lambda b=b, pb=pb, tsl=tsl: nc.vector.tensor_tensor(
                        out=modg2[:, b, (tsl - 6) * 512:(tsl - 5) * 512], in0=ps[pb][:, :],
                        in1=brow[:, tsl * 512:(tsl + 1) * 512], op=ALU.add), [psr[pb], r_brow], [r_modg2])
        dve.op(lambda: nc.vector.tensor_scalar(out=modrow[:, :, 2048:3072], in0=modrow[:, :, 2048:3072],
                                               scalar1=1.0, scalar2=None, op0=ALU.add), [r_modrow], [r_modrow])

    barrier()
    if stage == "0":
        sp.dma([(out_d[0:128, :], modrow[:, 0, 0:1024])], [r_modrow], [], r_modrow)
        sp.dma([(out_d[128:256, :], modg2[:, 1, :])], [r_modg2], [], r_modg2)
        sp.dma([(out_d[256:384, 0:32], modfm[:].rearrange("p a b -> p (a b)"))], [r_modfm], [], r_modfm)
        sp.deps([], [r_modrow, r_modg2, r_modfm])
        raise EarlyExit()

    xT_v = xT_d.rearrange("(k p) t -> p k t", p=128)
    UF_v = UF_d.rearrange("(k p) t -> p k t", p=128)
    YN_v = YN_d.rearrange("(k p) t -> p k t", p=128)

    def load_hT(st_tiles, tok0, nt, bsel):
        xTf, r_xTf, hT, r_hT = st_tiles
        sp.dma([(xTf[:, :, 0:nt], xT_v[:, :, tok0:tok0 + nt])], [], [r_xTf], r_xTf)
        for k in range(8):
            eng, E = (nc.vector, dve) if k % 2 == 0 else (nc.gpsimd, pool)
            E.op(lambda k=k, eng=eng: eng.tensor_scalar(
                out=hT[:, k, 0:nt], in0=xTf[:, k, 0:nt], scalar1=modfm[:, 8 + k, bsel:bsel + 1],
                scalar2=modfm[:, k, bsel:bsel + 1], op0=ALU.mult, op1=ALU.add), [r_xTf, r_modfm], [r_hT])

    TB = 256
    with contextlib.ExitStack() as stA:
        winA, r_winA = sb(stA, "winA", [128, 8, 2048], BF16)
        dgA, r_dgA = sb(stA, "dgA", [128, 8, 31, 128], BF16)
        r_p6 = [psr[6], psr[6]]
        xTf, r_xTf = sb(stA, "xTfA", [128, 8, TB])
        hT, r_hT = sb(stA, "hTA", [128, 8, TB], BF16)
        sig = [sb(stA, f"sig{i}", [128, TB]) for i in range(2)]
        ub, r_ub = sb(stA, "ub", [128, 8, 30 + TB], BF16)
        r_uc = [Res(f"uc{c}") for c in range(8)]
        cv, _ = sb(stA, "cv", [128, 8, TB])
        r_cv = [Res(f"cv{c}") for c in range(8)]
        cvq = [sb(stA, f"cvq{i}", [128, 2, TB], BF16) for i in range(2)]
        mean, r_mean = sb(stA, "mean", [128, TB])
        var, r_var = sb(stA, "var", [128, TB])
        rstd, r_rstd = sb(stA, "rstd", [128, TB])
        uf, r_uf = sb(stA, "uf", [128, 8, TB], BF16)
        stgA = [sb(stA, f"stgA{i}", [128, 1024]) for i in range(2)]
        load_cast([winA[:, k, h * 1024:(h + 1) * 1024] for k in range(8) for h in range(2)],
                  [win_d[k * 128:(k + 1) * 128, h * 1024:(h + 1) * 1024] for k in range(8) for h in range(2)],
                  stgA, r_winA)
        for c in range(8):
            for k in range(31):
                dve.op(lambda c=c, k=k: nc.vector.tensor_scalar(
                    out=dgA[:, c, k, :], in0=identb[:], scalar1=ccw[:, c, k:k + 1], scalar2=None, op0=ALU.mult),
                    [r_identb, r_ccw], [r_dgA])
        for s in range(2):
            for c in range(8):
                dve.op(lambda c=c: nc.vector.memset(ub[:, c, 0:30], 0.0), [], [r_uc[c]])
            for j in range(SEQ // TB):
                tok0 = s * SEQ + j * TB
                load_hT((xTf, r_xTf, hT, r_hT), tok0, TB, s)
                for c in range(8):
                    pv, pg = (0, 1) if c % 2 == 0 else (2, 3)
                    for (pi, col0) in ((pv, c * 128), (pg, 1024 + c * 128)):
                        def mm(pi=pi, col0=col0):
                            ins = None
                            for k in range(8):
                                ins = nc.tensor.matmul(ps[pi][:, 0:TB], lhsT=winA[:, k, col0:col0 + 128],
                                                       rhs=hT[:, k, :], start=(k == 0), stop=(k == 7))
                            return ins
                        pe.op(mm, [r_winA, r_hT], [psr[pi]])
                    sg, r_sg = sig[c % 2]
                    act.op(lambda pg=pg, sg=sg: nc.scalar.activation(out=sg[:], in_=ps[pg][:, 0:TB], func=AF.Sigmoid),
                           [psr[pg]], [r_sg])
                    dve.op(lambda pv=pv, sg=sg, c=c: nc.vector.tensor_tensor(
                        out=ub[:, c, 30:30 + TB], in0=ps[pv][:, 0:TB], in1=sg[:], op=ALU.mult),
                        [psr[pv], r_sg], [r_uc[c]])
                ck(51)
                for c in range(8):
                    hp = c % 2

                    def mm(c=c, hp=hp):
                        ins = None
                        for k in range(31):
                            ins = nc.tensor.matmul(ps[6][:, hp * TB:(hp + 1) * TB], lhsT=dgA[:, c, k, :],
                                                   rhs=ub[:, c, k:k + TB], start=(k == 0), stop=(k == 30))
                        return ins
                    pe.op(mm, [r_dgA, r_uc[c]], [r_p6[hp]])
                    ck(52)
                    act.op(lambda c=c, hp=hp: nc.scalar.activation(
                        out=cv[:, c, :], in_=ps[6][:, hp * TB:(hp + 1) * TB], func=AF.Identity,
                        bias=pfm[:, c:c + 1]), [r_p6[hp], r_pfm], [r_cv[c]])
                    ck(53)
                ck(54)
                for c in range(8):
                    eng, E = (nc.vector, dve) if c < 5 else (nc.gpsimd, pool)
                    E.op(lambda c=c, eng=eng: eng.tensor_copy(out=ub[:, c, 0:30], in_=ub[:, c, TB:TB + 30]),
                         [r_uc[c]], [r_uc[c]])
                for c in range(8):
                    q, r_q = cvq[c % 2]
                    act.op(lambda c=c, q=q: nc.scalar.activation(out=q[:, 0, :], in_=cv[:, c, :], func=AF.Copy),
                           [r_cv[c]], [r_q])
                    act.op(lambda c=c, q=q: nc.scalar.activation(out=q[:, 1, :], in_=cv[:, c, :], func=AF.Square),
                           [r_cv[c]], [r_q])

                    def mm(c=c, q=q):
                        nc.tensor.matmul(ps[4][:, 0:TB], lhsT=onesb[:], rhs=q[:, 0, :], start=(c == 0), stop=(c == 7))
                        return nc.tensor.matmul(ps[5][:, 0:TB], lhsT=onesb[:], rhs=q[:, 1, :], start=(c == 0),
                                                stop=(c == 7))
                    pe.op(mm, [r_q, r_onesb], [psr[4], psr[5]])
                ck(55)
                dve.op(lambda: nc.vector.tensor_scalar(out=mean[:], in0=ps[4][:, 0:TB], scalar1=1.0 / 1024.0,
                                                       scalar2=None, op0=ALU.mult), [psr[4]], [r_mean])
                dve.op(lambda: nc.vector.tensor_tensor(out=var[:], in0=mean[:], in1=mean[:], op=ALU.mult),
                       [r_mean], [r_var])
                dve.op(lambda: nc.vector.scalar_tensor_tensor(out=var[:], in0=ps[5][:, 0:TB], scalar=1.0 / 1024.0,
                                                              in1=var[:], op0=ALU.mult, op1=ALU.subtract),
                       [psr[5], r_var], [r_var])
                dve.op(lambda: nc.vector.tensor_scalar(out=var[:], in0=var[:], scalar1=0.0, scalar2=EPS,
                                                       op0=ALU.max, op1=ALU.add), [r_var], [r_var])
                act.op(lambda: nc.scalar.activation(out=var[:], in_=var[:], func=AF.Sqrt), [r_var], [r_var])
                dve.op(lambda: nc.vector.reciprocal(out=rstd[:], in_=var[:]), [r_var], [r_rstd])
                for c in range(8):
                    eng, E = (nc.vector, dve) if c < 5 else (nc.gpsimd, pool)
                    E.op(lambda c=c, eng=eng: eng.tensor_tensor(out=cv[:, c, :], in0=cv[:, c, :], in1=mean[:],
                                                                op=ALU.subtract), [r_cv[c], r_mean], [r_cv[c]])
                    E.op(lambda c=c, eng=eng: eng.tensor_tensor(out=cv[:, c, :], in0=cv[:, c, :], in1=rstd[:],
                                                                op=ALU.mult), [r_cv[c], r_rstd], [r_cv[c]])
                    act.op(lambda c=c: nc.scalar.activation(out=uf[:, c, :], in_=cv[:, c, :], func=AF.Silu,
                                                            bias=pfm[:, 16 + c:17 + c], scale=pfm[:, 8 + c:9 + c]),
                           [r_cv[c], r_pfm], [r_uf])
                act.dma([(UF_v[:, :, tok0:tok0 + TB], uf[:])], [r_uf], [r_UF], r_uf)
                ck(56)
                if stage == "A1":
                    dbg, r_dbg = sb(stA, "dbg", [128, 1024])
                    def dump(row, src_ap, n, rs):
                        dve.op(lambda: nc.vector.tensor_copy(out=dbg[:, 0:n], in_=src_ap), rs, [r_dbg])
                        sp.dma([(out_d[row:row + 128, 0:n], dbg[:, 0:n])], [r_dbg], [], r_dbg)
                    dump(0, winA[:, 0, 0:1024], 1024, [r_winA])
                    dump(128, hT[:, 0, :], 512, [r_hT])
                    dump(256, ub[:, 0, 0:542], 542, [r_uc[0]])
                    dump(384, cv[:, 0, :], 512, [r_cv[0]])
                    dump(512, mean[:], 512, [r_mean])
                    dump(640, var[:], 512, [r_var])
                    dump(768, rstd[:], 512, [r_rstd])
                    dump(896, uf[:, 0, :], 512, [r_uf])
                    sp.deps([], [r_dbg, r_uf])
                    raise EarlyExit()
                if stage == "A":
                    j4 = tok0 // 1024
                    col = tok0 % 1024
                    dve.op(lambda: nc.vector.tensor_copy(out=xTf[:], in_=uf[:]), [r_uf], [r_xTf])
                    sp.dma([(out_d[j4 * 1024:(j4 + 1) * 1024, col:col + TB].rearrange("(c p) t -> p c t", p=128),
                             xTf[:])], [r_xTf], [], r_xTf)

    barrier()
    if stage == "A":
        sp.deps([], [r_uf, r_xTf])
        raise EarlyExit()

    TBB = 128
    with contextlib.ExitStack() as stB:
        winB, r_winB = sb(stB, "winB", [128, 8, 3088], BF16)
        dg, r_dg = sb(stB, "dg", [128, 16, 4, 128], BF16)
        scw, r_scw = sb(stB, "scw", [128, 16, 4])
        normw, r_normw = sb(stB, "normw", [128, D])
        xTf, r_xTf = sb(stB, "xTfB", [128, 8, TBB])
        hT, r_hT = sb(stB, "hTB", [128, 8, TBB], BF16)
        xpre, r_xpre = sb(stB, "xpre", [128, 16, 3 + TBB], BF16)
        xpost, r_xpost = sb(stB, "xpost", [128, 16, TBB], BF16)
        tA, r_tA = sb(stB, "tA", [128, D])
        tB, r_tB = sb(stB, "tB", [128, D])
        tC, r_tC = sb(stB, "tC", [128, D])
        Rm, r_Rm = sb(stB, "Rm", [128, D])
        dec, r_dec = sb(stB, "dec", [128, D])
        MT, r_MT = sb(stB, "MT", [128, 16, 128], BF16)
        cbs, r_cbs = sb(stB, "cbs", [128, 512])
        xc, r_xc = sb(stB, "xc", [128, D], BF16)
        xcd, r_xcd = sb(stB, "xcd", [128, D], BF16)
        ynb, r_ynb = sb(stB, "ynb", [128, D], BF16)
        ynT, r_ynT = sb(stB, "ynT", [128, 8, 128], BF16)
        Btok, r_Btok = sb(stB, "Btok", [128, 512], BF16)
        S, r_S = sb(stB, "S", [128, D])
        Sb, r_Sb = sb(stB, "Sb", [128, D], BF16)
        sm, r_sm = sb(stB, "smB", [128, 16 * 12])
        dtp = sm[:, 0:16]
        dtv = sm[:, 16:32]
        av = sm[:, 32:48]
        acs = sm[:, 48:64]
        nacs = sm[:, 64:80]
        eacs = sm[:, 80:96]
        dif = sm[:, 96:112]
        dte = sm[:, 112:128]
        cdr = sm[:, 128:144]
        ss4 = sm[:, 144:148]
        rs4 = sm[:, 148:152]
        mv = sm[:, 152:154]
        rs1 = sm[:, 154:155]
        junk = dec

        stgB = [sb(stB, f"stgB{i}", [128, 1544]) for i in range(2)]
        for half in range(2):
            c0 = 2048 + half * 1544
            load_cast([winB[:, k, half * 1544:(half + 1) * 1544] for k in range(8)],
                      [win_d[k * 128:(k + 1) * 128, c0:c0 + 1544] for k in range(8)], stgB, r_winB)
        sp.dma([(scw[:], scw_d[:, :, :])], [], [r_scw], r_scw)
        sp.dma([(normw[:], normw_d[:, :])], [], [r_normw], r_normw)
        for c in range(16):
            for k in range(4):
                dve.op(lambda c=c, k=k: nc.vector.tensor_scalar(
                    out=dg[:, c, k, :], in0=identb[:], scalar1=scw[:, c, k:k + 1], scalar2=None, op0=ALU.mult),
                    [r_identb, r_scw], [r_dg])

        ZC0, XC0, DC0 = 0, 1024, 3072
        for s in range(2):
            dve.op(lambda: nc.vector.memset(xpre[:, :, 0:3], 0.0), [], [r_xpre])
            dve.op(lambda: nc.vector.memset(S[:], 0.0), [], [r_S])
            dve.op(lambda: nc.vector.memset(Sb[:], 0.0), [], [r_Sb])
            for j in range(SEQ // TBB):
                tok0 = s * SEQ + j * TBB
                load_hT((xTf, r_xTf, hT, r_hT), tok0, TBB, s)
                for cp in range(8):
                    pi = cp % 2
                    for hh in range(2):
                        c = cp * 2 + hh

                        def mm(pi=pi, hh=hh, c=c):
                            ins = None
                            for k in range(8):
                                ins = nc.tensor.matmul(ps[pi][:, hh * TBB:(hh + 1) * TBB],
                                                       lhsT=winB[:, k, XC0 + c * 128:XC0 + (c + 1) * 128],
                                                       rhs=hT[:, k, :], start=(k == 0), stop=(k == 7))
                            return ins
                        pe.op(mm, [r_winB, r_hT], [psr[pi]])
                    act.op(lambda pi=pi, cp=cp: nc.scalar.activation(
                        out=xpre[:, 2 * cp:2 * cp + 2, 3:3 + TBB],
                        in_=ps[pi][:, 0:2 * TBB].rearrange("p (a t) -> p a t", a=2), func=AF.Copy),
                        [psr[pi]], [r_xpre])
                for cp in range(8):
                    pi = 2 + cp % 2
                    for hh in range(2):
                        c = cp * 2 + hh

                        def mm(pi=pi, hh=hh, c=c):
                            ins = None
                            for k in range(4):
                                ins = nc.tensor.matmul(ps[pi][:, hh * TBB:(hh + 1) * TBB], lhsT=dg[:, c, k, :],
                                                       rhs=xpre[:, c, k:k + TBB], start=(k == 0), stop=(k == 3))
                            return ins
                        pe.op(mm, [r_dg, r_xpre], [psr[pi]])
                        act.op(lambda pi=pi, hh=hh, c=c: nc.scalar.activation(
                            out=xpost[:, c, :], in_=ps[pi][:, hh * TBB:(hh + 1) * TBB], func=AF.Silu,
                            bias=pfm[:, 24 + c:25 + c]), [psr[pi], r_pfm], [r_xpost])
                dve.op(lambda: nc.vector.tensor_copy(out=xpre[:, :, 0:3], in_=xpre[:, :, TBB:TBB + 3]),
                       [r_xpre], [r_xpre])

                ck(1)
                for q in range(TBB // 128):
                    o = q * 128
                    t0 = tok0 + o
                    tile_idx = t0 // 128
                    for hf in range(2):
                        def mm(hf=hf, o=o):
                            ins = None
                            for k in range(8):
                                ins = nc.tensor.matmul(ps[hf][:, :], lhsT=hT[:, k, o:o + 128],
                                                       rhs=winB[:, k, ZC0 + hf * 512:ZC0 + (hf + 1) * 512],
                                                       start=(k == 0), stop=(k == 7))
                            return ins
                        pe.op(mm, [r_hT, r_winB], [psr[hf]])

                    def mm(o=o):
                        ins = None
                        for k in range(8):
                            ins = nc.tensor.matmul(ps[2][:, 0:16], lhsT=hT[:, k, o:o + 128],
                                                   rhs=winB[:, k, DC0:DC0 + 16], start=(k == 0), stop=(k == 7))
                        return ins
                    pe.op(mm, [r_hT, r_winB], [psr[2]])
                    ck(2)
                    dve.op(lambda: nc.vector.tensor_tensor(out=dtp, in0=ps[2][:, 0:16], in1=prow[:, 0:16],
                                                           op=ALU.add), [psr[2], r_prow], [r_sm])
                    act.op(lambda: nc.scalar.activation(out=dtp, in_=dtp, func=AF.Exp), [r_sm], [r_sm])
                    act.op(lambda: nc.scalar.activation(out=dtv, in_=dtp, func=AF.Ln, bias=1.0), [r_sm], [r_sm])
                    dve.op(lambda: nc.vector.tensor_tensor(out=av, in0=dtv, in1=prow[:, 16:32], op=ALU.mult),
                           [r_sm, r_prow], [r_sm])

                    def mm():
                        nc.tensor.matmul(ps[2][:, 16:32], lhsT=tri[:], rhs=av, start=True, stop=True)
                        return nc.tensor.matmul(ps[2][:, 32:48], lhsT=onesf[:], rhs=av, start=True, stop=True)
                    pe.op(mm, [r_tri, r_onesf, r_sm], [psr[2]])
                    dve.op(lambda: nc.vector.tensor_copy(out=acs, in_=ps[2][:, 16:32]), [psr[2]], [r_sm])
                    dve.op(lambda: nc.vector.tensor_scalar(out=nacs, in0=acs, scalar1=-1.0, scalar2=None,
                                                           op0=ALU.mult), [r_sm], [r_sm])
                    dve.op(lambda: nc.vector.tensor_tensor(out=dif, in0=ps[2][:, 32:48], in1=acs, op=ALU.subtract),
                           [psr[2], r_sm], [r_sm])
                    act.op(lambda: nc.scalar.activation(out=eacs, in_=acs, func=AF.Exp), [r_sm], [r_sm])
                    act.op(lambda: nc.scalar.activation(out=dte, in_=dif, func=AF.Exp), [r_sm], [r_sm])
                    act.op(lambda: nc.scalar.activation(out=cdr, in_=ps[2][:, 32:48], func=AF.Exp),
                           [psr[2], r_sm], [r_sm])
                    ck(3)
                    def mm(o=o):
                        ins = None
                        for g in range(4):
                            ins = nc.tensor.matmul(ps[3][:, g * 128:(g + 1) * 128], lhsT=xpost[:, 8 + g, o:o + 128],
                                                   rhs=xpost[:, 12 + g, o:o + 128], start=True, stop=True)
                        return ins
                    pe.op(mm, [r_xpost], [psr[3]])
                    act.op(lambda: nc.scalar.activation(out=cbs[:], in_=ps[3][:, :], func=AF.Copy), [psr[3]], [r_cbs])
                    ck(4)
                    def mm(o=o):
                        ins = None
                        for c in range(8):
                            ins = nc.tensor.transpose(out=psbf(7)[:, c * 128:(c + 1) * 128], in_=xpost[:, c, o:o + 128],
                                                      identity=identb[:])
                        return ins
                    pe.op(mm, [r_xpost, r_identb], [psr[7]])
                    ck(41)
                    act.op(lambda: nc.scalar.activation(out=tB[:], in_=psbf(7)[:, :], func=AF.Copy), [psr[7]], [r_tB])
                    ck(42)
                    dve.op(lambda: nc.vector.tensor_tensor(
                        out=xc[:].rearrange("p (h d) -> p h d", d=64),
                        in0=tB[:].rearrange("p (h d) -> p h d", d=64),
                        in1=dtv.unsqueeze(2).broadcast_to([128, 16, 64]), op=ALU.mult), [r_tB, r_sm], [r_xc])
                    ck(43)
                    dve.op(lambda: nc.vector.tensor_tensor(
                        out=xcd[:].rearrange("p (h d) -> p h d", d=64),
                        in0=xc[:].rearrange("p (h d) -> p h d", d=64),
                        in1=dte.unsqueeze(2).broadcast_to([128, 16, 64]), op=ALU.mult), [r_xc, r_sm], [r_xcd])
                    ck(5)
                    for hh in range(2):
                        dve.op(lambda hh=hh: nc.vector.tensor_tensor(
                            out=Rm[:].rearrange("p (h l) -> p h l", l=128),
                            in0=tri[:].unsqueeze(1).broadcast_to([128, 8, 128]),
                            in1=av[:, 8 * hh:8 * hh + 8].unsqueeze(2).broadcast_to([128, 8, 128]), op=ALU.mult),
                            [r_tri, r_sm], [r_Rm])
                        for jb in range(2):
                            def mm(jb=jb):
                                nc.tensor.matmul(ps[4 + jb][:, :], lhsT=onesf[:], rhs=Rm[:, jb * 512:(jb + 1) * 512],
                                                 start=True, stop=False)
                                return nc.tensor.matmul(ps[4 + jb][:, :], lhsT=identf[:], rhs=negm[:],
                                                        start=False, stop=True)
                            pe.op(mm, [r_onesf, r_identf, r_negm, r_Rm], [psr[4 + jb]])
                        for jj in range(8):
                            h = 8 * hh + jj
                            act.op(lambda jj=jj, h=h: nc.scalar.activation(
                                out=dec[:, jj * 128:(jj + 1) * 128],
                                in_=ps[4 + jj // 4][:, (jj % 4) * 128:(jj % 4 + 1) * 128], func=AF.Exp,
                                bias=nacs[:, h:h + 1]), [psr[4 + jj // 4], r_sm], [r_dec])
                        for jb in range(2):
                            g = 2 * hh + jb
                            dve.op(lambda jb=jb, g=g, hh=hh: nc.vector.tensor_tensor(
                                out=MT[:, 8 * hh + 4 * jb:8 * hh + 4 * jb + 4, :],
                                in0=dec[:, jb * 512:(jb + 1) * 512].rearrange("p (h l) -> p h l", l=128),
                                in1=cbs[:, g * 128:(g + 1) * 128].unsqueeze(1).broadcast_to([128, 4, 128]),
                                op=ALU.mult), [r_dec, r_cbs], [r_MT])
                    ck(6)
                    for bk in range(2):
                        def mm(bk=bk, o=o):
                            ins = None
                            for gg in range(2):
                                g = 2 * bk + gg
                                ins = nc.tensor.matmul(ps[4 + bk][:, gg * 256:(gg + 1) * 256],
                                                       lhsT=xpost[:, 12 + g, o:o + 128],
                                                       rhs=Sb[:, g * 256:(g + 1) * 256], start=True, stop=True)
                            return ins
                        pe.op(mm, [r_xpost, r_Sb], [psr[4 + bk]])
                        dve.op(lambda bk=bk: nc.vector.tensor_tensor(
                            out=tC[:, bk * 512:(bk + 1) * 512].rearrange("p (h d) -> p h d", d=64),
                            in0=ps[4 + bk][:, :].rearrange("p (h d) -> p h d", d=64),
                            in1=eacs[:, 8 * bk:8 * bk + 8].unsqueeze(2).broadcast_to([128, 8, 64]), op=ALU.mult),
                            [psr[4 + bk], r_sm], [r_tC])
                    ck(7)
                    for bk in range(2):
                        def mm(bk=bk):
                            ins = None
                            for hh8 in range(8):
                                h = 8 * bk + hh8
                                ins = nc.tensor.matmul(ps[(6, 3)[bk]][:, hh8 * 64:(hh8 + 1) * 64], lhsT=MT[:, h, :],
                                                       rhs=xc[:, h * 64:(h + 1) * 64], start=True, stop=True)
                            return ins
                        pe.op(mm, [r_MT, r_xc], [psr[(6, 3)[bk]]])
                        dve.op(lambda bk=bk: nc.vector.tensor_tensor(
                            out=tC[:, bk * 512:(bk + 1) * 512], in0=ps[(6, 3)[bk]][:, :],
                            in1=tC[:, bk * 512:(bk + 1) * 512], op=ALU.add), [psr[(6, 3)[bk]], r_tC], [r_tC])
                    ck(8)
                    dve.op(lambda: nc.vector.tensor_tensor(
                        out=tB[:].rearrange("p (h d) -> p h d", d=64), in0=tB[:].rearrange("p (h d) -> p h d", d=64),
                        in1=prow[:, 32:48].unsqueeze(2).broadcast_to([128, 16, 64]), op=ALU.mult),
                        [r_tB, r_prow], [r_tB])
                    dve.op(lambda: nc.vector.tensor_tensor(out=tC[:], in0=tC[:], in1=tB[:], op=ALU.add),
                           [r_tC, r_tB], [r_tC])
                    ck(9)
                    for hf in range(2):
                        act.op(lambda hf=hf: nc.scalar.activation(out=tA[:, hf * 512:(hf + 1) * 512], in_=ps[hf][:, :],
                                                                  func=AF.Silu), [psr[hf]], [r_tA])
                    dve.op(lambda: nc.vector.tensor_tensor(out=tC[:], in0=tC[:], in1=tA[:], op=ALU.mult),
                           [r_tC, r_tA], [r_tC])
                    for g4 in range(4):
                        act.op(lambda g4=g4: nc.scalar.activation(
                            out=junk[:, 0:256], in_=tC[:, g4 * 256:(g4 + 1) * 256], func=AF.Square,
                            accum_out=ss4[:, g4:g4 + 1]), [r_tC], [r_dec, r_sm])
                    dve.op(lambda: nc.vector.tensor_scalar(out=ss4, in0=ss4, scalar1=1.0 / 256.0, scalar2=EPS,
                                                           op0=ALU.mult, op1=ALU.add), [r_sm], [r_sm])
                    act.op(lambda: nc.scalar.activation(out=ss4, in_=ss4, func=AF.Sqrt), [r_sm], [r_sm])
                    dve.op(lambda: nc.vector.reciprocal(out=rs4, in_=ss4), [r_sm], [r_sm])
                    dve.op(lambda: nc.vector.tensor_tensor(
                        out=tC[:].rearrange("p (g d) -> p g d", d=256), in0=tC[:].rearrange("p (g d) -> p g d", d=256),
                        in1=rs4.unsqueeze(2).broadcast_to([128, 4, 256]), op=ALU.mult), [r_tC, r_sm], [r_tC])
                    dve.op(lambda: nc.vector.tensor_tensor(out=ynb[:], in0=tC[:], in1=normw[:], op=ALU.mult),
                           [r_tC, r_normw], [r_ynb])

                    def mm():
                        ins = None
                        for c in range(8):
                            ins = nc.tensor.transpose(out=psbf(3)[:, c * 128:(c + 1) * 128],
                                                      in_=ynb[:, c * 128:(c + 1) * 128], identity=identb[:])
                        return ins
                    pe.op(mm, [r_ynb, r_identb], [psr[7]])
                    act.op(lambda: nc.scalar.activation(out=ynT[:].rearrange("p c t -> p (c t)"), in_=psbf(3)[:, :],
                                                        func=AF.Copy), [psr[7]], [r_ynT])
                    ck(10)
                    def mm(o=o):
                        ins = None
                        for g in range(4):
                            ins = nc.tensor.transpose(out=psbf(2)[:, g * 128:(g + 1) * 128],
                                                      in_=xpost[:, 8 + g, o:o + 128], identity=identb[:])
                        return ins
                    pe.op(mm, [r_xpost, r_identb], [psr[7]])
                    act.op(lambda: nc.scalar.activation(out=Btok[:], in_=psbf(2)[:, 0:512], func=AF.Copy),
                           [psr[7]], [r_Btok])
                    dve.op(lambda: nc.vector.tensor_tensor(
                        out=S[:].rearrange("p (h d) -> p h d", d=64), in0=S[:].rearrange("p (h d) -> p h d", d=64),
                        in1=cdr.unsqueeze(2).broadcast_to([128, 16, 64]), op=ALU.mult), [r_S, r_sm], [r_S])
                    for bk in range(2):
                        def mm(bk=bk):
                            ins = None
                            for gg in range(2):
                                g = 2 * bk + gg
                                ins = nc.tensor.matmul(ps[4 + bk][:, gg * 256:(gg + 1) * 256],
                                                       lhsT=Btok[:, g * 128:(g + 1) * 128],
                                                       rhs=xcd[:, g * 256:(g + 1) * 256], start=True, stop=True)
                            return ins
                        pe.op(mm, [r_Btok, r_xcd], [psr[4 + bk]])
                        dve.op(lambda bk=bk: nc.vector.tensor_tensor(
                            out=S[:, bk * 512:(bk + 1) * 512], in0=ps[4 + bk][:, :],
                            in1=S[:, bk * 512:(bk + 1) * 512], op=ALU.add), [psr[4 + bk], r_S], [r_S])
                    act.op(lambda: nc.scalar.activation(out=Sb[:], in_=S[:], func=AF.Copy), [r_S], [r_Sb])
                    act.dma([(YN_v[:, :, t0:t0 + 128], ynT[:])], [r_ynT], [r_YN], r_ynT)
                    if stage == "B1":
                        j4 = t0 // 1024
                        col = t0 % 1024
                        dve.op(lambda: nc.vector.tensor_copy(out=xTf[:], in_=ynT[:]), [r_ynT], [r_xTf])
                        sp.dma([(out_d[j4 * 1024:(j4 + 1) * 1024, col:col + 128].rearrange("(c p) t -> p c t", p=128),
                                 xTf[:])], [r_xTf], [], r_xTf)

    barrier()
    if stage == "B1":
        raise EarlyExit()
    with contextlib.ExitStack() as stC:
        woutb, r_woutb = sb(stC, "woutb", [128, 16, 1024], BF16)
        ln1g, r_ln1g = sb(stC, "ln1g", [128, D])
        ln1b, r_ln1b = sb(stC, "ln1b", [128, D])
        wr, r_wr = sb(stC, "wr", [128, 8, 72])
        setsC = []
        for i in range(2):
            setsC.append((sb(stC, f"ufb{i}", [128, 8, 128], BF16), sb(stC, f"ynTC{i}", [128, 8, 128], BF16),
                          sb(stC, f"xtok{i}", [128, D]), sb(stC, f"tAC{i}", [128, D]), sb(stC, f"tCC{i}", [128, D]),
                          sb(stC, f"h2Tf{i}", [128, 8, 128]), sb(stC, f"h2b{i}", [128, D], BF16),
                          sb(stC, f"bst{i}", [128, 2, 6]), sb(stC, f"smC{i}", [128, 8])))
        stgC = [sb(stC, f"stgC{i}", [128, 1024]) for i in range(2)]
        load_cast([woutb[:, c, :] for c in range(16)], [wout_d[c * 128:(c + 1) * 128, :] for c in range(16)],
                  stgC, r_woutb)
        sp.dma([(ln1g[:], ln1g_d[:, :])], [], [r_ln1g], r_ln1g)
        sp.dma([(ln1b[:], ln1b_d[:, :])], [], [r_ln1b], r_ln1b)
        sp.dma([(wr[:], wr_d[:, :, :])], [], [r_wr], r_wr)
        for tile_idx in range(32):
            ((ufb, r_ufb), (ynT, r_ynT), (xtok, r_xtok), (tA, r_tA), (tC, r_tC), (h2Tf, r_h2Tf), (h2b, r_h2b),
             (bst, r_bst), (sm, r_sm)) = setsC[tile_idx % 2]
            mv = sm[:, 0:2]
            rs1 = sm[:, 2:3]
            po = 0 if tile_idx % 2 == 0 else 4
            s = tile_idx // 16
            t0 = tile_idx * 128
            g1row = modrow[:, s, 0:1024]
            sh2row = modrow[:, s, 1024:2048]
            sc2row = modrow[:, s, 2048:3072]
            sp.dma([(xtok[:], x_d[t0:t0 + 128, :])], [], [r_xtok], r_xtok)
            sp.dma([(ufb[:], UF_v[:, :, t0:t0 + 128])], [r_UF], [r_ufb], r_ufb)
            sp.dma([(ynT[:], YN_v[:, :, t0:t0 + 128])], [r_YN], [r_ynT], r_ynT)
            for hf in range(2):
                def mm(hf=hf):
                    ins = None
                    for c in range(8):
                        ins = nc.tensor.matmul(ps[po + hf][:, :], lhsT=ufb[:, c, :],
                                               rhs=woutb[:, c, hf * 512:(hf + 1) * 512],
                                               start=(c == 0), stop=False)
                    for c in range(8):
                        ins = nc.tensor.matmul(ps[po + hf][:, :], lhsT=ynT[:, c, :],
                                               rhs=woutb[:, 8 + c, hf * 512:(hf + 1) * 512],
                                               start=False, stop=(c == 7))
                    return ins
                pe.op(mm, [r_ufb, r_ynT, r_woutb], [psr[po + hf]])
                dve.op(lambda hf=hf: nc.vector.tensor_tensor(
                    out=tA[:, hf * 512:(hf + 1) * 512], in0=ps[po + hf][:, :],
                    in1=g1row[:, hf * 512:(hf + 1) * 512], op=ALU.mult), [psr[po + hf], r_modrow], [r_tA])
            dve.op(lambda: nc.vector.scalar_tensor_tensor(out=tA[:], in0=xtok[:], scalar=ALPHA, in1=tA[:],
                                                          op0=ALU.mult, op1=ALU.add), [r_xtok, r_tA], [r_tA])
            ck(20)
            dve.op(lambda: nc.vector.bn_stats(out=bst[:, 0, :], in_=tA[:, 0:512]), [r_tA], [r_bst])
            dve.op(lambda: nc.vector.bn_stats(out=bst[:, 1, :], in_=tA[:, 512:1024]), [r_tA], [r_bst])
            dve.op(lambda: nc.vector.bn_aggr(out=mv, in_=bst[:].rearrange("p a b -> p (a b)")),
                   [r_bst], [r_sm])
            ck(21)
            dve.op(lambda: nc.vector.tensor_scalar(out=rs1, in0=mv[:, 1:2], scalar1=EPS, scalar2=None,
                                                   op0=ALU.add), [r_sm], [r_sm])
            act.op(lambda: nc.scalar.activation(out=rs1, in_=rs1, func=AF.Sqrt), [r_sm], [r_sm])
            dve.op(lambda: nc.vector.reciprocal(out=rs1, in_=rs1), [r_sm], [r_sm])
            dve.op(lambda: nc.vector.tensor_scalar(out=tC[:], in0=tA[:], scalar1=mv[:, 0:1], scalar2=rs1,
                                                   op0=ALU.subtract, op1=ALU.mult), [r_tA, r_sm], [r_tC])
            dve.op(lambda: nc.vector.tensor_tensor(out=tC[:], in0=tC[:], in1=ln1g[:], op=ALU.mult),
                   [r_tC, r_ln1g], [r_tC])
            dve.op(lambda: nc.vector.tensor_tensor(out=tC[:], in0=tC[:], in1=ln1b[:], op=ALU.add),
                   [r_tC, r_ln1b], [r_tC])
            pool.dma([(X1_d[t0:t0 + 128, :], tC[:])], [r_tC], [r_X1], r_tC)
            if stage == "B":
                sp.dma([(out_d[t0:t0 + 128, :], tC[:])], [r_tC], [], r_tC)
            ck(22)
            dve.op(lambda: nc.vector.tensor_tensor(out=tA[:], in0=tC[:], in1=sc2row, op=ALU.mult),
                   [r_tC, r_modrow], [r_tA])
            dve.op(lambda: nc.vector.tensor_tensor(out=tA[:], in0=tA[:], in1=sh2row, op=ALU.add),
                   [r_tA, r_modrow], [r_tA])
            ck(23)
            for bk in range(2):
                def mm(bk=bk):
                    ins = None
                    for cc in range(4):
                        c = 4 * bk + cc
                        ins = nc.tensor.matmul(ps[2 + bk][:, cc * 128:(cc + 1) * 128],
                                               lhsT=tA[:, c * 128:(c + 1) * 128], rhs=identf[:],
                                               start=True, stop=True)
                    return ins
                pe.op(mm, [r_tA, r_identf], [psr[2 + bk]])
                ck(241)
                act.op(lambda bk=bk: nc.scalar.activation(
                    out=h2Tf[:, 4 * bk:4 * bk + 4, :].rearrange("p c t -> p (c t)"), in_=ps[2 + bk][:, :],
                    func=AF.Copy), [psr[2 + bk]], [r_h2Tf])
                ck(242)
            ck(24)
            pool.op(lambda: nc.gpsimd.tensor_copy(out=h2b[:], in_=tA[:]), [r_tA], [r_h2b])
            pool.dma([(H2_d[t0:t0 + 128, :], h2b[:])], [r_h2b], [r_H2T], r_h2b)

            def mm():
                ins = None
                for k in range(8):
                    ins = nc.tensor.matmul(ps[6][:, 0:72], lhsT=h2Tf[:, k, :], rhs=wr[:, k, :],
                                           start=(k == 0), stop=(k == 7))
                return ins
            pe.op(mm, [r_h2Tf, r_wr], [psr[6]])
            dve.op(lambda tile_idx=tile_idx: nc.vector.tensor_tensor(
                out=Lg[:, tile_idx, :], in0=ps[6][:, 0:72], in1=rb[:], op=ALU.add), [psr[6], r_rb], [r_Lg])
            ck(25)

    barrier()
    if stage == "B":
        sp.deps([], [r_tC])
        raise EarlyExit()

    I32 = mybir.dt.int32
    V = nc.vector
    gmax = rsm[:, 0:32]
    pgt = rsm[:, 32:64]
    m1 = rsm[:, 64:96]
    m2 = rsm[:, 96:128]
    pe1 = rsm[:, 128:160]
    pe2 = rsm[:, 160:192]
    d1f = rsm[:, 192:224]
    d2f = rsm[:, 224:256]
    with contextlib.ExitStack() as stM:
        stR = contextlib.ExitStack()
        wk, r_wk = sb(stR, "wk", [128, 32, 64])
        oh1, r_oh1 = sb(stR, "oh1", [128, 32, 64])
        oh2, r_oh2 = sb(stR, "oh2", [128, 32, 64])
        cum, r_cum = sb(stR, "cum", [128, 32, 64])
        cs, r_cs = sb(stR, "cs", [128, 32, 64])
        ohb, r_ohb = sb(stR, "ohb", [128, 32, 64], BF16)
        triSb, r_triSb = sb(stR, "triSb", [128, 128], BF16)
        gm, r_gm = sb(stR, "gm", [128, 32, 8])
        ohg, r_ohg = sb(stR, "ohg", [128, 32, 8])
        thr, r_thr = sb(stR, "thr", [128, 64])
        iotab, r_iotab = sb(stR, "iotab", [128, 128])
        pid, r_pid = sb(stR, "pid", [128, 1])
        e64, r_e64 = sb(stR, "e64", [128, 6, 64])
        bef, r_bef = sb(stR, "bef", [128, 128])
        big, r_big = sb(stR, "big", [128, 4096])
        sp.dma([(thr[:], thr_d[:, :])], [], [r_thr], r_thr)
        sp.dma([(iotab[:], iotab_d[:, :])], [], [r_iotab], r_iotab)
        sp.dma([(pid[:], pid_d[:, :])], [], [r_pid], r_pid)
        dve.op(lambda: V.tensor_reduce(out=gmax, in_=Lg[:, :, 0:8], axis=AX.X, op=ALU.max), [r_Lg], [r_rsm])
        dve.op(lambda: V.tensor_tensor(out=gm[:], in0=Lg[:, :, 0:8], in1=gmax.unsqueeze(2).broadcast_to([128, 32, 8]),
                                       op=ALU.subtract), [r_Lg, r_rsm], [r_gm])
        dve.op(lambda: V.tensor_single_scalar(out=ohg[:], in_=gm[:], scalar=0.0, op=ALU.is_ge), [r_gm], [r_ohg])
        act.op(lambda: nc.scalar.activation(out=gm[:], in_=gm[:], func=AF.Exp), [r_gm], [r_gm])
        dve.op(lambda: V.tensor_reduce(out=pgt, in_=gm[:], axis=AX.X, op=ALU.add), [r_gm], [r_rsm])
        dve.op(lambda: V.reciprocal(out=pgt, in_=pgt), [r_rsm], [r_rsm])
        dve.op(lambda: V.tensor_scalar(out=ohg[:], in0=ohg[:], scalar1=-1.0, scalar2=BIG, op0=ALU.add, op1=ALU.mult),
               [r_ohg], [r_ohg])
        dve.op(lambda: V.tensor_tensor(
            out=wk[:].rearrange("p t (g e) -> p t g e", e=8), in0=Lg[:, :, 8:72].rearrange("p t (g e) -> p t g e", e=8),
            in1=ohg[:].unsqueeze(3).broadcast_to([128, 32, 8, 8]), op=ALU.add), [r_Lg, r_ohg], [r_wk])
        dve.op(lambda: V.tensor_reduce(out=m1, in_=wk[:], axis=AX.X, op=ALU.max), [r_wk], [r_rsm])
        dve.op(lambda: V.tensor_tensor(out=oh1[:], in0=wk[:], in1=m1.unsqueeze(2).broadcast_to([128, 32, 64]),
                                       op=ALU.is_ge), [r_wk, r_rsm], [r_oh1])
        dve.op(lambda: V.scalar_tensor_tensor(out=wk[:], in0=oh1[:], scalar=-BIG, in1=wk[:], op0=ALU.mult,
                                              op1=ALU.add), [r_oh1, r_wk], [r_wk])
        dve.op(lambda: V.tensor_reduce(out=m2, in_=wk[:], axis=AX.X, op=ALU.max), [r_wk], [r_rsm])
        dve.op(lambda: V.tensor_tensor(out=oh2[:], in0=wk[:], in1=m2.unsqueeze(2).broadcast_to([128, 32, 64]),
                                       op=ALU.is_ge), [r_wk, r_rsm], [r_oh2])
        dve.op(lambda: V.tensor_tensor(out=pe1, in0=m2, in1=m1, op=ALU.subtract), [r_rsm], [r_rsm])
        act.op(lambda: nc.scalar.activation(out=pe1, in_=pe1, func=AF.Exp), [r_rsm], [r_rsm])
        dve.op(lambda: V.tensor_scalar(out=pe1, in0=pe1, scalar1=1.0, scalar2=None, op0=ALU.add), [r_rsm], [r_rsm])
        dve.op(lambda: V.reciprocal(out=pe1, in_=pe1), [r_rsm], [r_rsm])
        dve.op(lambda: V.tensor_scalar(out=pe2, in0=pe1, scalar1=-1.0, scalar2=1.0, op0=ALU.mult, op1=ALU.add),
               [r_rsm], [r_rsm])
        dve.op(lambda: V.tensor_tensor(out=pe1, in0=pe1, in1=pgt, op=ALU.mult), [r_rsm], [r_rsm])
        dve.op(lambda: V.tensor_tensor(out=pe2, in0=pe2, in1=pgt, op=ALU.mult), [r_rsm], [r_rsm])
        dve.op(lambda: V.tensor_tensor(out=ohb[:], in0=oh1[:], in1=oh2[:], op=ALU.add), [r_oh1, r_oh2], [r_ohb])
        dve.op(lambda: V.tensor_tensor(out=triSb[:], in0=tri[:], in1=identf[:], op=ALU.subtract),
               [r_tri, r_identf], [r_triSb])
        for q4 in range(4):
            def mm(q4=q4):
                return nc.tensor.matmul(ps[q4][:, :], lhsT=triSb[:], rhs=ohb[:, 8 * q4:8 * q4 + 8, :],
                                        start=True, stop=True)
            pe.op(mm, [r_triSb, r_ohb], [psr[q4]])
            act.op(lambda q4=q4: nc.scalar.activation(
                out=cum[:, 8 * q4:8 * q4 + 8, :].rearrange("p t e -> p (t e)"), in_=ps[q4][:, :], func=AF.Copy),
                [psr[q4]], [r_cum])
        for q4 in range(4):
            def mm(q4=q4):
                return nc.tensor.matmul(ps[q4][:, :], lhsT=onesb[:], rhs=ohb[:, 8 * q4:8 * q4 + 8, :],
                                        start=True, stop=True)
            pe.op(mm, [r_onesb, r_ohb], [psr[q4]])
            act.op(lambda q4=q4: nc.scalar.activation(
                out=cs[:, 8 * q4:8 * q4 + 8, :].rearrange("p t e -> p (t e)"), in_=ps[q4][:, :], func=AF.Copy),
                [psr[q4]], [r_cs])
        cnt = e64[:, 0, :]
        nb = e64[:, 1, :]
        ci = e64[:, 2, :]
        sbase = e64[:, 3, :]
        ones64 = e64[:, 4, :]
        dve.op(lambda: V.memset(e64[:], 0.0), [], [r_e64])
        dve.op(lambda: V.memset(ones64, 1.0), [r_e64], [r_e64])
        for t in range(32):
            dve.op(lambda t=t: V.tensor_tensor(out=cum[:, t, :], in0=cum[:, t, :], in1=cnt, op=ALU.add),
                   [r_cum, r_e64], [r_cum])
            dve.op(lambda t=t: V.tensor_tensor(out=cnt, in0=cnt, in1=cs[:, t, :], op=ALU.add), [r_cs, r_e64], [r_e64])
        dve.op(lambda: V.tensor_tensor(out=big[:].rearrange("p (e j) -> p e j", j=64),
                                       in0=cnt.unsqueeze(2).broadcast_to([128, 64, 64]),
                                       in1=thr[:].unsqueeze(1).broadcast_to([128, 64, 64]), op=ALU.is_gt),
               [r_e64, r_thr], [r_big])
        dve.op(lambda: V.tensor_reduce(out=nb, in_=big[:].rearrange("p (e j) -> p e j", j=64), axis=AX.X, op=ALU.add),
               [r_big], [r_e64])
        dve.op(lambda: V.tensor_tensor_scan(out=ci, data0=ones64, data1=nb, initial=0.0, op0=ALU.mult, op1=ALU.add),
               [r_e64], [r_e64])
        dve.op(lambda: V.tensor_tensor(out=sbase, in0=ci, in1=nb, op=ALU.subtract), [r_e64], [r_e64])
        dve.op(lambda: V.tensor_scalar(out=sbase, in0=sbase, scalar1=128.0, scalar2=None, op0=ALU.mult),
               [r_e64], [r_e64])
        dve.op(lambda: V.tensor_tensor(out=cum[:], in0=cum[:], in1=sbase.unsqueeze(1).broadcast_to([128, 32, 64]),
                                       op=ALU.add), [r_cum, r_e64], [r_cum])
        dve.op(lambda: V.tensor_tensor(out=wk[:], in0=cum[:], in1=oh1[:], op=ALU.mult), [r_cum, r_oh1], [r_wk])
        dve.op(lambda: V.tensor_reduce(out=d1f, in_=wk[:], axis=AX.X, op=ALU.add), [r_wk], [r_rsm])
        dve.op(lambda: V.tensor_tensor(out=wk[:], in0=cum[:], in1=oh2[:], op=ALU.mult), [r_cum, r_oh2], [r_wk])
        dve.op(lambda: V.tensor_reduce(out=d2f, in_=wk[:], axis=AX.X, op=ALU.add), [r_wk], [r_rsm])
        dve.op(lambda: V.tensor_copy(out=d1i[:], in_=d1f), [r_rsm], [r_d1i])
        dve.op(lambda: V.tensor_copy(out=d2i[:], in_=d2f), [r_rsm], [r_d2i])
        for hb in range(2):
            dve.op(lambda hb=hb: V.tensor_tensor(
                out=big[:].rearrange("p (b e) -> p b e", e=64),
                in0=ci.unsqueeze(1).broadcast_to([128, 64, 64]),
                in1=iotab[:, 64 * hb:64 * hb + 64].unsqueeze(2).broadcast_to([128, 64, 64]), op=ALU.is_le),
                [r_e64, r_iotab], [r_big])
            dve.op(lambda hb=hb: V.tensor_reduce(
                out=bef[:, 64 * hb:64 * hb + 64], in_=big[:].rearrange("p (b e) -> p b e", e=64),
                axis=AX.X, op=ALU.add), [r_big], [r_bef])
        dve.op(lambda: V.tensor_scalar(out=bef[:], in0=bef[:], scalar1=63.0, scalar2=None, op0=ALU.min),
               [r_bef], [r_bef])
        dve.op(lambda: V.memset(big[:, 0:128], 0.0), [r_big], [r_big])
        dve.op(lambda: V.tensor_tensor(out=big[:, 1:128], in0=bef[:, 1:128], in1=bef[:, 0:127], op=ALU.is_equal),
               [r_bef, r_big], [r_big])
        dve.op(lambda: V.tensor_scalar(out=bef[:], in0=bef[:], scalar1=128.0, scalar2=None, op0=ALU.mult),
               [r_bef], [r_bef])
        dve.op(lambda: V.scalar_tensor_tensor(out=bef[:], in0=big[:, 0:128], scalar=1.0e6, in1=bef[:], op0=ALU.mult,
                                              op1=ALU.add), [r_big, r_bef], [r_bef])
        dve.op(lambda: V.tensor_scalar(out=bef[:], in0=bef[:], scalar1=pid[:, 0:1], scalar2=None, op0=ALU.add),
               [r_bef, r_pid], [r_bef])
        dve.op(lambda: V.tensor_copy(out=widx[:], in_=bef[:]), [r_bef], [r_widx])
        barrier()
        stR.close()
        stMid.close()
        if stage == "R":
            dbg, r_dbg = sb(stM, "dbgR", [128, 256])
            dve.op(lambda: V.tensor_copy(out=dbg[:, 0:32], in_=d1i[:]), [r_d1i], [r_dbg])
            dve.op(lambda: V.tensor_copy(out=dbg[:, 32:64], in_=d2i[:]), [r_d2i], [r_dbg])
            dve.op(lambda: V.tensor_copy(out=dbg[:, 64:128], in_=rsm[:, 128:192]), [r_rsm], [r_dbg])
            dve.op(lambda: V.tensor_copy(out=dbg[:, 128:256], in_=widx[:]), [r_widx], [r_dbg])
            sp.dma([(out_d[0:128, 0:256], dbg[:])], [r_dbg], [], r_dbg)
            sp.deps([], [r_dbg])
            barrier()
            raise EarlyExit()
        r_Xs, r_Ys = Res("Xs"), Res("Ys")
        with contextlib.ExitStack() as stS:
            hb2 = [sb(stS, f"hb2_{i}", [128, D], BF16) for i in range(2)]
            scat2 = [Res(f"scat2_{i}") for i in range(2)]
            for t in range(32):
                tl, r_tl = hb2[t % 2]
                sp.dma([(tl[:], H2_d[t * 128:(t + 1) * 128, :])], [r_H2T], [r_tl], r_tl)
                for si, (di, r_di) in enumerate(((d1i, r_d1i), (d2i, r_d2i))):
                    sres = r_tl if si == 0 else scat2[t % 2]
                    pool.deps([r_tl, r_di], [])
                    if sres.dsem is None:
                        sres.dsem = nc.alloc_semaphore(name=f"d{len(ALL_DMA_RES)}_" + sres.name)
                        ALL_DMA_RES.append(sres)
                    if sres.dcnt:
                        pool.wait({id(sres.dsem): (sres.dsem, sres.dcnt)})
                    nc.gpsimd.indirect_dma_start(
                        out=Xs_d[:, :], out_offset=bass.IndirectOffsetOnAxis(ap=di[:, t:t + 1], axis=0),
                        in_=tl[:, :], in_offset=None).then_inc(sres.dsem, 16)
                    sres.dcnt += 16
                    pool.mark([r_tl, r_di], [r_Xs], sres.dsem, sres.dcnt)
            barrier()
        with contextlib.ExitStack() as stE:
            stg = [sb(stE, f"stgE{i}", [128, 4096]) for i in range(3)]
            wbf = []
            for i in range(2):
                a1, ra1 = sb(stE, f"w1b{i}", [128, 8, 512], BF16)
                a3, ra3 = sb(stE, f"w3b{i}", [128, 8, 512], BF16)
                a2, ra2 = sb(stE, f"w2b{i}", [128, 4, 1024], BF16)
                wbf.append((a1, ra1, a3, ra3, a2, ra2))
            xblk = [sb(stE, f"xblk{i}", [128, D], BF16) for i in range(2)]
            XT, r_XT = sb(stE, "XT", [128, 8, 128], BF16)
            sgE, r_sgE = sb(stE, "sgE", [128, 512])
            actb, r_actb = sb(stE, "actb", [128, 4, 128], BF16)
            yb = [sb(stE, f"yb{i}", [128, D]) for i in range(2)]
            bc_reg = nc.gpsimd.alloc_register("bc_reg")
            nc.gpsimd.reg_mov(bc_reg, nexp_decl * 128 - 1)
            for b in range(128):
                a1, ra1, a3, ra3, a2, ra2 = wbf[b % 2]
                for (wd, (tg, r_tg), (ab, r_ab), nk) in ((w1_d, stg[0], (a1, ra1), 8), (w3_d, stg[1], (a3, ra3), 8),
                                                        (w2_d, stg[2], (a2, ra2), 4)):
                    pool.deps([r_widx], [r_tg])
                    if r_tg.dsem is None:
                        r_tg.dsem = nc.alloc_semaphore(name=f"d{len(ALL_DMA_RES)}_" + r_tg.name)
                        ALL_DMA_RES.append(r_tg)
                    if r_tg.dcnt:
                        pool.wait({id(r_tg.dsem): (r_tg.dsem, r_tg.dcnt)})
                    nc.gpsimd.indirect_dma_start(
                        out=tg[:, :], out_offset=None, in_=wd[:, :],
                        in_offset=bass.IndirectOffsetOnAxis(ap=widx[:, b:b + 1], axis=0),
                        bounds_check=bc_reg, oob_is_err=False).then_inc(r_tg.dsem, 16)
                    r_tg.dcnt += 16
                    pool.mark([r_widx], [r_tg], r_tg.dsem, r_tg.dcnt)
                    if nk == 8:
                        dve.op(lambda ab=ab, tg=tg: V.tensor_copy(out=ab[:].rearrange("p k n -> p (k n)"), in_=tg[:]),
                               [r_tg], [r_ab])
                    else:
                        act.op(lambda ab=ab, tg=tg: nc.scalar.activation(out=ab[:].rearrange("p k n -> p (k n)"),
                                                                         in_=tg[:], func=AF.Copy), [r_tg], [r_ab])
                xb_, r_xb = xblk[b % 2]
                sp.dma([(xb_[:], Xs_d[b * 128:(b + 1) * 128, :])], [r_Xs], [r_xb], r_xb)

                def mm(xb_=xb_):
                    ins = None
                    for c in range(8):
                        ins = nc.tensor.transpose(out=ps7b[:, c * 128:(c + 1) * 128], in_=xb_[:, c * 128:(c + 1) * 128],
                                                  identity=identb[:])
                    return ins
                pe.op(mm, [r_xb, r_identb], [psr[7]])
                act.op(lambda: nc.scalar.activation(out=XT[:].rearrange("p c t -> p (c t)"), in_=ps7b[:, :],
                                                    func=AF.Copy), [psr[7]], [r_XT])
                pg_, pu_ = (0, 1) if b % 2 == 0 else (2, 3)

                def mm(a1=a1, a3=a3, pg_=pg_, pu_=pu_):
                    ins = None
                    for hc in range(4):
                        for k in range(8):
                            ins = nc.tensor.matmul(ps[pg_][:, hc * 128:(hc + 1) * 128],
                                                   lhsT=a1[:, k, hc * 128:(hc + 1) * 128], rhs=XT[:, k, :],
                                                   start=(k == 0), stop=(k == 7))
                    for hc in range(4):
                        for k in range(8):
                            ins = nc.tensor.matmul(ps[pu_][:, hc * 128:(hc + 1) * 128],
                                                   lhsT=a3[:, k, hc * 128:(hc + 1) * 128], rhs=XT[:, k, :],
                                                   start=(k == 0), stop=(k == 7))
                    return ins
                pe.op(mm, [ra1, ra3, r_XT], [psr[pg_], psr[pu_]])
                act.op(lambda pg_=pg_: nc.scalar.activation(out=sgE[:], in_=ps[pg_][:, :], func=AF.Silu),
                       [psr[pg_]], [r_sgE])
                dve.op(lambda pu_=pu_: V.tensor_tensor(out=actb[:].rearrange("p c t -> p (c t)"), in0=ps[pu_][:, :],
                                                       in1=sgE[:], op=ALU.mult), [psr[pu_], r_sgE], [r_actb])
                yb_, r_yb = yb[b % 2]
                for hf in range(2):
                    py = 4 + hf

                    def mm(hf=hf, py=py, a2=a2):
                        ins = None
                        for hc in range(4):
                            ins = nc.tensor.matmul(ps[py][:, :], lhsT=actb[:, hc, :],
                                                   rhs=a2[:, hc, hf * 512:(hf + 1) * 512],
                                                   start=(hc == 0), stop=(hc == 3))
                        return ins
                    pe.op(mm, [r_actb, ra2], [psr[py]])
                    act.op(lambda hf=hf, py=py, yb_=yb_: nc.scalar.activation(
                        out=yb_[:, hf * 512:(hf + 1) * 512], in_=ps[py][:, :], func=AF.Copy), [psr[py]], [r_yb])
                act.dma([(Ys_d[b * 128:(b + 1) * 128, :], yb_[:])], [r_yb], [r_Ys], r_yb)
            barrier()
        with contextlib.ExitStack() as stF:
            ln2g, r_ln2g = sb(stF, "ln2g", [128, D])
            ln2b, r_ln2b = sb(stF, "ln2b", [128, D])
            x1t, r_x1t = sb(stF, "x1t", [128, D])
            fo, r_fo = sb(stF, "fo", [128, D])
            ya = [sb(stF, f"ya{i}", [128, D]) for i in range(2)]
            bst2, r_bst2 = sb(stF, "bst2", [128, 2, 6])
            sm2, r_sm2 = sb(stF, "sm2", [128, 4])
            sp.dma([(ln2g[:], ln2g_d[:, :])], [], [r_ln2g], r_ln2g)
            sp.dma([(ln2b[:], ln2b_d[:, :])], [], [r_ln2b], r_ln2b)
            for t in range(32):
                s_ = t // 16
                t0 = t * 128
                g2row = modg2[:, s_, :]
                for (yt_, r_yt), (di, r_di) in zip(ya, ((d1i, r_d1i), (d2i, r_d2i))):
                    pool.deps([r_di, r_Ys], [r_yt])
                    if r_yt.dsem is None:
                        r_yt.dsem = nc.alloc_semaphore(name=f"d{len(ALL_DMA_RES)}_" + r_yt.name)
                        ALL_DMA_RES.append(r_yt)
                    if r_yt.dcnt:
                        pool.wait({id(r_yt.dsem): (r_yt.dsem, r_yt.dcnt)})
                    nc.gpsimd.indirect_dma_start(
                        out=yt_[:, :], out_offset=None, in_=Ys_d[:, :],
                        in_offset=bass.IndirectOffsetOnAxis(ap=di[:, t:t + 1], axis=0)).then_inc(r_yt.dsem, 16)
                    r_yt.dcnt += 16
                    pool.mark([r_di, r_Ys], [r_yt], r_yt.dsem, r_yt.dcnt)
                sp.dma([(x1t[:], X1_d[t0:t0 + 128, :])], [r_X1], [r_x1t], r_x1t)
                dve.op(lambda t=t: V.tensor_scalar(out=fo[:], in0=ya[0][0][:], scalar1=pe1[:, t:t + 1], scalar2=None,
                                                   op0=ALU.mult), [ya[0][1], r_rsm], [r_fo])
                dve.op(lambda t=t: V.scalar_tensor_tensor(out=fo[:], in0=ya[1][0][:], scalar=pe2[:, t:t + 1], in1=fo[:],
                                                          op0=ALU.mult, op1=ALU.add), [ya[1][1], r_rsm, r_fo], [r_fo])
                dve.op(lambda g2row=g2row: V.tensor_tensor(out=fo[:], in0=fo[:], in1=g2row, op=ALU.mult),
                       [r_fo, r_modg2], [r_fo])
                dve.op(lambda: V.scalar_tensor_tensor(out=fo[:], in0=x1t[:], scalar=ALPHA, in1=fo[:], op0=ALU.mult,
                                                      op1=ALU.add), [r_x1t, r_fo], [r_fo])
                dve.op(lambda: V.bn_stats(out=bst2[:, 0, :], in_=fo[:, 0:512]), [r_fo], [r_bst2])
                dve.op(lambda: V.bn_stats(out=bst2[:, 1, :], in_=fo[:, 512:1024]), [r_fo], [r_bst2])
                dve.op(lambda: V.bn_aggr(out=sm2[:, 0:2], in_=bst2[:].rearrange("p a b -> p (a b)")),
                       [r_bst2], [r_sm2])
                dve.op(lambda: V.tensor_scalar(out=sm2[:, 2:3], in0=sm2[:, 1:2], scalar1=EPS, scalar2=None,
                                               op0=ALU.add), [r_sm2], [r_sm2])
                act.op(lambda: nc.scalar.activation(out=sm2[:, 2:3], in_=sm2[:, 2:3], func=AF.Sqrt), [r_sm2], [r_sm2])
                dve.op(lambda: V.reciprocal(out=sm2[:, 2:3], in_=sm2[:, 2:3]), [r_sm2], [r_sm2])
                dve.op(lambda: V.tensor_scalar(out=fo[:], in0=fo[:], scalar1=sm2[:, 0:1], scalar2=sm2[:, 2:3],
                                               op0=ALU.subtract, op1=ALU.mult), [r_fo, r_sm2], [r_fo])
                dve.op(lambda: V.tensor_tensor(out=fo[:], in0=fo[:], in1=ln2g[:], op=ALU.mult), [r_fo, r_ln2g], [r_fo])
                dve.op(lambda: V.tensor_tensor(out=fo[:], in0=fo[:], in1=ln2b[:], op=ALU.add), [r_fo, r_ln2b], [r_fo])
                act.dma([(out_d[t0:t0 + 128, :], fo[:])], [r_fo], [], r_fo)
            sp.deps([], [r_fo])
            barrier()
    return nc


def make_inputs(core, x, c, w_ada, b_ada, w_in, conf_conv_w, conf_conv_b, conf_ln_g, conf_ln_b,
                ssm_conv_w, ssm_conv_b, ssm_dt_bias, ssm_A_log, ssm_D, ssm_norm_w, w_out,
                ln1_g, ln1_b, router_group_w, router_group_b, router_expert_w, router_expert_b,
                expert_w1, expert_w3, expert_w2, ln2_g, ln2_b, shared):
    f = np.float32
    xc = np.ascontiguousarray(x[2 * core:2 * core + 2].reshape(NTOK, D), dtype=f)
    cc = c[2 * core:2 * core + 2]
    m = dict(shared)
    m["x"] = xc
    m["xT"] = np.ascontiguousarray(xc.T)
    m["cT"] = np.ascontiguousarray(cc.reshape(2, 8, 128).transpose(2, 1, 0), dtype=f)
    return m


def rep(v, n=128):
    return np.ascontiguousarray(np.broadcast_to(np.asarray(v, dtype=np.float32).reshape(1, -1), (n, v.size)))


def fm(v, nch):
    return np.ascontiguousarray(np.asarray(v, dtype=np.float32).reshape(nch, 128).T)


def shared_inputs(w_ada, b_ada, w_in, conf_conv_w, conf_conv_b, conf_ln_g, conf_ln_b,
                  ssm_conv_w, ssm_conv_b, ssm_dt_bias, ssm_A_log, ssm_D, ssm_norm_w, w_out,
                  ln1_g, ln1_b, router_group_w, router_group_b, router_expert_w, router_expert_b,
                  expert_w1, expert_w3, expert_w2, ln2_g, ln2_b):
    f = np.float32
    sh = {}
    sh["w_ada"] = np.ascontiguousarray(w_ada[0], dtype=f)
    sh["b_ada_fm"] = fm(b_ada[0][:2048], 16)
    sh["b_ada_row"] = rep(b_ada[0][2048:])
    sh["w_in"] = np.ascontiguousarray(w_in[0], dtype=f)
    sh["ccw"] = np.ascontiguousarray(conf_conv_w[0].reshape(31, 8, 128).transpose(2, 1, 0), dtype=f)
    sh["ccb"] = fm(conf_conv_b[0], 8)
    sh["clg"] = fm(conf_ln_g[0], 8)
    sh["clb"] = fm(conf_ln_b[0], 8)
    sh["scw"] = np.ascontiguousarray(ssm_conv_w[0].reshape(4, 16, 128).transpose(2, 1, 0), dtype=f)
    sh["scb"] = fm(ssm_conv_b[0], 16)
    sh["dtb"] = rep(ssm_dt_bias[0])
    sh["alog"] = rep(ssm_A_log[0])
    sh["dsk"] = rep(ssm_D[0])
    sh["normw"] = rep(ssm_norm_w[0])
    sh["w_out"] = np.ascontiguousarray(w_out[0], dtype=f)
    sh["ln1g"] = rep(ln1_g[0])
    sh["ln1b"] = rep(ln1_b[0])
    sh["ln2g"] = rep(ln2_g[0])
    sh["ln2b"] = rep(ln2_b[0])
    wrr = np.concatenate([router_group_w[0], router_expert_w[0]], axis=1).astype(f)
    sh["wr"] = np.ascontiguousarray(wrr.reshape(8, 128, 72).transpose(1, 0, 2))
    sh["rb"] = rep(np.concatenate([router_group_b[0], router_expert_b[0]]))
    sh["w1"] = np.ascontiguousarray(np.asarray(expert_w1[0], dtype=f).reshape(NEXP, 8, 128, 512).transpose(0, 2, 1, 3)
                                    ).reshape(NEXP * 128, 4096)
    sh["w3"] = np.ascontiguousarray(np.asarray(expert_w3[0], dtype=f).reshape(NEXP, 8, 128, 512).transpose(0, 2, 1, 3)
                                    ).reshape(NEXP * 128, 4096)
    sh["w2"] = np.ascontiguousarray(np.asarray(expert_w2[0], dtype=f).reshape(NEXP, 4, 128, 1024).transpose(0, 2, 1, 3)
                                    ).reshape(NEXP * 128, 4096)
    sh["thr"] = rep(np.arange(64, dtype=f) * 128.0)
    sh["iotab"] = rep(np.arange(128, dtype=f))
    sh["pid"] = np.arange(128, dtype=f).reshape(128, 1)
    sh["identf"] = np.eye(128, dtype=f)
    sh["tri"] = np.triu(np.ones((128, 128), dtype=f))
    nm = np.where(np.triu(np.ones((128, 128), dtype=bool)), 0.0, -30000.0).astype(f)
    sh["negm"] = np.ascontiguousarray(np.tile(nm, (1, 4)))
    return sh


def kernel(x, c, w_ada, b_ada, w_in, conf_conv_w, conf_conv_b, conf_ln_g, conf_ln_b,
           ssm_conv_w, ssm_conv_b, ssm_dt_bias, ssm_A_log, ssm_D, ssm_norm_w, w_out,
           ln1_g, ln1_b, router_group_w, router_group_b, router_expert_w, router_expert_b,
           expert_w1, expert_w3, expert_w2, ln2_g, ln2_b, _stage="full", _trace=False):
    args = [np.asarray(a) for a in (w_ada, b_ada, w_in, conf_conv_w, conf_conv_b, conf_ln_g, conf_ln_b,
                                    ssm_conv_w, ssm_conv_b, ssm_dt_bias, ssm_A_log, ssm_D, ssm_norm_w, w_out,
                                    ln1_g, ln1_b, router_group_w, router_group_b, router_expert_w, router_expert_b,
                                    expert_w1, expert_w3, expert_w2, ln2_g, ln2_b)]
    x = np.asarray(x)
    c = np.asarray(c)
    sh = shared_inputs(*args)
    if _stage != "full":
        for k in ("w1", "w3", "w2"):
            sh[k] = np.ascontiguousarray(sh[k][0:128])
    in_maps = []
    for core in range(8):
        m = dict(sh)
        xc = np.ascontiguousarray(x[2 * core:2 * core + 2].reshape(NTOK, D), dtype=np.float32)
        m["x"] = xc
        m["xT"] = np.ascontiguousarray(xc.T)
        m["cT"] = np.ascontiguousarray(c[2 * core:2 * core + 2].reshape(2, 8, 128).transpose(2, 1, 0),
                                       dtype=np.float32)
        in_maps.append(m)
    nc = build_nc(_stage)
    if _trace:
        res = run_bass_kernel_spmd(nc, in_maps, core_ids=list(range(8)), trace=True)
        print("EXEC_TIME_NS", _stage, res.exec_time_ns)
    else:
        res = run_bass_kernel_spmd(nc, in_maps, core_ids=list(range(8)))
    outs = [np.asarray(r["out"], dtype=np.float32).reshape(2, SEQ, D) for r in res.results]
    return np.concatenate(outs, axis=0)
```
